# Optimizing a Trainium2 kernel written in Bass

```python
import math
import jax, jax.numpy as jnp
from jax import lax
import numpy as np

D_MODEL = 1024
BATCH = 8
SEQ = 4096
DEPTH = 2

GRID_W = 64
CTX_LEN = 256
BLOCK = 128
WINDOW = 128
ROPE_THETA = 10000.0
EPS = 1e-6
NEG_INF = -1e30

HEAD_DIM = 64
A_HEADS = 6
A_KV_HEADS = 2
B_HEADS = 6
B_Q_RANK = 192
B_KV_RANK = 128
B_NOPE = 64
B_ROPE = 32
B_V = 64
C_HEADS = 4
C_KV_HEADS = 2
N_EXPERTS = 16
CAP_FACTOR = 2
D_EXPERT = 1024

IN_SPLITS = (A_HEADS * HEAD_DIM, A_KV_HEADS * HEAD_DIM, A_KV_HEADS * HEAD_DIM,
             B_Q_RANK, B_KV_RANK, B_ROPE,
             C_HEADS * HEAD_DIM, C_KV_HEADS * HEAD_DIM, C_KV_HEADS * HEAD_DIM)
IN_WIDTH = sum(IN_SPLITS)
IN_OFFSETS = [int(v) for v in np.cumsum(IN_SPLITS)[:-1]]
MIX_WIDTH = A_HEADS * HEAD_DIM + B_HEADS * B_V + C_HEADS * HEAD_DIM

kernel_name = "hybrid_dit_gqa_mla_swa_ecmoe"


def rms_norm(x, g):
    xf = x.astype(jnp.float32)
    y = xf * lax.rsqrt(jnp.mean(xf * xf, axis=-1, keepdims=True) + EPS)
    return y.astype(x.dtype) * g


def modulate(x, shift, scale):
    return x * (1 + scale) + shift


def axial_rope_tables(length, dim):
    rows = length // GRID_W
    t = jnp.arange(rows * GRID_W)
    row = jnp.repeat(jnp.arange(rows), GRID_W).astype(jnp.float32)
    col = (t % GRID_W).astype(jnp.float32)
    axis_dim = dim // 2
    inv = ROPE_THETA ** (-jnp.arange(0, axis_dim, 2, dtype=jnp.float32) / axis_dim)
    ang_r = row[:, None] * inv[None, :]
    ang_c = col[:, None] * inv[None, :]
    return (jnp.cos(ang_r), jnp.sin(ang_r), jnp.cos(ang_c), jnp.sin(ang_c))


def _rotate(x, cos, sin):
    half = x.shape[-1] // 2
    x1, x2 = x[..., :half], x[..., half:]
    cos = cos[:, None, :].astype(x.dtype)
    sin = sin[:, None, :].astype(x.dtype)
    return jnp.concatenate([x1 * cos - x2 * sin, x2 * cos + x1 * sin], axis=-1)


def apply_axial_rope(x, tables):
    cos_r, sin_r, cos_c, sin_c = tables
    a = x.shape[-1] // 2
    return jnp.concatenate([_rotate(x[..., :a], cos_r, sin_r), _rotate(x[..., a:], cos_c, sin_c)], axis=-1)


def gqa_heads(pq, pk, pv, gq, gk, n_heads, n_kv, tables):
    b, l, _ = pq.shape
    q = rms_norm(pq.reshape(b, l, n_heads, HEAD_DIM), gq)
    k = rms_norm(pk.reshape(b, l, n_kv, HEAD_DIM), gk)
    v = pv.reshape(b, l, n_kv, HEAD_DIM)
    if tables is not None:
        q = apply_axial_rope(q, tables)
        k = apply_axial_rope(k, tables)
    return q, k, v


def mla_heads(pcq, pckv, pkr, g_cq, g_ckv, w_uq, w_ukv, g_qn, g_kn, g_qr, g_kr, tables):
    b, l, _ = pcq.shape
    q = (rms_norm(pcq, g_cq) @ w_uq).reshape(b, l, B_HEADS, B_NOPE + B_ROPE)
    kv = (rms_norm(pckv, g_ckv) @ w_ukv).reshape(b, l, B_HEADS, B_NOPE + B_V)
    q_nope = rms_norm(q[..., :B_NOPE], g_qn)
    q_pe = rms_norm(q[..., B_NOPE:], g_qr)
    k_nope = rms_norm(kv[..., :B_NOPE], g_kn)
    v = kv[..., B_NOPE:]
    k_pe = rms_norm(pkr.reshape(b, l, 1, B_ROPE), g_kr)
    if tables is not None:
        q_pe = apply_axial_rope(q_pe, tables)
        k_pe = apply_axial_rope(k_pe, tables)
    q = jnp.concatenate([q_nope, q_pe], axis=-1)
    k = jnp.concatenate([k_nope, jnp.broadcast_to(k_pe, (b, l, B_HEADS, B_ROPE))], axis=-1)
    return q, k, v


def attend(q, k, v):
    b, t, h, dq = q.shape
    kvh = k.shape[2]
    qg = q.reshape(b, t, kvh, h // kvh, dq)
    s = jnp.einsum('btkgd,bskd->bkgts', qg, k).astype(jnp.float32) * (dq ** -0.5)
    p = jax.nn.softmax(s, axis=-1).astype(v.dtype)
    o = jnp.einsum('bkgts,bskd->btkgd', p, v)
    return o.reshape(b, t, h * v.shape[-1])


def attend_blocked(q, k, v):
    b, l, h, dq = q.shape
    nb = l // BLOCK
    qb = jnp.swapaxes(q.reshape(b, nb, BLOCK, h, dq), 0, 1)
    ob = lax.map(lambda qi: attend(qi, k, v), qb)
    return jnp.swapaxes(ob, 0, 1).reshape(b, l, -1)


def sink_softmax(s, sink):
    sink = jnp.broadcast_to(sink.astype(jnp.float32), s.shape[:-1] + (1,))
    return jax.nn.softmax(jnp.concatenate([s, sink], axis=-1), axis=-1)[..., :-1]


def attend_sink(q, k, v, sink):
    b, t, h, dq = q.shape
    kvh = k.shape[2]
    g = h // kvh
    s = jnp.einsum('btkgd,bskd->bkgts', q.reshape(b, t, kvh, g, dq), k).astype(jnp.float32) * (dq ** -0.5)
    p = sink_softmax(s, sink.reshape(kvh, g, 1, 1)).astype(v.dtype)
    return jnp.einsum('bkgts,bskd->btkgd', p, v).reshape(b, t, h * v.shape[-1])


def attend_window(q, k, v, k_ctx, v_ctx, sink):
    b, l, h, dq = q.shape
    kvh = k.shape[2]
    g = h // kvh
    nb = l // BLOCK
    n_ctx = k_ctx.shape[1]
    span = BLOCK + 2 * WINDOW
    qb = q.reshape(b, nb, BLOCK, kvh, g, dq)
    pad = ((0, 0), (WINDOW, WINDOW), (0, 0), (0, 0))
    idx = jnp.arange(nb)[:, None] * BLOCK + jnp.arange(span)[None, :]
    kb = jnp.pad(k, pad)[:, idx]
    vb = jnp.pad(v, pad)[:, idx]
    qpos = jnp.arange(nb)[:, None] * BLOCK + jnp.arange(BLOCK)[None, :]
    kpos = (idx - WINDOW)[:, None, :]
    valid = (jnp.abs(qpos[:, :, None] - kpos) <= WINDOW) & (kpos >= 0) & (kpos < l)
    scale = dq ** -0.5
    s_win = jnp.einsum('bntkgd,bnskd->bnkgts', qb, kb).astype(jnp.float32) * scale
    s_win = jnp.where(valid[None, :, None, None], s_win, NEG_INF)
    s_ctx = jnp.einsum('bntkgd,bskd->bnkgts', qb, k_ctx).astype(jnp.float32) * scale
    p = sink_softmax(jnp.concatenate([s_ctx, s_win], axis=-1), sink.reshape(1, 1, kvh, g, 1, 1)).astype(v.dtype)
    o = (jnp.einsum('bnkgts,bskd->bntkgd', p[..., :n_ctx], v_ctx)
         + jnp.einsum('bnkgts,bnskd->bntkgd', p[..., n_ctx:], vb))
    return o.reshape(b, l, h * v.shape[-1])


def expert_choice_ffn(h, w_router, w_gate, w_up, w_down):
    b, n, d = h.shape
    cap = max(1, CAP_FACTOR * n // N_EXPERTS)
    aff = jax.nn.softmax((h @ w_router).astype(jnp.float32), axis=-1)
    gate, idx = lax.top_k(jnp.swapaxes(aff, 1, 2), cap)
    xe = jax.vmap(lambda hb, ib: hb[ib])(h, idx)
    hid = jax.nn.silu(jnp.einsum('becd,edf->becf', xe, w_gate)) * jnp.einsum('becd,edf->becf', xe, w_up)
    ye = jnp.einsum('becf,efd->becd', hid, w_down) * gate[..., None].astype(h.dtype)
    return jax.vmap(lambda ib, yb: jnp.zeros((n, d), yb.dtype).at[ib.reshape(-1)].add(yb.reshape(-1, d)))(idx, ye)


def setup_inputs(seed: int = 0) -> dict:
    key = jax.random.key(seed)
    ks = jax.random.split(key, 32)

    def nrm(k, shape, scale=1.0):
        return jax.random.normal(k, shape, jnp.float32) * scale

    def gain(k, shape):
        return 1.0 + 0.05 * jax.random.normal(k, shape, jnp.float32)

    L, D = DEPTH, D_MODEL
    return {
        "x": nrm(ks[0], (BATCH, SEQ, D)),
        "c": nrm(ks[1], (BATCH, D)),
        "ctx": nrm(ks[2], (BATCH, CTX_LEN, D)),
        "c_ctx": nrm(ks[3], (D,)),
        "w_ada": nrm(ks[4], (L, D, 6 * D), D ** -0.5),
        "b_ada": nrm(ks[5], (L, 6 * D), 0.01),
        "norm1_g": gain(ks[6], (L, D)),
        "norm2_g": gain(ks[7], (L, D)),
        "w_in": nrm(ks[8], (L, D, IN_WIDTH), D ** -0.5),
        "a_q_norm": gain(ks[9], (L, HEAD_DIM)),
        "a_k_norm": gain(ks[10], (L, HEAD_DIM)),
        "b_cq_norm": gain(ks[11], (L, B_Q_RANK)),
        "b_ckv_norm": gain(ks[12], (L, B_KV_RANK)),
        "w_uq": nrm(ks[13], (L, B_Q_RANK, B_HEADS * (B_NOPE + B_ROPE)), B_Q_RANK ** -0.5),
        "w_ukv": nrm(ks[14], (L, B_KV_RANK, B_HEADS * (B_NOPE + B_V)), B_KV_RANK ** -0.5),
        "b_qn_norm": gain(ks[15], (L, B_NOPE)),
        "b_kn_norm": gain(ks[16], (L, B_NOPE)),
        "b_qr_norm": gain(ks[17], (L, B_ROPE)),
        "b_kr_norm": gain(ks[18], (L, B_ROPE)),
        "c_q_norm": gain(ks[19], (L, HEAD_DIM)),
        "c_k_norm": gain(ks[20], (L, HEAD_DIM)),
        "c_sink": nrm(ks[21], (L, C_HEADS), 0.5),
        "w_out": nrm(ks[22], (L, MIX_WIDTH, D), MIX_WIDTH ** -0.5),
        "w_router": nrm(ks[23], (L, D, N_EXPERTS), D ** -0.5),
        "w_e_gate": nrm(ks[24], (L, N_EXPERTS, D, D_EXPERT), D ** -0.5),
        "w_e_up": nrm(ks[25], (L, N_EXPERTS, D, D_EXPERT), D ** -0.5),
        "w_e_down": nrm(ks[26], (L, N_EXPERTS, D_EXPERT, D), D_EXPERT ** -0.5),
    }


def reference(x, c, ctx, c_ctx, w_ada, b_ada, norm1_g, norm2_g, w_in, a_q_norm, a_k_norm,
              b_cq_norm, b_ckv_norm, w_uq, w_ukv, b_qn_norm, b_kn_norm, b_qr_norm, b_kr_norm,
              c_q_norm, c_k_norm, c_sink, w_out, w_router, w_e_gate, w_e_up, w_e_down):
    length = x.shape[1]
    rope_hd = axial_rope_tables(length, HEAD_DIM)
    rope_b = axial_rope_tables(length, B_ROPE)
    for l in range(DEPTH):
        last = l == DEPTH - 1
        mod = (jax.nn.silu(c) @ w_ada[l] + b_ada[l])[:, None, :]
        mod_c = (jax.nn.silu(c_ctx) @ w_ada[l] + b_ada[l])[None, None, :]
        sh1, sc1, g1, sh2, sc2, g2 = jnp.split(mod, 6, axis=-1)
        csh1, csc1, cg1, csh2, csc2, cg2 = jnp.split(mod_c, 6, axis=-1)

        h = modulate(rms_norm(x, norm1_g[l]), sh1, sc1)
        hc = modulate(rms_norm(ctx, norm1_g[l]), csh1, csc1)
        aq, ak, av, bcq, bckv, bkr, cq, ck, cv = jnp.split(h @ w_in[l], IN_OFFSETS, axis=-1)
        aqc, akc, avc, bcqc, bckvc, bkrc, cqc, ckc, cvc = jnp.split(hc @ w_in[l], IN_OFFSETS, axis=-1)

        qa, ka, va = gqa_heads(aq, ak, av, a_q_norm[l], a_k_norm[l], A_HEADS, A_KV_HEADS, rope_hd)
        qac, kac, vac = gqa_heads(aqc, akc, avc, a_q_norm[l], a_k_norm[l], A_HEADS, A_KV_HEADS, None)
        o_a = attend_blocked(qa, jnp.concatenate([kac, ka], axis=1), jnp.concatenate([vac, va], axis=1))

        qb, kb, vb = mla_heads(bcq, bckv, bkr, b_cq_norm[l], b_ckv_norm[l], w_uq[l], w_ukv[l],
                               b_qn_norm[l], b_kn_norm[l], b_qr_norm[l], b_kr_norm[l], rope_b)
        qbc, kbc, vbc = mla_heads(bcqc, bckvc, bkrc, b_cq_norm[l], b_ckv_norm[l], w_uq[l], w_ukv[l],
                                  b_qn_norm[l], b_kn_norm[l], b_qr_norm[l], b_kr_norm[l], None)
        o_b = attend_blocked(qb, jnp.concatenate([kbc, kb], axis=1), jnp.concatenate([vbc, vb], axis=1))

        qc, kc, vc = gqa_heads(cq, ck, cv, c_q_norm[l], c_k_norm[l], C_HEADS, C_KV_HEADS, rope_hd)
        qcc, kcc, vcc = gqa_heads(cqc, ckc, cvc, c_q_norm[l], c_k_norm[l], C_HEADS, C_KV_HEADS, None)
        o_c = attend_window(qc, kc, vc, kcc, vcc, c_sink[l])

        x = x + g1 * (jnp.concatenate([o_a, o_b, o_c], axis=-1) @ w_out[l])
        if not last:
            o_ctx = jnp.concatenate([attend(qac, kac, vac), attend(qbc, kbc, vbc),
                                     attend_sink(qcc, kcc, vcc, c_sink[l])], axis=-1) @ w_out[l]
            ctx = ctx + cg1 * o_ctx

        h2 = modulate(rms_norm(x, norm2_g[l]), sh2, sc2)
        x = x + g2 * expert_choice_ffn(h2, w_router[l], w_e_gate[l], w_e_up[l], w_e_down[l])
        if not last:
            hc2 = modulate(rms_norm(ctx, norm2_g[l]), csh2, csc2)
            ctx = ctx + cg2 * expert_choice_ffn(hc2, w_router[l], w_e_gate[l], w_e_up[l], w_e_down[l])
    return x
```

```python
import numpy as np
from contextlib import ExitStack
import concourse.bass as bass
import concourse.mybir as mybir
from concourse.bass_utils import run_bass_kernel_spmd

F32 = mybir.dt.float32
BF16 = mybir.dt.bfloat16
I32 = mybir.dt.int32
AF = mybir.ActivationFunctionType
ALU = mybir.AluOpType
AX = mybir.AxisListType

D = 1024
SEQ = 4096
CTX = 256
T = SEQ + CTX
NT = T // 128
NCT = CTX // 128
DEPTH = 2
INW = 1504
NE = 16
CAP_L = 512
CAP_C = 32
EPS = 1e-6
BIG = 8192.0


def _is_psum(k):
    return k.startswith("pmod") or (len(k) > 1 and k[0] == "p" and k[1].isupper())


class Prog:
    ENG = ("pe", "dve", "act", "pool", "sp")

    def __init__(self, nc, stack, same_engine_sync=True, ring=(("sp", 16), ("pool", 16))):
        self.nc = nc
        self.stack = stack
        self.e = {"pe": nc.tensor, "dve": nc.vector, "act": nc.scalar, "pool": nc.gpsimd, "sp": nc.sync}
        self.same = same_engine_sync
        self.sems = {}
        self.semval = {}
        self.cur = {}
        self.epoch = {n: 0 for n in self.ENG}
        for n in self.ENG:
            self.cur[n] = self._mk("E_" + n + "_0")
        self.ring = {}
        self.ringpos = {}
        for q, k in ring:
            self.ring[q] = [self._mk(f"D_{q}{i}") for i in range(k)]
            self.ringpos[q] = 0
        self.waited = {n: {} for n in self.ENG}
        self.lastw = {}
        self.readers = {}
        self.ninstr = {n: 0 for n in self.ENG}

    def _mk(self, name):
        s = self.stack.enter_context(self.nc.semaphore(name))
        self.sems[name] = s
        self.semval[name] = 0
        return name

    def _wait(self, eng, ev):
        sem, val = ev
        if val <= 0 or self.waited[eng].get(sem, 0) >= val:
            return
        if sem == self.cur[eng] and (eng == "pe" or not self.same):
            return
        self.e[eng].wait_ge(self.sems[sem], val)
        self.waited[eng][sem] = val

    def _deps(self, eng, r, w):
        for res in list(r) + list(w):
            ev = self.lastw.get(res)
            if ev is not None:
                self._wait(eng, ev)
        for res in w:
            for sem, val in self.readers.get(res, {}).items():
                self._wait(eng, (sem, val))

    def _record(self, ev, r, w):
        for res in w:
            self.lastw[res] = ev
            self.readers[res] = {}
        for res in r:
            d = self.readers.setdefault(res, {})
            if d.get(ev[0], 0) < ev[1]:
                d[ev[0]] = ev[1]

    LIMIT = None
    total = 0

    def op(self, eng, fn, r=(), w=()):
        self.total += 1
        if self.LIMIT is not None and self.total > self.LIMIT:
            return
        w = list(w) + [k for k in r if _is_psum(k)]
        r = [k for k in r if not _is_psum(k)]
        self._deps(eng, r, w)
        ins = fn(self.e[eng])
        sem = self.cur[eng]
        self.semval[sem] += 1
        ins.then_inc(self.sems[sem], 1)
        self.ninstr[eng] += 1
        self._record((sem, self.semval[sem]), r, w)

    def dma(self, q, fn, r=(), w=()):
        self.total += 1
        if self.LIMIT is not None and self.total > self.LIMIT:
            return
        self._deps(q, r, w)
        k = self.ringpos[q]
        sem = self.ring[q][k]
        self.ringpos[q] = (k + 1) % len(self.ring[q])
        self._wait(q, (sem, self.semval[sem]))
        ins = fn(self.e[q])
        self.semval[sem] += 16
        ins.then_inc(self.sems[sem], 16)
        self.ninstr[q] += 1
        self._record((sem, self.semval[sem]), r, w)

    def barrier(self):
        for eng in self.ENG:
            for sem, val in self.semval.items():
                self._wait(eng, (sem, val))
        self.lastw.clear()
        self.readers.clear()
        for eng in self.ENG:
            if self.semval[self.cur[eng]] > 15000:
                self.epoch[eng] += 1
                self.cur[eng] = self._mk(f"E_{eng}_{self.epoch[eng]}")


def build(stop_after=None, dbg=(), limit=None, light=False):
    nc = bass.Bass("TRN2", target_bir_lowering=False)
    Prog.LIMIT = limit

    def din(name, shape, dt=F32):
        return nc.dram_tensor(name, list(shape), dt, kind="ExternalInput").ap()

    x_in = din("x", [SEQ, D]); ctx_in = din("ctx", [CTX, D]); c_in = din("c", [D]); cctx_in = din("c_ctx", [D])
    w_ada = din("w_ada", [DEPTH, D, 6 * D]); b_ada = din("b_ada", [DEPTH, 6 * D])
    norm1_g = din("norm1_g", [DEPTH, D]); norm2_g = din("norm2_g", [DEPTH, D])
    w_in = din("w_in", [DEPTH, D, INW])
    a_q_norm = din("a_q_norm", [DEPTH, 64]); a_k_norm = din("a_k_norm", [DEPTH, 64])
    b_cq_norm = din("b_cq_norm", [DEPTH, 192]); b_ckv_norm = din("b_ckv_norm", [DEPTH, 128])
    w_uq = din("w_uq", [DEPTH, 192, 576]); w_ukv = din("w_ukv", [DEPTH, 128, 768])
    b_qn_norm = din("b_qn_norm", [DEPTH, 64]); b_kn_norm = din("b_kn_norm", [DEPTH, 64])
    b_qr_norm = din("b_qr_norm", [DEPTH, 32]); b_kr_norm = din("b_kr_norm", [DEPTH, 32])
    c_q_norm = din("c_q_norm", [DEPTH, 64]); c_k_norm = din("c_k_norm", [DEPTH, 64])
    c_sink = din("c_sink", [DEPTH, 4])
    w_out = din("w_out", [DEPTH, D, D]); w_router = din("w_router", [DEPTH, D, NE])
    if not light:
        w_e_gate = din("w_e_gate", [DEPTH, NE, D, D]); w_e_up = din("w_e_up", [DEPTH, NE, D, D])
        w_e_down = din("w_e_down", [DEPTH, NE, D, D])
    rope = din("rope", [SEQ, 192])
    y_out = nc.dram_tensor("y", [SEQ, D], F32, kind="ExternalOutput").ap()

    def dscr(name, shape, dt):
        kind = "ExternalOutput" if name in dbg else "Internal"
        return nc.dram_tensor(name, list(shape), dt, kind=kind).ap()

    xall = dscr("xall", [T, D], F32)
    modv = dscr("modv", [2, 6 * D], F32)
    qkT_A = dscr("qkT_A", [512, T], BF16)
    qkT_B = dscr("qkT_B", [12, 96, T], BF16)
    qkT_C = dscr("qkT_C", [384, T], BF16)
    vall = dscr("vall", [T, 640], BF16)
    mix = dscr("mix", [T, D], BF16)
    h2d = dscr("h2d", [T, D], BF16)
    xe_l = [dscr(f"xe_l{i}", [CAP_L, D], BF16) for i in range(NE)]
    xe_c = [dscr(f"xe_c{i}", [CAP_C, D], BF16) for i in range(NE)]
    ye_l = [dscr(f"ye_l{i}", [CAP_L, D], BF16) for i in range(NE)]
    ye_c = [dscr(f"ye_c{i}", [CAP_C, D], BF16) for i in range(NE)]
    dbg_aff = dscr("dbg_aff", [128, NT * NE], F32)
    dbg_slot = dscr("dbg_slot", [128, NT * NE], I32)

    with ExitStack() as top:
        P = Prog(nc, top)
        V = lambda fn, r=(), w=(): P.op("dve", fn, r, w)
        A = lambda fn, r=(), w=(): P.op("act", fn, r, w)
        G = lambda fn, r=(), w=(): P.op("pool", fn, r, w)
        M = lambda fn, r=(), w=(): P.op("pe", fn, r, w)
        L = lambda fn, r=(), w=(): P.dma("sp", fn, r, w)
        S = lambda fn, r=(), w=(): P.dma("pool", fn, r, w)

        LAYER = [-1]

        def tsb(stk, name, shape, dt):
            return stk.enter_context(nc.sbuf_tensor(f"{name}_L{LAYER[0]}", list(shape), dt))

        def tps(stk, name, shape, dt=F32):
            return stk.enter_context(nc.psum_tensor(f"{name}_L{LAYER[0]}", list(shape), dt))

        REG = {CAP_L - 1: nc.gpsimd.to_reg(CAP_L - 1), CAP_C - 1: nc.gpsimd.to_reg(CAP_C - 1)}
        identf = tsb(top, "identf", [128, 128], F32)
        ident = tsb(top, "ident", [128, 128], BF16)
        onesf = tsb(top, "onesf", [128, 128], F32)
        ones_b = tsb(top, "ones_b", [128, 128], BF16)
        Ub = tsb(top, "Ub", [128, 128], BF16)
        M1 = tsb(top, "M1", [128, 128], BF16)
        M2 = tsb(top, "M2", [128, 128], BF16)
        ctmp = tsb(top, "ctmp", [128, 128], F32)
        cs = tsb(top, "cs", [128, 8, 2], F32)
        craw = tsb(top, "craw", [128, 2, 8], F32)

        G(lambda e: e.memset(identf[:], 0.0), w=["identf"])
        G(lambda e: e.affine_select(out=identf[:], in_=identf[:], pattern=[[-1, 128]], base=0, channel_multiplier=1,
                                    compare_op=ALU.not_equal, fill=1.0), r=["identf"], w=["identf"])
        V(lambda e: e.tensor_copy(out=ident[:], in_=identf[:]), r=["identf"], w=["ident"])
        G(lambda e: e.memset(onesf[:], 1.0), w=["onesf"])
        V(lambda e: e.tensor_copy(out=ones_b[:], in_=onesf[:]), r=["onesf"], w=["ones_b"])
        G(lambda e: e.affine_select(out=ctmp[:], in_=onesf[:], pattern=[[-1, 128]], base=0, channel_multiplier=1,
                                    compare_op=ALU.is_ge, fill=0.0), r=["onesf"], w=["ctmp"])
        V(lambda e: e.tensor_copy(out=M1[:], in_=ctmp[:]), r=["ctmp"], w=["M1"])
        G(lambda e: e.affine_select(out=ctmp[:], in_=onesf[:], pattern=[[1, 128]], base=0, channel_multiplier=-1,
                                    compare_op=ALU.is_ge, fill=0.0), r=["onesf"], w=["ctmp"])
        V(lambda e: e.tensor_copy(out=M2[:], in_=ctmp[:]), r=["ctmp"], w=["M2"])
        G(lambda e: e.affine_select(out=ctmp[:], in_=onesf[:], pattern=[[1, 128]], base=-1, channel_multiplier=-1,
                                    compare_op=ALU.is_ge, fill=0.0), r=["onesf"], w=["ctmp"])
        V(lambda e: e.tensor_copy(out=Ub[:], in_=ctmp[:]), r=["ctmp"], w=["Ub"])

        L(lambda e: e.dma_start(out=craw[:, 0, :], in_=c_in.rearrange("(k p) -> p k", p=128), allow_slow_non_contiguous=True), w=["craw"])
        L(lambda e: e.dma_start(out=craw[:, 1, :], in_=cctx_in.rearrange("(k p) -> p k", p=128), allow_slow_non_contiguous=True), w=["craw"])
        A(lambda e: e.activation(out=cs[:, :, :].rearrange("p k j -> p j k"), in_=craw[:, :, :], func=AF.Silu), r=["craw"], w=["cs"])

        L(lambda e: e.dma_start(out=xall[0:CTX, :], in_=ctx_in))
        for i in range(8):
            L(lambda e: e.dma_start(out=xall[CTX + i * 512:CTX + (i + 1) * 512, :], in_=x_in[i * 512:(i + 1) * 512, :]))
        P.barrier()

        def rstd_from_ss(ss_ap, out_ap, scale, r, w):
            V(lambda e: e.tensor_scalar(out=out_ap, in0=ss_ap, scalar1=scale, scalar2=EPS, op0=ALU.mult, op1=ALU.add), r=r, w=w)
            A(lambda e: e.activation(out=out_ap, in_=out_ap, func=AF.Sqrt), r=w, w=w)
            V(lambda e: e.reciprocal(out=out_ap, in_=out_ap), r=w, w=w)

        for l in range(DEPTH):
            last = l == DEPTH - 1
            LAYER[0] = l
            tiles_act = list(range(NT)) if not last else list(range(NCT, NT))

            with ExitStack() as ph:
                wst = [tsb(ph, f"wstA{i}", [128, 8, 512], F32) for i in range(2)]
                modrow = tsb(ph, "modrow", [2, 6 * D], F32)
                bada = tsb(ph, "bada", [2, 6 * D], F32)
                gn = tsb(ph, "gn", [2, 2, D], F32)
                pmod = [tps(ph, f"pmod{i}", [128, 512]) for i in range(2)]
                L(lambda e: e.dma_start(out=bada[:, :], in_=b_ada[l:l + 1, :].broadcast_to([2, 6 * D])), w=["bada"])
                L(lambda e: e.dma_start(out=gn[:, 0, :], in_=norm1_g[l:l + 1, :].broadcast_to([2, D])), w=["gn"])
                L(lambda e: e.dma_start(out=gn[:, 1, :], in_=norm2_g[l:l + 1, :].broadcast_to([2, D])), w=["gn"])
                for nb in range(12):
                    wb = wst[nb % 2]; pm = pmod[nb % 2]
                    for h in range(2):
                        L(lambda e: e.dma_start(out=wb[:, h * 4:(h + 1) * 4, :],
                                                in_=w_ada[l, h * 512:(h + 1) * 512, nb * 512:(nb + 1) * 512].rearrange("(k p) n -> p k n", p=128)),
                          w=[f"wstA{nb % 2}"])
                    for kc in range(8):
                        M(lambda e: e.matmul(pm[0:2, :], lhsT=cs[:, kc, :], rhs=wb[:, kc, :], start=(kc == 0), stop=(kc == 7)),
                          r=[f"wstA{nb % 2}", "cs"], w=[f"pmod{nb % 2}"])
                    V(lambda e: e.tensor_tensor(out=modrow[:, nb * 512:(nb + 1) * 512], in0=pm[0:2, :], in1=bada[:, nb * 512:(nb + 1) * 512], op=ALU.add),
                      r=[f"pmod{nb % 2}", "bada"], w=["modrow"])
                for j, off in ((0, 1 * D), (1, 4 * D)):
                    V(lambda e: e.scalar_tensor_tensor(out=modrow[:, off:off + D], in0=modrow[:, off:off + D], scalar=1.0, in1=gn[:, j, :],
                                                       op0=ALU.add, op1=ALU.mult), r=["modrow", "gn"], w=["modrow"])
                S(lambda e: e.dma_start(out=modv, in_=modrow[:, :]), r=["modrow"])
            P.barrier()
            if stop_after == ("A", l):
                break

            def load_bcast(dst, which, j, key):
                L(lambda e: e.dma_start(out=dst, in_=modv[which:which + 1, j * D:(j + 1) * D].broadcast_to([128, D])), w=[key])

            with ExitStack() as ph:
                wst = tsb(ph, "wstB", [128, 8, 512], F32)
                w_in_b = tsb(ph, "w_in_b", [128, 8, INW], BF16)
                wuq_f = tsb(ph, "wuq_f", [128, 2, 576], F32)
                wuq_b = tsb(ph, "wuq_b", [128, 2, 576], BF16)
                wukv_f = tsb(ph, "wukv_f", [128, 768], F32)
                wukv_b = tsb(ph, "wukv_b", [128, 768], BF16)
                gcq = tsb(ph, "gcq", [128, 2], F32)
                gckv = tsb(ph, "gckv", [128, 1], F32)
                gA = tsb(ph, "gA", [128, 8, 64], F32)
                gC = tsb(ph, "gC", [128, 6, 64], F32)
                gBq = tsb(ph, "gBq", [128, 96], F32)
                gBk = tsb(ph, "gBk", [128, 64], F32)
                gBkr = tsb(ph, "gBkr", [128, 32], F32)
                invB = tsb(ph, "invB", [128, 3], F32)
                invQ = tsb(ph, "invQ", [128, 12], F32)
                Ar = [tsb(ph, f"Ar{i}", [128, D], F32) for i in range(2)]
                Br = [tsb(ph, f"Br{i}", [128, D], F32) for i in range(2)]
                xt = [tsb(ph, f"xtB{i}", [128, D], F32) for i in range(2)]
                rp = [tsb(ph, f"rp{i}", [128, 192], F32) for i in range(2)]
                junk = tsb(ph, "junkB", [128, D], BF16)
                st1 = tsb(ph, "st1", [128, 4], F32)
                hf = tsb(ph, "hf", [128, D], F32)
                hb = tsb(ph, "hb", [128, D], BF16)
                hT = tsb(ph, "hT", [128, 8, 128], BF16)
                sq = tsb(ph, "sq", [128, 576], F32)
                ssh = tsb(ph, "ssh", [128, 16], F32)
                qn = tsb(ph, "qn", [128, 576], F32)
                t1 = tsb(ph, "t1", [128, 576], F32)
                t2 = tsb(ph, "t2", [128, 576], F32)
                qA = tsb(ph, "qA", [128, 8, 64], BF16)
                qC = tsb(ph, "qC", [128, 6, 64], BF16)
                qB = tsb(ph, "qB", [128, 12, 96], BF16)
                cqn = tsb(ph, "cqn", [128, 320], BF16)
                cT = tsb(ph, "cT", [128, 3, 128], BF16)
                kpe = tsb(ph, "kpe", [128, 32], F32)
                kpe2 = tsb(ph, "kpe2", [128, 32], F32)
                vst = tsb(ph, "vst", [128, 640], BF16)
                stA = tsb(ph, "stA", [128, 4, 512], BF16)
                stB = tsb(ph, "stB", [128, 12, 512], BF16)
                stC = tsb(ph, "stC", [128, 3, 512], BF16)
                pT = tps(ph, "pTB", [128, 1024], BF16)
                pP = [tps(ph, f"pP{i}", [128, 512]) for i in range(3)]
                pQ = [tps(ph, f"pQ{i}", [128, 512]) for i in range(2)]
                pK = [tps(ph, f"pK{i}", [128, 512]) for i in range(2)]

                for b, (c0, c1) in enumerate(((0, 512), (512, 1024), (1024, INW))):
                    n = c1 - c0
                    for h in range(2):
                        L(lambda e: e.dma_start(out=wst[:, h * 4:(h + 1) * 4, 0:n],
                                                in_=w_in[l, h * 512:(h + 1) * 512, c0:c1].rearrange("(k p) n -> p k n", p=128)), w=["wstB"])
                    V(lambda e: e.tensor_copy(out=w_in_b[:, 0:4, c0:c1], in_=wst[:, 0:4, 0:n]), r=["wstB"], w=["w_in_b"])
                    G(lambda e: e.tensor_copy(out=w_in_b[:, 4:8, c0:c1], in_=wst[:, 4:8, 0:n]), r=["wstB"], w=["w_in_b"])
                L(lambda e: e.dma_start(out=wuq_f[:, 0, :], in_=w_uq[l, 0:128, :]), w=["wuq_f"])
                L(lambda e: e.dma_start(out=wuq_f[0:64, 1, :], in_=w_uq[l, 128:192, :]), w=["wuq_f"])
                L(lambda e: e.dma_start(out=wukv_f[:, :], in_=w_ukv[l, :, :]), w=["wukv_f"])
                L(lambda e: e.dma_start(out=gcq[:, 0:1], in_=b_cq_norm[l, 0:128].rearrange("(p o) -> p o", o=1)), w=["gcq"])
                L(lambda e: e.dma_start(out=gcq[0:64, 1:2], in_=b_cq_norm[l, 128:192].rearrange("(p o) -> p o", o=1)), w=["gcq"])
                L(lambda e: e.dma_start(out=gckv[:, 0:1], in_=b_ckv_norm[l, :].rearrange("(p o) -> p o", o=1)), w=["gckv"])
                V(lambda e: e.tensor_scalar(out=wuq_b[:, 0, :], in0=wuq_f[:, 0, :], scalar1=gcq[:, 0:1], scalar2=None, op0=ALU.mult), r=["wuq_f", "gcq"], w=["wuq_b"])
                V(lambda e: e.tensor_scalar(out=wuq_b[0:64, 1, :], in0=wuq_f[0:64, 1, :], scalar1=gcq[0:64, 1:2], scalar2=None, op0=ALU.mult), r=["wuq_f", "gcq"], w=["wuq_b"])
                V(lambda e: e.tensor_scalar(out=wukv_b[:, :], in0=wukv_f[:, :], scalar1=gckv[:, 0:1], scalar2=None, op0=ALU.mult), r=["wukv_f", "gckv"], w=["wukv_b"])
                for hh in range(8):
                    src = a_q_norm if hh < 6 else a_k_norm
                    L(lambda e: e.dma_start(out=gA[:, hh, :], in_=src[l:l + 1, :].broadcast_to([128, 64])), w=["gA"])
                for hh in range(6):
                    src = c_q_norm if hh < 4 else c_k_norm
                    L(lambda e: e.dma_start(out=gC[:, hh, :], in_=src[l:l + 1, :].broadcast_to([128, 64])), w=["gC"])
                L(lambda e: e.dma_start(out=gBq[:, 0:64], in_=b_qn_norm[l:l + 1, :].broadcast_to([128, 64])), w=["gBq"])
                L(lambda e: e.dma_start(out=gBq[:, 64:96], in_=b_qr_norm[l:l + 1, :].broadcast_to([128, 32])), w=["gBq"])
                L(lambda e: e.dma_start(out=gBk[:, :], in_=b_kn_norm[l:l + 1, :].broadcast_to([128, 64])), w=["gBk"])
                L(lambda e: e.dma_start(out=gBkr[:, :], in_=b_kr_norm[l:l + 1, :].broadcast_to([128, 32])), w=["gBkr"])
                for j, v in enumerate((1.0 / 192, 1.0 / 128, 1.0 / 32)):
                    G(lambda e: e.memset(invB[:, j:j + 1], v), w=["invB"])
                G(lambda e: e.memset(invQ[:, 0:6], 1.0 / 64), w=["invQ"])
                G(lambda e: e.memset(invQ[:, 6:12], 1.0 / 32), w=["invQ"])
                for which in range(2):
                    load_bcast(Ar[which][:, :], which, 1, f"Ar{which}")
                    load_bcast(Br[which][:, :], which, 0, f"Br{which}")

                def rope_apply(src_ap, H, Dh, C_ap, S_ap, out_ap, rs):
                    c4 = Dh // 4
                    t1v = t1[:, 0:H * Dh].rearrange("p (h d) -> p h d", h=H)
                    t2v = t2[:, 0:H * Dh].rearrange("p (h a b c) -> p h a b c", h=H, a=2, b=2)
                    s5 = src_ap.rearrange("p h (a b c) -> p h a b c", a=2, b=2)
                    S4 = S_ap.rearrange("p (a b c) -> p a b c", a=2, b=2)
                    V(lambda e: e.tensor_tensor(out=t1v, in0=src_ap, in1=C_ap.unsqueeze(1).to_broadcast([128, H, Dh]), op=ALU.mult), r=rs, w=["t1"])
                    V(lambda e: e.tensor_tensor(out=t2v[:, :, :, 0, :], in0=s5[:, :, :, 1, :],
                                                in1=S4[:, :, 0, :].unsqueeze(1).to_broadcast([128, H, 2, c4]), op=ALU.mult), r=rs, w=["t2"])
                    V(lambda e: e.tensor_tensor(out=t2v[:, :, :, 1, :], in0=s5[:, :, :, 0, :],
                                                in1=S4[:, :, 1, :].unsqueeze(1).to_broadcast([128, H, 2, c4]), op=ALU.mult), r=rs, w=["t2"])
                    return t1v, t2[:, 0:H * Dh].rearrange("p (h d) -> p h d", h=H)

                blocks = [list(range(0, NCT))] + [list(range(NCT + 4 * b, NCT + 4 * b + 4)) for b in range(SEQ // 512)]
                for blk in blocks:
                    for bi, ti in enumerate(blk):
                        isc = ti < NCT
                        which = 1 if isc else 0
                        li = ti - NCT
                        xb = xt[ti % 2]; xk = f"xtB{ti % 2}"
                        rpb = rp[ti % 2]; rk = f"rp{ti % 2}"
                        L(lambda e: e.dma_start(out=xb[:, :], in_=xall[ti * 128:(ti + 1) * 128, :]), w=[xk])
                        if not isc:
                            L(lambda e: e.dma_start(out=rpb[:, :], in_=rope[li * 128:(li + 1) * 128, :]), w=[rk])
                        A(lambda e: e.activation(out=junk[:, :], in_=xb[:, :], func=AF.Square, accum_out=st1[:, 0:1]), r=[xk], w=["junkB", "st1"])
                        rstd_from_ss(st1[:, 0:1], st1[:, 1:2], 1.0 / D, ["st1"], ["st1b"])
                        V(lambda e: e.scalar_tensor_tensor(out=hf[:, :], in0=xb[:, :], scalar=st1[:, 1:2], in1=Ar[which][:, :], op0=ALU.mult, op1=ALU.mult),
                          r=[xk, "st1b", f"Ar{which}"], w=["hf"])
                        V(lambda e: e.tensor_tensor(out=hb[:, :], in0=hf[:, :], in1=Br[which][:, :], op=ALU.add), r=["hf", f"Br{which}"], w=["hb"])
                        for kc in range(8):
                            M(lambda e: e.transpose(out=pT[:, kc * 128:(kc + 1) * 128], in_=hb[:, kc * 128:(kc + 1) * 128], identity=ident[:, :]),
                              r=["hb", "ident"], w=["pTB"])
                        A(lambda e: e.copy(out=hT[:, :, :].rearrange("p k t -> p (k t)"), in_=pT[:, :]), r=["pTB"], w=["hT"])
                        for b, (c0, c1) in enumerate(((0, 512), (512, 992), (992, INW))):
                            for kc in range(8):
                                M(lambda e: e.matmul(pP[b][:, 0:c1 - c0], lhsT=hT[:, kc, :], rhs=w_in_b[:, kc, c0:c1], start=(kc == 0), stop=(kc == 7)),
                                  r=["hT", "w_in_b"], w=[f"pP{b}"])

                        for (pp, pk, H, gain, gk, dst, dk) in ((pP[0], "pP0", 8, gA, "gA", qA, "qA"), (pP[2], "pP2", 6, gC, "gC", qC, "qC")):
                            n = H * 64
                            src3 = pp[:, 0:n].rearrange("p (h d) -> p h d", h=H)
                            A(lambda e: e.activation(out=sq[:, 0:n], in_=pp[:, 0:n], func=AF.Square), r=[pk], w=["sq"])
                            V(lambda e: e.tensor_reduce(out=ssh[:, 0:H], in_=sq[:, 0:n].rearrange("p (h d) -> p h d", h=H), axis=AX.X, op=ALU.add), r=["sq"], w=["ssh"])
                            rstd_from_ss(ssh[:, 0:H], ssh[:, 0:H], 1.0 / 64, ["ssh"], ["ssh"])
                            qn3 = qn[:, 0:n].rearrange("p (h d) -> p h d", h=H)
                            V(lambda e: e.tensor_tensor(out=qn3, in0=src3, in1=ssh[:, 0:H].unsqueeze(2).to_broadcast([128, H, 64]), op=ALU.mult), r=[pk, "ssh"], w=["qn"])
                            if isc:
                                V(lambda e: e.tensor_tensor(out=dst[:, :, :], in0=qn3, in1=gain[:, :, :], op=ALU.mult), r=["qn", gk], w=[dk])
                            else:
                                V(lambda e: e.tensor_tensor(out=qn3, in0=qn3, in1=gain[:, :, :], op=ALU.mult), r=["qn", gk], w=["qn"])
                                a1, a2 = rope_apply(qn3, H, 64, rpb[:, 0:64], rpb[:, 64:128], None, ["qn", rk])
                                V(lambda e: e.tensor_tensor(out=dst[:, :, :], in0=a1, in1=a2, op=ALU.add), r=["t1", "t2"], w=[dk])
                        A(lambda e: e.copy(out=vst[:, 0:128], in_=pP[1][:, 0:128]), r=["pP1"], w=["vst"])
                        A(lambda e: e.copy(out=vst[:, 512:640], in_=pP[2][:, 384:512]), r=["pP2"], w=["vst"])

                        A(lambda e: e.activation(out=sq[:, 0:352], in_=pP[1][:, 128:480], func=AF.Square), r=["pP1"], w=["sq"])
                        V(lambda e: e.tensor_reduce(out=ssh[:, 0:1], in_=sq[:, 0:192], axis=AX.X, op=ALU.add), r=["sq"], w=["ssh"])
                        V(lambda e: e.tensor_reduce(out=ssh[:, 1:2], in_=sq[:, 192:320], axis=AX.X, op=ALU.add), r=["sq"], w=["ssh"])
                        V(lambda e: e.tensor_reduce(out=ssh[:, 2:3], in_=sq[:, 320:352], axis=AX.X, op=ALU.add), r=["sq"], w=["ssh"])
                        V(lambda e: e.tensor_tensor(out=ssh[:, 0:3], in0=ssh[:, 0:3], in1=invB[:, :], op=ALU.mult), r=["ssh", "invB"], w=["ssh"])
                        rstd_from_ss(ssh[:, 0:3], ssh[:, 0:3], 1.0, ["ssh"], ["ssh"])
                        V(lambda e: e.tensor_scalar(out=cqn[:, 0:192], in0=pP[1][:, 128:320], scalar1=ssh[:, 0:1], scalar2=None, op0=ALU.mult), r=["pP1", "ssh"], w=["cqn"])
                        V(lambda e: e.tensor_scalar(out=cqn[:, 192:320], in0=pP[1][:, 320:448], scalar1=ssh[:, 1:2], scalar2=None, op0=ALU.mult), r=["pP1", "ssh"], w=["cqn"])
                        V(lambda e: e.scalar_tensor_tensor(out=kpe[:, :], in0=pP[1][:, 448:480], scalar=ssh[:, 2:3], in1=gBkr[:, :], op0=ALU.mult, op1=ALU.mult),
                          r=["pP1", "ssh", "gBkr"], w=["kpe"])
                        if not isc:
                            a1, a2 = rope_apply(kpe[:, :].unsqueeze(1), 1, 32, rpb[:, 128:160], rpb[:, 160:192], None, ["kpe", rk])
                            V(lambda e: e.tensor_tensor(out=kpe2[:, :].unsqueeze(1), in0=a1, in1=a2, op=ALU.add), r=["t1", "t2"], w=["kpe2"])
                            kp = kpe2
                        else:
                            kp = kpe
                        V(lambda e: e.tensor_copy(out=qB[:, 6:12, 64:96], in_=kp[:, :].unsqueeze(1).to_broadcast([128, 6, 32])), r=["kpe", "kpe2"], w=["qBk"])
                        M(lambda e: e.transpose(out=pT[:, 0:128], in_=cqn[:, 0:128], identity=ident[:, :]), r=["cqn", "ident"], w=["pTB"])
                        M(lambda e: e.transpose(out=pT[0:64, 128:256], in_=cqn[:, 128:192], identity=ident[:, :]), r=["cqn", "ident"], w=["pTB"])
                        M(lambda e: e.transpose(out=pT[:, 256:384], in_=cqn[:, 192:320], identity=ident[:, :]), r=["cqn", "ident"], w=["pTB"])
                        A(lambda e: e.copy(out=cT[:, 0, :], in_=pT[:, 0:128]), r=["pTB"], w=["cT"])
                        A(lambda e: e.copy(out=cT[0:64, 1, :], in_=pT[0:64, 128:256]), r=["pTB"], w=["cT"])
                        A(lambda e: e.copy(out=cT[:, 2, :], in_=pT[:, 256:384]), r=["pTB"], w=["cT"])
                        for hf_ in range(2):
                            M(lambda e: e.matmul(pQ[hf_][:, 0:288], lhsT=cT[:, 0, :], rhs=wuq_b[:, 0, hf_ * 288:(hf_ + 1) * 288], start=True, stop=False),
                              r=["cT", "wuq_b"], w=[f"pQ{hf_}"])
                            M(lambda e: e.matmul(pQ[hf_][:, 0:288], lhsT=cT[0:64, 1, :], rhs=wuq_b[0:64, 1, hf_ * 288:(hf_ + 1) * 288], start=False, stop=True),
                              r=["cT", "wuq_b"], w=[f"pQ{hf_}"])
                            M(lambda e: e.matmul(pK[hf_][:, 0:384], lhsT=cT[:, 2, :], rhs=wukv_b[:, hf_ * 384:(hf_ + 1) * 384], start=True, stop=True),
                              r=["cT", "wukv_b"], w=[f"pK{hf_}"])
                        for hf_ in range(2):
                            A(lambda e: e.activation(out=sq[:, hf_ * 288:(hf_ + 1) * 288], in_=pQ[hf_][:, 0:288], func=AF.Square), r=[f"pQ{hf_}"], w=["sq"])
                        sq3 = sq[:, 0:576].rearrange("p (h d) -> p h d", h=6)
                        V(lambda e: e.tensor_reduce(out=ssh[:, 0:6], in_=sq3[:, :, 0:64], axis=AX.X, op=ALU.add), r=["sq"], w=["ssh"])
                        V(lambda e: e.tensor_reduce(out=ssh[:, 6:12], in_=sq3[:, :, 64:96], axis=AX.X, op=ALU.add), r=["sq"], w=["ssh"])
                        V(lambda e: e.tensor_tensor(out=ssh[:, 0:12], in0=ssh[:, 0:12], in1=invQ[:, :], op=ALU.mult), r=["ssh", "invQ"], w=["ssh"])
                        rstd_from_ss(ssh[:, 0:12], ssh[:, 0:12], 1.0, ["ssh"], ["ssh"])
                        qn3 = qn[:, 0:576].rearrange("p (h d) -> p h d", h=6)
                        for hf_ in range(2):
                            p3 = pQ[hf_][:, 0:288].rearrange("p (h d) -> p h d", h=3)
                            hs = slice(hf_ * 3, hf_ * 3 + 3)
                            V(lambda e: e.tensor_tensor(out=qn3[:, hs, 0:64], in0=p3[:, :, 0:64],
                                                        in1=ssh[:, hf_ * 3:hf_ * 3 + 3].unsqueeze(2).to_broadcast([128, 3, 64]), op=ALU.mult), r=[f"pQ{hf_}", "ssh"], w=["qn"])
                            V(lambda e: e.tensor_tensor(out=qn3[:, hs, 64:96], in0=p3[:, :, 64:96],
                                                        in1=ssh[:, 6 + hf_ * 3:6 + hf_ * 3 + 3].unsqueeze(2).to_broadcast([128, 3, 32]), op=ALU.mult), r=[f"pQ{hf_}", "ssh"], w=["qn"])
                        V(lambda e: e.tensor_tensor(out=qn3, in0=qn3, in1=gBq[:, :].unsqueeze(1).to_broadcast([128, 6, 96]), op=ALU.mult), r=["qn", "gBq"], w=["qn"])
                        V(lambda e: e.tensor_copy(out=qB[:, 0:6, 0:64], in_=qn3[:, :, 0:64]), r=["qn"], w=["qBq"])
                        if isc:
                            V(lambda e: e.tensor_copy(out=qB[:, 0:6, 64:96], in_=qn3[:, :, 64:96]), r=["qn"], w=["qBq"])
                        else:
                            a1, a2 = rope_apply(qn3[:, :, 64:96], 6, 32, rpb[:, 128:160], rpb[:, 160:192], None, ["qn", rk])
                            V(lambda e: e.tensor_tensor(out=qB[:, 0:6, 64:96], in0=a1, in1=a2, op=ALU.add), r=["t1", "t2"], w=["qBq"])
                        for hf_ in range(2):
                            A(lambda e: e.activation(out=sq[:, hf_ * 192:(hf_ + 1) * 192].rearrange("p (h d) -> p h d", h=3),
                                                     in_=pK[hf_][:, 0:384].rearrange("p (h d) -> p h d", h=3)[:, :, 0:64], func=AF.Square), r=[f"pK{hf_}"], w=["sq"])
                            A(lambda e: e.copy(out=vst[:, 128 + hf_ * 192:128 + (hf_ + 1) * 192].rearrange("p (h d) -> p h d", h=3),
                                               in_=pK[hf_][:, 0:384].rearrange("p (h d) -> p h d", h=3)[:, :, 64:128]), r=[f"pK{hf_}"], w=["vst"])
                        V(lambda e: e.tensor_reduce(out=ssh[:, 0:6], in_=sq[:, 0:384].rearrange("p (h d) -> p h d", h=6), axis=AX.X, op=ALU.add), r=["sq"], w=["ssh"])
                        rstd_from_ss(ssh[:, 0:6], ssh[:, 0:6], 1.0 / 64, ["ssh"], ["ssh"])
                        kn3 = qn[:, 0:384].rearrange("p (h d) -> p h d", h=6)
                        for hf_ in range(2):
                            p3 = pK[hf_][:, 0:384].rearrange("p (h d) -> p h d", h=3)
                            V(lambda e: e.tensor_tensor(out=kn3[:, hf_ * 3:hf_ * 3 + 3, :], in0=p3[:, :, 0:64],
                                                        in1=ssh[:, hf_ * 3:hf_ * 3 + 3].unsqueeze(2).to_broadcast([128, 3, 64]), op=ALU.mult), r=[f"pK{hf_}", "ssh"], w=["qn"])
                        V(lambda e: e.tensor_tensor(out=qB[:, 6:12, 0:64], in0=kn3, in1=gBk[:, :].unsqueeze(1).to_broadcast([128, 6, 64]), op=ALU.mult), r=["qn", "gBk"], w=["qBk"])

                        S(lambda e: e.dma_start(out=vall[ti * 128:(ti + 1) * 128, :], in_=vst[:, :]), r=["vst"])
                        c0 = bi * 128
                        for i in range(4):
                            M(lambda e: e.transpose(out=pT[:, i * 128:(i + 1) * 128], in_=qA[:, 2 * i:2 * i + 2, :].rearrange("p h d -> p (h d)"), identity=ident[:, :]),
                              r=["qA", "ident"], w=["pTB"])
                        A(lambda e: e.copy(out=stA[:, :, c0:c0 + 128], in_=pT[:, 0:512].rearrange("p (i t) -> p i t", i=4)), r=["pTB"], w=["stA"])
                        for i in range(3):
                            M(lambda e: e.transpose(out=pT[:, i * 128:(i + 1) * 128], in_=qC[:, 2 * i:2 * i + 2, :].rearrange("p h d -> p (h d)"), identity=ident[:, :]),
                              r=["qC", "ident"], w=["pTB"])
                        V(lambda e: e.tensor_copy(out=stC[:, :, c0:c0 + 128], in_=pT[:, 0:384].rearrange("p (i t) -> p i t", i=3)), r=["pTB"], w=["stC"])
                        for rnd in range(2):
                            for i in range(6):
                                M(lambda e: e.transpose(out=pT[0:96, i * 128:(i + 1) * 128], in_=qB[:, rnd * 6 + i, :], identity=ident[:, :]),
                                  r=["qBq", "qBk", "ident"], w=["pTB"])
                            A(lambda e: e.copy(out=stB[0:96, rnd * 6:rnd * 6 + 6, c0:c0 + 128], in_=pT[0:96, 0:768].rearrange("p (i t) -> p i t", i=6)), r=["pTB"], w=["stB"])
                    W = len(blk) * 128
                    t0 = blk[0] * 128
                    S(lambda e: e.dma_start(out=qkT_A[:, t0:t0 + W].rearrange("(i p) t -> p i t", p=128), in_=stA[:, :, 0:W]), r=["stA"])
                    S(lambda e: e.dma_start(out=qkT_C[:, t0:t0 + W].rearrange("(i p) t -> p i t", p=128), in_=stC[:, :, 0:W]), r=["stC"])
                    S(lambda e: e.dma_start(out=qkT_B[:, :, t0:t0 + W].rearrange("j p t -> p j t"), in_=stB[0:96, :, 0:W]), r=["stB"])
            P.barrier()
            if stop_after == ("B", l):
                break

            with ExitStack() as ph:
                kt = [tsb(ph, f"kt{i}", [96, T], BF16) for i in range(2)]
                vb = [tsb(ph, f"vb{i}", [128, NT, 65], BF16) for i in range(2)]
                qt = [tsb(ph, f"qt{i}", [96, 512], BF16) for i in range(2)]
                pt = [tsb(ph, f"pt{i}", [128, 512], BF16) for i in range(3)]
                ob = [tsb(ph, f"ob{i}", [128, 4, 64], BF16) for i in range(2)]
                den = tsb(ph, "den", [128, 8], F32)
                sinkr = tsb(ph, "sinkr", [128, 4], F32)
                sinke = tsb(ph, "sinke", [128, 4], F32)
                pS = [tps(ph, f"pS{i}", [128, 512]) for i in range(3)]
                pO = [tps(ph, f"pO{i}", [128, 512]) for i in range(2)]
                den2 = [tsb(ph, f"den2_{i}", [128, 4], F32) for i in range(2)]
                L(lambda e: e.dma_start(out=sinkr[:, :], in_=c_sink[l:l + 1, :].broadcast_to([128, 4])), w=["sinkr"])
                A(lambda e: e.activation(out=sinke[:, :], in_=sinkr[:, :], func=AF.Exp), r=["sinkr"], w=["sinke"])
                for i in range(2):
                    G(lambda e: e.memset(vb[i][:, :, 64:65], 1.0), w=[f"vb{i}"])
                cnt = {"kv": 0, "q": 0, "s": 0, "p": 0, "u": 0}
                mixv = mix.rearrange("(j p) c -> p j c", p=128)
                vallv = vall.rearrange("(j p) c -> p j c", p=128)
                pending = []

                def emit_tail(st):
                    (si, pi, Wq, scale, msk, subs, u, n, nkeys, vbuf, vk, j, sink_h, out_fn) = st
                    ui = u % 2
                    A(lambda e: e.activation(out=pt[pi][:, 0:Wq], in_=pS[si][:, 0:Wq], func=AF.Exp, scale=scale), r=[f"pS{si}"], w=[f"pt{pi}"])
                    if msk is not None:
                        V(lambda e: e.tensor_tensor(out=pt[pi][:, 0:Wq].rearrange("p (s t) -> p s t", s=subs), in0=pt[pi][:, 0:Wq].rearrange("p (s t) -> p s t", s=subs),
                                                    in1=msk[:, :].unsqueeze(1).to_broadcast([128, subs, 128]), op=ALU.mult), r=[f"pt{pi}"], w=[f"pt{pi}"])
                    for s_ in range(subs):
                        M(lambda e: e.matmul(pO[ui][:, s_ * 65:(s_ + 1) * 65], lhsT=pt[pi][:, s_ * 128:(s_ + 1) * 128], rhs=vbuf[:, j, :],
                                             start=(n == 0 and s_ == 0), stop=(n == nkeys - 1), skip_group_check=True),
                          r=[f"pt{pi}", vk], w=[f"pO{ui}"])
                    if n == nkeys - 1:
                        pov = pO[ui][:, 0:subs * 65].rearrange("p (s c) -> p s c", s=subs)
                        dn = den2[ui]
                        if sink_h is not None:
                            V(lambda e: e.tensor_tensor(out=dn[:, 0:subs].unsqueeze(2), in0=pov[:, :, 64:65], in1=sinke[:, sink_h:sink_h + subs].unsqueeze(2), op=ALU.add),
                              r=[f"pO{ui}", "sinke"], w=[f"den2_{ui}"])
                            V(lambda e: e.reciprocal(out=dn[:, 0:subs], in_=dn[:, 0:subs]), r=[f"den2_{ui}"], w=[f"den2_{ui}"])
                        else:
                            V(lambda e: e.reciprocal(out=dn[:, 0:subs].unsqueeze(2), in_=pov[:, :, 64:65]), r=[f"pO{ui}"], w=[f"den2_{ui}"])
                        V(lambda e: e.tensor_tensor(out=ob[ui][:, 0:subs, :], in0=pov[:, :, 0:64], in1=dn[:, 0:subs].unsqueeze(2).to_broadcast([128, subs, 64]), op=ALU.mult),
                          r=[f"pO{ui}", f"den2_{ui}"], w=[f"ob{ui}"])
                        out_fn(ob[ui], f"ob{ui}")

                def attn_unit(qsrc_fn, dq, Wq, keys, kb, kk, vbuf, vk, scale, subs, sink_h, out_fn):
                    qi = cnt["q"] % 2; cnt["q"] += 1
                    u = cnt["u"]; cnt["u"] += 1
                    qsrc_fn(qt[qi], f"qt{qi}")
                    for n, (j, msk) in enumerate(keys):
                        si = cnt["s"] % 3; cnt["s"] += 1
                        pi = cnt["p"] % 3; cnt["p"] += 1
                        M(lambda e: e.matmul(pS[si][:, 0:Wq], lhsT=kb[0:dq, j * 128:(j + 1) * 128], rhs=qt[qi][0:dq, 0:Wq], start=True, stop=True),
                          r=[kk, f"qt{qi}"], w=[f"pS{si}"])
                        pending.append((si, pi, Wq, scale, msk, subs, u, n, len(keys), vbuf, vk, j, sink_h, out_fn))
                        if len(pending) > 1:
                            emit_tail(pending.pop(0))

                def load_kv(krows_ap, dq, voff):
                    i = cnt["kv"] % 2; cnt["kv"] += 1
                    L(lambda e: e.dma_start(out=kt[i][0:dq, :], in_=krows_ap), w=[f"kt{i}"])
                    for hh in range(2):
                        L(lambda e: e.dma_start(out=vb[i][:, hh * 17:(hh + 1) * 17, 0:64], in_=vallv[:, hh * 17:(hh + 1) * 17, voff:voff + 64]), w=[f"vb{i}"])
                    return kt[i], f"kt{i}", vb[i], f"vb{i}"

                qblocks = ([(0, NCT)] if not last else []) + [(NCT + 4 * b, 4) for b in range(SEQ // 512)]
                for mixer in ("A", "B"):
                    nkv = 2 if mixer == "A" else 6
                    for kv in range(nkv):
                        if mixer == "A":
                            kb, kk, vbuf, vk = load_kv(qkT_A[(6 + kv) * 64:(7 + kv) * 64, :], 64, kv * 64)
                            heads = [3 * kv + g for g in range(3)]; dq = 64; scale = 64 ** -0.5
                        else:
                            kb, kk, vbuf, vk = load_kv(qkT_B[6 + kv, :, :], 96, 128 + kv * 64)
                            heads = [kv]; dq = 96; scale = 96 ** -0.5
                        for h in heads:
                            for (tb, nsub) in qblocks:
                                keys = [(j, None) for j in (range(NCT) if tb < NCT else range(NT))]
                                Wq = nsub * 128
                                if mixer == "A":
                                    qsrc = lambda dst, dk, h=h, tb=tb, Wq=Wq: L(lambda e: e.dma_start(out=dst[0:64, 0:Wq], in_=qkT_A[h * 64:(h + 1) * 64, tb * 128:tb * 128 + Wq]), w=[dk])
                                    col = h * 64
                                else:
                                    qsrc = lambda dst, dk, h=h, tb=tb, Wq=Wq: L(lambda e: e.dma_start(out=dst[0:96, 0:Wq], in_=qkT_B[h, :, tb * 128:tb * 128 + Wq]), w=[dk])
                                    col = 384 + h * 64
                                outf = lambda o, ok, tb=tb, nsub=nsub, col=col: S(lambda e: e.dma_start(out=mixv[:, tb:tb + nsub, col:col + 64], in_=o[:, 0:nsub, :]), r=[ok])
                                attn_unit(qsrc, dq, Wq, keys, kb, kk, vbuf, vk, scale, nsub, None, outf)
                for kv in range(2):
                    kb, kk, vbuf, vk = load_kv(qkT_C[(4 + kv) * 64:(5 + kv) * 64, :], 64, 512 + kv * 64)
                    for ti in (range(NT) if not last else range(NCT, NT)):
                        if ti < NCT:
                            keys = [(j, None) for j in range(NCT)]
                        else:
                            keys = [(j, None) for j in range(NCT)]
                            if ti - 1 >= NCT:
                                keys.append((ti - 1, M1))
                            keys.append((ti, None))
                            if ti + 1 < NT:
                                keys.append((ti + 1, M2))

                        def qsrc(dst, dk, kv=kv, ti=ti):
                            L(lambda e: e.dma_start(out=dst[0:64, 0:256].rearrange("p (h t) -> p h t", h=2),
                                                    in_=qkT_C[2 * kv * 64:(2 * kv + 2) * 64, ti * 128:(ti + 1) * 128].rearrange("(h p) t -> p h t", p=64)), w=[dk])
                        col = 768 + 2 * kv * 64
                        outf = lambda o, ok, ti=ti, col=col: S(lambda e: e.dma_start(out=mix[ti * 128:(ti + 1) * 128, col:col + 128], in_=o[:, 0:2, :].rearrange("p s d -> p (s d)")), r=[ok])
                        attn_unit(qsrc, 64, 256, keys, kb, kk, vbuf, vk, 64 ** -0.5, 2, 2 * kv, outf)
                while pending:
                    emit_tail(pending.pop(0))
            P.barrier()
            if stop_after == ("C", l):
                break

            with ExitStack() as ph:
                wst = tsb(ph, "wstD", [128, 8, 512], F32)
                w_out_b = tsb(ph, "w_out_b", [128, 8, D], BF16)
                G1 = [tsb(ph, f"G1_{i}", [128, D], F32) for i in range(2)]
                xt = [tsb(ph, f"xtD{i}", [128, D], F32) for i in range(2)]
                mt = [tsb(ph, f"mt{i}", [128, D], BF16) for i in range(2)]
                mT = tsb(ph, "mT", [128, 8, 128], BF16)
                yt = [tsb(ph, f"yt{i}", [128, D], F32) for i in range(2)]
                pT = tps(ph, "pTD", [128, 1024], BF16)
                pY = [tps(ph, f"pY{i}", [128, 512]) for i in range(2)]
                for b in range(2):
                    for h in range(2):
                        L(lambda e: e.dma_start(out=wst[:, h * 4:(h + 1) * 4, :],
                                                in_=w_out[l, h * 512:(h + 1) * 512, b * 512:(b + 1) * 512].rearrange("(k p) n -> p k n", p=128)), w=["wstD"])
                    V(lambda e: e.tensor_copy(out=w_out_b[:, 0:4, b * 512:(b + 1) * 512], in_=wst[:, 0:4, :]), r=["wstD"], w=["w_out_b"])
                    G(lambda e: e.tensor_copy(out=w_out_b[:, 4:8, b * 512:(b + 1) * 512], in_=wst[:, 4:8, :]), r=["wstD"], w=["w_out_b"])
                for which in range(2):
                    load_bcast(G1[which][:, :], which, 2, f"G1_{which}")
                for ti in tiles_act:
                    which = 1 if ti < NCT else 0
                    i2 = ti % 2
                    L(lambda e: e.dma_start(out=xt[i2][:, :], in_=xall[ti * 128:(ti + 1) * 128, :]), w=[f"xtD{i2}"])
                    L(lambda e: e.dma_start(out=mt[i2][:, :], in_=mix[ti * 128:(ti + 1) * 128, :]), w=[f"mt{i2}"])
                    for kc in range(8):
                        M(lambda e: e.transpose(out=pT[:, kc * 128:(kc + 1) * 128], in_=mt[i2][:, kc * 128:(kc + 1) * 128], identity=ident[:, :]), r=[f"mt{i2}", "ident"], w=["pTD"])
                    A(lambda e: e.copy(out=mT[:, :, :].rearrange("p k t -> p (k t)"), in_=pT[:, :]), r=["pTD"], w=["mT"])
                    for b in range(2):
                        for kc in range(8):
                            M(lambda e: e.matmul(pY[b][:, :], lhsT=mT[:, kc, :], rhs=w_out_b[:, kc, b * 512:(b + 1) * 512], start=(kc == 0), stop=(kc == 7)), r=["mT", "w_out_b"], w=[f"pY{b}"])
                        V(lambda e: e.tensor_tensor(out=yt[i2][:, b * 512:(b + 1) * 512], in0=pY[b][:, :], in1=G1[which][:, b * 512:(b + 1) * 512], op=ALU.mult),
                          r=[f"pY{b}", f"G1_{which}"], w=[f"yt{i2}"])
                    V(lambda e: e.tensor_tensor(out=yt[i2][:, :], in0=yt[i2][:, :], in1=xt[i2][:, :], op=ALU.add), r=[f"yt{i2}", f"xtD{i2}"], w=[f"yt{i2}"])
                    S(lambda e: e.dma_start(out=xall[ti * 128:(ti + 1) * 128, :], in_=yt[i2][:, :]), r=[f"yt{i2}"])
            P.barrier()
            if stop_after == ("D", l):
                break

            with ExitStack() as ph:
                aff = tsb(ph, "aff", [128, NT, NE], F32)
                gm = tsb(ph, "gm", [128, NT, NE], F32)
                sloti = tsb(ph, "sloti", [128, NT, NE], I32)
                slotc = tsb(ph, "slotc", [128, NT, NE], I32)
                G2 = [tsb(ph, f"G2_{i}", [128, D], F32) for i in range(2)]
                p13 = ExitStack()
                affT = tsb(p13, "affT", [NE, T], F32)
                maskT = tsb(p13, "maskT", [NE, T], BF16)
                with ExitStack() as p1:
                    wr_f = tsb(p1, "wr_f", [128, 8, NE], F32)
                    wr_b = tsb(p1, "wr_b", [128, 8, NE], BF16)
                    Ar = [tsb(p1, f"Ar2_{i}", [128, D], F32) for i in range(2)]
                    Br = [tsb(p1, f"Br2_{i}", [128, D], F32) for i in range(2)]
                    xt = [tsb(p1, f"xtE{i}", [128, D], F32) for i in range(2)]
                    junk = tsb(p1, "junkE", [128, D], BF16)
                    st1 = tsb(p1, "st1E", [128, 8], F32)
                    hf = tsb(p1, "hfE", [128, D], F32)
                    hb = [tsb(p1, f"hbE{i}", [128, D], BF16) for i in range(2)]
                    hT = tsb(p1, "hTE", [128, 8, 128], BF16)
                    ex = tsb(p1, "ex", [128, NE], F32)
                    pT = tps(p1, "pTE", [128, 1024], BF16)
                    pR = tps(p1, "pR", [128, 512])
                    pAT = tps(p1, "pAT", [128, 512])
                    L(lambda e: e.dma_start(out=wr_f[:, :, :], in_=w_router[l, :, :].rearrange("(k p) n -> p k n", p=128)), w=["wr_f"])
                    V(lambda e: e.tensor_copy(out=wr_b[:, :, :], in_=wr_f[:, :, :]), r=["wr_f"], w=["wr_b"])
                    for which in range(2):
                        load_bcast(Ar[which][:, :], which, 4, f"Ar2_{which}")
                        load_bcast(Br[which][:, :], which, 3, f"Br2_{which}")
                        load_bcast(G2[which][:, :], which, 5, f"G2_{which}")
                    for ti in tiles_act:
                        which = 1 if ti < NCT else 0
                        i2 = ti % 2
                        L(lambda e: e.dma_start(out=xt[i2][:, :], in_=xall[ti * 128:(ti + 1) * 128, :]), w=[f"xtE{i2}"])
                        A(lambda e: e.activation(out=junk[:, :], in_=xt[i2][:, :], func=AF.Square, accum_out=st1[:, 0:1]), r=[f"xtE{i2}"], w=["junkE", "st1E"])
                        rstd_from_ss(st1[:, 0:1], st1[:, 1:2], 1.0 / D, ["st1E"], ["st1Eb"])
                        V(lambda e: e.scalar_tensor_tensor(out=hf[:, :], in0=xt[i2][:, :], scalar=st1[:, 1:2], in1=Ar[which][:, :], op0=ALU.mult, op1=ALU.mult),
                          r=[f"xtE{i2}", "st1Eb", f"Ar2_{which}"], w=["hfE"])
                        V(lambda e: e.tensor_tensor(out=hb[i2][:, :], in0=hf[:, :], in1=Br[which][:, :], op=ALU.add), r=["hfE", f"Br2_{which}"], w=[f"hbE{i2}"])
                        S(lambda e: e.dma_start(out=h2d[ti * 128:(ti + 1) * 128, :], in_=hb[i2][:, :]), r=[f"hbE{i2}"])
                        for kc in range(8):
                            M(lambda e: e.transpose(out=pT[:, kc * 128:(kc + 1) * 128], in_=hb[i2][:, kc * 128:(kc + 1) * 128], identity=ident[:, :]), r=[f"hbE{i2}", "ident"], w=["pTE"])
                        A(lambda e: e.copy(out=hT[:, :, :].rearrange("p k t -> p (k t)"), in_=pT[:, :]), r=["pTE"], w=["hTE"])
                        for kc in range(8):
                            M(lambda e: e.matmul(pR[:, 0:NE], lhsT=hT[:, kc, :], rhs=wr_b[:, kc, :], start=(kc == 0), stop=(kc == 7)), r=["hTE", "wr_b"], w=["pR"])
                        V(lambda e: e.tensor_reduce(out=st1[:, 2:3], in_=pR[:, 0:NE], axis=AX.X, op=ALU.max, negate=True), r=["pR"], w=["st1Ec"])
                        A(lambda e: e.activation(out=ex[:, :], in_=pR[:, 0:NE], func=AF.Exp, bias=st1[:, 2:3], accum_out=st1[:, 3:4]), r=["pR", "st1Ec"], w=["ex", "st1Ed"])
                        V(lambda e: e.reciprocal(out=st1[:, 4:5], in_=st1[:, 3:4]), r=["st1Ed"], w=["st1Ee"])
                        V(lambda e: e.tensor_scalar(out=aff[:, ti, :], in0=ex[:, :], scalar1=st1[:, 4:5], scalar2=None, op0=ALU.mult), r=["ex", "st1Ee"], w=["aff"])
                        M(lambda e: e.transpose(out=pAT[0:NE, 0:128], in_=aff[:, ti, :], identity=identf[:, :]), r=["aff", "identf"], w=["pAT"])
                        A(lambda e: e.copy(out=affT[:, ti * 128:(ti + 1) * 128], in_=pAT[0:NE, 0:128]), r=["pAT"], w=["affT"])
                P.barrier()
                with ExitStack() as p2:
                    work = tsb(p2, "work", [NE, SEQ], F32)
                    mx = tsb(p2, "mx", [NE, 8], F32)
                    sets = [(NCT * 128, T, CAP_L)] + ([(0, NCT * 128, CAP_C)] if not last else [])
                    for (a0, a1, cap) in sets:
                        n = a1 - a0
                        V(lambda e: e.tensor_copy(out=work[:, 0:n], in_=affT[:, a0:a1]), r=["affT"], w=["work"])
                        for r_ in range(cap // 8):
                            V(lambda e: e.max(out=mx[:, :], in_=work[:, 0:n]), r=["work"], w=["mx"])
                            if r_ < cap // 8 - 1:
                                V(lambda e: e.match_replace(out=work[:, 0:n], in_to_replace=mx[:, :], in_values=work[:, 0:n], imm_value=-1.0), r=["work", "mx"], w=["work"])
                        V(lambda e: e.tensor_scalar(out=maskT[:, a0:a1], in0=affT[:, a0:a1], scalar1=mx[:, 7:8], scalar2=None, op0=ALU.is_ge), r=["affT", "mx"], w=["maskT"])
                P.barrier()
                with ExitStack() as p3:
                    mk = tsb(p3, "mk", [128, NE], F32)
                    mkb = tsb(p3, "mkb", [128, NE], BF16)
                    Sf = tsb(p3, "Sf", [128, NE], F32)
                    Sb = tsb(p3, "Sb", [128, NE], BF16)
                    sl = tsb(p3, "sl", [128, NE], F32)
                    hb = [tsb(p3, f"hbS{i}", [128, D], BF16) for i in range(2)]
                    pM = tps(p3, "pM", [128, 1024], BF16)
                    pPos = tps(p3, "pPos", [128, 512])
                    for ti in tiles_act:
                        isc = ti < NCT
                        i2 = ti % 2
                        if ti == 0 or ti == NCT:
                            V(lambda e: e.memset(Sf[:, :], 0.0), w=["Sf"])
                            V(lambda e: e.tensor_copy(out=Sb[:, :], in_=Sf[:, :]), r=["Sf"], w=["Sb"])
                        L(lambda e: e.dma_start(out=hb[i2][:, :], in_=h2d[ti * 128:(ti + 1) * 128, :]), w=[f"hbS{i2}"])
                        M(lambda e: e.transpose(out=pM[:, 0:NE], in_=maskT[:, ti * 128:(ti + 1) * 128], identity=ident[0:NE, 0:NE]), r=["maskT", "ident"], w=["pM"])
                        V(lambda e: e.tensor_copy(out=mk[:, :], in_=pM[:, 0:NE]), r=["pM"], w=["mk"])
                        A(lambda e: e.copy(out=mkb[:, :], in_=pM[:, 0:NE]), r=["pM"], w=["mkb"])
                        M(lambda e: e.matmul(pPos[:, 0:NE], lhsT=Ub[:, :], rhs=mkb[:, :], start=True, stop=False), r=["Ub", "mkb"], w=["pPos"])
                        M(lambda e: e.matmul(pPos[:, 0:NE], lhsT=ones_b[:, :], rhs=Sb[:, :], start=False, stop=True), r=["ones_b", "Sb"], w=["pPos"])
                        V(lambda e: e.tensor_scalar(out=sl[:, :], in0=mk[:, :], scalar1=-BIG, scalar2=BIG, op0=ALU.mult, op1=ALU.add), r=["mk"], w=["sl"])
                        V(lambda e: e.tensor_tensor(out=sl[:, :], in0=pPos[:, 0:NE], in1=sl[:, :], op=ALU.add), r=["pPos", "sl"], w=["sl"])
                        V(lambda e: e.tensor_copy(out=sloti[:, ti, :], in_=sl[:, :]), r=["sl"], w=["sloti"])
                        V(lambda e: e.tensor_scalar(out=sl[:, :], in0=sl[:, :], scalar1=float((CAP_C if isc else CAP_L) - 1), scalar2=None, op0=ALU.min), r=["sl"], w=["sl"])
                        V(lambda e: e.tensor_copy(out=slotc[:, ti, :], in_=sl[:, :]), r=["sl"], w=["slotc"])
                        V(lambda e: e.tensor_tensor(out=gm[:, ti, :], in0=aff[:, ti, :], in1=mk[:, :], op=ALU.mult), r=["aff", "mk"], w=["gm"])
                        V(lambda e: e.tensor_tensor(out=Sf[:, :], in0=Sf[:, :], in1=mk[:, :], op=ALU.add), r=["Sf", "mk"], w=["Sf"])
                        V(lambda e: e.tensor_copy(out=Sb[:, :], in_=Sf[:, :]), r=["Sf"], w=["Sb"])
                        for ex_ in range(NE):
                            dst = xe_c[ex_] if isc else xe_l[ex_]
                            bnd = (CAP_C if isc else CAP_L) - 1
                            S(lambda e: e.indirect_dma_start(out=dst, out_offset=bass.IndirectOffsetOnAxis(ap=sloti[:, ti, ex_:ex_ + 1], axis=0),
                                                             in_=hb[i2][:, :], in_offset=None, bounds_check=REG[bnd], oob_is_err=False),
                              r=[f"hbS{i2}", "sloti"])
                    if "dbg_aff" in dbg:
                        S(lambda e: e.dma_start(out=dbg_aff, in_=aff[:, :, :].rearrange("p t e -> p (t e)")), r=["aff"])
                        S(lambda e: e.dma_start(out=dbg_slot, in_=sloti[:, :, :].rearrange("p t e -> p (t e)")), r=["sloti"])
                P.barrier()
                p13.close()
                if stop_after == ("E3", l):
                    break
                with ExitStack() as p4:
                    NJ = CAP_L + (CAP_C if not last else 0)
                    stg = [tsb(p4, f"stg{i}", [128, 8, D], F32) for i in range(2)]
                    wb = [tsb(p4, f"wb{i}", [128, 8, D], BF16) for i in range(4)]
                    xet = [tsb(p4, f"xet{i}", [128, D], BF16) for i in range(2)]
                    xeT = [tsb(p4, f"xeT{i}", [128, 8, CAP_L + CAP_C], BF16) for i in range(2)]
                    hidT = tsb(p4, "hidT", [128, 8, CAP_L + CAP_C], BF16)
                    sg = [tsb(p4, f"sg{i}", [128, CAP_L + CAP_C], F32) for i in range(2)]
                    yeb = [tsb(p4, f"yeb{i}", [128, D], BF16) for i in range(2)]
                    pT = tps(p4, "pT4", [128, 1024], BF16)
                    pG = tps(p4, "pG", [128, 512]); pU = tps(p4, "pU", [128, 512])
                    pGc = tps(p4, "pGc", [128, 512])
                    pUc = tps(p4, "pUc", [128, 512])
                    pY = [tps(p4, f"pY4_{i}", [128, 512]) for i in range(2)]
                    cnt4 = {"w": 0, "s": 0, "x": 0, "g": 0, "y": 0}

                    def load_w(src):
                        si = cnt4["s"] % 2; cnt4["s"] += 1
                        wi = cnt4["w"] % 4; cnt4["w"] += 1
                        for h in range(4):
                            L(lambda e: e.dma_start(out=stg[si][:, 2 * h:2 * h + 2, :], in_=src[h * 256:(h + 1) * 256, :].rearrange("(k p) n -> p k n", p=128)), w=[f"stg{si}h{h}"])
                        V(lambda e: e.tensor_copy(out=wb[wi][:, 0:2, :], in_=stg[si][:, 0:2, :]), r=[f"stg{si}h0"], w=[f"wb{wi}a"])
                        A(lambda e: e.copy(out=wb[wi][:, 2:4, :], in_=stg[si][:, 2:4, :]), r=[f"stg{si}h1"], w=[f"wb{wi}b"])
                        V(lambda e: e.tensor_copy(out=wb[wi][:, 4:5, :], in_=stg[si][:, 4:5, :]), r=[f"stg{si}h2"], w=[f"wb{wi}c"])
                        A(lambda e: e.copy(out=wb[wi][:, 5:6, :], in_=stg[si][:, 5:6, :]), r=[f"stg{si}h2"], w=[f"wb{wi}d"])
                        G(lambda e: e.tensor_copy(out=wb[wi][:, 6:8, :], in_=stg[si][:, 6:8, :]), r=[f"stg{si}h3"], w=[f"wb{wi}e"])
                        return wb[wi], [f"wb{wi}{c}" for c in "abcde"]

                    def stage_x(ex_):
                        xT = xeT[ex_ % 2]; xk = f"xeT{ex_ % 2}"
                        jts = [(xe_l[ex_][j * 128:(j + 1) * 128, :], 128, j * 128) for j in range(4)]
                        if not last:
                            jts.append((xe_c[ex_][:, :], CAP_C, CAP_L))
                        for (src, nj, c0) in jts:
                            xi = cnt4["x"] % 2; cnt4["x"] += 1
                            L(lambda e: e.dma_start(out=xet[xi][0:nj, :], in_=src), w=[f"xet{xi}"])
                            for kc in range(8):
                                M(lambda e: e.transpose(out=pT[:, kc * 128:kc * 128 + nj], in_=xet[xi][0:nj, kc * 128:(kc + 1) * 128], identity=ident[0:nj, 0:nj]),
                                  r=[f"xet{xi}", "ident"], w=["pT4"])
                            V(lambda e: e.tensor_copy(out=xT[:, :, c0:c0 + nj], in_=pT[:, :].rearrange("p (k t) -> p k t", k=8)[:, :, 0:nj]), r=["pT4"], w=[xk])

                    def stage_hidden(ex_, wg, wgk, wu, wuk):
                        xT = xeT[ex_ % 2]; xk = f"xeT{ex_ % 2}"
                        for fc in range(8):
                            for kc in range(8):
                                M(lambda e: e.matmul(pG[:, :], lhsT=wg[:, kc, fc * 128:(fc + 1) * 128], rhs=xT[:, kc, 0:CAP_L], start=(kc == 0), stop=(kc == 7)), r=wgk + [xk], w=["pG"])
                            for kc in range(8):
                                M(lambda e: e.matmul(pU[:, :], lhsT=wu[:, kc, fc * 128:(fc + 1) * 128], rhs=xT[:, kc, 0:CAP_L], start=(kc == 0), stop=(kc == 7)), r=wuk + [xk], w=["pU"])
                            gi = cnt4["g"] % 2; cnt4["g"] += 1
                            A(lambda e: e.activation(out=sg[gi][:, 0:CAP_L], in_=pG[:, :], func=AF.Silu), r=["pG"], w=[f"sg{gi}"])
                            V(lambda e: e.tensor_tensor(out=hidT[:, fc, 0:CAP_L], in0=pU[:, :], in1=sg[gi][:, 0:CAP_L], op=ALU.mult), r=["pU", f"sg{gi}"], w=["hidT"])
                            if not last:
                                for kc in range(8):
                                    M(lambda e: e.matmul(pGc[:, 0:CAP_C], lhsT=wg[:, kc, fc * 128:(fc + 1) * 128], rhs=xT[:, kc, CAP_L:NJ], start=(kc == 0), stop=(kc == 7)), r=wgk + [xk], w=["pGc0"])
                                for kc in range(8):
                                    M(lambda e: e.matmul(pUc[:, 0:CAP_C], lhsT=wu[:, kc, fc * 128:(fc + 1) * 128], rhs=xT[:, kc, CAP_L:NJ], start=(kc == 0), stop=(kc == 7)), r=wuk + [xk], w=["pGc1"])
                                A(lambda e: e.activation(out=sg[gi][:, CAP_L:NJ], in_=pGc[:, 0:CAP_C], func=AF.Silu), r=["pGc0"], w=[f"sg{gi}c"])
                                V(lambda e: e.tensor_tensor(out=hidT[:, fc, CAP_L:NJ], in0=pUc[:, 0:CAP_C], in1=sg[gi][:, CAP_L:NJ], op=ALU.mult), r=["pGc1", f"sg{gi}c"], w=["hidTc"])

                    def stage_down(ex_, wd, wdk):
                        outs = [(ye_l[ex_][j * 128:(j + 1) * 128, :], 128, j * 128) for j in range(4)]
                        if not last:
                            outs.append((ye_c[ex_][:, :], CAP_C, CAP_L))
                        for (dst, nj, c0) in outs:
                            yi = cnt4["y"] % 2; cnt4["y"] += 1
                            for dh in range(2):
                                for fc in range(8):
                                    M(lambda e: e.matmul(pY[dh][0:nj, :], lhsT=hidT[:, fc, c0:c0 + nj], rhs=wd[:, fc, dh * 512:(dh + 1) * 512], start=(fc == 0), stop=(fc == 7)),
                                      r=["hidT", "hidTc"] + wdk, w=[f"pY4_{dh}"])
                            A(lambda e: e.copy(out=yeb[yi][0:nj, 0:512], in_=pY[0][0:nj, :]), r=["pY4_0"], w=[f"yeb{yi}a"])
                            V(lambda e: e.tensor_copy(out=yeb[yi][0:nj, 512:1024], in_=pY[1][0:nj, :]), r=["pY4_1"], w=[f"yeb{yi}b"])
                            S(lambda e: e.dma_start(out=dst, in_=yeb[yi][0:nj, :]), r=[f"yeb{yi}a", f"yeb{yi}b"])

                    wg, wgk = load_w(w_e_gate[l, 0])
                    wu, wuk = load_w(w_e_up[l, 0])
                    stage_x(0)
                    for ex_ in range(NE):
                        wd, wdk = load_w(w_e_down[l, ex_])
                        stage_hidden(ex_, wg, wgk, wu, wuk)
                        if ex_ + 1 < NE:
                            wg, wgk = load_w(w_e_gate[l, ex_ + 1])
                            wu, wuk = load_w(w_e_up[l, ex_ + 1])
                            stage_x(ex_ + 1)
                        stage_down(ex_, wd, wdk)
                P.barrier()
                if stop_after == ("E4", l):
                    break
                with ExitStack() as p5:
                    xt = [tsb(p5, f"xt5_{i}", [128, D], F32) for i in range(2)]
                    gt = [tsb(p5, f"gt{i}", [128, D], BF16) for i in range(4)]
                    acc = [tsb(p5, f"acc{i}", [128, D], F32) for i in range(2)]
                    for i in range(4):
                        V(lambda e: e.memset(gt[i][:, :], 0.0), w=[f"gt{i}"])
                    gc = 0
                    for ti in tiles_act:
                        isc = ti < NCT
                        which = 1 if isc else 0
                        i2 = ti % 2
                        L(lambda e: e.dma_start(out=xt[i2][:, :], in_=xall[ti * 128:(ti + 1) * 128, :]), w=[f"xt5_{i2}"])
                        for ex_ in range(NE):
                            gi = gc % 4; gc += 1
                            src = ye_c[ex_] if isc else ye_l[ex_]
                            S(lambda e: e.indirect_dma_start(out=gt[gi][:, :], out_offset=None, in_=src,
                                                             in_offset=bass.IndirectOffsetOnAxis(ap=slotc[:, ti, ex_:ex_ + 1], axis=0),
                                                             bounds_check=REG[(CAP_C if isc else CAP_L) - 1], oob_is_err=False),
                              r=["slotc"], w=[f"gt{gi}"])
                            if ex_ == 0:
                                V(lambda e: e.tensor_scalar(out=acc[i2][:, :], in0=gt[gi][:, :], scalar1=gm[:, ti, ex_:ex_ + 1], scalar2=None, op0=ALU.mult), r=[f"gt{gi}", "gm"], w=[f"acc{i2}"])
                            else:
                                V(lambda e: e.scalar_tensor_tensor(out=acc[i2][:, :], in0=gt[gi][:, :], scalar=gm[:, ti, ex_:ex_ + 1], in1=acc[i2][:, :], op0=ALU.mult, op1=ALU.add),
                                  r=[f"gt{gi}", "gm"], w=[f"acc{i2}"])
                        V(lambda e: e.tensor_tensor(out=acc[i2][:, :], in0=acc[i2][:, :], in1=G2[which][:, :], op=ALU.mult), r=[f"acc{i2}", f"G2_{which}"], w=[f"acc{i2}"])
                        V(lambda e: e.tensor_tensor(out=acc[i2][:, :], in0=acc[i2][:, :], in1=xt[i2][:, :], op=ALU.add), r=[f"acc{i2}", f"xt5_{i2}"], w=[f"acc{i2}"])
                        if last:
                            S(lambda e: e.dma_start(out=y_out[(ti - NCT) * 128:(ti - NCT + 1) * 128, :], in_=acc[i2][:, :]), r=[f"acc{i2}"])
                        else:
                            S(lambda e: e.dma_start(out=xall[ti * 128:(ti + 1) * 128, :], in_=acc[i2][:, :]), r=[f"acc{i2}"])
            P.barrier()
            if stop_after == ("L", l):
                break
        P.barrier()
        print("instr counts", P.ninstr, "total", P.total, flush=True)
    return nc


def _rope_table():
    t = np.arange(SEQ)
    row = (t // 64).astype(np.float64)
    col = (t % 64).astype(np.float64)
    out = np.zeros((SEQ, 192), np.float64)

    def fill(off, dim):
        ad = dim // 2
        inv = 10000.0 ** (-np.arange(0, ad, 2, dtype=np.float64) / ad)
        ar = row[:, None] * inv[None, :]
        ac = col[:, None] * inv[None, :]
        C = np.concatenate([np.cos(ar), np.cos(ar), np.cos(ac), np.cos(ac)], axis=1)
        S_ = np.concatenate([-np.sin(ar), np.sin(ar), -np.sin(ac), np.sin(ac)], axis=1)
        out[:, off:off + dim] = C
        out[:, off + dim:off + 2 * dim] = S_
    fill(0, 64)
    fill(128, 32)
    return out.astype(np.float32)


_NC_CACHE = {}


def kernel(**inputs):
    key = "full"
    if key not in _NC_CACHE:
        _NC_CACHE[key] = build()
    nc = _NC_CACHE[key]
    rope = _rope_table()
    shared = {k: np.ascontiguousarray(np.asarray(v, dtype=np.float32)) for k, v in inputs.items() if k not in ("x", "c", "ctx")}
    x = np.asarray(inputs["x"], dtype=np.float32); c = np.asarray(inputs["c"], dtype=np.float32); ctx = np.asarray(inputs["ctx"], dtype=np.float32)
    in_maps = []
    for b in range(8):
        m = dict(shared)
        m["x"] = np.ascontiguousarray(x[b]); m["ctx"] = np.ascontiguousarray(ctx[b]); m["c"] = np.ascontiguousarray(c[b])
        m["rope"] = rope
        in_maps.append(m)
    res = run_bass_kernel_spmd(nc, in_maps, core_ids=list(range(8)))
    return np.stack([np.asarray(r["y"], dtype=np.float32) for r in res.results], axis=0)
```

```python
import numpy as np
from contextlib import ExitStack
import concourse.bass as bass
import concourse.mybir as mybir
from concourse.bass_utils import run_bass_kernel_spmd

F32 = mybir.dt.float32
BF16 = mybir.dt.bfloat16
I32 = mybir.dt.int32
AF = mybir.ActivationFunctionType
ALU = mybir.AluOpType
AX = mybir.AxisListType

D = 1024
SEQ = 4096
CTX = 256
T = SEQ + CTX
NT = T // 128
NCT = CTX // 128
DEPTH = 2
INW = 1504
NE = 16
CAP_L = 512
CAP_C = 32
XW = D + 36
EPS = 1e-6
BIG = 8192.0


def _is_psum(k):
    return k.startswith("pmod") or (len(k) > 1 and k[0] == "p" and k[1].isupper())


class Prog:
    ENG = ("pe", "dve", "act", "pool", "sp")

    def __init__(self, nc, stack, same_engine_sync=True, ring=(("sp", 16), ("pool", 16))):
        self.nc = nc
        self.stack = stack
        self.e = {"pe": nc.tensor, "dve": nc.vector, "act": nc.scalar, "pool": nc.gpsimd, "sp": nc.sync}
        self.same = same_engine_sync
        self.sems = {}
        self.semval = {}
        self.cur = {}
        self.epoch = {n: 0 for n in self.ENG}
        for n in self.ENG:
            self.cur[n] = self._mk("E_" + n + "_0")
        self.ring = {}
        self.ringpos = {}
        for q, k in ring:
            self.ring[q] = [self._mk(f"D_{q}{i}") for i in range(k)]
            self.ringpos[q] = 0
        self.waited = {n: {} for n in self.ENG}
        self.lastw = {}
        self.readers = {}
        self.ninstr = {n: 0 for n in self.ENG}

    def _mk(self, name):
        s = self.stack.enter_context(self.nc.semaphore(name))
        self.sems[name] = s
        self.semval[name] = 0
        return name

    def _wait(self, eng, ev):
        sem, val = ev
        if val <= 0 or self.waited[eng].get(sem, 0) >= val:
            return
        if sem == self.cur[eng] and (eng == "pe" or not self.same):
            return
        self.e[eng].wait_ge(self.sems[sem], val)
        self.waited[eng][sem] = val

    def _deps(self, eng, r, w):
        for res in list(r) + list(w):
            ev = self.lastw.get(res)
            if ev is not None:
                self._wait(eng, ev)
        for res in w:
            for sem, val in self.readers.get(res, {}).items():
                self._wait(eng, (sem, val))

    def _record(self, ev, r, w):
        for res in w:
            self.lastw[res] = ev
            self.readers[res] = {}
        for res in r:
            d = self.readers.setdefault(res, {})
            if d.get(ev[0], 0) < ev[1]:
                d[ev[0]] = ev[1]

    LIMIT = None
    total = 0

    def op(self, eng, fn, r=(), w=()):
        self.total += 1
        if self.LIMIT is not None and self.total > self.LIMIT:
            return
        w = list(w) + [k for k in r if _is_psum(k)]
        r = [k for k in r if not _is_psum(k)]
        self._deps(eng, r, w)
        ins = fn(self.e[eng])
        sem = self.cur[eng]
        self.semval[sem] += 1
        ins.then_inc(self.sems[sem], 1)
        self.ninstr[eng] += 1
        self._record((sem, self.semval[sem]), r, w)

    def dma(self, q, fn, r=(), w=()):
        self.total += 1
        if self.LIMIT is not None and self.total > self.LIMIT:
            return
        self._deps(q, r, w)
        k = self.ringpos[q]
        sem = self.ring[q][k]
        self.ringpos[q] = (k + 1) % len(self.ring[q])
        self._wait(q, (sem, self.semval[sem]))
        ins = fn(self.e[q])
        self.semval[sem] += 16
        ins.then_inc(self.sems[sem], 16)
        self.ninstr[q] += 1
        self._record((sem, self.semval[sem]), r, w)

    def barrier(self):
        for eng in self.ENG:
            for sem, val in self.semval.items():
                self._wait(eng, (sem, val))
        self.lastw.clear()
        self.readers.clear()
        for eng in self.ENG:
            if self.semval[self.cur[eng]] > 15000:
                self.epoch[eng] += 1
                self.cur[eng] = self._mk(f"E_{eng}_{self.epoch[eng]}")


def build(stop_after=None, dbg=(), limit=None, light=False):
    nc = bass.Bass("TRN2", target_bir_lowering=False)
    Prog.LIMIT = limit

    def din(name, shape, dt=F32):
        return nc.dram_tensor(name, list(shape), dt, kind="ExternalInput").ap()

    x_in = din("x", [SEQ, D]); ctx_in = din("ctx", [CTX, D]); c_in = din("c", [D]); cctx_in = din("c_ctx", [D])
    w_ada = din("w_ada", [DEPTH, D, 6 * D]); b_ada = din("b_ada", [DEPTH, 6 * D])
    norm1_g = din("norm1_g", [DEPTH, D]); norm2_g = din("norm2_g", [DEPTH, D])
    w_in = din("w_in", [DEPTH, D, INW])
    a_q_norm = din("a_q_norm", [DEPTH, 64]); a_k_norm = din("a_k_norm", [DEPTH, 64])
    b_cq_norm = din("b_cq_norm", [DEPTH, 192]); b_ckv_norm = din("b_ckv_norm", [DEPTH, 128])
    w_uq = din("w_uq", [DEPTH, 192, 576]); w_ukv = din("w_ukv", [DEPTH, 128, 768])
    b_qn_norm = din("b_qn_norm", [DEPTH, 64]); b_kn_norm = din("b_kn_norm", [DEPTH, 64])
    b_qr_norm = din("b_qr_norm", [DEPTH, 32]); b_kr_norm = din("b_kr_norm", [DEPTH, 32])
    c_q_norm = din("c_q_norm", [DEPTH, 64]); c_k_norm = din("c_k_norm", [DEPTH, 64])
    c_sink = din("c_sink", [DEPTH, 4])
    w_out = din("w_out", [DEPTH, D, D]); w_router = din("w_router", [DEPTH, D, NE])
    if not light:
        w_e_gate = din("w_e_gate", [DEPTH, NE, D, D]); w_e_up = din("w_e_up", [DEPTH, NE, D, D])
        w_e_down = din("w_e_down", [DEPTH, NE, D, D])
    rope = din("rope", [SEQ, 192])
    y_out = nc.dram_tensor("y", [SEQ, D], F32, kind="ExternalOutput").ap()

    def dscr(name, shape, dt):
        kind = "ExternalOutput" if name in dbg else "Internal"
        return nc.dram_tensor(name, list(shape), dt, kind=kind).ap()

    xall = dscr("xall", [T, D], F32)
    modv = dscr("modv", [2, 6 * D], F32)
    qkT_A = dscr("qkT_A", [512, T], BF16)
    qkT_B = dscr("qkT_B", [12, 96, T], BF16)
    qkT_C = dscr("qkT_C", [384, T], BF16)
    vall = dscr("vall", [T, 640], BF16)
    mix = dscr("mix", [T, D], BF16)
    h2d = dscr("h2d", [T, D], BF16)
    xe_l = [dscr(f"xe_l{i}", [CAP_L, XW], BF16) for i in range(NE)]
    xe_c = [dscr(f"xe_c{i}", [CAP_C, XW], BF16) for i in range(NE)]
    accd = dscr("accd", [T, D], F32)
    ye_l = [dscr(f"ye_l{i}", [CAP_L, D], BF16) for i in range(NE)]
    ye_c = [dscr(f"ye_c{i}", [CAP_C, D], BF16) for i in range(NE)]
    dbg_aff = dscr("dbg_aff", [128, NT * NE], F32)
    dbg_slot = dscr("dbg_slot", [128, NT * NE], I32)

    with ExitStack() as top:
        P = Prog(nc, top)
        V = lambda fn, r=(), w=(): P.op("dve", fn, r, w)
        A = lambda fn, r=(), w=(): P.op("act", fn, r, w)
        G = lambda fn, r=(), w=(): P.op("pool", fn, r, w)
        M = lambda fn, r=(), w=(): P.op("pe", fn, r, w)
        L = lambda fn, r=(), w=(): P.dma("sp", fn, r, w)
        S = lambda fn, r=(), w=(): P.dma("pool", fn, r, w)

        LAYER = [-1]

        def tsb(stk, name, shape, dt):
            return stk.enter_context(nc.sbuf_tensor(f"{name}_L{LAYER[0]}", list(shape), dt))

        def tps(stk, name, shape, dt=F32):
            return stk.enter_context(nc.psum_tensor(f"{name}_L{LAYER[0]}", list(shape), dt))

        REG = {CAP_L - 1: nc.gpsimd.to_reg(CAP_L - 1), CAP_C - 1: nc.gpsimd.to_reg(CAP_C - 1)}
        identf = tsb(top, "identf", [128, 128], F32)
        ident = tsb(top, "ident", [128, 128], BF16)
        onesf = tsb(top, "onesf", [128, 128], F32)
        ones_b = tsb(top, "ones_b", [128, 128], BF16)
        Ub = tsb(top, "Ub", [128, 128], BF16)
        M1 = tsb(top, "M1", [128, 128], BF16)
        M2 = tsb(top, "M2", [128, 128], BF16)
        ctmp = tsb(top, "ctmp", [128, 128], F32)
        cs = tsb(top, "cs", [128, 8, 2], F32)
        craw = tsb(top, "craw", [128, 2, 8], F32)

        G(lambda e: e.memset(identf[:], 0.0), w=["identf"])
        G(lambda e: e.affine_select(out=identf[:], in_=identf[:], pattern=[[-1, 128]], base=0, channel_multiplier=1,
                                    compare_op=ALU.not_equal, fill=1.0), r=["identf"], w=["identf"])
        V(lambda e: e.tensor_copy(out=ident[:], in_=identf[:]), r=["identf"], w=["ident"])
        G(lambda e: e.memset(onesf[:], 1.0), w=["onesf"])
        V(lambda e: e.tensor_copy(out=ones_b[:], in_=onesf[:]), r=["onesf"], w=["ones_b"])
        G(lambda e: e.affine_select(out=ctmp[:], in_=onesf[:], pattern=[[-1, 128]], base=0, channel_multiplier=1,
                                    compare_op=ALU.is_ge, fill=0.0), r=["onesf"], w=["ctmp"])
        V(lambda e: e.tensor_copy(out=M1[:], in_=ctmp[:]), r=["ctmp"], w=["M1"])
        G(lambda e: e.affine_select(out=ctmp[:], in_=onesf[:], pattern=[[1, 128]], base=0, channel_multiplier=-1,
                                    compare_op=ALU.is_ge, fill=0.0), r=["onesf"], w=["ctmp"])
        V(lambda e: e.tensor_copy(out=M2[:], in_=ctmp[:]), r=["ctmp"], w=["M2"])
        G(lambda e: e.affine_select(out=ctmp[:], in_=onesf[:], pattern=[[1, 128]], base=-1, channel_multiplier=-1,
                                    compare_op=ALU.is_ge, fill=0.0), r=["onesf"], w=["ctmp"])
        V(lambda e: e.tensor_copy(out=Ub[:], in_=ctmp[:]), r=["ctmp"], w=["Ub"])

        L(lambda e: e.dma_start(out=craw[:, 0, :], in_=c_in.rearrange("(k p) -> p k", p=128), allow_slow_non_contiguous=True), w=["craw"])
        L(lambda e: e.dma_start(out=craw[:, 1, :], in_=cctx_in.rearrange("(k p) -> p k", p=128), allow_slow_non_contiguous=True), w=["craw"])
        A(lambda e: e.activation(out=cs[:, :, :].rearrange("p k j -> p j k"), in_=craw[:, :, :], func=AF.Silu), r=["craw"], w=["cs"])

        L(lambda e: e.dma_start(out=xall[0:CTX, :], in_=ctx_in))
        for i in range(8):
            L(lambda e: e.dma_start(out=xall[CTX + i * 512:CTX + (i + 1) * 512, :], in_=x_in[i * 512:(i + 1) * 512, :]))
        P.barrier()

        def rstd_from_ss(ss_ap, out_ap, scale, r, w):
            V(lambda e: e.tensor_scalar(out=out_ap, in0=ss_ap, scalar1=scale, scalar2=EPS, op0=ALU.mult, op1=ALU.add), r=r, w=w)
            A(lambda e: e.activation(out=out_ap, in_=out_ap, func=AF.Sqrt), r=w, w=w)
            V(lambda e: e.reciprocal(out=out_ap, in_=out_ap), r=w, w=w)

        for l in range(DEPTH):
            last = l == DEPTH - 1
            LAYER[0] = l
            tiles_act = list(range(NT)) if not last else list(range(NCT, NT))

            with ExitStack() as ph:
                wst = [tsb(ph, f"wstA{i}", [128, 8, 512], F32) for i in range(2)]
                modrow = tsb(ph, "modrow", [2, 6 * D], F32)
                bada = tsb(ph, "bada", [2, 6 * D], F32)
                gn = tsb(ph, "gn", [2, 2, D], F32)
                pmod = [tps(ph, f"pmod{i}", [128, 512]) for i in range(2)]
                L(lambda e: e.dma_start(out=bada[:, :], in_=b_ada[l:l + 1, :].broadcast_to([2, 6 * D])), w=["bada"])
                L(lambda e: e.dma_start(out=gn[:, 0, :], in_=norm1_g[l:l + 1, :].broadcast_to([2, D])), w=["gn"])
                L(lambda e: e.dma_start(out=gn[:, 1, :], in_=norm2_g[l:l + 1, :].broadcast_to([2, D])), w=["gn"])
                for nb in range(12):
                    wb = wst[nb % 2]; pm = pmod[nb % 2]
                    for h in range(2):
                        L(lambda e: e.dma_start(out=wb[:, h * 4:(h + 1) * 4, :],
                                                in_=w_ada[l, h * 512:(h + 1) * 512, nb * 512:(nb + 1) * 512].rearrange("(k p) n -> p k n", p=128)),
                          w=[f"wstA{nb % 2}"])
                    for kc in range(8):
                        M(lambda e: e.matmul(pm[0:2, :], lhsT=cs[:, kc, :], rhs=wb[:, kc, :], start=(kc == 0), stop=(kc == 7)),
                          r=[f"wstA{nb % 2}", "cs"], w=[f"pmod{nb % 2}"])
                    V(lambda e: e.tensor_tensor(out=modrow[:, nb * 512:(nb + 1) * 512], in0=pm[0:2, :], in1=bada[:, nb * 512:(nb + 1) * 512], op=ALU.add),
                      r=[f"pmod{nb % 2}", "bada"], w=["modrow"])
                for j, off in ((0, 1 * D), (1, 4 * D)):
                    V(lambda e: e.scalar_tensor_tensor(out=modrow[:, off:off + D], in0=modrow[:, off:off + D], scalar=1.0, in1=gn[:, j, :],
                                                       op0=ALU.add, op1=ALU.mult), r=["modrow", "gn"], w=["modrow"])
                S(lambda e: e.dma_start(out=modv, in_=modrow[:, :]), r=["modrow"])
            P.barrier()
            if stop_after == ("A", l):
                break

            def load_bcast(dst, which, j, key):
                L(lambda e: e.dma_start(out=dst, in_=modv[which:which + 1, j * D:(j + 1) * D].broadcast_to([128, D])), w=[key])

            with ExitStack() as ph:
                wst = tsb(ph, "wstB", [128, 8, 512], F32)
                w_in_b = tsb(ph, "w_in_b", [128, 8, INW], BF16)
                wuq_f = tsb(ph, "wuq_f", [128, 2, 576], F32)
                wuq_b = tsb(ph, "wuq_b", [128, 2, 576], BF16)
                wukv_f = tsb(ph, "wukv_f", [128, 768], F32)
                wukv_b = tsb(ph, "wukv_b", [128, 768], BF16)
                gcq = tsb(ph, "gcq", [128, 2], F32)
                gckv = tsb(ph, "gckv", [128, 1], F32)
                gA = tsb(ph, "gA", [128, 8, 64], F32)
                gC = tsb(ph, "gC", [128, 6, 64], F32)
                gBq = tsb(ph, "gBq", [128, 96], F32)
                gBk = tsb(ph, "gBk", [128, 64], F32)
                gBkr = tsb(ph, "gBkr", [128, 32], F32)
                invB = tsb(ph, "invB", [128, 3], F32)
                invQ = tsb(ph, "invQ", [128, 12], F32)
                Ar = [tsb(ph, f"Ar{i}", [128, D], F32) for i in range(2)]
                Br = [tsb(ph, f"Br{i}", [128, D], F32) for i in range(2)]
                xt = [tsb(ph, f"xtB{i}", [128, D], F32) for i in range(2)]
                rp = [tsb(ph, f"rp{i}", [128, 192], F32) for i in range(2)]
                junk = tsb(ph, "junkB", [128, D], BF16)
                st1 = tsb(ph, "st1", [128, 4], F32)
                hf = tsb(ph, "hf", [128, D], F32)
                hb = tsb(ph, "hb", [128, D], BF16)
                hT = tsb(ph, "hT", [128, 8, 128], BF16)
                sq = tsb(ph, "sq", [128, 576], F32)
                ssh = tsb(ph, "ssh", [128, 16], F32)
                qn = tsb(ph, "qn", [128, 576], F32)
                t1 = tsb(ph, "t1", [128, 576], F32)
                t2 = tsb(ph, "t2", [128, 576], F32)
                qA = tsb(ph, "qA", [128, 8, 64], BF16)
                qC = tsb(ph, "qC", [128, 6, 64], BF16)
                qB = tsb(ph, "qB", [128, 12, 96], BF16)
                cqn = tsb(ph, "cqn", [128, 320], BF16)
                cT = tsb(ph, "cT", [128, 3, 128], BF16)
                kpe = tsb(ph, "kpe", [128, 32], F32)
                kpe2 = tsb(ph, "kpe2", [128, 32], F32)
                vst = tsb(ph, "vst", [128, 640], BF16)
                stA = tsb(ph, "stA", [128, 4, 512], BF16)
                stB = tsb(ph, "stB", [128, 12, 512], BF16)
                stC = tsb(ph, "stC", [128, 3, 512], BF16)
                pT = tps(ph, "pTB", [128, 1024], BF16)
                pP = [tps(ph, f"pP{i}", [128, 512]) for i in range(3)]
                pQ = [tps(ph, f"pQ{i}", [128, 512]) for i in range(2)]
                pK = [tps(ph, f"pK{i}", [128, 512]) for i in range(2)]

                for b, (c0, c1) in enumerate(((0, 512), (512, 1024), (1024, INW))):
                    n = c1 - c0
                    for h in range(2):
                        L(lambda e: e.dma_start(out=wst[:, h * 4:(h + 1) * 4, 0:n],
                                                in_=w_in[l, h * 512:(h + 1) * 512, c0:c1].rearrange("(k p) n -> p k n", p=128)), w=["wstB"])
                    V(lambda e: e.tensor_copy(out=w_in_b[:, 0:4, c0:c1], in_=wst[:, 0:4, 0:n]), r=["wstB"], w=["w_in_b"])
                    G(lambda e: e.tensor_copy(out=w_in_b[:, 4:8, c0:c1], in_=wst[:, 4:8, 0:n]), r=["wstB"], w=["w_in_b"])
                L(lambda e: e.dma_start(out=wuq_f[:, 0, :], in_=w_uq[l, 0:128, :]), w=["wuq_f"])
                L(lambda e: e.dma_start(out=wuq_f[0:64, 1, :], in_=w_uq[l, 128:192, :]), w=["wuq_f"])
                L(lambda e: e.dma_start(out=wukv_f[:, :], in_=w_ukv[l, :, :]), w=["wukv_f"])
                L(lambda e: e.dma_start(out=gcq[:, 0:1], in_=b_cq_norm[l, 0:128].rearrange("(p o) -> p o", o=1)), w=["gcq"])
                L(lambda e: e.dma_start(out=gcq[0:64, 1:2], in_=b_cq_norm[l, 128:192].rearrange("(p o) -> p o", o=1)), w=["gcq"])
                L(lambda e: e.dma_start(out=gckv[:, 0:1], in_=b_ckv_norm[l, :].rearrange("(p o) -> p o", o=1)), w=["gckv"])
                V(lambda e: e.tensor_scalar(out=wuq_b[:, 0, :], in0=wuq_f[:, 0, :], scalar1=gcq[:, 0:1], scalar2=None, op0=ALU.mult), r=["wuq_f", "gcq"], w=["wuq_b"])
                V(lambda e: e.tensor_scalar(out=wuq_b[0:64, 1, :], in0=wuq_f[0:64, 1, :], scalar1=gcq[0:64, 1:2], scalar2=None, op0=ALU.mult), r=["wuq_f", "gcq"], w=["wuq_b"])
                V(lambda e: e.tensor_scalar(out=wukv_b[:, :], in0=wukv_f[:, :], scalar1=gckv[:, 0:1], scalar2=None, op0=ALU.mult), r=["wukv_f", "gckv"], w=["wukv_b"])
                for hh in range(8):
                    src = a_q_norm if hh < 6 else a_k_norm
                    L(lambda e: e.dma_start(out=gA[:, hh, :], in_=src[l:l + 1, :].broadcast_to([128, 64])), w=["gA"])
                for hh in range(6):
                    src = c_q_norm if hh < 4 else c_k_norm
                    L(lambda e: e.dma_start(out=gC[:, hh, :], in_=src[l:l + 1, :].broadcast_to([128, 64])), w=["gC"])
                L(lambda e: e.dma_start(out=gBq[:, 0:64], in_=b_qn_norm[l:l + 1, :].broadcast_to([128, 64])), w=["gBq"])
                L(lambda e: e.dma_start(out=gBq[:, 64:96], in_=b_qr_norm[l:l + 1, :].broadcast_to([128, 32])), w=["gBq"])
                L(lambda e: e.dma_start(out=gBk[:, :], in_=b_kn_norm[l:l + 1, :].broadcast_to([128, 64])), w=["gBk"])
                L(lambda e: e.dma_start(out=gBkr[:, :], in_=b_kr_norm[l:l + 1, :].broadcast_to([128, 32])), w=["gBkr"])
                for j, v in enumerate((1.0 / 192, 1.0 / 128, 1.0 / 32)):
                    G(lambda e: e.memset(invB[:, j:j + 1], v), w=["invB"])
                G(lambda e: e.memset(invQ[:, 0:6], 1.0 / 64), w=["invQ"])
                G(lambda e: e.memset(invQ[:, 6:12], 1.0 / 32), w=["invQ"])
                for which in range(2):
                    load_bcast(Ar[which][:, :], which, 1, f"Ar{which}")
                    load_bcast(Br[which][:, :], which, 0, f"Br{which}")

                def rope_apply(src_ap, H, Dh, C_ap, S_ap, out_ap, rs):
                    c4 = Dh // 4
                    t1v = t1[:, 0:H * Dh].rearrange("p (h d) -> p h d", h=H)
                    t2v = t2[:, 0:H * Dh].rearrange("p (h a b c) -> p h a b c", h=H, a=2, b=2)
                    s5 = src_ap.rearrange("p h (a b c) -> p h a b c", a=2, b=2)
                    S4 = S_ap.rearrange("p (a b c) -> p a b c", a=2, b=2)
                    V(lambda e: e.tensor_tensor(out=t1v, in0=src_ap, in1=C_ap.unsqueeze(1).to_broadcast([128, H, Dh]), op=ALU.mult), r=rs, w=["t1"])
                    V(lambda e: e.tensor_tensor(out=t2v[:, :, :, 0, :], in0=s5[:, :, :, 1, :],
                                                in1=S4[:, :, 0, :].unsqueeze(1).to_broadcast([128, H, 2, c4]), op=ALU.mult), r=rs, w=["t2"])
                    V(lambda e: e.tensor_tensor(out=t2v[:, :, :, 1, :], in0=s5[:, :, :, 0, :],
                                                in1=S4[:, :, 1, :].unsqueeze(1).to_broadcast([128, H, 2, c4]), op=ALU.mult), r=rs, w=["t2"])
                    return t1v, t2[:, 0:H * Dh].rearrange("p (h d) -> p h d", h=H)

                blocks = [list(range(0, NCT))] + [list(range(NCT + 4 * b, NCT + 4 * b + 4)) for b in range(SEQ // 512)]
                for blk in blocks:
                    for bi, ti in enumerate(blk):
                        isc = ti < NCT
                        which = 1 if isc else 0
                        li = ti - NCT
                        xb = xt[ti % 2]; xk = f"xtB{ti % 2}"
                        rpb = rp[ti % 2]; rk = f"rp{ti % 2}"
                        L(lambda e: e.dma_start(out=xb[:, :], in_=xall[ti * 128:(ti + 1) * 128, :]), w=[xk])
                        if not isc:
                            L(lambda e: e.dma_start(out=rpb[:, :], in_=rope[li * 128:(li + 1) * 128, :]), w=[rk])
                        A(lambda e: e.activation(out=junk[:, :], in_=xb[:, :], func=AF.Square, accum_out=st1[:, 0:1]), r=[xk], w=["junkB", "st1"])
                        rstd_from_ss(st1[:, 0:1], st1[:, 1:2], 1.0 / D, ["st1"], ["st1b"])
                        V(lambda e: e.scalar_tensor_tensor(out=hf[:, :], in0=xb[:, :], scalar=st1[:, 1:2], in1=Ar[which][:, :], op0=ALU.mult, op1=ALU.mult),
                          r=[xk, "st1b", f"Ar{which}"], w=["hf"])
                        V(lambda e: e.tensor_tensor(out=hb[:, :], in0=hf[:, :], in1=Br[which][:, :], op=ALU.add), r=["hf", f"Br{which}"], w=["hb"])
                        for kc in range(8):
                            M(lambda e: e.transpose(out=pT[:, kc * 128:(kc + 1) * 128], in_=hb[:, kc * 128:(kc + 1) * 128], identity=ident[:, :]),
                              r=["hb", "ident"], w=["pTB"])
                        A(lambda e: e.copy(out=hT[:, :, :].rearrange("p k t -> p (k t)"), in_=pT[:, :]), r=["pTB"], w=["hT"])
                        for b, (c0, c1) in enumerate(((0, 512), (512, 992), (992, INW))):
                            for kc in range(8):
                                M(lambda e: e.matmul(pP[b][:, 0:c1 - c0], lhsT=hT[:, kc, :], rhs=w_in_b[:, kc, c0:c1], start=(kc == 0), stop=(kc == 7)),
                                  r=["hT", "w_in_b"], w=[f"pP{b}"])

                        for (pp, pk, H, gain, gk, dst, dk) in ((pP[0], "pP0", 8, gA, "gA", qA, "qA"), (pP[2], "pP2", 6, gC, "gC", qC, "qC")):
                            n = H * 64
                            src3 = pp[:, 0:n].rearrange("p (h d) -> p h d", h=H)
                            A(lambda e: e.activation(out=sq[:, 0:n], in_=pp[:, 0:n], func=AF.Square), r=[pk], w=["sq"])
                            V(lambda e: e.tensor_reduce(out=ssh[:, 0:H], in_=sq[:, 0:n].rearrange("p (h d) -> p h d", h=H), axis=AX.X, op=ALU.add), r=["sq"], w=["ssh"])
                            rstd_from_ss(ssh[:, 0:H], ssh[:, 0:H], 1.0 / 64, ["ssh"], ["ssh"])
                            qn3 = qn[:, 0:n].rearrange("p (h d) -> p h d", h=H)
                            V(lambda e: e.tensor_tensor(out=qn3, in0=src3, in1=ssh[:, 0:H].unsqueeze(2).to_broadcast([128, H, 64]), op=ALU.mult), r=[pk, "ssh"], w=["qn"])
                            if isc:
                                V(lambda e: e.tensor_tensor(out=dst[:, :, :], in0=qn3, in1=gain[:, :, :], op=ALU.mult), r=["qn", gk], w=[dk])
                            else:
                                V(lambda e: e.tensor_tensor(out=qn3, in0=qn3, in1=gain[:, :, :], op=ALU.mult), r=["qn", gk], w=["qn"])
                                a1, a2 = rope_apply(qn3, H, 64, rpb[:, 0:64], rpb[:, 64:128], None, ["qn", rk])
                                V(lambda e: e.tensor_tensor(out=dst[:, :, :], in0=a1, in1=a2, op=ALU.add), r=["t1", "t2"], w=[dk])
                        A(lambda e: e.copy(out=vst[:, 0:128], in_=pP[1][:, 0:128]), r=["pP1"], w=["vst"])
                        A(lambda e: e.copy(out=vst[:, 512:640], in_=pP[2][:, 384:512]), r=["pP2"], w=["vst"])

                        A(lambda e: e.activation(out=sq[:, 0:352], in_=pP[1][:, 128:480], func=AF.Square), r=["pP1"], w=["sq"])
                        V(lambda e: e.tensor_reduce(out=ssh[:, 0:1], in_=sq[:, 0:192], axis=AX.X, op=ALU.add), r=["sq"], w=["ssh"])
                        V(lambda e: e.tensor_reduce(out=ssh[:, 1:2], in_=sq[:, 192:320], axis=AX.X, op=ALU.add), r=["sq"], w=["ssh"])
                        V(lambda e: e.tensor_reduce(out=ssh[:, 2:3], in_=sq[:, 320:352], axis=AX.X, op=ALU.add), r=["sq"], w=["ssh"])
                        V(lambda e: e.tensor_tensor(out=ssh[:, 0:3], in0=ssh[:, 0:3], in1=invB[:, :], op=ALU.mult), r=["ssh", "invB"], w=["ssh"])
                        rstd_from_ss(ssh[:, 0:3], ssh[:, 0:3], 1.0, ["ssh"], ["ssh"])
                        V(lambda e: e.tensor_scalar(out=cqn[:, 0:192], in0=pP[1][:, 128:320], scalar1=ssh[:, 0:1], scalar2=None, op0=ALU.mult), r=["pP1", "ssh"], w=["cqn"])
                        V(lambda e: e.tensor_scalar(out=cqn[:, 192:320], in0=pP[1][:, 320:448], scalar1=ssh[:, 1:2], scalar2=None, op0=ALU.mult), r=["pP1", "ssh"], w=["cqn"])
                        V(lambda e: e.scalar_tensor_tensor(out=kpe[:, :], in0=pP[1][:, 448:480], scalar=ssh[:, 2:3], in1=gBkr[:, :], op0=ALU.mult, op1=ALU.mult),
                          r=["pP1", "ssh", "gBkr"], w=["kpe"])
                        if not isc:
                            a1, a2 = rope_apply(kpe[:, :].unsqueeze(1), 1, 32, rpb[:, 128:160], rpb[:, 160:192], None, ["kpe", rk])
                            V(lambda e: e.tensor_tensor(out=kpe2[:, :].unsqueeze(1), in0=a1, in1=a2, op=ALU.add), r=["t1", "t2"], w=["kpe2"])
                            kp = kpe2
                        else:
                            kp = kpe
                        V(lambda e: e.tensor_copy(out=qB[:, 6:12, 64:96], in_=kp[:, :].unsqueeze(1).to_broadcast([128, 6, 32])), r=["kpe", "kpe2"], w=["qBk"])
                        M(lambda e: e.transpose(out=pT[:, 0:128], in_=cqn[:, 0:128], identity=ident[:, :]), r=["cqn", "ident"], w=["pTB"])
                        M(lambda e: e.transpose(out=pT[0:64, 128:256], in_=cqn[:, 128:192], identity=ident[:, :]), r=["cqn", "ident"], w=["pTB"])
                        M(lambda e: e.transpose(out=pT[:, 256:384], in_=cqn[:, 192:320], identity=ident[:, :]), r=["cqn", "ident"], w=["pTB"])
                        A(lambda e: e.copy(out=cT[:, 0, :], in_=pT[:, 0:128]), r=["pTB"], w=["cT"])
                        A(lambda e: e.copy(out=cT[0:64, 1, :], in_=pT[0:64, 128:256]), r=["pTB"], w=["cT"])
                        A(lambda e: e.copy(out=cT[:, 2, :], in_=pT[:, 256:384]), r=["pTB"], w=["cT"])
                        for hf_ in range(2):
                            M(lambda e: e.matmul(pQ[hf_][:, 0:288], lhsT=cT[:, 0, :], rhs=wuq_b[:, 0, hf_ * 288:(hf_ + 1) * 288], start=True, stop=False),
                              r=["cT", "wuq_b"], w=[f"pQ{hf_}"])
                            M(lambda e: e.matmul(pQ[hf_][:, 0:288], lhsT=cT[0:64, 1, :], rhs=wuq_b[0:64, 1, hf_ * 288:(hf_ + 1) * 288], start=False, stop=True),
                              r=["cT", "wuq_b"], w=[f"pQ{hf_}"])
                            M(lambda e: e.matmul(pK[hf_][:, 0:384], lhsT=cT[:, 2, :], rhs=wukv_b[:, hf_ * 384:(hf_ + 1) * 384], start=True, stop=True),
                              r=["cT", "wukv_b"], w=[f"pK{hf_}"])
                        for hf_ in range(2):
                            A(lambda e: e.activation(out=sq[:, hf_ * 288:(hf_ + 1) * 288], in_=pQ[hf_][:, 0:288], func=AF.Square), r=[f"pQ{hf_}"], w=["sq"])
                        sq3 = sq[:, 0:576].rearrange("p (h d) -> p h d", h=6)
                        V(lambda e: e.tensor_reduce(out=ssh[:, 0:6], in_=sq3[:, :, 0:64], axis=AX.X, op=ALU.add), r=["sq"], w=["ssh"])
                        V(lambda e: e.tensor_reduce(out=ssh[:, 6:12], in_=sq3[:, :, 64:96], axis=AX.X, op=ALU.add), r=["sq"], w=["ssh"])
                        V(lambda e: e.tensor_tensor(out=ssh[:, 0:12], in0=ssh[:, 0:12], in1=invQ[:, :], op=ALU.mult), r=["ssh", "invQ"], w=["ssh"])
                        rstd_from_ss(ssh[:, 0:12], ssh[:, 0:12], 1.0, ["ssh"], ["ssh"])
                        qn3 = qn[:, 0:576].rearrange("p (h d) -> p h d", h=6)
                        for hf_ in range(2):
                            p3 = pQ[hf_][:, 0:288].rearrange("p (h d) -> p h d", h=3)
                            hs = slice(hf_ * 3, hf_ * 3 + 3)
                            V(lambda e: e.tensor_tensor(out=qn3[:, hs, 0:64], in0=p3[:, :, 0:64],
                                                        in1=ssh[:, hf_ * 3:hf_ * 3 + 3].unsqueeze(2).to_broadcast([128, 3, 64]), op=ALU.mult), r=[f"pQ{hf_}", "ssh"], w=["qn"])
                            V(lambda e: e.tensor_tensor(out=qn3[:, hs, 64:96], in0=p3[:, :, 64:96],
                                                        in1=ssh[:, 6 + hf_ * 3:6 + hf_ * 3 + 3].unsqueeze(2).to_broadcast([128, 3, 32]), op=ALU.mult), r=[f"pQ{hf_}", "ssh"], w=["qn"])
                        V(lambda e: e.tensor_tensor(out=qn3, in0=qn3, in1=gBq[:, :].unsqueeze(1).to_broadcast([128, 6, 96]), op=ALU.mult), r=["qn", "gBq"], w=["qn"])
                        V(lambda e: e.tensor_copy(out=qB[:, 0:6, 0:64], in_=qn3[:, :, 0:64]), r=["qn"], w=["qBq"])
                        if isc:
                            V(lambda e: e.tensor_copy(out=qB[:, 0:6, 64:96], in_=qn3[:, :, 64:96]), r=["qn"], w=["qBq"])
                        else:
                            a1, a2 = rope_apply(qn3[:, :, 64:96], 6, 32, rpb[:, 128:160], rpb[:, 160:192], None, ["qn", rk])
                            V(lambda e: e.tensor_tensor(out=qB[:, 0:6, 64:96], in0=a1, in1=a2, op=ALU.add), r=["t1", "t2"], w=["qBq"])
                        for hf_ in range(2):
                            A(lambda e: e.activation(out=sq[:, hf_ * 192:(hf_ + 1) * 192].rearrange("p (h d) -> p h d", h=3),
                                                     in_=pK[hf_][:, 0:384].rearrange("p (h d) -> p h d", h=3)[:, :, 0:64], func=AF.Square), r=[f"pK{hf_}"], w=["sq"])
                            A(lambda e: e.copy(out=vst[:, 128 + hf_ * 192:128 + (hf_ + 1) * 192].rearrange("p (h d) -> p h d", h=3),
                                               in_=pK[hf_][:, 0:384].rearrange("p (h d) -> p h d", h=3)[:, :, 64:128]), r=[f"pK{hf_}"], w=["vst"])
                        V(lambda e: e.tensor_reduce(out=ssh[:, 0:6], in_=sq[:, 0:384].rearrange("p (h d) -> p h d", h=6), axis=AX.X, op=ALU.add), r=["sq"], w=["ssh"])
                        rstd_from_ss(ssh[:, 0:6], ssh[:, 0:6], 1.0 / 64, ["ssh"], ["ssh"])
                        kn3 = qn[:, 0:384].rearrange("p (h d) -> p h d", h=6)
                        for hf_ in range(2):
                            p3 = pK[hf_][:, 0:384].rearrange("p (h d) -> p h d", h=3)
                            V(lambda e: e.tensor_tensor(out=kn3[:, hf_ * 3:hf_ * 3 + 3, :], in0=p3[:, :, 0:64],
                                                        in1=ssh[:, hf_ * 3:hf_ * 3 + 3].unsqueeze(2).to_broadcast([128, 3, 64]), op=ALU.mult), r=[f"pK{hf_}", "ssh"], w=["qn"])
                        V(lambda e: e.tensor_tensor(out=qB[:, 6:12, 0:64], in0=kn3, in1=gBk[:, :].unsqueeze(1).to_broadcast([128, 6, 64]), op=ALU.mult), r=["qn", "gBk"], w=["qBk"])

                        S(lambda e: e.dma_start(out=vall[ti * 128:(ti + 1) * 128, :], in_=vst[:, :]), r=["vst"])
                        c0 = bi * 128
                        for i in range(4):
                            M(lambda e: e.transpose(out=pT[:, i * 128:(i + 1) * 128], in_=qA[:, 2 * i:2 * i + 2, :].rearrange("p h d -> p (h d)"), identity=ident[:, :]),
                              r=["qA", "ident"], w=["pTB"])
                        A(lambda e: e.copy(out=stA[:, :, c0:c0 + 128], in_=pT[:, 0:512].rearrange("p (i t) -> p i t", i=4)), r=["pTB"], w=["stA"])
                        for i in range(3):
                            M(lambda e: e.transpose(out=pT[:, i * 128:(i + 1) * 128], in_=qC[:, 2 * i:2 * i + 2, :].rearrange("p h d -> p (h d)"), identity=ident[:, :]),
                              r=["qC", "ident"], w=["pTB"])
                        V(lambda e: e.tensor_copy(out=stC[:, :, c0:c0 + 128], in_=pT[:, 0:384].rearrange("p (i t) -> p i t", i=3)), r=["pTB"], w=["stC"])
                        for rnd in range(2):
                            for i in range(6):
                                M(lambda e: e.transpose(out=pT[0:96, i * 128:(i + 1) * 128], in_=qB[:, rnd * 6 + i, :], identity=ident[:, :]),
                                  r=["qBq", "qBk", "ident"], w=["pTB"])
                            A(lambda e: e.copy(out=stB[0:96, rnd * 6:rnd * 6 + 6, c0:c0 + 128], in_=pT[0:96, 0:768].rearrange("p (i t) -> p i t", i=6)), r=["pTB"], w=["stB"])
                    W = len(blk) * 128
                    t0 = blk[0] * 128
                    S(lambda e: e.dma_start(out=qkT_A[:, t0:t0 + W].rearrange("(i p) t -> p i t", p=128), in_=stA[:, :, 0:W]), r=["stA"])
                    S(lambda e: e.dma_start(out=qkT_C[:, t0:t0 + W].rearrange("(i p) t -> p i t", p=128), in_=stC[:, :, 0:W]), r=["stC"])
                    S(lambda e: e.dma_start(out=qkT_B[:, :, t0:t0 + W].rearrange("j p t -> p j t"), in_=stB[0:96, :, 0:W]), r=["stB"])
            P.barrier()
            if stop_after == ("B", l):
                break

            with ExitStack() as ph:
                kt = [tsb(ph, f"kt{i}", [96, T], BF16) for i in range(2)]
                vb = [tsb(ph, f"vb{i}", [128, NT, 65], BF16) for i in range(2)]
                qt = [tsb(ph, f"qt{i}", [96, 512], BF16) for i in range(2)]
                pt = [tsb(ph, f"pt{i}", [128, 512], BF16) for i in range(3)]
                ob = [tsb(ph, f"ob{i}", [128, 4, 64], BF16) for i in range(2)]
                den = tsb(ph, "den", [128, 8], F32)
                sinkr = tsb(ph, "sinkr", [128, 4], F32)
                sinke = tsb(ph, "sinke", [128, 4], F32)
                pS = [tps(ph, f"pS{i}", [128, 512]) for i in range(3)]
                pO = [tps(ph, f"pO{i}", [128, 512]) for i in range(2)]
                den2 = [tsb(ph, f"den2_{i}", [128, 4], F32) for i in range(2)]
                L(lambda e: e.dma_start(out=sinkr[:, :], in_=c_sink[l:l + 1, :].broadcast_to([128, 4])), w=["sinkr"])
                A(lambda e: e.activation(out=sinke[:, :], in_=sinkr[:, :], func=AF.Exp), r=["sinkr"], w=["sinke"])
                for i in range(2):
                    G(lambda e: e.memset(vb[i][:, :, 64:65], 1.0), w=[f"vb{i}"])
                cnt = {"kv": 0, "q": 0, "s": 0, "p": 0, "u": 0}
                mixv = mix.rearrange("(j p) c -> p j c", p=128)
                vallv = vall.rearrange("(j p) c -> p j c", p=128)
                pending = []

                def emit_tail(st):
                    (si, pi, Wq, scale, msk, subs, u, n, nkeys, vbuf, vk, j, sink_h, out_fn) = st
                    ui = u % 2
                    A(lambda e: e.activation(out=pt[pi][:, 0:Wq], in_=pS[si][:, 0:Wq], func=AF.Exp, scale=scale), r=[f"pS{si}"], w=[f"pt{pi}"])
                    if msk is not None:
                        V(lambda e: e.tensor_tensor(out=pt[pi][:, 0:Wq].rearrange("p (s t) -> p s t", s=subs), in0=pt[pi][:, 0:Wq].rearrange("p (s t) -> p s t", s=subs),
                                                    in1=msk[:, :].unsqueeze(1).to_broadcast([128, subs, 128]), op=ALU.mult), r=[f"pt{pi}"], w=[f"pt{pi}"])
                    for s_ in range(subs):
                        M(lambda e: e.matmul(pO[ui][:, s_ * 65:(s_ + 1) * 65], lhsT=pt[pi][:, s_ * 128:(s_ + 1) * 128], rhs=vbuf[:, j, :],
                                             start=(n == 0 and s_ == 0), stop=(n == nkeys - 1), skip_group_check=True),
                          r=[f"pt{pi}", vk], w=[f"pO{ui}"])
                    if n == nkeys - 1:
                        pov = pO[ui][:, 0:subs * 65].rearrange("p (s c) -> p s c", s=subs)
                        dn = den2[ui]
                        if sink_h is not None:
                            V(lambda e: e.tensor_tensor(out=dn[:, 0:subs].unsqueeze(2), in0=pov[:, :, 64:65], in1=sinke[:, sink_h:sink_h + subs].unsqueeze(2), op=ALU.add),
                              r=[f"pO{ui}", "sinke"], w=[f"den2_{ui}"])
                            V(lambda e: e.reciprocal(out=dn[:, 0:subs], in_=dn[:, 0:subs]), r=[f"den2_{ui}"], w=[f"den2_{ui}"])
                        else:
                            V(lambda e: e.reciprocal(out=dn[:, 0:subs].unsqueeze(2), in_=pov[:, :, 64:65]), r=[f"pO{ui}"], w=[f"den2_{ui}"])
                        V(lambda e: e.tensor_tensor(out=ob[ui][:, 0:subs, :], in0=pov[:, :, 0:64], in1=dn[:, 0:subs].unsqueeze(2).to_broadcast([128, subs, 64]), op=ALU.mult),
                          r=[f"pO{ui}", f"den2_{ui}"], w=[f"ob{ui}"])
                        out_fn(ob[ui], f"ob{ui}")

                def attn_unit(qsrc_fn, dq, Wq, keys, kb, kk, vbuf, vk, scale, subs, sink_h, out_fn):
                    qi = cnt["q"] % 2; cnt["q"] += 1
                    u = cnt["u"]; cnt["u"] += 1
                    qsrc_fn(qt[qi], f"qt{qi}")
                    for n, (j, msk) in enumerate(keys):
                        si = cnt["s"] % 3; cnt["s"] += 1
                        pi = cnt["p"] % 3; cnt["p"] += 1
                        M(lambda e: e.matmul(pS[si][:, 0:Wq], lhsT=kb[0:dq, j * 128:(j + 1) * 128], rhs=qt[qi][0:dq, 0:Wq], start=True, stop=True),
                          r=[kk, f"qt{qi}"], w=[f"pS{si}"])
                        pending.append((si, pi, Wq, scale, msk, subs, u, n, len(keys), vbuf, vk, j, sink_h, out_fn))
                        if len(pending) > 1:
                            emit_tail(pending.pop(0))

                def load_kv(krows_ap, dq, voff):
                    i = cnt["kv"] % 2; cnt["kv"] += 1
                    L(lambda e: e.dma_start(out=kt[i][0:dq, :], in_=krows_ap), w=[f"kt{i}"])
                    for hh in range(2):
                        L(lambda e: e.dma_start(out=vb[i][:, hh * 17:(hh + 1) * 17, 0:64], in_=vallv[:, hh * 17:(hh + 1) * 17, voff:voff + 64]), w=[f"vb{i}"])
                    return kt[i], f"kt{i}", vb[i], f"vb{i}"

                qblocks = ([(0, NCT)] if not last else []) + [(NCT + 4 * b, 4) for b in range(SEQ // 512)]
                for mixer in ("A", "B"):
                    nkv = 2 if mixer == "A" else 6
                    for kv in range(nkv):
                        if mixer == "A":
                            kb, kk, vbuf, vk = load_kv(qkT_A[(6 + kv) * 64:(7 + kv) * 64, :], 64, kv * 64)
                            heads = [3 * kv + g for g in range(3)]; dq = 64; scale = 64 ** -0.5
                        else:
                            kb, kk, vbuf, vk = load_kv(qkT_B[6 + kv, :, :], 96, 128 + kv * 64)
                            heads = [kv]; dq = 96; scale = 96 ** -0.5
                        for h in heads:
                            for (tb, nsub) in qblocks:
                                keys = [(j, None) for j in (range(NCT) if tb < NCT else range(NT))]
                                Wq = nsub * 128
                                if mixer == "A":
                                    qsrc = lambda dst, dk, h=h, tb=tb, Wq=Wq: L(lambda e: e.dma_start(out=dst[0:64, 0:Wq], in_=qkT_A[h * 64:(h + 1) * 64, tb * 128:tb * 128 + Wq]), w=[dk])
                                    col = h * 64
                                else:
                                    qsrc = lambda dst, dk, h=h, tb=tb, Wq=Wq: L(lambda e: e.dma_start(out=dst[0:96, 0:Wq], in_=qkT_B[h, :, tb * 128:tb * 128 + Wq]), w=[dk])
                                    col = 384 + h * 64
                                outf = lambda o, ok, tb=tb, nsub=nsub, col=col: S(lambda e: e.dma_start(out=mixv[:, tb:tb + nsub, col:col + 64], in_=o[:, 0:nsub, :]), r=[ok])
                                attn_unit(qsrc, dq, Wq, keys, kb, kk, vbuf, vk, scale, nsub, None, outf)
                for kv in range(2):
                    kb, kk, vbuf, vk = load_kv(qkT_C[(4 + kv) * 64:(5 + kv) * 64, :], 64, 512 + kv * 64)
                    for ti in (range(NT) if not last else range(NCT, NT)):
                        if ti < NCT:
                            keys = [(j, None) for j in range(NCT)]
                        else:
                            keys = [(j, None) for j in range(NCT)]
                            if ti - 1 >= NCT:
                                keys.append((ti - 1, M1))
                            keys.append((ti, None))
                            if ti + 1 < NT:
                                keys.append((ti + 1, M2))

                        def qsrc(dst, dk, kv=kv, ti=ti):
                            L(lambda e: e.dma_start(out=dst[0:64, 0:256].rearrange("p (h t) -> p h t", h=2),
                                                    in_=qkT_C[2 * kv * 64:(2 * kv + 2) * 64, ti * 128:(ti + 1) * 128].rearrange("(h p) t -> p h t", p=64)), w=[dk])
                        col = 768 + 2 * kv * 64
                        outf = lambda o, ok, ti=ti, col=col: S(lambda e: e.dma_start(out=mix[ti * 128:(ti + 1) * 128, col:col + 128], in_=o[:, 0:2, :].rearrange("p s d -> p (s d)")), r=[ok])
                        attn_unit(qsrc, 64, 256, keys, kb, kk, vbuf, vk, 64 ** -0.5, 2, 2 * kv, outf)
                while pending:
                    emit_tail(pending.pop(0))
            P.barrier()
            if stop_after == ("C", l):
                break

            with ExitStack() as ph:
                wst = tsb(ph, "wstD", [128, 8, 512], F32)
                w_out_b = tsb(ph, "w_out_b", [128, 8, D], BF16)
                G1 = [tsb(ph, f"G1_{i}", [128, D], F32) for i in range(2)]
                xt = [tsb(ph, f"xtD{i}", [128, D], F32) for i in range(2)]
                mt = [tsb(ph, f"mt{i}", [128, D], BF16) for i in range(2)]
                mT = tsb(ph, "mT", [128, 8, 128], BF16)
                yt = [tsb(ph, f"yt{i}", [128, D], F32) for i in range(2)]
                pT = tps(ph, "pTD", [128, 1024], BF16)
                pY = [tps(ph, f"pY{i}", [128, 512]) for i in range(2)]
                for b in range(2):
                    for h in range(2):
                        L(lambda e: e.dma_start(out=wst[:, h * 4:(h + 1) * 4, :],
                                                in_=w_out[l, h * 512:(h + 1) * 512, b * 512:(b + 1) * 512].rearrange("(k p) n -> p k n", p=128)), w=["wstD"])
                    V(lambda e: e.tensor_copy(out=w_out_b[:, 0:4, b * 512:(b + 1) * 512], in_=wst[:, 0:4, :]), r=["wstD"], w=["w_out_b"])
                    G(lambda e: e.tensor_copy(out=w_out_b[:, 4:8, b * 512:(b + 1) * 512], in_=wst[:, 4:8, :]), r=["wstD"], w=["w_out_b"])
                for which in range(2):
                    load_bcast(G1[which][:, :], which, 2, f"G1_{which}")
                for ti in tiles_act:
                    which = 1 if ti < NCT else 0
                    i2 = ti % 2
                    L(lambda e: e.dma_start(out=xt[i2][:, :], in_=xall[ti * 128:(ti + 1) * 128, :]), w=[f"xtD{i2}"])
                    L(lambda e: e.dma_start(out=mt[i2][:, :], in_=mix[ti * 128:(ti + 1) * 128, :]), w=[f"mt{i2}"])
                    for kc in range(8):
                        M(lambda e: e.transpose(out=pT[:, kc * 128:(kc + 1) * 128], in_=mt[i2][:, kc * 128:(kc + 1) * 128], identity=ident[:, :]), r=[f"mt{i2}", "ident"], w=["pTD"])
                    A(lambda e: e.copy(out=mT[:, :, :].rearrange("p k t -> p (k t)"), in_=pT[:, :]), r=["pTD"], w=["mT"])
                    for b in range(2):
                        for kc in range(8):
                            M(lambda e: e.matmul(pY[b][:, :], lhsT=mT[:, kc, :], rhs=w_out_b[:, kc, b * 512:(b + 1) * 512], start=(kc == 0), stop=(kc == 7)), r=["mT", "w_out_b"], w=[f"pY{b}"])
                        V(lambda e: e.tensor_tensor(out=yt[i2][:, b * 512:(b + 1) * 512], in0=pY[b][:, :], in1=G1[which][:, b * 512:(b + 1) * 512], op=ALU.mult),
                          r=[f"pY{b}", f"G1_{which}"], w=[f"yt{i2}"])
                    V(lambda e: e.tensor_tensor(out=yt[i2][:, :], in0=yt[i2][:, :], in1=xt[i2][:, :], op=ALU.add), r=[f"yt{i2}", f"xtD{i2}"], w=[f"yt{i2}"])
                    S(lambda e: e.dma_start(out=xall[ti * 128:(ti + 1) * 128, :], in_=yt[i2][:, :]), r=[f"yt{i2}"])
            P.barrier()
            if stop_after == ("D", l):
                break

            with ExitStack() as ph:
                aff = tsb(ph, "aff", [128, NT, NE], F32)
                gm = tsb(ph, "gm", [128, NT, NE], F32)
                sloti = tsb(ph, "sloti", [128, NT, NE], I32)
                slotc = tsb(ph, "slotc", [128, NT, NE], I32)
                G2 = [tsb(ph, f"G2_{i}", [128, D], F32) for i in range(2)]
                p13 = ExitStack()
                affT = tsb(p13, "affT", [NE, T], F32)
                maskT = tsb(p13, "maskT", [NE, T], BF16)
                with ExitStack() as p1:
                    wr_f = tsb(p1, "wr_f", [128, 8, NE], F32)
                    wr_b = tsb(p1, "wr_b", [128, 8, NE], BF16)
                    Ar = [tsb(p1, f"Ar2_{i}", [128, D], F32) for i in range(2)]
                    Br = [tsb(p1, f"Br2_{i}", [128, D], F32) for i in range(2)]
                    xt = [tsb(p1, f"xtE{i}", [128, D], F32) for i in range(2)]
                    junk = tsb(p1, "junkE", [128, D], BF16)
                    st1 = tsb(p1, "st1E", [128, 8], F32)
                    hf = tsb(p1, "hfE", [128, D], F32)
                    hb = [tsb(p1, f"hbE{i}", [128, D], BF16) for i in range(2)]
                    hT = tsb(p1, "hTE", [128, 8, 128], BF16)
                    ex = tsb(p1, "ex", [128, NE], F32)
                    pT = tps(p1, "pTE", [128, 1024], BF16)
                    pR = tps(p1, "pR", [128, 512])
                    pAT = tps(p1, "pAT", [128, 512])
                    L(lambda e: e.dma_start(out=wr_f[:, :, :], in_=w_router[l, :, :].rearrange("(k p) n -> p k n", p=128)), w=["wr_f"])
                    V(lambda e: e.tensor_copy(out=wr_b[:, :, :], in_=wr_f[:, :, :]), r=["wr_f"], w=["wr_b"])
                    for which in range(2):
                        load_bcast(Ar[which][:, :], which, 4, f"Ar2_{which}")
                        load_bcast(Br[which][:, :], which, 3, f"Br2_{which}")
                        load_bcast(G2[which][:, :], which, 5, f"G2_{which}")
                    for ti in tiles_act:
                        which = 1 if ti < NCT else 0
                        i2 = ti % 2
                        L(lambda e: e.dma_start(out=xt[i2][:, :], in_=xall[ti * 128:(ti + 1) * 128, :]), w=[f"xtE{i2}"])
                        A(lambda e: e.activation(out=junk[:, :], in_=xt[i2][:, :], func=AF.Square, accum_out=st1[:, 0:1]), r=[f"xtE{i2}"], w=["junkE", "st1E"])
                        rstd_from_ss(st1[:, 0:1], st1[:, 1:2], 1.0 / D, ["st1E"], ["st1Eb"])
                        V(lambda e: e.scalar_tensor_tensor(out=hf[:, :], in0=xt[i2][:, :], scalar=st1[:, 1:2], in1=Ar[which][:, :], op0=ALU.mult, op1=ALU.mult),
                          r=[f"xtE{i2}", "st1Eb", f"Ar2_{which}"], w=["hfE"])
                        V(lambda e: e.tensor_tensor(out=hb[i2][:, :], in0=hf[:, :], in1=Br[which][:, :], op=ALU.add), r=["hfE", f"Br2_{which}"], w=[f"hbE{i2}"])
                        S(lambda e: e.dma_start(out=h2d[ti * 128:(ti + 1) * 128, :], in_=hb[i2][:, :]), r=[f"hbE{i2}"])
                        for kc in range(8):
                            M(lambda e: e.transpose(out=pT[:, kc * 128:(kc + 1) * 128], in_=hb[i2][:, kc * 128:(kc + 1) * 128], identity=ident[:, :]), r=[f"hbE{i2}", "ident"], w=["pTE"])
                        A(lambda e: e.copy(out=hT[:, :, :].rearrange("p k t -> p (k t)"), in_=pT[:, :]), r=["pTE"], w=["hTE"])
                        for kc in range(8):
                            M(lambda e: e.matmul(pR[:, 0:NE], lhsT=hT[:, kc, :], rhs=wr_b[:, kc, :], start=(kc == 0), stop=(kc == 7)), r=["hTE", "wr_b"], w=["pR"])
                        V(lambda e: e.tensor_reduce(out=st1[:, 2:3], in_=pR[:, 0:NE], axis=AX.X, op=ALU.max, negate=True), r=["pR"], w=["st1Ec"])
                        A(lambda e: e.activation(out=ex[:, :], in_=pR[:, 0:NE], func=AF.Exp, bias=st1[:, 2:3], accum_out=st1[:, 3:4]), r=["pR", "st1Ec"], w=["ex", "st1Ed"])
                        V(lambda e: e.reciprocal(out=st1[:, 4:5], in_=st1[:, 3:4]), r=["st1Ed"], w=["st1Ee"])
                        V(lambda e: e.tensor_scalar(out=aff[:, ti, :], in0=ex[:, :], scalar1=st1[:, 4:5], scalar2=None, op0=ALU.mult), r=["ex", "st1Ee"], w=["aff"])
                        M(lambda e: e.transpose(out=pAT[0:NE, 0:128], in_=aff[:, ti, :], identity=identf[:, :]), r=["aff", "identf"], w=["pAT"])
                        A(lambda e: e.copy(out=affT[:, ti * 128:(ti + 1) * 128], in_=pAT[0:NE, 0:128]), r=["pAT"], w=["affT"])
                P.barrier()
                with ExitStack() as p2:
                    work = tsb(p2, "work", [NE, SEQ], F32)
                    mx = tsb(p2, "mx", [NE, 8], F32)
                    sets = [(NCT * 128, T, CAP_L)] + ([(0, NCT * 128, CAP_C)] if not last else [])
                    for (a0, a1, cap) in sets:
                        n = a1 - a0
                        V(lambda e: e.tensor_copy(out=work[:, 0:n], in_=affT[:, a0:a1]), r=["affT"], w=["work"])
                        for r_ in range(cap // 8):
                            V(lambda e: e.max(out=mx[:, :], in_=work[:, 0:n]), r=["work"], w=["mx"])
                            if r_ < cap // 8 - 1:
                                V(lambda e: e.match_replace(out=work[:, 0:n], in_to_replace=mx[:, :], in_values=work[:, 0:n], imm_value=-1.0), r=["work", "mx"], w=["work"])
                        V(lambda e: e.tensor_scalar(out=maskT[:, a0:a1], in0=affT[:, a0:a1], scalar1=mx[:, 7:8], scalar2=None, op0=ALU.is_ge), r=["affT", "mx"], w=["maskT"])
                P.barrier()
                with ExitStack() as p3:
                    mk = tsb(p3, "mk", [128, NE], F32)
                    mkb = tsb(p3, "mkb", [128, NE], BF16)
                    Sf = tsb(p3, "Sf", [128, NE], F32)
                    Sb = tsb(p3, "Sb", [128, NE], BF16)
                    sl = tsb(p3, "sl", [128, NE], F32)
                    hb = [tsb(p3, f"hbS{i}", [128, XW], BF16) for i in range(2)]
                    tok_i = tsb(p3, "tok_i", [128, 2, NT], I32)
                    tokb = tsb(p3, "tokb", [128, NT, 2], BF16)
                    ghi = tsb(p3, "ghi", [128, NE], BF16)
                    zt = tsb(p3, "zt", [128, 2 * D], F32)
                    for hh in range(2):
                        G(lambda e: e.iota(tok_i[hh * 64:(hh + 1) * 64, 0, :], pattern=[[2, NT]], base=hh, channel_multiplier=0), w=["tok_i"])
                        G(lambda e: e.iota(tok_i[hh * 64:(hh + 1) * 64, 1, :], pattern=[[0, NT]], base=0, channel_multiplier=1), w=["tok_i"])
                    V(lambda e: e.tensor_copy(out=tokb[:, :, :].rearrange("p t j -> p j t"), in_=tok_i[:, :, :]), r=["tok_i"], w=["tokb"])
                    V(lambda e: e.memset(zt[:, :], 0.0), w=["zt"])
                    for i in range(2):
                        V(lambda e: e.memset(hb[i][:, D:XW], 0.0), w=[f"hbS{i}"])
                    for ti in range(0, NT, 2):
                        L(lambda e: e.dma_start(out=accd[ti * 128:(ti + 2) * 128, :].rearrange("(j p) c -> p j c", p=128), in_=zt[:, :].rearrange("p (j c) -> p j c", j=2)), r=["zt"])
                    pM = tps(p3, "pM", [128, 1024], BF16)
                    pPos = tps(p3, "pPos", [128, 512])
                    for ti in tiles_act:
                        isc = ti < NCT
                        i2 = ti % 2
                        if ti == 0 or ti == NCT:
                            V(lambda e: e.memset(Sf[:, :], 0.0), w=["Sf"])
                            V(lambda e: e.tensor_copy(out=Sb[:, :], in_=Sf[:, :]), r=["Sf"], w=["Sb"])
                        L(lambda e: e.dma_start(out=hb[i2][:, 0:D], in_=h2d[ti * 128:(ti + 1) * 128, :]), w=[f"hbS{i2}"])
                        M(lambda e: e.transpose(out=pM[:, 0:NE], in_=maskT[:, ti * 128:(ti + 1) * 128], identity=ident[0:NE, 0:NE]), r=["maskT", "ident"], w=["pM"])
                        V(lambda e: e.tensor_copy(out=mk[:, :], in_=pM[:, 0:NE]), r=["pM"], w=["mk"])
                        A(lambda e: e.copy(out=mkb[:, :], in_=pM[:, 0:NE]), r=["pM"], w=["mkb"])
                        M(lambda e: e.matmul(pPos[:, 0:NE], lhsT=Ub[:, :], rhs=mkb[:, :], start=True, stop=False), r=["Ub", "mkb"], w=["pPos"])
                        M(lambda e: e.matmul(pPos[:, 0:NE], lhsT=ones_b[:, :], rhs=Sb[:, :], start=False, stop=True), r=["ones_b", "Sb"], w=["pPos"])
                        V(lambda e: e.tensor_scalar(out=sl[:, :], in0=mk[:, :], scalar1=-BIG, scalar2=BIG, op0=ALU.mult, op1=ALU.add), r=["mk"], w=["sl"])
                        V(lambda e: e.tensor_tensor(out=sl[:, :], in0=pPos[:, 0:NE], in1=sl[:, :], op=ALU.add), r=["pPos", "sl"], w=["sl"])
                        V(lambda e: e.tensor_copy(out=sloti[:, ti, :], in_=sl[:, :]), r=["sl"], w=["sloti"])
                        V(lambda e: e.tensor_scalar(out=sl[:, :], in0=sl[:, :], scalar1=float((CAP_C if isc else CAP_L) - 1), scalar2=None, op0=ALU.min), r=["sl"], w=["sl"])
                        V(lambda e: e.tensor_copy(out=slotc[:, ti, :], in_=sl[:, :]), r=["sl"], w=["slotc"])
                        V(lambda e: e.tensor_tensor(out=gm[:, ti, :], in0=aff[:, ti, :], in1=mk[:, :], op=ALU.mult), r=["aff", "mk"], w=["gm"])
                        V(lambda e: e.tensor_tensor(out=Sf[:, :], in0=Sf[:, :], in1=mk[:, :], op=ALU.add), r=["Sf", "mk"], w=["Sf"])
                        V(lambda e: e.tensor_copy(out=Sb[:, :], in_=Sf[:, :]), r=["Sf"], w=["Sb"])
                        V(lambda e: e.tensor_copy(out=hb[i2][:, D:D + 2], in_=tokb[:, ti, :]), r=["tokb"], w=[f"hbS{i2}"])
                        V(lambda e: e.tensor_copy(out=ghi[:, :], in_=gm[:, ti, :]), r=["gm"], w=["ghi"])
                        V(lambda e: e.tensor_copy(out=hb[i2][:, D + 2:D + 18], in_=ghi[:, :]), r=["ghi"], w=[f"hbS{i2}"])
                        V(lambda e: e.tensor_tensor(out=hb[i2][:, D + 18:D + 34], in0=gm[:, ti, :], in1=ghi[:, :], op=ALU.subtract), r=["gm", "ghi"], w=[f"hbS{i2}"])
                        for ex_ in range(NE):
                            dst = xe_c[ex_] if isc else xe_l[ex_]
                            bnd = (CAP_C if isc else CAP_L) - 1
                            S(lambda e: e.indirect_dma_start(out=dst, out_offset=bass.IndirectOffsetOnAxis(ap=sloti[:, ti, ex_:ex_ + 1], axis=0),
                                                             in_=hb[i2][:, :], in_offset=None, bounds_check=REG[bnd], oob_is_err=False),
                              r=[f"hbS{i2}", "sloti"])
                    if "dbg_aff" in dbg:
                        S(lambda e: e.dma_start(out=dbg_aff, in_=aff[:, :, :].rearrange("p t e -> p (t e)")), r=["aff"])
                        S(lambda e: e.dma_start(out=dbg_slot, in_=sloti[:, :, :].rearrange("p t e -> p (t e)")), r=["sloti"])
                P.barrier()
                p13.close()
                if stop_after == ("E3", l):
                    break
                with ExitStack() as p4:
                    NJ = CAP_L + (CAP_C if not last else 0)
                    stg = [tsb(p4, f"stg{i}", [128, 8, D], F32) for i in range(2)]
                    wb = [tsb(p4, f"wb{i}", [128, 8, D], BF16) for i in range(4)]
                    xet = [tsb(p4, f"xet{i}", [128, XW], BF16) for i in range(2)]
                    sideb = [tsb(p4, f"sideb{i}", [128, 5, 36], BF16) for i in range(2)]
                    gatef = [tsb(p4, f"gatef{i}", [128, 5], F32) for i in range(2)]
                    idxf = [tsb(p4, f"idxf{i}", [128, 5], F32) for i in range(2)]
                    idxi = [tsb(p4, f"idxi{i}", [128, 5], I32) for i in range(2)]
                    xeT = [tsb(p4, f"xeT{i}", [128, 8, CAP_L + CAP_C], BF16) for i in range(2)]
                    hidT = tsb(p4, "hidT", [128, 8, CAP_L + CAP_C], BF16)
                    sg = [tsb(p4, f"sg{i}", [128, CAP_L + CAP_C], F32) for i in range(2)]
                    yeb = [tsb(p4, f"yeb{i}", [128, D], BF16) for i in range(2)]
                    pT = tps(p4, "pT4", [128, 1024], BF16)
                    pG = tps(p4, "pG", [128, 512]); pU = tps(p4, "pU", [128, 512])
                    pGc = tps(p4, "pGc", [128, 512])
                    pUc = tps(p4, "pUc", [128, 512])
                    pY = [tps(p4, f"pY4_{i}", [128, 512]) for i in range(2)]
                    cnt4 = {"w": 0, "s": 0, "x": 0, "g": 0, "y": 0}

                    def load_w(src):
                        si = cnt4["s"] % 2; cnt4["s"] += 1
                        wi = cnt4["w"] % 4; cnt4["w"] += 1
                        for h in range(4):
                            L(lambda e: e.dma_start(out=stg[si][:, 2 * h:2 * h + 2, :], in_=src[h * 256:(h + 1) * 256, :].rearrange("(k p) n -> p k n", p=128)), w=[f"stg{si}h{h}"])
                        V(lambda e: e.tensor_copy(out=wb[wi][:, 0:2, :], in_=stg[si][:, 0:2, :]), r=[f"stg{si}h0"], w=[f"wb{wi}a"])
                        A(lambda e: e.copy(out=wb[wi][:, 2:4, :], in_=stg[si][:, 2:4, :]), r=[f"stg{si}h1"], w=[f"wb{wi}b"])
                        V(lambda e: e.tensor_copy(out=wb[wi][:, 4:5, :], in_=stg[si][:, 4:5, :]), r=[f"stg{si}h2"], w=[f"wb{wi}c"])
                        A(lambda e: e.copy(out=wb[wi][:, 5:6, :], in_=stg[si][:, 5:6, :]), r=[f"stg{si}h2"], w=[f"wb{wi}d"])
                        G(lambda e: e.tensor_copy(out=wb[wi][:, 6:8, :], in_=stg[si][:, 6:8, :]), r=[f"stg{si}h3"], w=[f"wb{wi}e"])
                        return wb[wi], [f"wb{wi}{c}" for c in "abcde"]

                    def stage_x(ex_):
                        xT = xeT[ex_ % 2]; xk = f"xeT{ex_ % 2}"
                        jts = [(xe_l[ex_][j * 128:(j + 1) * 128, :], 128, j * 128) for j in range(4)]
                        if not last:
                            jts.append((xe_c[ex_][:, :], CAP_C, CAP_L))
                        for jt, (src, nj, c0) in enumerate(jts):
                            xi = cnt4["x"] % 2; cnt4["x"] += 1
                            L(lambda e: e.dma_start(out=xet[xi][0:nj, :], in_=src), w=[f"xet{xi}"])
                            e2 = ex_ % 2
                            V(lambda e: e.tensor_copy(out=sideb[e2][0:nj, jt, :], in_=xet[xi][0:nj, D:XW]), r=[f"xet{xi}"], w=[f"sideb{e2}"])
                            V(lambda e: e.tensor_tensor(out=gatef[e2][0:nj, jt:jt + 1], in0=sideb[e2][0:nj, jt, 2 + ex_:3 + ex_], in1=sideb[e2][0:nj, jt, 18 + ex_:19 + ex_], op=ALU.add),
                              r=[f"sideb{e2}"], w=[f"gatef{e2}"])
                            V(lambda e: e.scalar_tensor_tensor(out=idxf[e2][0:nj, jt:jt + 1], in0=sideb[e2][0:nj, jt, 0:1], scalar=64.0, in1=sideb[e2][0:nj, jt, 1:2], op0=ALU.mult, op1=ALU.add),
                              r=[f"sideb{e2}"], w=[f"idxf{e2}"])
                            V(lambda e: e.tensor_copy(out=idxi[e2][0:nj, jt:jt + 1], in_=idxf[e2][0:nj, jt:jt + 1]), r=[f"idxf{e2}"], w=[f"idxi{e2}"])
                            for kc in range(8):
                                M(lambda e: e.transpose(out=pT[:, kc * 128:kc * 128 + nj], in_=xet[xi][0:nj, kc * 128:(kc + 1) * 128], identity=ident[0:nj, 0:nj]),
                                  r=[f"xet{xi}", "ident"], w=["pT4"])
                            V(lambda e: e.tensor_copy(out=xT[:, :, c0:c0 + nj], in_=pT[:, :].rearrange("p (k t) -> p k t", k=8)[:, :, 0:nj]), r=["pT4"], w=[xk])

                    def stage_hidden(ex_, wg, wgk, wu, wuk):
                        xT = xeT[ex_ % 2]; xk = f"xeT{ex_ % 2}"
                        for fc in range(8):
                            for kc in range(8):
                                M(lambda e: e.matmul(pG[:, :], lhsT=wg[:, kc, fc * 128:(fc + 1) * 128], rhs=xT[:, kc, 0:CAP_L], start=(kc == 0), stop=(kc == 7)), r=wgk + [xk], w=["pG"])
                            for kc in range(8):
                                M(lambda e: e.matmul(pU[:, :], lhsT=wu[:, kc, fc * 128:(fc + 1) * 128], rhs=xT[:, kc, 0:CAP_L], start=(kc == 0), stop=(kc == 7)), r=wuk + [xk], w=["pU"])
                            gi = cnt4["g"] % 2; cnt4["g"] += 1
                            A(lambda e: e.activation(out=sg[gi][:, 0:CAP_L], in_=pG[:, :], func=AF.Silu), r=["pG"], w=[f"sg{gi}"])
                            V(lambda e: e.tensor_tensor(out=hidT[:, fc, 0:CAP_L], in0=pU[:, :], in1=sg[gi][:, 0:CAP_L], op=ALU.mult), r=["pU", f"sg{gi}"], w=["hidT"])
                            if not last:
                                for kc in range(8):
                                    M(lambda e: e.matmul(pGc[:, 0:CAP_C], lhsT=wg[:, kc, fc * 128:(fc + 1) * 128], rhs=xT[:, kc, CAP_L:NJ], start=(kc == 0), stop=(kc == 7)), r=wgk + [xk], w=["pGc0"])
                                for kc in range(8):
                                    M(lambda e: e.matmul(pUc[:, 0:CAP_C], lhsT=wu[:, kc, fc * 128:(fc + 1) * 128], rhs=xT[:, kc, CAP_L:NJ], start=(kc == 0), stop=(kc == 7)), r=wuk + [xk], w=["pGc1"])
                                A(lambda e: e.activation(out=sg[gi][:, CAP_L:NJ], in_=pGc[:, 0:CAP_C], func=AF.Silu), r=["pGc0"], w=[f"sg{gi}c"])
                                V(lambda e: e.tensor_tensor(out=hidT[:, fc, CAP_L:NJ], in0=pUc[:, 0:CAP_C], in1=sg[gi][:, CAP_L:NJ], op=ALU.mult), r=["pGc1", f"sg{gi}c"], w=["hidTc"])

                    def stage_down(ex_, wd, wdk):
                        outs = [(None, 128, j * 128) for j in range(4)]
                        if not last:
                            outs.append((None, CAP_C, CAP_L))
                        e2 = ex_ % 2
                        for jt, (dst, nj, c0) in enumerate(outs):
                            gate_ap = gatef[e2][0:nj, jt:jt + 1]
                            idx_ap = idxi[e2][0:nj, jt:jt + 1]
                            yi = cnt4["y"] % 2; cnt4["y"] += 1
                            for dh in range(2):
                                for fc in range(8):
                                    M(lambda e: e.matmul(pY[dh][0:nj, :], lhsT=hidT[:, fc, c0:c0 + nj], rhs=wd[:, fc, dh * 512:(dh + 1) * 512], start=(fc == 0), stop=(fc == 7)),
                                      r=["hidT", "hidTc"] + wdk, w=[f"pY4_{dh}"])
                            A(lambda e: e.activation(out=yeb[yi][0:nj, 0:512], in_=pY[0][0:nj, :], func=AF.Identity, scale=gate_ap), r=["pY4_0", f"gatef{e2}"], w=[f"yeb{yi}a"])
                            V(lambda e: e.tensor_scalar(out=yeb[yi][0:nj, 512:1024], in0=pY[1][0:nj, :], scalar1=gate_ap, scalar2=None, op0=ALU.mult), r=["pY4_1", f"gatef{e2}"], w=[f"yeb{yi}b"])
                            S(lambda e: e.indirect_dma_start(out=accd[:, :], out_offset=bass.IndirectOffsetOnAxis(ap=idx_ap, axis=0),
                                                             in_=yeb[yi][0:nj, :], in_offset=None, compute_op=ALU.add),
                              r=[f"yeb{yi}a", f"yeb{yi}b", f"idxi{e2}"], w=["accd"])

                    wg, wgk = load_w(w_e_gate[l, 0])
                    wu, wuk = load_w(w_e_up[l, 0])
                    stage_x(0)
                    for ex_ in range(NE):
                        wd, wdk = load_w(w_e_down[l, ex_])
                        stage_hidden(ex_, wg, wgk, wu, wuk)
                        if ex_ + 1 < NE:
                            wg, wgk = load_w(w_e_gate[l, ex_ + 1])
                            wu, wuk = load_w(w_e_up[l, ex_ + 1])
                            stage_x(ex_ + 1)
                        stage_down(ex_, wd, wdk)
                P.barrier()
                if stop_after == ("E4", l):
                    break
                with ExitStack() as p5:
                    xt = [tsb(p5, f"xt5_{i}", [128, D], F32) for i in range(3)]
                    acc = [tsb(p5, f"acc{i}", [128, D], F32) for i in range(3)]
                    for n_, ti in enumerate(tiles_act):
                        which = 1 if ti < NCT else 0
                        i2 = n_ % 3
                        L(lambda e: e.dma_start(out=xt[i2][:, :], in_=xall[ti * 128:(ti + 1) * 128, :]), w=[f"xt5_{i2}"])
                        L(lambda e: e.dma_start(out=acc[i2][:, :], in_=accd[ti * 128:(ti + 1) * 128, :]), w=[f"acc{i2}"])
                        V(lambda e: e.tensor_tensor(out=acc[i2][:, :], in0=acc[i2][:, :], in1=G2[which][:, :], op=ALU.mult), r=[f"acc{i2}", f"G2_{which}"], w=[f"acc{i2}"])
                        V(lambda e: e.tensor_tensor(out=acc[i2][:, :], in0=acc[i2][:, :], in1=xt[i2][:, :], op=ALU.add), r=[f"acc{i2}", f"xt5_{i2}"], w=[f"acc{i2}"])
                        if last:
                            S(lambda e: e.dma_start(out=y_out[(ti - NCT) * 128:(ti - NCT + 1) * 128, :], in_=acc[i2][:, :]), r=[f"acc{i2}"])
                        else:
                            S(lambda e: e.dma_start(out=xall[ti * 128:(ti + 1) * 128, :], in_=acc[i2][:, :]), r=[f"acc{i2}"])
            P.barrier()
            if stop_after == ("L", l):
                break
        P.barrier()
        print("instr counts", P.ninstr, "total", P.total, flush=True)
    return nc


def _rope_table():
    t = np.arange(SEQ)
    row = (t // 64).astype(np.float64)
    col = (t % 64).astype(np.float64)
    out = np.zeros((SEQ, 192), np.float64)

    def fill(off, dim):
        ad = dim // 2
        inv = 10000.0 ** (-np.arange(0, ad, 2, dtype=np.float64) / ad)
        ar = row[:, None] * inv[None, :]
        ac = col[:, None] * inv[None, :]
        C = np.concatenate([np.cos(ar), np.cos(ar), np.cos(ac), np.cos(ac)], axis=1)
        S_ = np.concatenate([-np.sin(ar), np.sin(ar), -np.sin(ac), np.sin(ac)], axis=1)
        out[:, off:off + dim] = C
        out[:, off + dim:off + 2 * dim] = S_
    fill(0, 64)
    fill(128, 32)
    return out.astype(np.float32)


_NC_CACHE = {}


def kernel(**inputs):
    key = "full"
    if key not in _NC_CACHE:
        _NC_CACHE[key] = build()
    nc = _NC_CACHE[key]
    rope = _rope_table()
    shared = {k: np.ascontiguousarray(np.asarray(v, dtype=np.float32)) for k, v in inputs.items() if k not in ("x", "c", "ctx")}
    x = np.asarray(inputs["x"], dtype=np.float32); c = np.asarray(inputs["c"], dtype=np.float32); ctx = np.asarray(inputs["ctx"], dtype=np.float32)
    in_maps = []
    for b in range(8):
        m = dict(shared)
        m["x"] = np.ascontiguousarray(x[b]); m["ctx"] = np.ascontiguousarray(ctx[b]); m["c"] = np.ascontiguousarray(c[b])
        m["rope"] = rope
        in_maps.append(m)
    res = run_bass_kernel_spmd(nc, in_maps, core_ids=list(range(8)))
    return np.stack([np.asarray(r["y"], dtype=np.float32) for r in res.results], axis=0)
```

```python
import numpy as np
from contextlib import ExitStack
import concourse.bass as bass
import concourse.mybir as mybir
from concourse.bass_utils import run_bass_kernel_spmd

F32 = mybir.dt.float32
BF16 = mybir.dt.bfloat16
I32 = mybir.dt.int32
AF = mybir.ActivationFunctionType
ALU = mybir.AluOpType
AX = mybir.AxisListType

D = 1024
SEQ = 4096
CTX = 256
T = SEQ + CTX
NT = T // 128
NCT = CTX // 128
DEPTH = 2
INW = 1504
NE = 16
CAP_L = 512
CAP_C = 32
XW = D + 36
EPS = 1e-6
BIG = 8192.0


def _is_psum(k):
    return k.startswith("pmod") or (len(k) > 1 and k[0] == "p" and k[1].isupper())


class Prog:
    ENG = ("pe", "dve", "act", "pool", "sp")

    def __init__(self, nc, stack, same_engine_sync=True, ring=(("sp", 16), ("pool", 16))):
        self.nc = nc
        self.stack = stack
        self.e = {"pe": nc.tensor, "dve": nc.vector, "act": nc.scalar, "pool": nc.gpsimd, "sp": nc.sync}
        self.same = same_engine_sync
        self.sems = {}
        self.semval = {}
        self.cur = {}
        self.epoch = {n: 0 for n in self.ENG}
        for n in self.ENG:
            self.cur[n] = self._mk("E_" + n + "_0")
        self.ring = {}
        self.ringpos = {}
        for q, k in ring:
            self.ring[q] = [self._mk(f"D_{q}{i}") for i in range(k)]
            self.ringpos[q] = 0
        self.waited = {n: {} for n in self.ENG}
        self.lastw = {}
        self.readers = {}
        self.ninstr = {n: 0 for n in self.ENG}

    def _mk(self, name):
        s = self.stack.enter_context(self.nc.semaphore(name))
        self.sems[name] = s
        self.semval[name] = 0
        return name

    def _wait(self, eng, ev):
        sem, val = ev
        if val <= 0 or self.waited[eng].get(sem, 0) >= val:
            return
        if sem == self.cur[eng] and (eng == "pe" or not self.same):
            return
        self.e[eng].wait_ge(self.sems[sem], val)
        self.waited[eng][sem] = val

    def _deps(self, eng, r, w):
        for res in list(r) + list(w):
            ev = self.lastw.get(res)
            if ev is not None:
                self._wait(eng, ev)
        for res in w:
            for sem, val in self.readers.get(res, {}).items():
                self._wait(eng, (sem, val))

    def _record(self, ev, r, w):
        for res in w:
            self.lastw[res] = ev
            self.readers[res] = {}
        for res in r:
            d = self.readers.setdefault(res, {})
            if d.get(ev[0], 0) < ev[1]:
                d[ev[0]] = ev[1]

    LIMIT = None
    total = 0

    def op(self, eng, fn, r=(), w=()):
        self.total += 1
        if self.LIMIT is not None and self.total > self.LIMIT:
            return
        w = list(w) + [k for k in r if _is_psum(k)]
        r = [k for k in r if not _is_psum(k)]
        self._deps(eng, r, w)
        ins = fn(self.e[eng])
        sem = self.cur[eng]
        self.semval[sem] += 1
        ins.then_inc(self.sems[sem], 1)
        self.ninstr[eng] += 1
        self._record((sem, self.semval[sem]), r, w)

    def dma(self, q, fn, r=(), w=()):
        self.total += 1
        if self.LIMIT is not None and self.total > self.LIMIT:
            return
        self._deps(q, r, w)
        k = self.ringpos[q]
        sem = self.ring[q][k]
        self.ringpos[q] = (k + 1) % len(self.ring[q])
        self._wait(q, (sem, self.semval[sem]))
        ins = fn(self.e[q])
        self.semval[sem] += 16
        ins.then_inc(self.sems[sem], 16)
        self.ninstr[q] += 1
        self._record((sem, self.semval[sem]), r, w)

    def barrier(self):
        for eng in self.ENG:
            for sem, val in self.semval.items():
                self._wait(eng, (sem, val))
        self.lastw.clear()
        self.readers.clear()
        for eng in self.ENG:
            if self.semval[self.cur[eng]] > 15000:
                self.epoch[eng] += 1
                self.cur[eng] = self._mk(f"E_{eng}_{self.epoch[eng]}")


def build(stop_after=None, dbg=(), limit=None, light=False):
    nc = bass.Bass("TRN2", target_bir_lowering=False)
    Prog.LIMIT = limit

    def din(name, shape, dt=F32):
        return nc.dram_tensor(name, list(shape), dt, kind="ExternalInput").ap()

    x_in = din("x", [SEQ, D]); ctx_in = din("ctx", [CTX, D]); c_in = din("c", [D]); cctx_in = din("c_ctx", [D])
    w_ada = din("w_ada", [DEPTH, D, 6 * D]); b_ada = din("b_ada", [DEPTH, 6 * D])
    norm1_g = din("norm1_g", [DEPTH, D]); norm2_g = din("norm2_g", [DEPTH, D])
    w_in = din("w_in", [DEPTH, D, INW])
    a_q_norm = din("a_q_norm", [DEPTH, 64]); a_k_norm = din("a_k_norm", [DEPTH, 64])
    b_cq_norm = din("b_cq_norm", [DEPTH, 192]); b_ckv_norm = din("b_ckv_norm", [DEPTH, 128])
    w_uq = din("w_uq", [DEPTH, 192, 576]); w_ukv = din("w_ukv", [DEPTH, 128, 768])
    b_qn_norm = din("b_qn_norm", [DEPTH, 64]); b_kn_norm = din("b_kn_norm", [DEPTH, 64])
    b_qr_norm = din("b_qr_norm", [DEPTH, 32]); b_kr_norm = din("b_kr_norm", [DEPTH, 32])
    c_q_norm = din("c_q_norm", [DEPTH, 64]); c_k_norm = din("c_k_norm", [DEPTH, 64])
    c_sink = din("c_sink", [DEPTH, 4])
    w_out = din("w_out", [DEPTH, D, D]); w_router = din("w_router", [DEPTH, D, NE])
    if not light:
        w_e_gate = din("w_e_gate", [DEPTH, NE, D, D]); w_e_up = din("w_e_up", [DEPTH, NE, D, D])
        w_e_down = din("w_e_down", [DEPTH, NE, D, D])
    rope = din("rope", [SEQ, 192])
    y_out = nc.dram_tensor("y", [SEQ, D], F32, kind="ExternalOutput").ap()

    def dscr(name, shape, dt):
        kind = "ExternalOutput" if name in dbg else "Internal"
        return nc.dram_tensor(name, list(shape), dt, kind=kind).ap()

    xall = dscr("xall", [T, D], F32)
    modv = dscr("modv", [2, 6 * D], F32)
    qkT_A = dscr("qkT_A", [512, T], BF16)
    qkT_B = dscr("qkT_B", [12, 96, T], BF16)
    qkT_C = dscr("qkT_C", [384, T], BF16)
    vall = dscr("vall", [T, 640], BF16)
    mix = dscr("mix", [T, D], BF16)
    h2d = dscr("h2d", [T, D], BF16)
    xe_l = [dscr(f"xe_l{i}", [CAP_L, XW], BF16) for i in range(NE)]
    xe_c = [dscr(f"xe_c{i}", [CAP_C, XW], BF16) for i in range(NE)]
    accd = dscr("accd", [T, D], F32)
    aT_d = dscr("aT_d", [NE, SEQ], F32)
    mk_d = dscr("mk_d", [NE, SEQ], BF16)
    ye_l = [dscr(f"ye_l{i}", [CAP_L, D], BF16) for i in range(NE)]
    ye_c = [dscr(f"ye_c{i}", [CAP_C, D], BF16) for i in range(NE)]
    dbg_aff = dscr("dbg_aff", [128, NT * NE], F32)
    dbg_slot = dscr("dbg_slot", [128, NT * NE], I32)

    with ExitStack() as top:
        P = Prog(nc, top)
        V = lambda fn, r=(), w=(): P.op("dve", fn, r, w)
        A = lambda fn, r=(), w=(): P.op("act", fn, r, w)
        G = lambda fn, r=(), w=(): P.op("pool", fn, r, w)
        M = lambda fn, r=(), w=(): P.op("pe", fn, r, w)
        L = lambda fn, r=(), w=(): P.dma("sp", fn, r, w)
        S = lambda fn, r=(), w=(): P.dma("pool", fn, r, w)

        LAYER = [-1]

        def tsb(stk, name, shape, dt):
            return stk.enter_context(nc.sbuf_tensor(f"{name}_L{LAYER[0]}", list(shape), dt))

        def tps(stk, name, shape, dt=F32):
            return stk.enter_context(nc.psum_tensor(f"{name}_L{LAYER[0]}", list(shape), dt))

        REG = {CAP_L - 1: nc.gpsimd.to_reg(CAP_L - 1), CAP_C - 1: nc.gpsimd.to_reg(CAP_C - 1)}
        identf = tsb(top, "identf", [128, 128], F32)
        ident = tsb(top, "ident", [128, 128], BF16)
        onesf = tsb(top, "onesf", [128, 128], F32)
        ones_b = tsb(top, "ones_b", [128, 128], BF16)
        Ub = tsb(top, "Ub", [128, 128], BF16)
        M1 = tsb(top, "M1", [128, 128], BF16)
        M2 = tsb(top, "M2", [128, 128], BF16)
        ctmp = tsb(top, "ctmp", [128, 128], F32)
        cs = tsb(top, "cs", [128, 8, 2], F32)
        craw = tsb(top, "craw", [128, 2, 8], F32)

        G(lambda e: e.memset(identf[:], 0.0), w=["identf"])
        G(lambda e: e.affine_select(out=identf[:], in_=identf[:], pattern=[[-1, 128]], base=0, channel_multiplier=1,
                                    compare_op=ALU.not_equal, fill=1.0), r=["identf"], w=["identf"])
        V(lambda e: e.tensor_copy(out=ident[:], in_=identf[:]), r=["identf"], w=["ident"])
        G(lambda e: e.memset(onesf[:], 1.0), w=["onesf"])
        V(lambda e: e.tensor_copy(out=ones_b[:], in_=onesf[:]), r=["onesf"], w=["ones_b"])
        G(lambda e: e.affine_select(out=ctmp[:], in_=onesf[:], pattern=[[-1, 128]], base=0, channel_multiplier=1,
                                    compare_op=ALU.is_ge, fill=0.0), r=["onesf"], w=["ctmp"])
        V(lambda e: e.tensor_copy(out=M1[:], in_=ctmp[:]), r=["ctmp"], w=["M1"])
        G(lambda e: e.affine_select(out=ctmp[:], in_=onesf[:], pattern=[[1, 128]], base=0, channel_multiplier=-1,
                                    compare_op=ALU.is_ge, fill=0.0), r=["onesf"], w=["ctmp"])
        V(lambda e: e.tensor_copy(out=M2[:], in_=ctmp[:]), r=["ctmp"], w=["M2"])
        G(lambda e: e.affine_select(out=ctmp[:], in_=onesf[:], pattern=[[1, 128]], base=-1, channel_multiplier=-1,
                                    compare_op=ALU.is_ge, fill=0.0), r=["onesf"], w=["ctmp"])
        V(lambda e: e.tensor_copy(out=Ub[:], in_=ctmp[:]), r=["ctmp"], w=["Ub"])

        L(lambda e: e.dma_start(out=craw[:, 0, :], in_=c_in.rearrange("(k p) -> p k", p=128), allow_slow_non_contiguous=True), w=["craw"])
        L(lambda e: e.dma_start(out=craw[:, 1, :], in_=cctx_in.rearrange("(k p) -> p k", p=128), allow_slow_non_contiguous=True), w=["craw"])
        A(lambda e: e.activation(out=cs[:, :, :].rearrange("p k j -> p j k"), in_=craw[:, :, :], func=AF.Silu), r=["craw"], w=["cs"])

        L(lambda e: e.dma_start(out=xall[0:CTX, :], in_=ctx_in))
        for i in range(8):
            L(lambda e: e.dma_start(out=xall[CTX + i * 512:CTX + (i + 1) * 512, :], in_=x_in[i * 512:(i + 1) * 512, :]))
        P.barrier()

        def rstd_from_ss(ss_ap, out_ap, scale, r, w):
            V(lambda e: e.tensor_scalar(out=out_ap, in0=ss_ap, scalar1=scale, scalar2=EPS, op0=ALU.mult, op1=ALU.add), r=r, w=w)
            A(lambda e: e.activation(out=out_ap, in_=out_ap, func=AF.Sqrt), r=w, w=w)
            V(lambda e: e.reciprocal(out=out_ap, in_=out_ap), r=w, w=w)

        for l in range(DEPTH):
            last = l == DEPTH - 1
            LAYER[0] = l
            tiles_act = list(range(NT)) if not last else list(range(NCT, NT))

            with ExitStack() as ph:
                wst = [tsb(ph, f"wstA{i}", [128, 8, 512], F32) for i in range(2)]
                modrow = tsb(ph, "modrow", [2, 6 * D], F32)
                bada = tsb(ph, "bada", [2, 6 * D], F32)
                gn = tsb(ph, "gn", [2, 2, D], F32)
                pmod = [tps(ph, f"pmod{i}", [128, 512]) for i in range(2)]
                L(lambda e: e.dma_start(out=bada[:, :], in_=b_ada[l:l + 1, :].broadcast_to([2, 6 * D])), w=["bada"])
                L(lambda e: e.dma_start(out=gn[:, 0, :], in_=norm1_g[l:l + 1, :].broadcast_to([2, D])), w=["gn"])
                L(lambda e: e.dma_start(out=gn[:, 1, :], in_=norm2_g[l:l + 1, :].broadcast_to([2, D])), w=["gn"])
                for nb in range(12):
                    wb = wst[nb % 2]; pm = pmod[nb % 2]
                    for h in range(2):
                        L(lambda e: e.dma_start(out=wb[:, h * 4:(h + 1) * 4, :],
                                                in_=w_ada[l, h * 512:(h + 1) * 512, nb * 512:(nb + 1) * 512].rearrange("(k p) n -> p k n", p=128)),
                          w=[f"wstA{nb % 2}"])
                    for kc in range(8):
                        M(lambda e: e.matmul(pm[0:2, :], lhsT=cs[:, kc, :], rhs=wb[:, kc, :], start=(kc == 0), stop=(kc == 7)),
                          r=[f"wstA{nb % 2}", "cs"], w=[f"pmod{nb % 2}"])
                    V(lambda e: e.tensor_tensor(out=modrow[:, nb * 512:(nb + 1) * 512], in0=pm[0:2, :], in1=bada[:, nb * 512:(nb + 1) * 512], op=ALU.add),
                      r=[f"pmod{nb % 2}", "bada"], w=["modrow"])
                for j, off in ((0, 1 * D), (1, 4 * D)):
                    V(lambda e: e.scalar_tensor_tensor(out=modrow[:, off:off + D], in0=modrow[:, off:off + D], scalar=1.0, in1=gn[:, j, :],
                                                       op0=ALU.add, op1=ALU.mult), r=["modrow", "gn"], w=["modrow"])
                S(lambda e: e.dma_start(out=modv, in_=modrow[:, :]), r=["modrow"])
            P.barrier()
            if stop_after == ("A", l):
                break

            def load_bcast(dst, which, j, key):
                L(lambda e: e.dma_start(out=dst, in_=modv[which:which + 1, j * D:(j + 1) * D].broadcast_to([128, D])), w=[key])

            with ExitStack() as ph:
                wst = tsb(ph, "wstB", [128, 8, 512], F32)
                w_in_b = tsb(ph, "w_in_b", [128, 8, INW], BF16)
                wuq_f = tsb(ph, "wuq_f", [128, 2, 576], F32)
                wuq_b = tsb(ph, "wuq_b", [128, 2, 576], BF16)
                wukv_f = tsb(ph, "wukv_f", [128, 768], F32)
                wukv_b = tsb(ph, "wukv_b", [128, 768], BF16)
                gcq = tsb(ph, "gcq", [128, 2], F32)
                gckv = tsb(ph, "gckv", [128, 1], F32)
                gA = tsb(ph, "gA", [128, 8, 64], F32)
                gC = tsb(ph, "gC", [128, 6, 64], F32)
                gBq = tsb(ph, "gBq", [128, 96], F32)
                gBk = tsb(ph, "gBk", [128, 64], F32)
                gBkr = tsb(ph, "gBkr", [128, 32], F32)
                invB = tsb(ph, "invB", [128, 3], F32)
                invQ = tsb(ph, "invQ", [128, 12], F32)
                Ar = [tsb(ph, f"Ar{i}", [128, D], F32) for i in range(2)]
                Br = [tsb(ph, f"Br{i}", [128, D], F32) for i in range(2)]
                xt = [tsb(ph, f"xtB{i}", [128, D], F32) for i in range(2)]
                rp = [tsb(ph, f"rp{i}", [128, 192], F32) for i in range(2)]
                junk = tsb(ph, "junkB", [128, D], BF16)
                st1 = tsb(ph, "st1", [128, 4], F32)
                hf = tsb(ph, "hf", [128, D], F32)
                hb = tsb(ph, "hb", [128, D], BF16)
                hT = tsb(ph, "hT", [128, 8, 128], BF16)
                sq = tsb(ph, "sq", [128, 576], F32)
                ssh = tsb(ph, "ssh", [128, 16], F32)
                qn = tsb(ph, "qn", [128, 576], F32)
                t1 = tsb(ph, "t1", [128, 576], F32)
                t2 = tsb(ph, "t2", [128, 576], F32)
                qA = tsb(ph, "qA", [128, 8, 64], BF16)
                qC = tsb(ph, "qC", [128, 6, 64], BF16)
                qB = tsb(ph, "qB", [128, 12, 96], BF16)
                cqn = tsb(ph, "cqn", [128, 320], BF16)
                cT = tsb(ph, "cT", [128, 3, 128], BF16)
                kpe = tsb(ph, "kpe", [128, 32], F32)
                kpe2 = tsb(ph, "kpe2", [128, 32], F32)
                vst = tsb(ph, "vst", [128, 640], BF16)
                stA = tsb(ph, "stA", [128, 4, 512], BF16)
                stB = tsb(ph, "stB", [128, 12, 512], BF16)
                stC = tsb(ph, "stC", [128, 3, 512], BF16)
                pT = tps(ph, "pTB", [128, 1024], BF16)
                pP = [tps(ph, f"pP{i}", [128, 512]) for i in range(3)]
                pQ = [tps(ph, f"pQ{i}", [128, 512]) for i in range(2)]
                pK = [tps(ph, f"pK{i}", [128, 512]) for i in range(2)]

                for b, (c0, c1) in enumerate(((0, 512), (512, 1024), (1024, INW))):
                    n = c1 - c0
                    for h in range(2):
                        L(lambda e: e.dma_start(out=wst[:, h * 4:(h + 1) * 4, 0:n],
                                                in_=w_in[l, h * 512:(h + 1) * 512, c0:c1].rearrange("(k p) n -> p k n", p=128)), w=["wstB"])
                    V(lambda e: e.tensor_copy(out=w_in_b[:, 0:4, c0:c1], in_=wst[:, 0:4, 0:n]), r=["wstB"], w=["w_in_b"])
                    G(lambda e: e.tensor_copy(out=w_in_b[:, 4:8, c0:c1], in_=wst[:, 4:8, 0:n]), r=["wstB"], w=["w_in_b"])
                L(lambda e: e.dma_start(out=wuq_f[:, 0, :], in_=w_uq[l, 0:128, :]), w=["wuq_f"])
                L(lambda e: e.dma_start(out=wuq_f[0:64, 1, :], in_=w_uq[l, 128:192, :]), w=["wuq_f"])
                L(lambda e: e.dma_start(out=wukv_f[:, :], in_=w_ukv[l, :, :]), w=["wukv_f"])
                L(lambda e: e.dma_start(out=gcq[:, 0:1], in_=b_cq_norm[l, 0:128].rearrange("(p o) -> p o", o=1)), w=["gcq"])
                L(lambda e: e.dma_start(out=gcq[0:64, 1:2], in_=b_cq_norm[l, 128:192].rearrange("(p o) -> p o", o=1)), w=["gcq"])
                L(lambda e: e.dma_start(out=gckv[:, 0:1], in_=b_ckv_norm[l, :].rearrange("(p o) -> p o", o=1)), w=["gckv"])
                V(lambda e: e.tensor_scalar(out=wuq_b[:, 0, :], in0=wuq_f[:, 0, :], scalar1=gcq[:, 0:1], scalar2=None, op0=ALU.mult), r=["wuq_f", "gcq"], w=["wuq_b"])
                V(lambda e: e.tensor_scalar(out=wuq_b[0:64, 1, :], in0=wuq_f[0:64, 1, :], scalar1=gcq[0:64, 1:2], scalar2=None, op0=ALU.mult), r=["wuq_f", "gcq"], w=["wuq_b"])
                V(lambda e: e.tensor_scalar(out=wukv_b[:, :], in0=wukv_f[:, :], scalar1=gckv[:, 0:1], scalar2=None, op0=ALU.mult), r=["wukv_f", "gckv"], w=["wukv_b"])
                for hh in range(8):
                    src = a_q_norm if hh < 6 else a_k_norm
                    L(lambda e: e.dma_start(out=gA[:, hh, :], in_=src[l:l + 1, :].broadcast_to([128, 64])), w=["gA"])
                for hh in range(6):
                    src = c_q_norm if hh < 4 else c_k_norm
                    L(lambda e: e.dma_start(out=gC[:, hh, :], in_=src[l:l + 1, :].broadcast_to([128, 64])), w=["gC"])
                L(lambda e: e.dma_start(out=gBq[:, 0:64], in_=b_qn_norm[l:l + 1, :].broadcast_to([128, 64])), w=["gBq"])
                L(lambda e: e.dma_start(out=gBq[:, 64:96], in_=b_qr_norm[l:l + 1, :].broadcast_to([128, 32])), w=["gBq"])
                L(lambda e: e.dma_start(out=gBk[:, :], in_=b_kn_norm[l:l + 1, :].broadcast_to([128, 64])), w=["gBk"])
                L(lambda e: e.dma_start(out=gBkr[:, :], in_=b_kr_norm[l:l + 1, :].broadcast_to([128, 32])), w=["gBkr"])
                for j, v in enumerate((1.0 / 192, 1.0 / 128, 1.0 / 32)):
                    G(lambda e: e.memset(invB[:, j:j + 1], v), w=["invB"])
                G(lambda e: e.memset(invQ[:, 0:6], 1.0 / 64), w=["invQ"])
                G(lambda e: e.memset(invQ[:, 6:12], 1.0 / 32), w=["invQ"])
                for which in range(2):
                    load_bcast(Ar[which][:, :], which, 1, f"Ar{which}")
                    load_bcast(Br[which][:, :], which, 0, f"Br{which}")

                def rope_apply(src_ap, H, Dh, C_ap, S_ap, out_ap, rs):
                    c4 = Dh // 4
                    t1v = t1[:, 0:H * Dh].rearrange("p (h d) -> p h d", h=H)
                    t2v = t2[:, 0:H * Dh].rearrange("p (h a b c) -> p h a b c", h=H, a=2, b=2)
                    s5 = src_ap.rearrange("p h (a b c) -> p h a b c", a=2, b=2)
                    S4 = S_ap.rearrange("p (a b c) -> p a b c", a=2, b=2)
                    V(lambda e: e.tensor_tensor(out=t1v, in0=src_ap, in1=C_ap.unsqueeze(1).to_broadcast([128, H, Dh]), op=ALU.mult), r=rs, w=["t1"])
                    V(lambda e: e.tensor_tensor(out=t2v[:, :, :, 0, :], in0=s5[:, :, :, 1, :],
                                                in1=S4[:, :, 0, :].unsqueeze(1).to_broadcast([128, H, 2, c4]), op=ALU.mult), r=rs, w=["t2"])
                    V(lambda e: e.tensor_tensor(out=t2v[:, :, :, 1, :], in0=s5[:, :, :, 0, :],
                                                in1=S4[:, :, 1, :].unsqueeze(1).to_broadcast([128, H, 2, c4]), op=ALU.mult), r=rs, w=["t2"])
                    return t1v, t2[:, 0:H * Dh].rearrange("p (h d) -> p h d", h=H)

                blocks = [list(range(0, NCT))] + [list(range(NCT + 4 * b, NCT + 4 * b + 4)) for b in range(SEQ // 512)]
                for blk in blocks:
                    for bi, ti in enumerate(blk):
                        isc = ti < NCT
                        which = 1 if isc else 0
                        li = ti - NCT
                        xb = xt[ti % 2]; xk = f"xtB{ti % 2}"
                        rpb = rp[ti % 2]; rk = f"rp{ti % 2}"
                        L(lambda e: e.dma_start(out=xb[:, :], in_=xall[ti * 128:(ti + 1) * 128, :]), w=[xk])
                        if not isc:
                            L(lambda e: e.dma_start(out=rpb[:, :], in_=rope[li * 128:(li + 1) * 128, :]), w=[rk])
                        A(lambda e: e.activation(out=junk[:, :], in_=xb[:, :], func=AF.Square, accum_out=st1[:, 0:1]), r=[xk], w=["junkB", "st1"])
                        rstd_from_ss(st1[:, 0:1], st1[:, 1:2], 1.0 / D, ["st1"], ["st1b"])
                        V(lambda e: e.scalar_tensor_tensor(out=hf[:, :], in0=xb[:, :], scalar=st1[:, 1:2], in1=Ar[which][:, :], op0=ALU.mult, op1=ALU.mult),
                          r=[xk, "st1b", f"Ar{which}"], w=["hf"])
                        V(lambda e: e.tensor_tensor(out=hb[:, :], in0=hf[:, :], in1=Br[which][:, :], op=ALU.add), r=["hf", f"Br{which}"], w=["hb"])
                        for kc in range(8):
                            M(lambda e: e.transpose(out=pT[:, kc * 128:(kc + 1) * 128], in_=hb[:, kc * 128:(kc + 1) * 128], identity=ident[:, :]),
                              r=["hb", "ident"], w=["pTB"])
                        A(lambda e: e.copy(out=hT[:, :, :].rearrange("p k t -> p (k t)"), in_=pT[:, :]), r=["pTB"], w=["hT"])
                        for b, (c0, c1) in enumerate(((0, 512), (512, 992), (992, INW))):
                            for kc in range(8):
                                M(lambda e: e.matmul(pP[b][:, 0:c1 - c0], lhsT=hT[:, kc, :], rhs=w_in_b[:, kc, c0:c1], start=(kc == 0), stop=(kc == 7)),
                                  r=["hT", "w_in_b"], w=[f"pP{b}"])

                        for (pp, pk, H, gain, gk, dst, dk) in ((pP[0], "pP0", 8, gA, "gA", qA, "qA"), (pP[2], "pP2", 6, gC, "gC", qC, "qC")):
                            n = H * 64
                            src3 = pp[:, 0:n].rearrange("p (h d) -> p h d", h=H)
                            A(lambda e: e.activation(out=sq[:, 0:n], in_=pp[:, 0:n], func=AF.Square), r=[pk], w=["sq"])
                            V(lambda e: e.tensor_reduce(out=ssh[:, 0:H], in_=sq[:, 0:n].rearrange("p (h d) -> p h d", h=H), axis=AX.X, op=ALU.add), r=["sq"], w=["ssh"])
                            rstd_from_ss(ssh[:, 0:H], ssh[:, 0:H], 1.0 / 64, ["ssh"], ["ssh"])
                            qn3 = qn[:, 0:n].rearrange("p (h d) -> p h d", h=H)
                            V(lambda e: e.tensor_tensor(out=qn3, in0=src3, in1=ssh[:, 0:H].unsqueeze(2).to_broadcast([128, H, 64]), op=ALU.mult), r=[pk, "ssh"], w=["qn"])
                            if isc:
                                V(lambda e: e.tensor_tensor(out=dst[:, :, :], in0=qn3, in1=gain[:, :, :], op=ALU.mult), r=["qn", gk], w=[dk])
                            else:
                                V(lambda e: e.tensor_tensor(out=qn3, in0=qn3, in1=gain[:, :, :], op=ALU.mult), r=["qn", gk], w=["qn"])
                                a1, a2 = rope_apply(qn3, H, 64, rpb[:, 0:64], rpb[:, 64:128], None, ["qn", rk])
                                V(lambda e: e.tensor_tensor(out=dst[:, :, :], in0=a1, in1=a2, op=ALU.add), r=["t1", "t2"], w=[dk])
                        A(lambda e: e.copy(out=vst[:, 0:128], in_=pP[1][:, 0:128]), r=["pP1"], w=["vst"])
                        A(lambda e: e.copy(out=vst[:, 512:640], in_=pP[2][:, 384:512]), r=["pP2"], w=["vst"])

                        A(lambda e: e.activation(out=sq[:, 0:352], in_=pP[1][:, 128:480], func=AF.Square), r=["pP1"], w=["sq"])
                        V(lambda e: e.tensor_reduce(out=ssh[:, 0:1], in_=sq[:, 0:192], axis=AX.X, op=ALU.add), r=["sq"], w=["ssh"])
                        V(lambda e: e.tensor_reduce(out=ssh[:, 1:2], in_=sq[:, 192:320], axis=AX.X, op=ALU.add), r=["sq"], w=["ssh"])
                        V(lambda e: e.tensor_reduce(out=ssh[:, 2:3], in_=sq[:, 320:352], axis=AX.X, op=ALU.add), r=["sq"], w=["ssh"])
                        V(lambda e: e.tensor_tensor(out=ssh[:, 0:3], in0=ssh[:, 0:3], in1=invB[:, :], op=ALU.mult), r=["ssh", "invB"], w=["ssh"])
                        rstd_from_ss(ssh[:, 0:3], ssh[:, 0:3], 1.0, ["ssh"], ["ssh"])
                        V(lambda e: e.tensor_scalar(out=cqn[:, 0:192], in0=pP[1][:, 128:320], scalar1=ssh[:, 0:1], scalar2=None, op0=ALU.mult), r=["pP1", "ssh"], w=["cqn"])
                        V(lambda e: e.tensor_scalar(out=cqn[:, 192:320], in0=pP[1][:, 320:448], scalar1=ssh[:, 1:2], scalar2=None, op0=ALU.mult), r=["pP1", "ssh"], w=["cqn"])
                        V(lambda e: e.scalar_tensor_tensor(out=kpe[:, :], in0=pP[1][:, 448:480], scalar=ssh[:, 2:3], in1=gBkr[:, :], op0=ALU.mult, op1=ALU.mult),
                          r=["pP1", "ssh", "gBkr"], w=["kpe"])
                        if not isc:
                            a1, a2 = rope_apply(kpe[:, :].unsqueeze(1), 1, 32, rpb[:, 128:160], rpb[:, 160:192], None, ["kpe", rk])
                            V(lambda e: e.tensor_tensor(out=kpe2[:, :].unsqueeze(1), in0=a1, in1=a2, op=ALU.add), r=["t1", "t2"], w=["kpe2"])
                            kp = kpe2
                        else:
                            kp = kpe
                        V(lambda e: e.tensor_copy(out=qB[:, 6:12, 64:96], in_=kp[:, :].unsqueeze(1).to_broadcast([128, 6, 32])), r=["kpe", "kpe2"], w=["qBk"])
                        M(lambda e: e.transpose(out=pT[:, 0:128], in_=cqn[:, 0:128], identity=ident[:, :]), r=["cqn", "ident"], w=["pTB"])
                        M(lambda e: e.transpose(out=pT[0:64, 128:256], in_=cqn[:, 128:192], identity=ident[:, :]), r=["cqn", "ident"], w=["pTB"])
                        M(lambda e: e.transpose(out=pT[:, 256:384], in_=cqn[:, 192:320], identity=ident[:, :]), r=["cqn", "ident"], w=["pTB"])
                        A(lambda e: e.copy(out=cT[:, 0, :], in_=pT[:, 0:128]), r=["pTB"], w=["cT"])
                        A(lambda e: e.copy(out=cT[0:64, 1, :], in_=pT[0:64, 128:256]), r=["pTB"], w=["cT"])
                        A(lambda e: e.copy(out=cT[:, 2, :], in_=pT[:, 256:384]), r=["pTB"], w=["cT"])
                        for hf_ in range(2):
                            M(lambda e: e.matmul(pQ[hf_][:, 0:288], lhsT=cT[:, 0, :], rhs=wuq_b[:, 0, hf_ * 288:(hf_ + 1) * 288], start=True, stop=False),
                              r=["cT", "wuq_b"], w=[f"pQ{hf_}"])
                            M(lambda e: e.matmul(pQ[hf_][:, 0:288], lhsT=cT[0:64, 1, :], rhs=wuq_b[0:64, 1, hf_ * 288:(hf_ + 1) * 288], start=False, stop=True),
                              r=["cT", "wuq_b"], w=[f"pQ{hf_}"])
                            M(lambda e: e.matmul(pK[hf_][:, 0:384], lhsT=cT[:, 2, :], rhs=wukv_b[:, hf_ * 384:(hf_ + 1) * 384], start=True, stop=True),
                              r=["cT", "wukv_b"], w=[f"pK{hf_}"])
                        for hf_ in range(2):
                            A(lambda e: e.activation(out=sq[:, hf_ * 288:(hf_ + 1) * 288], in_=pQ[hf_][:, 0:288], func=AF.Square), r=[f"pQ{hf_}"], w=["sq"])
                        sq3 = sq[:, 0:576].rearrange("p (h d) -> p h d", h=6)
                        V(lambda e: e.tensor_reduce(out=ssh[:, 0:6], in_=sq3[:, :, 0:64], axis=AX.X, op=ALU.add), r=["sq"], w=["ssh"])
                        V(lambda e: e.tensor_reduce(out=ssh[:, 6:12], in_=sq3[:, :, 64:96], axis=AX.X, op=ALU.add), r=["sq"], w=["ssh"])
                        V(lambda e: e.tensor_tensor(out=ssh[:, 0:12], in0=ssh[:, 0:12], in1=invQ[:, :], op=ALU.mult), r=["ssh", "invQ"], w=["ssh"])
                        rstd_from_ss(ssh[:, 0:12], ssh[:, 0:12], 1.0, ["ssh"], ["ssh"])
                        qn3 = qn[:, 0:576].rearrange("p (h d) -> p h d", h=6)
                        for hf_ in range(2):
                            p3 = pQ[hf_][:, 0:288].rearrange("p (h d) -> p h d", h=3)
                            hs = slice(hf_ * 3, hf_ * 3 + 3)
                            V(lambda e: e.tensor_tensor(out=qn3[:, hs, 0:64], in0=p3[:, :, 0:64],
                                                        in1=ssh[:, hf_ * 3:hf_ * 3 + 3].unsqueeze(2).to_broadcast([128, 3, 64]), op=ALU.mult), r=[f"pQ{hf_}", "ssh"], w=["qn"])
                            V(lambda e: e.tensor_tensor(out=qn3[:, hs, 64:96], in0=p3[:, :, 64:96],
                                                        in1=ssh[:, 6 + hf_ * 3:6 + hf_ * 3 + 3].unsqueeze(2).to_broadcast([128, 3, 32]), op=ALU.mult), r=[f"pQ{hf_}", "ssh"], w=["qn"])
                        V(lambda e: e.tensor_tensor(out=qn3, in0=qn3, in1=gBq[:, :].unsqueeze(1).to_broadcast([128, 6, 96]), op=ALU.mult), r=["qn", "gBq"], w=["qn"])
                        V(lambda e: e.tensor_copy(out=qB[:, 0:6, 0:64], in_=qn3[:, :, 0:64]), r=["qn"], w=["qBq"])
                        if isc:
                            V(lambda e: e.tensor_copy(out=qB[:, 0:6, 64:96], in_=qn3[:, :, 64:96]), r=["qn"], w=["qBq"])
                        else:
                            a1, a2 = rope_apply(qn3[:, :, 64:96], 6, 32, rpb[:, 128:160], rpb[:, 160:192], None, ["qn", rk])
                            V(lambda e: e.tensor_tensor(out=qB[:, 0:6, 64:96], in0=a1, in1=a2, op=ALU.add), r=["t1", "t2"], w=["qBq"])
                        for hf_ in range(2):
                            A(lambda e: e.activation(out=sq[:, hf_ * 192:(hf_ + 1) * 192].rearrange("p (h d) -> p h d", h=3),
                                                     in_=pK[hf_][:, 0:384].rearrange("p (h d) -> p h d", h=3)[:, :, 0:64], func=AF.Square), r=[f"pK{hf_}"], w=["sq"])
                            A(lambda e: e.copy(out=vst[:, 128 + hf_ * 192:128 + (hf_ + 1) * 192].rearrange("p (h d) -> p h d", h=3),
                                               in_=pK[hf_][:, 0:384].rearrange("p (h d) -> p h d", h=3)[:, :, 64:128]), r=[f"pK{hf_}"], w=["vst"])
                        V(lambda e: e.tensor_reduce(out=ssh[:, 0:6], in_=sq[:, 0:384].rearrange("p (h d) -> p h d", h=6), axis=AX.X, op=ALU.add), r=["sq"], w=["ssh"])
                        rstd_from_ss(ssh[:, 0:6], ssh[:, 0:6], 1.0 / 64, ["ssh"], ["ssh"])
                        kn3 = qn[:, 0:384].rearrange("p (h d) -> p h d", h=6)
                        for hf_ in range(2):
                            p3 = pK[hf_][:, 0:384].rearrange("p (h d) -> p h d", h=3)
                            V(lambda e: e.tensor_tensor(out=kn3[:, hf_ * 3:hf_ * 3 + 3, :], in0=p3[:, :, 0:64],
                                                        in1=ssh[:, hf_ * 3:hf_ * 3 + 3].unsqueeze(2).to_broadcast([128, 3, 64]), op=ALU.mult), r=[f"pK{hf_}", "ssh"], w=["qn"])
                        V(lambda e: e.tensor_tensor(out=qB[:, 6:12, 0:64], in0=kn3, in1=gBk[:, :].unsqueeze(1).to_broadcast([128, 6, 64]), op=ALU.mult), r=["qn", "gBk"], w=["qBk"])

                        S(lambda e: e.dma_start(out=vall[ti * 128:(ti + 1) * 128, :], in_=vst[:, :]), r=["vst"])
                        c0 = bi * 128
                        for i in range(4):
                            M(lambda e: e.transpose(out=pT[:, i * 128:(i + 1) * 128], in_=qA[:, 2 * i:2 * i + 2, :].rearrange("p h d -> p (h d)"), identity=ident[:, :]),
                              r=["qA", "ident"], w=["pTB"])
                        A(lambda e: e.copy(out=stA[:, :, c0:c0 + 128], in_=pT[:, 0:512].rearrange("p (i t) -> p i t", i=4)), r=["pTB"], w=["stA"])
                        for i in range(3):
                            M(lambda e: e.transpose(out=pT[:, i * 128:(i + 1) * 128], in_=qC[:, 2 * i:2 * i + 2, :].rearrange("p h d -> p (h d)"), identity=ident[:, :]),
                              r=["qC", "ident"], w=["pTB"])
                        V(lambda e: e.tensor_copy(out=stC[:, :, c0:c0 + 128], in_=pT[:, 0:384].rearrange("p (i t) -> p i t", i=3)), r=["pTB"], w=["stC"])
                        for rnd in range(2):
                            for i in range(6):
                                M(lambda e: e.transpose(out=pT[0:96, i * 128:(i + 1) * 128], in_=qB[:, rnd * 6 + i, :], identity=ident[:, :]),
                                  r=["qBq", "qBk", "ident"], w=["pTB"])
                            A(lambda e: e.copy(out=stB[0:96, rnd * 6:rnd * 6 + 6, c0:c0 + 128], in_=pT[0:96, 0:768].rearrange("p (i t) -> p i t", i=6)), r=["pTB"], w=["stB"])
                    W = len(blk) * 128
                    t0 = blk[0] * 128
                    S(lambda e: e.dma_start(out=qkT_A[:, t0:t0 + W].rearrange("(i p) t -> p i t", p=128), in_=stA[:, :, 0:W]), r=["stA"])
                    S(lambda e: e.dma_start(out=qkT_C[:, t0:t0 + W].rearrange("(i p) t -> p i t", p=128), in_=stC[:, :, 0:W]), r=["stC"])
                    S(lambda e: e.dma_start(out=qkT_B[:, :, t0:t0 + W].rearrange("j p t -> p j t"), in_=stB[0:96, :, 0:W]), r=["stB"])
            P.barrier()
            if stop_after == ("B", l):
                break

            with ExitStack() as ph:
                kt = [tsb(ph, f"kt{i}", [96, T], BF16) for i in range(2)]
                vb = [tsb(ph, f"vb{i}", [128, NT, 65], BF16) for i in range(2)]
                qt = [tsb(ph, f"qt{i}", [96, 512], BF16) for i in range(2)]
                pt = [tsb(ph, f"pt{i}", [128, 512], BF16) for i in range(3)]
                ob = [tsb(ph, f"ob{i}", [128, 4, 64], BF16) for i in range(2)]
                den = tsb(ph, "den", [128, 8], F32)
                sinkr = tsb(ph, "sinkr", [128, 4], F32)
                sinke = tsb(ph, "sinke", [128, 4], F32)
                pS = [tps(ph, f"pS{i}", [128, 512]) for i in range(3)]
                pO = [tps(ph, f"pO{i}", [128, 512]) for i in range(2)]
                den2 = [tsb(ph, f"den2_{i}", [128, 4], F32) for i in range(2)]
                L(lambda e: e.dma_start(out=sinkr[:, :], in_=c_sink[l:l + 1, :].broadcast_to([128, 4])), w=["sinkr"])
                A(lambda e: e.activation(out=sinke[:, :], in_=sinkr[:, :], func=AF.Exp), r=["sinkr"], w=["sinke"])
                for i in range(2):
                    G(lambda e: e.memset(vb[i][:, :, 64:65], 1.0), w=[f"vb{i}"])
                cnt = {"kv": 0, "q": 0, "s": 0, "p": 0, "u": 0}
                mixv = mix.rearrange("(j p) c -> p j c", p=128)
                vallv = vall.rearrange("(j p) c -> p j c", p=128)
                pending = []

                def emit_tail(st):
                    (si, pi, Wq, scale, msk, subs, u, n, nkeys, vbuf, vk, j, sink_h, out_fn) = st
                    ui = u % 2
                    A(lambda e: e.activation(out=pt[pi][:, 0:Wq], in_=pS[si][:, 0:Wq], func=AF.Exp, scale=scale), r=[f"pS{si}"], w=[f"pt{pi}"])
                    if msk is not None:
                        V(lambda e: e.tensor_tensor(out=pt[pi][:, 0:Wq].rearrange("p (s t) -> p s t", s=subs), in0=pt[pi][:, 0:Wq].rearrange("p (s t) -> p s t", s=subs),
                                                    in1=msk[:, :].unsqueeze(1).to_broadcast([128, subs, 128]), op=ALU.mult), r=[f"pt{pi}"], w=[f"pt{pi}"])
                    for s_ in range(subs):
                        M(lambda e: e.matmul(pO[ui][:, s_ * 65:(s_ + 1) * 65], lhsT=pt[pi][:, s_ * 128:(s_ + 1) * 128], rhs=vbuf[:, j, :],
                                             start=(n == 0 and s_ == 0), stop=(n == nkeys - 1), skip_group_check=True),
                          r=[f"pt{pi}", vk], w=[f"pO{ui}"])
                    if n == nkeys - 1:
                        pov = pO[ui][:, 0:subs * 65].rearrange("p (s c) -> p s c", s=subs)
                        dn = den2[ui]
                        if sink_h is not None:
                            V(lambda e: e.tensor_tensor(out=dn[:, 0:subs].unsqueeze(2), in0=pov[:, :, 64:65], in1=sinke[:, sink_h:sink_h + subs].unsqueeze(2), op=ALU.add),
                              r=[f"pO{ui}", "sinke"], w=[f"den2_{ui}"])
                            V(lambda e: e.reciprocal(out=dn[:, 0:subs], in_=dn[:, 0:subs]), r=[f"den2_{ui}"], w=[f"den2_{ui}"])
                        else:
                            V(lambda e: e.reciprocal(out=dn[:, 0:subs].unsqueeze(2), in_=pov[:, :, 64:65]), r=[f"pO{ui}"], w=[f"den2_{ui}"])
                        V(lambda e: e.tensor_tensor(out=ob[ui][:, 0:subs, :], in0=pov[:, :, 0:64], in1=dn[:, 0:subs].unsqueeze(2).to_broadcast([128, subs, 64]), op=ALU.mult),
                          r=[f"pO{ui}", f"den2_{ui}"], w=[f"ob{ui}"])
                        out_fn(ob[ui], f"ob{ui}")

                def attn_unit(qsrc_fn, dq, Wq, keys, kb, kk, vbuf, vk, scale, subs, sink_h, out_fn):
                    qi = cnt["q"] % 2; cnt["q"] += 1
                    u = cnt["u"]; cnt["u"] += 1
                    qsrc_fn(qt[qi], f"qt{qi}")
                    for n, (j, msk) in enumerate(keys):
                        si = cnt["s"] % 3; cnt["s"] += 1
                        pi = cnt["p"] % 3; cnt["p"] += 1
                        M(lambda e: e.matmul(pS[si][:, 0:Wq], lhsT=kb[0:dq, j * 128:(j + 1) * 128], rhs=qt[qi][0:dq, 0:Wq], start=True, stop=True),
                          r=[kk, f"qt{qi}"], w=[f"pS{si}"])
                        pending.append((si, pi, Wq, scale, msk, subs, u, n, len(keys), vbuf, vk, j, sink_h, out_fn))
                        if len(pending) > 1:
                            emit_tail(pending.pop(0))

                def load_kv(krows_ap, dq, voff):
                    i = cnt["kv"] % 2; cnt["kv"] += 1
                    L(lambda e: e.dma_start(out=kt[i][0:dq, :], in_=krows_ap), w=[f"kt{i}"])
                    for hh in range(2):
                        L(lambda e: e.dma_start(out=vb[i][:, hh * 17:(hh + 1) * 17, 0:64], in_=vallv[:, hh * 17:(hh + 1) * 17, voff:voff + 64]), w=[f"vb{i}"])
                    return kt[i], f"kt{i}", vb[i], f"vb{i}"

                qblocks = ([(0, NCT)] if not last else []) + [(NCT + 4 * b, 4) for b in range(SEQ // 512)]
                for mixer in ("A", "B"):
                    nkv = 2 if mixer == "A" else 6
                    for kv in range(nkv):
                        if mixer == "A":
                            kb, kk, vbuf, vk = load_kv(qkT_A[(6 + kv) * 64:(7 + kv) * 64, :], 64, kv * 64)
                            heads = [3 * kv + g for g in range(3)]; dq = 64; scale = 64 ** -0.5
                        else:
                            kb, kk, vbuf, vk = load_kv(qkT_B[6 + kv, :, :], 96, 128 + kv * 64)
                            heads = [kv]; dq = 96; scale = 96 ** -0.5
                        for h in heads:
                            for (tb, nsub) in qblocks:
                                keys = [(j, None) for j in (range(NCT) if tb < NCT else range(NT))]
                                Wq = nsub * 128
                                if mixer == "A":
                                    qsrc = lambda dst, dk, h=h, tb=tb, Wq=Wq: L(lambda e: e.dma_start(out=dst[0:64, 0:Wq], in_=qkT_A[h * 64:(h + 1) * 64, tb * 128:tb * 128 + Wq]), w=[dk])
                                    col = h * 64
                                else:
                                    qsrc = lambda dst, dk, h=h, tb=tb, Wq=Wq: L(lambda e: e.dma_start(out=dst[0:96, 0:Wq], in_=qkT_B[h, :, tb * 128:tb * 128 + Wq]), w=[dk])
                                    col = 384 + h * 64
                                outf = lambda o, ok, tb=tb, nsub=nsub, col=col: S(lambda e: e.dma_start(out=mixv[:, tb:tb + nsub, col:col + 64], in_=o[:, 0:nsub, :]), r=[ok])
                                attn_unit(qsrc, dq, Wq, keys, kb, kk, vbuf, vk, scale, nsub, None, outf)
                for kv in range(2):
                    kb, kk, vbuf, vk = load_kv(qkT_C[(4 + kv) * 64:(5 + kv) * 64, :], 64, 512 + kv * 64)
                    for ti in (range(NT) if not last else range(NCT, NT)):
                        if ti < NCT:
                            keys = [(j, None) for j in range(NCT)]
                        else:
                            keys = [(j, None) for j in range(NCT)]
                            if ti - 1 >= NCT:
                                keys.append((ti - 1, M1))
                            keys.append((ti, None))
                            if ti + 1 < NT:
                                keys.append((ti + 1, M2))

                        def qsrc(dst, dk, kv=kv, ti=ti):
                            L(lambda e: e.dma_start(out=dst[0:64, 0:256].rearrange("p (h t) -> p h t", h=2),
                                                    in_=qkT_C[2 * kv * 64:(2 * kv + 2) * 64, ti * 128:(ti + 1) * 128].rearrange("(h p) t -> p h t", p=64)), w=[dk])
                        col = 768 + 2 * kv * 64
                        outf = lambda o, ok, ti=ti, col=col: S(lambda e: e.dma_start(out=mix[ti * 128:(ti + 1) * 128, col:col + 128], in_=o[:, 0:2, :].rearrange("p s d -> p (s d)")), r=[ok])
                        attn_unit(qsrc, 64, 256, keys, kb, kk, vbuf, vk, 64 ** -0.5, 2, 2 * kv, outf)
                while pending:
                    emit_tail(pending.pop(0))
            P.barrier()
            if stop_after == ("C", l):
                break

            with ExitStack() as ph:
                wst = tsb(ph, "wstD", [128, 8, 512], F32)
                w_out_b = tsb(ph, "w_out_b", [128, 8, D], BF16)
                G1 = [tsb(ph, f"G1_{i}", [128, D], F32) for i in range(2)]
                xt = [tsb(ph, f"xtD{i}", [128, D], F32) for i in range(2)]
                mt = [tsb(ph, f"mt{i}", [128, D], BF16) for i in range(2)]
                mT = tsb(ph, "mT", [128, 8, 128], BF16)
                yt = [tsb(ph, f"yt{i}", [128, D], F32) for i in range(2)]
                pT = tps(ph, "pTD", [128, 1024], BF16)
                pY = [tps(ph, f"pY{i}", [128, 512]) for i in range(2)]
                for b in range(2):
                    for h in range(2):
                        L(lambda e: e.dma_start(out=wst[:, h * 4:(h + 1) * 4, :],
                                                in_=w_out[l, h * 512:(h + 1) * 512, b * 512:(b + 1) * 512].rearrange("(k p) n -> p k n", p=128)), w=["wstD"])
                    V(lambda e: e.tensor_copy(out=w_out_b[:, 0:4, b * 512:(b + 1) * 512], in_=wst[:, 0:4, :]), r=["wstD"], w=["w_out_b"])
                    G(lambda e: e.tensor_copy(out=w_out_b[:, 4:8, b * 512:(b + 1) * 512], in_=wst[:, 4:8, :]), r=["wstD"], w=["w_out_b"])
                for which in range(2):
                    load_bcast(G1[which][:, :], which, 2, f"G1_{which}")
                for ti in tiles_act:
                    which = 1 if ti < NCT else 0
                    i2 = ti % 2
                    L(lambda e: e.dma_start(out=xt[i2][:, :], in_=xall[ti * 128:(ti + 1) * 128, :]), w=[f"xtD{i2}"])
                    L(lambda e: e.dma_start(out=mt[i2][:, :], in_=mix[ti * 128:(ti + 1) * 128, :]), w=[f"mt{i2}"])
                    for kc in range(8):
                        M(lambda e: e.transpose(out=pT[:, kc * 128:(kc + 1) * 128], in_=mt[i2][:, kc * 128:(kc + 1) * 128], identity=ident[:, :]), r=[f"mt{i2}", "ident"], w=["pTD"])
                    A(lambda e: e.copy(out=mT[:, :, :].rearrange("p k t -> p (k t)"), in_=pT[:, :]), r=["pTD"], w=["mT"])
                    for b in range(2):
                        for kc in range(8):
                            M(lambda e: e.matmul(pY[b][:, :], lhsT=mT[:, kc, :], rhs=w_out_b[:, kc, b * 512:(b + 1) * 512], start=(kc == 0), stop=(kc == 7)), r=["mT", "w_out_b"], w=[f"pY{b}"])
                        V(lambda e: e.tensor_tensor(out=yt[i2][:, b * 512:(b + 1) * 512], in0=pY[b][:, :], in1=G1[which][:, b * 512:(b + 1) * 512], op=ALU.mult),
                          r=[f"pY{b}", f"G1_{which}"], w=[f"yt{i2}"])
                    V(lambda e: e.tensor_tensor(out=yt[i2][:, :], in0=yt[i2][:, :], in1=xt[i2][:, :], op=ALU.add), r=[f"yt{i2}", f"xtD{i2}"], w=[f"yt{i2}"])
                    S(lambda e: e.dma_start(out=xall[ti * 128:(ti + 1) * 128, :], in_=yt[i2][:, :]), r=[f"yt{i2}"])
            P.barrier()
            if stop_after == ("D", l):
                break

            with ExitStack() as ph:
                aff = tsb(ph, "aff", [128, NT, NE], F32)
                gm = tsb(ph, "gm", [128, NT, NE], F32)
                sloti = tsb(ph, "sloti", [128, NT, NE], I32)
                slotc = tsb(ph, "slotc", [128, NT, NE], I32)
                G2 = [tsb(ph, f"G2_{i}", [128, D], F32) for i in range(2)]
                p13 = ExitStack()
                affT = tsb(p13, "affT", [NE, T], F32)
                maskT = tsb(p13, "maskT", [NE, T], BF16)
                with ExitStack() as p1:
                    wr_f = tsb(p1, "wr_f", [128, 8, NE], F32)
                    wr_b = tsb(p1, "wr_b", [128, 8, NE], BF16)
                    Ar = [tsb(p1, f"Ar2_{i}", [128, D], F32) for i in range(2)]
                    Br = [tsb(p1, f"Br2_{i}", [128, D], F32) for i in range(2)]
                    xt = [tsb(p1, f"xtE{i}", [128, D], F32) for i in range(2)]
                    junk = tsb(p1, "junkE", [128, D], BF16)
                    st1 = tsb(p1, "st1E", [128, 8], F32)
                    hf = tsb(p1, "hfE", [128, D], F32)
                    hb = [tsb(p1, f"hbE{i}", [128, D], BF16) for i in range(2)]
                    hT = tsb(p1, "hTE", [128, 8, 128], BF16)
                    ex = tsb(p1, "ex", [128, NE], F32)
                    pT = tps(p1, "pTE", [128, 1024], BF16)
                    pR = tps(p1, "pR", [128, 512])
                    pAT = tps(p1, "pAT", [128, 512])
                    L(lambda e: e.dma_start(out=wr_f[:, :, :], in_=w_router[l, :, :].rearrange("(k p) n -> p k n", p=128)), w=["wr_f"])
                    V(lambda e: e.tensor_copy(out=wr_b[:, :, :], in_=wr_f[:, :, :]), r=["wr_f"], w=["wr_b"])
                    for which in range(2):
                        load_bcast(Ar[which][:, :], which, 4, f"Ar2_{which}")
                        load_bcast(Br[which][:, :], which, 3, f"Br2_{which}")
                        load_bcast(G2[which][:, :], which, 5, f"G2_{which}")
                    for ti in tiles_act:
                        which = 1 if ti < NCT else 0
                        i2 = ti % 2
                        L(lambda e: e.dma_start(out=xt[i2][:, :], in_=xall[ti * 128:(ti + 1) * 128, :]), w=[f"xtE{i2}"])
                        A(lambda e: e.activation(out=junk[:, :], in_=xt[i2][:, :], func=AF.Square, accum_out=st1[:, 0:1]), r=[f"xtE{i2}"], w=["junkE", "st1E"])
                        rstd_from_ss(st1[:, 0:1], st1[:, 1:2], 1.0 / D, ["st1E"], ["st1Eb"])
                        V(lambda e: e.scalar_tensor_tensor(out=hf[:, :], in0=xt[i2][:, :], scalar=st1[:, 1:2], in1=Ar[which][:, :], op0=ALU.mult, op1=ALU.mult),
                          r=[f"xtE{i2}", "st1Eb", f"Ar2_{which}"], w=["hfE"])
                        V(lambda e: e.tensor_tensor(out=hb[i2][:, :], in0=hf[:, :], in1=Br[which][:, :], op=ALU.add), r=["hfE", f"Br2_{which}"], w=[f"hbE{i2}"])
                        S(lambda e: e.dma_start(out=h2d[ti * 128:(ti + 1) * 128, :], in_=hb[i2][:, :]), r=[f"hbE{i2}"])
                        for kc in range(8):
                            M(lambda e: e.transpose(out=pT[:, kc * 128:(kc + 1) * 128], in_=hb[i2][:, kc * 128:(kc + 1) * 128], identity=ident[:, :]), r=[f"hbE{i2}", "ident"], w=["pTE"])
                        A(lambda e: e.copy(out=hT[:, :, :].rearrange("p k t -> p (k t)"), in_=pT[:, :]), r=["pTE"], w=["hTE"])
                        for kc in range(8):
                            M(lambda e: e.matmul(pR[:, 0:NE], lhsT=hT[:, kc, :], rhs=wr_b[:, kc, :], start=(kc == 0), stop=(kc == 7)), r=["hTE", "wr_b"], w=["pR"])
                        V(lambda e: e.tensor_reduce(out=st1[:, 2:3], in_=pR[:, 0:NE], axis=AX.X, op=ALU.max, negate=True), r=["pR"], w=["st1Ec"])
                        A(lambda e: e.activation(out=ex[:, :], in_=pR[:, 0:NE], func=AF.Exp, bias=st1[:, 2:3], accum_out=st1[:, 3:4]), r=["pR", "st1Ec"], w=["ex", "st1Ed"])
                        V(lambda e: e.reciprocal(out=st1[:, 4:5], in_=st1[:, 3:4]), r=["st1Ed"], w=["st1Ee"])
                        V(lambda e: e.tensor_scalar(out=aff[:, ti, :], in0=ex[:, :], scalar1=st1[:, 4:5], scalar2=None, op0=ALU.mult), r=["ex", "st1Ee"], w=["aff"])
                        M(lambda e: e.transpose(out=pAT[0:NE, 0:128], in_=aff[:, ti, :], identity=identf[:, :]), r=["aff", "identf"], w=["pAT"])
                        A(lambda e: e.copy(out=affT[:, ti * 128:(ti + 1) * 128], in_=pAT[0:NE, 0:128]), r=["pAT"], w=["affT"])
                P.barrier()
                with ExitStack() as p2:
                    work = tsb(p2, "work", [NE, CTX], F32)
                    mx = tsb(p2, "mx", [NE, 8], F32)
                    if not last:
                        V(lambda e: e.tensor_copy(out=work[:, :], in_=affT[:, 0:CTX]), r=["affT"], w=["work"])
                        for r_ in range(CAP_C // 8):
                            V(lambda e: e.max(out=mx[:, :], in_=work[:, :]), r=["work"], w=["mx"])
                            if r_ < CAP_C // 8 - 1:
                                V(lambda e: e.match_replace(out=work[:, :], in_to_replace=mx[:, :], in_values=work[:, :], imm_value=-1.0), r=["work", "mx"], w=["work"])
                        V(lambda e: e.tensor_scalar(out=maskT[:, 0:CTX], in0=affT[:, 0:CTX], scalar1=mx[:, 7:8], scalar2=None, op0=ALU.is_ge), r=["affT", "mx"], w=["maskT"])
                    A2 = tsb(p2, "A2", [128, 512], F32)
                    jk = tsb(p2, "jk2", [128, 512], F32)
                    mk2 = tsb(p2, "mk2", [128, 512], BF16)
                    Bsel = tsb(p2, "Bsel", [NE, 128], F32)
                    Gm = tsb(p2, "Gm", [128, 128], F32)
                    bs = tsb(p2, "bs", [128, 8], F32)
                    pG2 = tps(p2, "pGm", [128, 512])
                    pC2 = tps(p2, "pCnt", [128, 512])
                    S(lambda e: e.dma_start(out=aT_d, in_=affT[:, CTX:T]), r=["affT"], w=["aT_d"])
                    L(lambda e: e.dma_start(out=A2[:, :], in_=aT_d.rearrange("e (c t) -> (e c) t", c=8)), r=["aT_d"], w=["A2"])
                    G(lambda e: e.affine_select(out=Bsel[:, :], in_=onesf[0:NE, :], pattern=[[1, 128]], base=0, channel_multiplier=-8, compare_op=ALU.is_ge, fill=0.0), r=["onesf"], w=["Bsel"])
                    G(lambda e: e.affine_select(out=Bsel[:, :], in_=Bsel[:, :], pattern=[[-1, 128]], base=7, channel_multiplier=8, compare_op=ALU.is_ge, fill=0.0), r=["Bsel"], w=["Bsel"])
                    M(lambda e: e.matmul(pG2[:, 0:128], lhsT=Bsel[:, :], rhs=Bsel[:, :], start=True, stop=True), r=["Bsel"], w=["pGm"])
                    V(lambda e: e.tensor_copy(out=Gm[:, :], in_=pG2[:, 0:128]), r=["pGm"], w=["Gm"])
                    V(lambda e: e.memset(bs[:, 0:1], 0.0), w=["bs"])
                    V(lambda e: e.memset(bs[:, 1:2], 1.0), w=["bs"])
                    for it in range(32):
                        V(lambda e: e.tensor_scalar(out=bs[:, 2:3], in0=bs[:, 0:1], scalar1=bs[:, 1:2], scalar2=0.5, op0=ALU.add, op1=ALU.mult), r=["bs"], w=["bs"])
                        V(lambda e: e.tensor_scalar(out=jk[:, :], in0=A2[:, :], scalar1=bs[:, 2:3], scalar2=0.0, op0=ALU.is_ge, op1=ALU.add, accum_out=bs[:, 3:4]), r=["A2", "bs"], w=["jk2", "bs"])
                        M(lambda e: e.matmul(pC2[:, 0:1], lhsT=Gm[:, :], rhs=bs[:, 3:4], start=True, stop=True), r=["Gm", "bs"], w=["pCnt"])
                        V(lambda e: e.tensor_scalar(out=bs[:, 4:5], in0=pC2[:, 0:1], scalar1=float(CAP_L) - 0.5, scalar2=None, op0=ALU.is_ge), r=["pCnt"], w=["bs"])
                        V(lambda e: e.tensor_tensor(out=bs[:, 5:6], in0=bs[:, 4:5], in1=bs[:, 2:3], op=ALU.mult), r=["bs"], w=["bs"])
                        V(lambda e: e.tensor_tensor(out=bs[:, 0:1], in0=bs[:, 0:1], in1=bs[:, 5:6], op=ALU.max), r=["bs"], w=["bs"])
                        V(lambda e: e.scalar_tensor_tensor(out=bs[:, 6:7], in0=bs[:, 4:5], scalar=2.0, in1=bs[:, 2:3], op0=ALU.mult, op1=ALU.add), r=["bs"], w=["bs"])
                        V(lambda e: e.tensor_tensor(out=bs[:, 1:2], in0=bs[:, 1:2], in1=bs[:, 6:7], op=ALU.min), r=["bs"], w=["bs"])
                    V(lambda e: e.tensor_scalar(out=mk2[:, :], in0=A2[:, :], scalar1=bs[:, 0:1], scalar2=None, op0=ALU.is_ge), r=["A2", "bs"], w=["mk2"])
                    S(lambda e: e.dma_start(out=mk_d.rearrange("e (c t) -> (e c) t", c=8), in_=mk2[:, :]), r=["mk2"], w=["mk_d"])
                    L(lambda e: e.dma_start(out=maskT[:, CTX:T], in_=mk_d), r=["mk_d"], w=["maskT"])
                P.barrier()
                with ExitStack() as p3:
                    mk = tsb(p3, "mk", [128, NE], F32)
                    mkb = tsb(p3, "mkb", [128, NE], BF16)
                    Sf = tsb(p3, "Sf", [128, NE], F32)
                    Sb = tsb(p3, "Sb", [128, NE], BF16)
                    sl = tsb(p3, "sl", [128, NE], F32)
                    hb = [tsb(p3, f"hbS{i}", [128, XW], BF16) for i in range(2)]
                    tok_i = tsb(p3, "tok_i", [128, 2, NT], I32)
                    tokb = tsb(p3, "tokb", [128, NT, 2], BF16)
                    ghi = tsb(p3, "ghi", [128, NE], BF16)
                    zt = tsb(p3, "zt", [128, 2 * D], F32)
                    for hh in range(2):
                        G(lambda e: e.iota(tok_i[hh * 64:(hh + 1) * 64, 0, :], pattern=[[2, NT]], base=hh, channel_multiplier=0), w=["tok_i"])
                        G(lambda e: e.iota(tok_i[hh * 64:(hh + 1) * 64, 1, :], pattern=[[0, NT]], base=0, channel_multiplier=1), w=["tok_i"])
                    V(lambda e: e.tensor_copy(out=tokb[:, :, :].rearrange("p t j -> p j t"), in_=tok_i[:, :, :]), r=["tok_i"], w=["tokb"])
                    V(lambda e: e.memset(zt[:, :], 0.0), w=["zt"])
                    for i in range(2):
                        V(lambda e: e.memset(hb[i][:, D:XW], 0.0), w=[f"hbS{i}"])
                    for ti in range(0, NT, 2):
                        L(lambda e: e.dma_start(out=accd[ti * 128:(ti + 2) * 128, :].rearrange("(j p) c -> p j c", p=128), in_=zt[:, :].rearrange("p (j c) -> p j c", j=2)), r=["zt"])
                    pM = tps(p3, "pM", [128, 1024], BF16)
                    pPos = tps(p3, "pPos", [128, 512])
                    for ti in tiles_act:
                        isc = ti < NCT
                        i2 = ti % 2
                        if ti == 0 or ti == NCT:
                            V(lambda e: e.memset(Sf[:, :], 0.0), w=["Sf"])
                            V(lambda e: e.tensor_copy(out=Sb[:, :], in_=Sf[:, :]), r=["Sf"], w=["Sb"])
                        L(lambda e: e.dma_start(out=hb[i2][:, 0:D], in_=h2d[ti * 128:(ti + 1) * 128, :]), w=[f"hbS{i2}"])
                        M(lambda e: e.transpose(out=pM[:, 0:NE], in_=maskT[:, ti * 128:(ti + 1) * 128], identity=ident[0:NE, 0:NE]), r=["maskT", "ident"], w=["pM"])
                        V(lambda e: e.tensor_copy(out=mk[:, :], in_=pM[:, 0:NE]), r=["pM"], w=["mk"])
                        A(lambda e: e.copy(out=mkb[:, :], in_=pM[:, 0:NE]), r=["pM"], w=["mkb"])
                        M(lambda e: e.matmul(pPos[:, 0:NE], lhsT=Ub[:, :], rhs=mkb[:, :], start=True, stop=False), r=["Ub", "mkb"], w=["pPos"])
                        M(lambda e: e.matmul(pPos[:, 0:NE], lhsT=ones_b[:, :], rhs=Sb[:, :], start=False, stop=True), r=["ones_b", "Sb"], w=["pPos"])
                        V(lambda e: e.tensor_scalar(out=sl[:, :], in0=mk[:, :], scalar1=-BIG, scalar2=BIG, op0=ALU.mult, op1=ALU.add), r=["mk"], w=["sl"])
                        V(lambda e: e.tensor_tensor(out=sl[:, :], in0=pPos[:, 0:NE], in1=sl[:, :], op=ALU.add), r=["pPos", "sl"], w=["sl"])
                        V(lambda e: e.tensor_copy(out=sloti[:, ti, :], in_=sl[:, :]), r=["sl"], w=["sloti"])
                        V(lambda e: e.tensor_scalar(out=sl[:, :], in0=sl[:, :], scalar1=float((CAP_C if isc else CAP_L) - 1), scalar2=None, op0=ALU.min), r=["sl"], w=["sl"])
                        V(lambda e: e.tensor_copy(out=slotc[:, ti, :], in_=sl[:, :]), r=["sl"], w=["slotc"])
                        V(lambda e: e.tensor_tensor(out=gm[:, ti, :], in0=aff[:, ti, :], in1=mk[:, :], op=ALU.mult), r=["aff", "mk"], w=["gm"])
                        V(lambda e: e.tensor_tensor(out=Sf[:, :], in0=Sf[:, :], in1=mk[:, :], op=ALU.add), r=["Sf", "mk"], w=["Sf"])
                        V(lambda e: e.tensor_copy(out=Sb[:, :], in_=Sf[:, :]), r=["Sf"], w=["Sb"])
                        V(lambda e: e.tensor_copy(out=hb[i2][:, D:D + 2], in_=tokb[:, ti, :]), r=["tokb"], w=[f"hbS{i2}"])
                        V(lambda e: e.tensor_copy(out=ghi[:, :], in_=gm[:, ti, :]), r=["gm"], w=["ghi"])
                        V(lambda e: e.tensor_copy(out=hb[i2][:, D + 2:D + 18], in_=ghi[:, :]), r=["ghi"], w=[f"hbS{i2}"])
                        V(lambda e: e.tensor_tensor(out=hb[i2][:, D + 18:D + 34], in0=gm[:, ti, :], in1=ghi[:, :], op=ALU.subtract), r=["gm", "ghi"], w=[f"hbS{i2}"])
                        for ex_ in range(NE):
                            dst = xe_c[ex_] if isc else xe_l[ex_]
                            bnd = (CAP_C if isc else CAP_L) - 1
                            S(lambda e: e.indirect_dma_start(out=dst, out_offset=bass.IndirectOffsetOnAxis(ap=sloti[:, ti, ex_:ex_ + 1], axis=0),
                                                             in_=hb[i2][:, :], in_offset=None, bounds_check=REG[bnd], oob_is_err=False),
                              r=[f"hbS{i2}", "sloti"])
                    if "dbg_aff" in dbg:
                        S(lambda e: e.dma_start(out=dbg_aff, in_=aff[:, :, :].rearrange("p t e -> p (t e)")), r=["aff"])
                        S(lambda e: e.dma_start(out=dbg_slot, in_=sloti[:, :, :].rearrange("p t e -> p (t e)")), r=["sloti"])
                P.barrier()
                p13.close()
                if stop_after == ("E3", l):
                    break
                with ExitStack() as p4:
                    NJ = CAP_L + (CAP_C if not last else 0)
                    stg = [tsb(p4, f"stg{i}", [128, 8, D], F32) for i in range(2)]
                    wb = [tsb(p4, f"wb{i}", [128, 8, D], BF16) for i in range(4)]
                    xet = [tsb(p4, f"xet{i}", [128, XW], BF16) for i in range(2)]
                    sideb = [tsb(p4, f"sideb{i}", [128, 5, 36], BF16) for i in range(2)]
                    gatef = [tsb(p4, f"gatef{i}", [128, 5], F32) for i in range(2)]
                    idxf = [tsb(p4, f"idxf{i}", [128, 5], F32) for i in range(2)]
                    idxi = [tsb(p4, f"idxi{i}", [128, 5], I32) for i in range(2)]
                    xeT = [tsb(p4, f"xeT{i}", [128, 8, CAP_L + CAP_C], BF16) for i in range(2)]
                    hidT = tsb(p4, "hidT", [128, 8, CAP_L + CAP_C], BF16)
                    sg = [tsb(p4, f"sg{i}", [128, CAP_L + CAP_C], F32) for i in range(2)]
                    yeb = [tsb(p4, f"yeb{i}", [128, D], BF16) for i in range(2)]
                    pT = tps(p4, "pT4", [128, 1024], BF16)
                    pG = tps(p4, "pG", [128, 512]); pU = tps(p4, "pU", [128, 512])
                    pGc = tps(p4, "pGc", [128, 512])
                    pUc = tps(p4, "pUc", [128, 512])
                    pY = [tps(p4, f"pY4_{i}", [128, 512]) for i in range(2)]
                    cnt4 = {"w": 0, "s": 0, "x": 0, "g": 0, "y": 0}

                    def load_w(src):
                        si = cnt4["s"] % 2; cnt4["s"] += 1
                        wi = cnt4["w"] % 4; cnt4["w"] += 1
                        for h in range(4):
                            L(lambda e: e.dma_start(out=stg[si][:, 2 * h:2 * h + 2, :], in_=src[h * 256:(h + 1) * 256, :].rearrange("(k p) n -> p k n", p=128)), w=[f"stg{si}h{h}"])
                        V(lambda e: e.tensor_copy(out=wb[wi][:, 0:2, :], in_=stg[si][:, 0:2, :]), r=[f"stg{si}h0"], w=[f"wb{wi}a"])
                        A(lambda e: e.copy(out=wb[wi][:, 2:4, :], in_=stg[si][:, 2:4, :]), r=[f"stg{si}h1"], w=[f"wb{wi}b"])
                        V(lambda e: e.tensor_copy(out=wb[wi][:, 4:5, :], in_=stg[si][:, 4:5, :]), r=[f"stg{si}h2"], w=[f"wb{wi}c"])
                        A(lambda e: e.copy(out=wb[wi][:, 5:6, :], in_=stg[si][:, 5:6, :]), r=[f"stg{si}h2"], w=[f"wb{wi}d"])
                        G(lambda e: e.tensor_copy(out=wb[wi][:, 6:8, :], in_=stg[si][:, 6:8, :]), r=[f"stg{si}h3"], w=[f"wb{wi}e"])
                        return wb[wi], [f"wb{wi}{c}" for c in "abcde"]

                    def stage_x(ex_):
                        xT = xeT[ex_ % 2]; xk = f"xeT{ex_ % 2}"
                        jts = [(xe_l[ex_][j * 128:(j + 1) * 128, :], 128, j * 128) for j in range(4)]
                        if not last:
                            jts.append((xe_c[ex_][:, :], CAP_C, CAP_L))
                        for jt, (src, nj, c0) in enumerate(jts):
                            xi = cnt4["x"] % 2; cnt4["x"] += 1
                            L(lambda e: e.dma_start(out=xet[xi][0:nj, :], in_=src), w=[f"xet{xi}"])
                            e2 = ex_ % 2
                            V(lambda e: e.tensor_copy(out=sideb[e2][0:nj, jt, :], in_=xet[xi][0:nj, D:XW]), r=[f"xet{xi}"], w=[f"sideb{e2}"])
                            V(lambda e: e.tensor_tensor(out=gatef[e2][0:nj, jt:jt + 1], in0=sideb[e2][0:nj, jt, 2 + ex_:3 + ex_], in1=sideb[e2][0:nj, jt, 18 + ex_:19 + ex_], op=ALU.add),
                              r=[f"sideb{e2}"], w=[f"gatef{e2}"])
                            V(lambda e: e.scalar_tensor_tensor(out=idxf[e2][0:nj, jt:jt + 1], in0=sideb[e2][0:nj, jt, 0:1], scalar=64.0, in1=sideb[e2][0:nj, jt, 1:2], op0=ALU.mult, op1=ALU.add),
                              r=[f"sideb{e2}"], w=[f"idxf{e2}"])
                            V(lambda e: e.tensor_copy(out=idxi[e2][0:nj, jt:jt + 1], in_=idxf[e2][0:nj, jt:jt + 1]), r=[f"idxf{e2}"], w=[f"idxi{e2}"])
                            for kc in range(8):
                                M(lambda e: e.transpose(out=pT[:, kc * 128:kc * 128 + nj], in_=xet[xi][0:nj, kc * 128:(kc + 1) * 128], identity=ident[0:nj, 0:nj]),
                                  r=[f"xet{xi}", "ident"], w=["pT4"])
                            V(lambda e: e.tensor_copy(out=xT[:, :, c0:c0 + nj], in_=pT[:, :].rearrange("p (k t) -> p k t", k=8)[:, :, 0:nj]), r=["pT4"], w=[xk])

                    def stage_hidden(ex_, wg, wgk, wu, wuk):
                        xT = xeT[ex_ % 2]; xk = f"xeT{ex_ % 2}"
                        for fc in range(8):
                            for kc in range(8):
                                M(lambda e: e.matmul(pG[:, :], lhsT=wg[:, kc, fc * 128:(fc + 1) * 128], rhs=xT[:, kc, 0:CAP_L], start=(kc == 0), stop=(kc == 7)), r=wgk + [xk], w=["pG"])
                            for kc in range(8):
                                M(lambda e: e.matmul(pU[:, :], lhsT=wu[:, kc, fc * 128:(fc + 1) * 128], rhs=xT[:, kc, 0:CAP_L], start=(kc == 0), stop=(kc == 7)), r=wuk + [xk], w=["pU"])
                            gi = cnt4["g"] % 2; cnt4["g"] += 1
                            A(lambda e: e.activation(out=sg[gi][:, 0:CAP_L], in_=pG[:, :], func=AF.Silu), r=["pG"], w=[f"sg{gi}"])
                            V(lambda e: e.tensor_tensor(out=hidT[:, fc, 0:CAP_L], in0=pU[:, :], in1=sg[gi][:, 0:CAP_L], op=ALU.mult), r=["pU", f"sg{gi}"], w=["hidT"])
                            if not last:
                                for kc in range(8):
                                    M(lambda e: e.matmul(pGc[:, 0:CAP_C], lhsT=wg[:, kc, fc * 128:(fc + 1) * 128], rhs=xT[:, kc, CAP_L:NJ], start=(kc == 0), stop=(kc == 7)), r=wgk + [xk], w=["pGc0"])
                                for kc in range(8):
                                    M(lambda e: e.matmul(pUc[:, 0:CAP_C], lhsT=wu[:, kc, fc * 128:(fc + 1) * 128], rhs=xT[:, kc, CAP_L:NJ], start=(kc == 0), stop=(kc == 7)), r=wuk + [xk], w=["pGc1"])
                                A(lambda e: e.activation(out=sg[gi][:, CAP_L:NJ], in_=pGc[:, 0:CAP_C], func=AF.Silu), r=["pGc0"], w=[f"sg{gi}c"])
                                V(lambda e: e.tensor_tensor(out=hidT[:, fc, CAP_L:NJ], in0=pUc[:, 0:CAP_C], in1=sg[gi][:, CAP_L:NJ], op=ALU.mult), r=["pGc1", f"sg{gi}c"], w=["hidTc"])

                    def stage_down(ex_, wd, wdk):
                        outs = [(None, 128, j * 128) for j in range(4)]
                        if not last:
                            outs.append((None, CAP_C, CAP_L))
                        e2 = ex_ % 2
                        for jt, (dst, nj, c0) in enumerate(outs):
                            gate_ap = gatef[e2][0:nj, jt:jt + 1]
                            idx_ap = idxi[e2][0:nj, jt:jt + 1]
                            yi = cnt4["y"] % 2; cnt4["y"] += 1
                            for dh in range(2):
                                for fc in range(8):
                                    M(lambda e: e.matmul(pY[dh][0:nj, :], lhsT=hidT[:, fc, c0:c0 + nj], rhs=wd[:, fc, dh * 512:(dh + 1) * 512], start=(fc == 0), stop=(fc == 7)),
                                      r=["hidT", "hidTc"] + wdk, w=[f"pY4_{dh}"])
                            A(lambda e: e.activation(out=yeb[yi][0:nj, 0:512], in_=pY[0][0:nj, :], func=AF.Identity, scale=gate_ap), r=["pY4_0", f"gatef{e2}"], w=[f"yeb{yi}a"])
                            V(lambda e: e.tensor_scalar(out=yeb[yi][0:nj, 512:1024], in0=pY[1][0:nj, :], scalar1=gate_ap, scalar2=None, op0=ALU.mult), r=["pY4_1", f"gatef{e2}"], w=[f"yeb{yi}b"])
                            S(lambda e: e.indirect_dma_start(out=accd[:, :], out_offset=bass.IndirectOffsetOnAxis(ap=idx_ap, axis=0),
                                                             in_=yeb[yi][0:nj, :], in_offset=None, compute_op=ALU.add),
                              r=[f"yeb{yi}a", f"yeb{yi}b", f"idxi{e2}"], w=["accd"])

                    wg, wgk = load_w(w_e_gate[l, 0])
                    wu, wuk = load_w(w_e_up[l, 0])
                    stage_x(0)
                    for ex_ in range(NE):
                        wd, wdk = load_w(w_e_down[l, ex_])
                        stage_hidden(ex_, wg, wgk, wu, wuk)
                        if ex_ + 1 < NE:
                            wg, wgk = load_w(w_e_gate[l, ex_ + 1])
                            wu, wuk = load_w(w_e_up[l, ex_ + 1])
                            stage_x(ex_ + 1)
                        stage_down(ex_, wd, wdk)
                P.barrier()
                if stop_after == ("E4", l):
                    break
                with ExitStack() as p5:
                    xt = [tsb(p5, f"xt5_{i}", [128, D], F32) for i in range(3)]
                    acc = [tsb(p5, f"acc{i}", [128, D], F32) for i in range(3)]
                    for n_, ti in enumerate(tiles_act):
                        which = 1 if ti < NCT else 0
                        i2 = n_ % 3
                        L(lambda e: e.dma_start(out=xt[i2][:, :], in_=xall[ti * 128:(ti + 1) * 128, :]), w=[f"xt5_{i2}"])
                        L(lambda e: e.dma_start(out=acc[i2][:, :], in_=accd[ti * 128:(ti + 1) * 128, :]), w=[f"acc{i2}"])
                        V(lambda e: e.tensor_tensor(out=acc[i2][:, :], in0=acc[i2][:, :], in1=G2[which][:, :], op=ALU.mult), r=[f"acc{i2}", f"G2_{which}"], w=[f"acc{i2}"])
                        V(lambda e: e.tensor_tensor(out=acc[i2][:, :], in0=acc[i2][:, :], in1=xt[i2][:, :], op=ALU.add), r=[f"acc{i2}", f"xt5_{i2}"], w=[f"acc{i2}"])
                        if last:
                            S(lambda e: e.dma_start(out=y_out[(ti - NCT) * 128:(ti - NCT + 1) * 128, :], in_=acc[i2][:, :]), r=[f"acc{i2}"])
                        else:
                            S(lambda e: e.dma_start(out=xall[ti * 128:(ti + 1) * 128, :], in_=acc[i2][:, :]), r=[f"acc{i2}"])
            P.barrier()
            if stop_after == ("L", l):
                break
        P.barrier()
        print("instr counts", P.ninstr, "total", P.total, flush=True)
    return nc


def _rope_table():
    t = np.arange(SEQ)
    row = (t // 64).astype(np.float64)
    col = (t % 64).astype(np.float64)
    out = np.zeros((SEQ, 192), np.float64)

    def fill(off, dim):
        ad = dim // 2
        inv = 10000.0 ** (-np.arange(0, ad, 2, dtype=np.float64) / ad)
        ar = row[:, None] * inv[None, :]
        ac = col[:, None] * inv[None, :]
        C = np.concatenate([np.cos(ar), np.cos(ar), np.cos(ac), np.cos(ac)], axis=1)
        S_ = np.concatenate([-np.sin(ar), np.sin(ar), -np.sin(ac), np.sin(ac)], axis=1)
        out[:, off:off + dim] = C
        out[:, off + dim:off + 2 * dim] = S_
    fill(0, 64)
    fill(128, 32)
    return out.astype(np.float32)


_NC_CACHE = {}


def kernel(**inputs):
    key = "full"
    if key not in _NC_CACHE:
        _NC_CACHE[key] = build()
    nc = _NC_CACHE[key]
    rope = _rope_table()
    shared = {k: np.ascontiguousarray(np.asarray(v, dtype=np.float32)) for k, v in inputs.items() if k not in ("x", "c", "ctx")}
    x = np.asarray(inputs["x"], dtype=np.float32); c = np.asarray(inputs["c"], dtype=np.float32); ctx = np.asarray(inputs["ctx"], dtype=np.float32)
    in_maps = []
    for b in range(8):
        m = dict(shared)
        m["x"] = np.ascontiguousarray(x[b]); m["ctx"] = np.ascontiguousarray(ctx[b]); m["c"] = np.ascontiguousarray(c[b])
        m["rope"] = rope
        in_maps.append(m)
    res = run_bass_kernel_spmd(nc, in_maps, core_ids=list(range(8)))
    return np.stack([np.asarray(r["y"], dtype=np.float32) for r in res.results], axis=0)
```

```python
import numpy as np
from contextlib import ExitStack
import concourse.bass as bass
import concourse.mybir as mybir
from concourse.bass_utils import run_bass_kernel_spmd

F32 = mybir.dt.float32
BF16 = mybir.dt.bfloat16
I32 = mybir.dt.int32
AF = mybir.ActivationFunctionType
ALU = mybir.AluOpType
AX = mybir.AxisListType

D = 1024
SEQ = 4096
CTX = 256
T = SEQ + CTX
NT = T // 128
NCT = CTX // 128
DEPTH = 2
INW = 1504
NE = 16
CAP_L = 512
CAP_C = 32
XW = D + 36
EPS = 1e-6
BIG = 8192.0


def _is_psum(k):
    return k.startswith("pmod") or (len(k) > 1 and k[0] == "p" and k[1].isupper())


class Prog:
    ENG = ("pe", "dve", "act", "pool", "sp")

    def __init__(self, nc, stack, same_engine_sync=True, ring=(("sp", 16), ("pool", 16))):
        self.nc = nc
        self.stack = stack
        self.e = {"pe": nc.tensor, "dve": nc.vector, "act": nc.scalar, "pool": nc.gpsimd, "sp": nc.sync}
        self.same = same_engine_sync
        self.sems = {}
        self.semval = {}
        self.cur = {}
        self.epoch = {n: 0 for n in self.ENG}
        for n in self.ENG:
            self.cur[n] = self._mk("E_" + n + "_0")
        self.ring = {}
        self.ringpos = {}
        for q, k in ring:
            self.ring[q] = [self._mk(f"D_{q}{i}") for i in range(k)]
            self.ringpos[q] = 0
        self.waited = {n: {} for n in self.ENG}
        self.lastw = {}
        self.readers = {}
        self.ninstr = {n: 0 for n in self.ENG}

    def _mk(self, name):
        s = self.stack.enter_context(self.nc.semaphore(name))
        self.sems[name] = s
        self.semval[name] = 0
        return name

    def _wait(self, eng, ev):
        sem, val = ev
        if val <= 0 or self.waited[eng].get(sem, 0) >= val:
            return
        if sem == self.cur[eng] and (eng == "pe" or not self.same):
            return
        self.e[eng].wait_ge(self.sems[sem], val)
        self.waited[eng][sem] = val

    def _deps(self, eng, r, w):
        for res in list(r) + list(w):
            ev = self.lastw.get(res)
            if ev is not None:
                self._wait(eng, ev)
        for res in w:
            for sem, val in self.readers.get(res, {}).items():
                self._wait(eng, (sem, val))

    def _record(self, ev, r, w):
        for res in w:
            self.lastw[res] = ev
            self.readers[res] = {}
        for res in r:
            d = self.readers.setdefault(res, {})
            if d.get(ev[0], 0) < ev[1]:
                d[ev[0]] = ev[1]

    LIMIT = None
    total = 0

    def op(self, eng, fn, r=(), w=()):
        self.total += 1
        if self.LIMIT is not None and self.total > self.LIMIT:
            return
        w = list(w) + [k for k in r if _is_psum(k)]
        r = [k for k in r if not _is_psum(k)]
        self._deps(eng, r, w)
        ins = fn(self.e[eng])
        sem = self.cur[eng]
        self.semval[sem] += 1
        ins.then_inc(self.sems[sem], 1)
        self.ninstr[eng] += 1
        self._record((sem, self.semval[sem]), r, w)

    def dma(self, q, fn, r=(), w=()):
        self.total += 1
        if self.LIMIT is not None and self.total > self.LIMIT:
            return
        self._deps(q, r, w)
        k = self.ringpos[q]
        sem = self.ring[q][k]
        self.ringpos[q] = (k + 1) % len(self.ring[q])
        self._wait(q, (sem, self.semval[sem]))
        ins = fn(self.e[q])
        self.semval[sem] += 16
        ins.then_inc(self.sems[sem], 16)
        self.ninstr[q] += 1
        self._record((sem, self.semval[sem]), r, w)

    def barrier(self):
        for eng in self.ENG:
            for sem, val in self.semval.items():
                self._wait(eng, (sem, val))
        self.lastw.clear()
        self.readers.clear()
        for eng in self.ENG:
            if self.semval[self.cur[eng]] > 15000:
                self.epoch[eng] += 1
                self.cur[eng] = self._mk(f"E_{eng}_{self.epoch[eng]}")


def build(stop_after=None, dbg=(), limit=None, light=False):
    nc = bass.Bass("TRN2", target_bir_lowering=False)
    Prog.LIMIT = limit

    def din(name, shape, dt=F32):
        return nc.dram_tensor(name, list(shape), dt, kind="ExternalInput").ap()

    x_in = din("x", [SEQ, D]); ctx_in = din("ctx", [CTX, D]); c_in = din("c", [D]); cctx_in = din("c_ctx", [D])
    w_ada = din("w_ada", [DEPTH, D, 6 * D]); b_ada = din("b_ada", [DEPTH, 6 * D])
    norm1_g = din("norm1_g", [DEPTH, D]); norm2_g = din("norm2_g", [DEPTH, D])
    w_in = din("w_in", [DEPTH, D, INW])
    a_q_norm = din("a_q_norm", [DEPTH, 64]); a_k_norm = din("a_k_norm", [DEPTH, 64])
    b_cq_norm = din("b_cq_norm", [DEPTH, 192]); b_ckv_norm = din("b_ckv_norm", [DEPTH, 128])
    w_uq = din("w_uq", [DEPTH, 192, 576]); w_ukv = din("w_ukv", [DEPTH, 128, 768])
    b_qn_norm = din("b_qn_norm", [DEPTH, 64]); b_kn_norm = din("b_kn_norm", [DEPTH, 64])
    b_qr_norm = din("b_qr_norm", [DEPTH, 32]); b_kr_norm = din("b_kr_norm", [DEPTH, 32])
    c_q_norm = din("c_q_norm", [DEPTH, 64]); c_k_norm = din("c_k_norm", [DEPTH, 64])
    c_sink = din("c_sink", [DEPTH, 4])
    w_out = din("w_out", [DEPTH, D, D]); w_router = din("w_router", [DEPTH, D, NE])
    if not light:
        w_e_gate = din("w_e_gate", [DEPTH, NE, D, D]); w_e_up = din("w_e_up", [DEPTH, NE, D, D])
        w_e_down = din("w_e_down", [DEPTH, NE, D, D])
    rope = din("rope", [SEQ, 192])
    y_out = nc.dram_tensor("y", [SEQ, D], F32, kind="ExternalOutput").ap()

    def dscr(name, shape, dt):
        kind = "ExternalOutput" if name in dbg else "Internal"
        return nc.dram_tensor(name, list(shape), dt, kind=kind).ap()

    xall = dscr("xall", [T, D], F32)
    modv = dscr("modv", [2, 6 * D], F32)
    qkT_A = dscr("qkT_A", [512, T], BF16)
    qkT_B = dscr("qkT_B", [12, 96, T], BF16)
    qkT_C = dscr("qkT_C", [384, T], BF16)
    vall = dscr("vall", [T, 640], BF16)
    mix = dscr("mix", [T, D], BF16)
    h2d = dscr("h2d", [T, D], BF16)
    xe_l = [dscr(f"xe_l{i}", [CAP_L, XW], BF16) for i in range(NE)]
    xe_c = [dscr(f"xe_c{i}", [CAP_C, XW], BF16) for i in range(NE)]
    accd = dscr("accd", [T, D], F32)
    aT_d = dscr("aT_d", [NE, SEQ], F32)
    mk_d = dscr("mk_d", [NE, SEQ], BF16)
    ye_l = [dscr(f"ye_l{i}", [CAP_L, D], BF16) for i in range(NE)]
    ye_c = [dscr(f"ye_c{i}", [CAP_C, D], BF16) for i in range(NE)]
    dbg_aff = dscr("dbg_aff", [128, NT * NE], F32)
    dbg_slot = dscr("dbg_slot", [128, NT * NE], I32)

    with ExitStack() as top:
        P = Prog(nc, top)
        V = lambda fn, r=(), w=(): P.op("dve", fn, r, w)
        A = lambda fn, r=(), w=(): P.op("act", fn, r, w)
        G = lambda fn, r=(), w=(): P.op("pool", fn, r, w)
        M = lambda fn, r=(), w=(): P.op("pe", fn, r, w)
        L = lambda fn, r=(), w=(): P.dma("sp", fn, r, w)
        S = lambda fn, r=(), w=(): P.dma("pool", fn, r, w)

        LAYER = [-1]

        def tsb(stk, name, shape, dt):
            return stk.enter_context(nc.sbuf_tensor(f"{name}_L{LAYER[0]}", list(shape), dt))

        def tps(stk, name, shape, dt=F32):
            return stk.enter_context(nc.psum_tensor(f"{name}_L{LAYER[0]}", list(shape), dt))

        REG = {CAP_L - 1: nc.gpsimd.to_reg(CAP_L - 1), CAP_C - 1: nc.gpsimd.to_reg(CAP_C - 1)}
        identf = tsb(top, "identf", [128, 128], F32)
        ident = tsb(top, "ident", [128, 128], BF16)
        onesf = tsb(top, "onesf", [128, 128], F32)
        ones_b = tsb(top, "ones_b", [128, 128], BF16)
        Ub = tsb(top, "Ub", [128, 128], BF16)
        M1 = tsb(top, "M1", [128, 128], BF16)
        M2 = tsb(top, "M2", [128, 128], BF16)
        ctmp = tsb(top, "ctmp", [128, 128], F32)
        cs = tsb(top, "cs", [128, 8, 2], F32)
        craw = tsb(top, "craw", [128, 2, 8], F32)

        G(lambda e: e.memset(identf[:], 0.0), w=["identf"])
        G(lambda e: e.affine_select(out=identf[:], in_=identf[:], pattern=[[-1, 128]], base=0, channel_multiplier=1,
                                    compare_op=ALU.not_equal, fill=1.0), r=["identf"], w=["identf"])
        V(lambda e: e.tensor_copy(out=ident[:], in_=identf[:]), r=["identf"], w=["ident"])
        G(lambda e: e.memset(onesf[:], 1.0), w=["onesf"])
        V(lambda e: e.tensor_copy(out=ones_b[:], in_=onesf[:]), r=["onesf"], w=["ones_b"])
        G(lambda e: e.affine_select(out=ctmp[:], in_=onesf[:], pattern=[[-1, 128]], base=0, channel_multiplier=1,
                                    compare_op=ALU.is_ge, fill=0.0), r=["onesf"], w=["ctmp"])
        V(lambda e: e.tensor_copy(out=M1[:], in_=ctmp[:]), r=["ctmp"], w=["M1"])
        G(lambda e: e.affine_select(out=ctmp[:], in_=onesf[:], pattern=[[1, 128]], base=0, channel_multiplier=-1,
                                    compare_op=ALU.is_ge, fill=0.0), r=["onesf"], w=["ctmp"])
        V(lambda e: e.tensor_copy(out=M2[:], in_=ctmp[:]), r=["ctmp"], w=["M2"])
        G(lambda e: e.affine_select(out=ctmp[:], in_=onesf[:], pattern=[[1, 128]], base=-1, channel_multiplier=-1,
                                    compare_op=ALU.is_ge, fill=0.0), r=["onesf"], w=["ctmp"])
        V(lambda e: e.tensor_copy(out=Ub[:], in_=ctmp[:]), r=["ctmp"], w=["Ub"])

        L(lambda e: e.dma_start(out=craw[:, 0, :], in_=c_in.rearrange("(k p) -> p k", p=128), allow_slow_non_contiguous=True), w=["craw"])
        L(lambda e: e.dma_start(out=craw[:, 1, :], in_=cctx_in.rearrange("(k p) -> p k", p=128), allow_slow_non_contiguous=True), w=["craw"])
        A(lambda e: e.activation(out=cs[:, :, :].rearrange("p k j -> p j k"), in_=craw[:, :, :], func=AF.Silu), r=["craw"], w=["cs"])

        L(lambda e: e.dma_start(out=xall[0:CTX, :], in_=ctx_in))
        for i in range(8):
            L(lambda e: e.dma_start(out=xall[CTX + i * 512:CTX + (i + 1) * 512, :], in_=x_in[i * 512:(i + 1) * 512, :]))
        P.barrier()

        def rstd_from_ss(ss_ap, out_ap, scale, r, w):
            A(lambda e: e.activation(out=out_ap, in_=ss_ap, func=AF.Sqrt, scale=scale, bias=EPS), r=r, w=w)
            V(lambda e: e.reciprocal(out=out_ap, in_=out_ap), r=w, w=w)

        for l in range(DEPTH):
            last = l == DEPTH - 1
            LAYER[0] = l
            tiles_act = list(range(NT)) if not last else list(range(NCT, NT))

            with ExitStack() as ph:
                wst = [tsb(ph, f"wstA{i}", [128, 8, 512], F32) for i in range(2)]
                modrow = tsb(ph, "modrow", [2, 6 * D], F32)
                bada = tsb(ph, "bada", [2, 6 * D], F32)
                gn = tsb(ph, "gn", [2, 2, D], F32)
                pmod = [tps(ph, f"pmod{i}", [128, 512]) for i in range(2)]
                L(lambda e: e.dma_start(out=bada[:, :], in_=b_ada[l:l + 1, :].broadcast_to([2, 6 * D])), w=["bada"])
                L(lambda e: e.dma_start(out=gn[:, 0, :], in_=norm1_g[l:l + 1, :].broadcast_to([2, D])), w=["gn"])
                L(lambda e: e.dma_start(out=gn[:, 1, :], in_=norm2_g[l:l + 1, :].broadcast_to([2, D])), w=["gn"])
                for nb in range(12):
                    wb = wst[nb % 2]; pm = pmod[nb % 2]
                    for h in range(2):
                        L(lambda e: e.dma_start(out=wb[:, h * 4:(h + 1) * 4, :],
                                                in_=w_ada[l, h * 512:(h + 1) * 512, nb * 512:(nb + 1) * 512].rearrange("(k p) n -> p k n", p=128)),
                          w=[f"wstA{nb % 2}"])
                    for kc in range(8):
                        M(lambda e: e.matmul(pm[0:2, :], lhsT=cs[:, kc, :], rhs=wb[:, kc, :], start=(kc == 0), stop=(kc == 7)),
                          r=[f"wstA{nb % 2}", "cs"], w=[f"pmod{nb % 2}"])
                    V(lambda e: e.tensor_tensor(out=modrow[:, nb * 512:(nb + 1) * 512], in0=pm[0:2, :], in1=bada[:, nb * 512:(nb + 1) * 512], op=ALU.add),
                      r=[f"pmod{nb % 2}", "bada"], w=["modrow"])
                for j, off in ((0, 1 * D), (1, 4 * D)):
                    V(lambda e: e.scalar_tensor_tensor(out=modrow[:, off:off + D], in0=modrow[:, off:off + D], scalar=1.0, in1=gn[:, j, :],
                                                       op0=ALU.add, op1=ALU.mult), r=["modrow", "gn"], w=["modrow"])
                S(lambda e: e.dma_start(out=modv, in_=modrow[:, :]), r=["modrow"])
            P.barrier()
            if stop_after == ("A", l):
                break

            def load_bcast(dst, which, j, key):
                L(lambda e: e.dma_start(out=dst, in_=modv[which:which + 1, j * D:(j + 1) * D].broadcast_to([128, D])), w=[key])

            with ExitStack() as ph:
                wst = tsb(ph, "wstB", [128, 8, 512], F32)
                w_in_b = tsb(ph, "w_in_b", [128, 8, INW], BF16)
                wuq_f = tsb(ph, "wuq_f", [128, 2, 576], F32)
                wuq_b = tsb(ph, "wuq_b", [128, 2, 576], BF16)
                wukv_f = tsb(ph, "wukv_f", [128, 768], F32)
                wukv_b = tsb(ph, "wukv_b", [128, 768], BF16)
                gcq = tsb(ph, "gcq", [128, 2], F32)
                gckv = tsb(ph, "gckv", [128, 1], F32)
                gA = tsb(ph, "gA", [128, 8, 64], F32)
                gC = tsb(ph, "gC", [128, 6, 64], F32)
                gBq = tsb(ph, "gBq", [128, 96], F32)
                gBk = tsb(ph, "gBk", [128, 64], F32)
                gBkr = tsb(ph, "gBkr", [128, 32], F32)
                invB = tsb(ph, "invB", [128, 3], F32)
                invQ = tsb(ph, "invQ", [128, 12], F32)
                Ar = [tsb(ph, f"Ar{i}", [128, D], F32) for i in range(2)]
                Br = [tsb(ph, f"Br{i}", [128, D], F32) for i in range(2)]
                xt = [tsb(ph, f"xtB{i}", [128, D], F32) for i in range(2)]
                rp = [tsb(ph, f"rp{i}", [128, 192], F32) for i in range(2)]
                junk = tsb(ph, "junkB", [128, D], BF16)
                st1 = tsb(ph, "st1", [128, 4], F32)
                hf = tsb(ph, "hf", [128, D], F32)
                hb = tsb(ph, "hb", [128, D], BF16)
                hT = tsb(ph, "hT", [128, 8, 128], BF16)
                sq = tsb(ph, "sq", [128, 576], F32)
                ssh = tsb(ph, "ssh", [128, 16], F32)
                qn = tsb(ph, "qn", [128, 576], F32)
                t1 = tsb(ph, "t1", [128, 576], F32)
                t2 = tsb(ph, "t2", [128, 576], F32)
                qA = tsb(ph, "qA", [128, 8, 64], BF16)
                qC = tsb(ph, "qC", [128, 6, 64], BF16)
                qB = tsb(ph, "qB", [128, 12, 96], BF16)
                cqn = tsb(ph, "cqn", [128, 320], BF16)
                cT = tsb(ph, "cT", [128, 3, 128], BF16)
                kpe = tsb(ph, "kpe", [128, 32], F32)
                kpe2 = tsb(ph, "kpe2", [128, 32], F32)
                vst = tsb(ph, "vst", [128, 640], BF16)
                stA = tsb(ph, "stA", [128, 4, 512], BF16)
                stB = tsb(ph, "stB", [128, 12, 512], BF16)
                stC = tsb(ph, "stC", [128, 3, 512], BF16)
                pT = tps(ph, "pTB", [128, 1024], BF16)
                pP = [tps(ph, f"pP{i}", [128, 512]) for i in range(3)]
                pQ = [tps(ph, f"pQ{i}", [128, 512]) for i in range(2)]
                pK = [tps(ph, f"pK{i}", [128, 512]) for i in range(2)]

                for b, (c0, c1) in enumerate(((0, 512), (512, 1024), (1024, INW))):
                    n = c1 - c0
                    for h in range(2):
                        L(lambda e: e.dma_start(out=wst[:, h * 4:(h + 1) * 4, 0:n],
                                                in_=w_in[l, h * 512:(h + 1) * 512, c0:c1].rearrange("(k p) n -> p k n", p=128)), w=["wstB"])
                    V(lambda e: e.tensor_copy(out=w_in_b[:, 0:4, c0:c1], in_=wst[:, 0:4, 0:n]), r=["wstB"], w=["w_in_b"])
                    G(lambda e: e.tensor_copy(out=w_in_b[:, 4:8, c0:c1], in_=wst[:, 4:8, 0:n]), r=["wstB"], w=["w_in_b"])
                L(lambda e: e.dma_start(out=wuq_f[:, 0, :], in_=w_uq[l, 0:128, :]), w=["wuq_f"])
                L(lambda e: e.dma_start(out=wuq_f[0:64, 1, :], in_=w_uq[l, 128:192, :]), w=["wuq_f"])
                L(lambda e: e.dma_start(out=wukv_f[:, :], in_=w_ukv[l, :, :]), w=["wukv_f"])
                L(lambda e: e.dma_start(out=gcq[:, 0:1], in_=b_cq_norm[l, 0:128].rearrange("(p o) -> p o", o=1)), w=["gcq"])
                L(lambda e: e.dma_start(out=gcq[0:64, 1:2], in_=b_cq_norm[l, 128:192].rearrange("(p o) -> p o", o=1)), w=["gcq"])
                L(lambda e: e.dma_start(out=gckv[:, 0:1], in_=b_ckv_norm[l, :].rearrange("(p o) -> p o", o=1)), w=["gckv"])
                V(lambda e: e.tensor_scalar(out=wuq_b[:, 0, :], in0=wuq_f[:, 0, :], scalar1=gcq[:, 0:1], scalar2=None, op0=ALU.mult), r=["wuq_f", "gcq"], w=["wuq_b"])
                V(lambda e: e.tensor_scalar(out=wuq_b[0:64, 1, :], in0=wuq_f[0:64, 1, :], scalar1=gcq[0:64, 1:2], scalar2=None, op0=ALU.mult), r=["wuq_f", "gcq"], w=["wuq_b"])
                V(lambda e: e.tensor_scalar(out=wukv_b[:, :], in0=wukv_f[:, :], scalar1=gckv[:, 0:1], scalar2=None, op0=ALU.mult), r=["wukv_f", "gckv"], w=["wukv_b"])
                for hh in range(8):
                    src = a_q_norm if hh < 6 else a_k_norm
                    L(lambda e: e.dma_start(out=gA[:, hh, :], in_=src[l:l + 1, :].broadcast_to([128, 64])), w=["gA"])
                for hh in range(6):
                    src = c_q_norm if hh < 4 else c_k_norm
                    L(lambda e: e.dma_start(out=gC[:, hh, :], in_=src[l:l + 1, :].broadcast_to([128, 64])), w=["gC"])
                L(lambda e: e.dma_start(out=gBq[:, 0:64], in_=b_qn_norm[l:l + 1, :].broadcast_to([128, 64])), w=["gBq"])
                L(lambda e: e.dma_start(out=gBq[:, 64:96], in_=b_qr_norm[l:l + 1, :].broadcast_to([128, 32])), w=["gBq"])
                L(lambda e: e.dma_start(out=gBk[:, :], in_=b_kn_norm[l:l + 1, :].broadcast_to([128, 64])), w=["gBk"])
                L(lambda e: e.dma_start(out=gBkr[:, :], in_=b_kr_norm[l:l + 1, :].broadcast_to([128, 32])), w=["gBkr"])
                for j, v in enumerate((1.0 / 192, 1.0 / 128, 1.0 / 32)):
                    G(lambda e: e.memset(invB[:, j:j + 1], v), w=["invB"])
                G(lambda e: e.memset(invQ[:, 0:6], 1.0 / 64), w=["invQ"])
                G(lambda e: e.memset(invQ[:, 6:12], 1.0 / 32), w=["invQ"])
                for which in range(2):
                    load_bcast(Ar[which][:, :], which, 1, f"Ar{which}")
                    load_bcast(Br[which][:, :], which, 0, f"Br{which}")

                def rope_apply(src_ap, H, Dh, C_ap, S_ap, out_ap, rs):
                    c4 = Dh // 4
                    t1v = t1[:, 0:H * Dh].rearrange("p (h d) -> p h d", h=H)
                    t2v = t2[:, 0:H * Dh].rearrange("p (h a b c) -> p h a b c", h=H, a=2, b=2)
                    s5 = src_ap.rearrange("p h (a b c) -> p h a b c", a=2, b=2)
                    S4 = S_ap.rearrange("p (a b c) -> p a b c", a=2, b=2)
                    V(lambda e: e.tensor_tensor(out=t1v, in0=src_ap, in1=C_ap.unsqueeze(1).to_broadcast([128, H, Dh]), op=ALU.mult), r=rs, w=["t1"])
                    V(lambda e: e.tensor_tensor(out=t2v[:, :, :, 0, :], in0=s5[:, :, :, 1, :],
                                                in1=S4[:, :, 0, :].unsqueeze(1).to_broadcast([128, H, 2, c4]), op=ALU.mult), r=rs, w=["t2"])
                    V(lambda e: e.tensor_tensor(out=t2v[:, :, :, 1, :], in0=s5[:, :, :, 0, :],
                                                in1=S4[:, :, 1, :].unsqueeze(1).to_broadcast([128, H, 2, c4]), op=ALU.mult), r=rs, w=["t2"])
                    return t1v, t2[:, 0:H * Dh].rearrange("p (h d) -> p h d", h=H)

                blocks = [list(range(0, NCT))] + [list(range(NCT + 4 * b, NCT + 4 * b + 4)) for b in range(SEQ // 512)]
                for blk in blocks:
                    for bi, ti in enumerate(blk):
                        isc = ti < NCT
                        which = 1 if isc else 0
                        li = ti - NCT
                        xb = xt[ti % 2]; xk = f"xtB{ti % 2}"
                        rpb = rp[ti % 2]; rk = f"rp{ti % 2}"
                        L(lambda e: e.dma_start(out=xb[:, :], in_=xall[ti * 128:(ti + 1) * 128, :]), w=[xk])
                        if not isc:
                            L(lambda e: e.dma_start(out=rpb[:, :], in_=rope[li * 128:(li + 1) * 128, :]), w=[rk])
                        A(lambda e: e.activation(out=junk[:, :], in_=xb[:, :], func=AF.Square, accum_out=st1[:, 0:1]), r=[xk], w=["junkB", "st1"])
                        rstd_from_ss(st1[:, 0:1], st1[:, 1:2], 1.0 / D, ["st1"], ["st1b"])
                        V(lambda e: e.scalar_tensor_tensor(out=hf[:, :], in0=xb[:, :], scalar=st1[:, 1:2], in1=Ar[which][:, :], op0=ALU.mult, op1=ALU.mult),
                          r=[xk, "st1b", f"Ar{which}"], w=["hf"])
                        V(lambda e: e.tensor_tensor(out=hb[:, :], in0=hf[:, :], in1=Br[which][:, :], op=ALU.add), r=["hf", f"Br{which}"], w=["hb"])
                        for kc in range(8):
                            M(lambda e: e.transpose(out=pT[:, kc * 128:(kc + 1) * 128], in_=hb[:, kc * 128:(kc + 1) * 128], identity=ident[:, :]),
                              r=["hb", "ident"], w=["pTB"])
                        A(lambda e: e.copy(out=hT[:, :, :].rearrange("p k t -> p (k t)"), in_=pT[:, :]), r=["pTB"], w=["hT"])
                        for b, (c0, c1) in enumerate(((0, 512), (512, 992), (992, INW))):
                            for kc in range(8):
                                M(lambda e: e.matmul(pP[b][:, 0:c1 - c0], lhsT=hT[:, kc, :], rhs=w_in_b[:, kc, c0:c1], start=(kc == 0), stop=(kc == 7)),
                                  r=["hT", "w_in_b"], w=[f"pP{b}"])

                        for (pp, pk, H, gain, gk, dst, dk) in ((pP[0], "pP0", 8, gA, "gA", qA, "qA"), (pP[2], "pP2", 6, gC, "gC", qC, "qC")):
                            n = H * 64
                            src3 = pp[:, 0:n].rearrange("p (h d) -> p h d", h=H)
                            A(lambda e: e.activation(out=sq[:, 0:n], in_=pp[:, 0:n], func=AF.Square), r=[pk], w=["sq"])
                            V(lambda e: e.tensor_reduce(out=ssh[:, 0:H], in_=sq[:, 0:n].rearrange("p (h d) -> p h d", h=H), axis=AX.X, op=ALU.add), r=["sq"], w=["ssh"])
                            rstd_from_ss(ssh[:, 0:H], ssh[:, 0:H], 1.0 / 64, ["ssh"], ["ssh"])
                            qn3 = qn[:, 0:n].rearrange("p (h d) -> p h d", h=H)
                            V(lambda e: e.tensor_tensor(out=qn3, in0=src3, in1=ssh[:, 0:H].unsqueeze(2).to_broadcast([128, H, 64]), op=ALU.mult), r=[pk, "ssh"], w=["qn"])
                            if isc:
                                V(lambda e: e.tensor_tensor(out=dst[:, :, :], in0=qn3, in1=gain[:, :, :], op=ALU.mult), r=["qn", gk], w=[dk])
                            else:
                                V(lambda e: e.tensor_tensor(out=qn3, in0=qn3, in1=gain[:, :, :], op=ALU.mult), r=["qn", gk], w=["qn"])
                                a1, a2 = rope_apply(qn3, H, 64, rpb[:, 0:64], rpb[:, 64:128], None, ["qn", rk])
                                V(lambda e: e.tensor_tensor(out=dst[:, :, :], in0=a1, in1=a2, op=ALU.add), r=["t1", "t2"], w=[dk])
                        A(lambda e: e.copy(out=vst[:, 0:128], in_=pP[1][:, 0:128]), r=["pP1"], w=["vst"])
                        A(lambda e: e.copy(out=vst[:, 512:640], in_=pP[2][:, 384:512]), r=["pP2"], w=["vst"])

                        A(lambda e: e.activation(out=sq[:, 0:352], in_=pP[1][:, 128:480], func=AF.Square), r=["pP1"], w=["sq"])
                        V(lambda e: e.tensor_reduce(out=ssh[:, 0:1], in_=sq[:, 0:192], axis=AX.X, op=ALU.add), r=["sq"], w=["ssh"])
                        V(lambda e: e.tensor_reduce(out=ssh[:, 1:2], in_=sq[:, 192:320], axis=AX.X, op=ALU.add), r=["sq"], w=["ssh"])
                        V(lambda e: e.tensor_reduce(out=ssh[:, 2:3], in_=sq[:, 320:352], axis=AX.X, op=ALU.add), r=["sq"], w=["ssh"])
                        V(lambda e: e.tensor_tensor(out=ssh[:, 0:3], in0=ssh[:, 0:3], in1=invB[:, :], op=ALU.mult), r=["ssh", "invB"], w=["ssh"])
                        rstd_from_ss(ssh[:, 0:3], ssh[:, 0:3], 1.0, ["ssh"], ["ssh"])
                        V(lambda e: e.tensor_scalar(out=cqn[:, 0:192], in0=pP[1][:, 128:320], scalar1=ssh[:, 0:1], scalar2=None, op0=ALU.mult), r=["pP1", "ssh"], w=["cqn"])
                        V(lambda e: e.tensor_scalar(out=cqn[:, 192:320], in0=pP[1][:, 320:448], scalar1=ssh[:, 1:2], scalar2=None, op0=ALU.mult), r=["pP1", "ssh"], w=["cqn"])
                        V(lambda e: e.scalar_tensor_tensor(out=kpe[:, :], in0=pP[1][:, 448:480], scalar=ssh[:, 2:3], in1=gBkr[:, :], op0=ALU.mult, op1=ALU.mult),
                          r=["pP1", "ssh", "gBkr"], w=["kpe"])
                        if not isc:
                            a1, a2 = rope_apply(kpe[:, :].unsqueeze(1), 1, 32, rpb[:, 128:160], rpb[:, 160:192], None, ["kpe", rk])
                            V(lambda e: e.tensor_tensor(out=kpe2[:, :].unsqueeze(1), in0=a1, in1=a2, op=ALU.add), r=["t1", "t2"], w=["kpe2"])
                            kp = kpe2
                        else:
                            kp = kpe
                        V(lambda e: e.tensor_copy(out=qB[:, 6:12, 64:96], in_=kp[:, :].unsqueeze(1).to_broadcast([128, 6, 32])), r=["kpe", "kpe2"], w=["qBk"])
                        M(lambda e: e.transpose(out=pT[:, 0:128], in_=cqn[:, 0:128], identity=ident[:, :]), r=["cqn", "ident"], w=["pTB"])
                        M(lambda e: e.transpose(out=pT[0:64, 128:256], in_=cqn[:, 128:192], identity=ident[:, :]), r=["cqn", "ident"], w=["pTB"])
                        M(lambda e: e.transpose(out=pT[:, 256:384], in_=cqn[:, 192:320], identity=ident[:, :]), r=["cqn", "ident"], w=["pTB"])
                        A(lambda e: e.copy(out=cT[:, 0, :], in_=pT[:, 0:128]), r=["pTB"], w=["cT"])
                        A(lambda e: e.copy(out=cT[0:64, 1, :], in_=pT[0:64, 128:256]), r=["pTB"], w=["cT"])
                        A(lambda e: e.copy(out=cT[:, 2, :], in_=pT[:, 256:384]), r=["pTB"], w=["cT"])
                        for hf_ in range(2):
                            M(lambda e: e.matmul(pQ[hf_][:, 0:288], lhsT=cT[:, 0, :], rhs=wuq_b[:, 0, hf_ * 288:(hf_ + 1) * 288], start=True, stop=False),
                              r=["cT", "wuq_b"], w=[f"pQ{hf_}"])
                            M(lambda e: e.matmul(pQ[hf_][:, 0:288], lhsT=cT[0:64, 1, :], rhs=wuq_b[0:64, 1, hf_ * 288:(hf_ + 1) * 288], start=False, stop=True),
                              r=["cT", "wuq_b"], w=[f"pQ{hf_}"])
                            M(lambda e: e.matmul(pK[hf_][:, 0:384], lhsT=cT[:, 2, :], rhs=wukv_b[:, hf_ * 384:(hf_ + 1) * 384], start=True, stop=True),
                              r=["cT", "wukv_b"], w=[f"pK{hf_}"])
                        for hf_ in range(2):
                            A(lambda e: e.activation(out=sq[:, hf_ * 288:(hf_ + 1) * 288], in_=pQ[hf_][:, 0:288], func=AF.Square), r=[f"pQ{hf_}"], w=["sq"])
                        sq3 = sq[:, 0:576].rearrange("p (h d) -> p h d", h=6)
                        V(lambda e: e.tensor_reduce(out=ssh[:, 0:6], in_=sq3[:, :, 0:64], axis=AX.X, op=ALU.add), r=["sq"], w=["ssh"])
                        V(lambda e: e.tensor_reduce(out=ssh[:, 6:12], in_=sq3[:, :, 64:96], axis=AX.X, op=ALU.add), r=["sq"], w=["ssh"])
                        V(lambda e: e.tensor_tensor(out=ssh[:, 0:12], in0=ssh[:, 0:12], in1=invQ[:, :], op=ALU.mult), r=["ssh", "invQ"], w=["ssh"])
                        rstd_from_ss(ssh[:, 0:12], ssh[:, 0:12], 1.0, ["ssh"], ["ssh"])
                        qn3 = qn[:, 0:576].rearrange("p (h d) -> p h d", h=6)
                        for hf_ in range(2):
                            p3 = pQ[hf_][:, 0:288].rearrange("p (h d) -> p h d", h=3)
                            hs = slice(hf_ * 3, hf_ * 3 + 3)
                            V(lambda e: e.tensor_tensor(out=qn3[:, hs, 0:64], in0=p3[:, :, 0:64],
                                                        in1=ssh[:, hf_ * 3:hf_ * 3 + 3].unsqueeze(2).to_broadcast([128, 3, 64]), op=ALU.mult), r=[f"pQ{hf_}", "ssh"], w=["qn"])
                            V(lambda e: e.tensor_tensor(out=qn3[:, hs, 64:96], in0=p3[:, :, 64:96],
                                                        in1=ssh[:, 6 + hf_ * 3:6 + hf_ * 3 + 3].unsqueeze(2).to_broadcast([128, 3, 32]), op=ALU.mult), r=[f"pQ{hf_}", "ssh"], w=["qn"])
                        V(lambda e: e.tensor_tensor(out=qn3, in0=qn3, in1=gBq[:, :].unsqueeze(1).to_broadcast([128, 6, 96]), op=ALU.mult), r=["qn", "gBq"], w=["qn"])
                        V(lambda e: e.tensor_copy(out=qB[:, 0:6, 0:64], in_=qn3[:, :, 0:64]), r=["qn"], w=["qBq"])
                        if isc:
                            V(lambda e: e.tensor_copy(out=qB[:, 0:6, 64:96], in_=qn3[:, :, 64:96]), r=["qn"], w=["qBq"])
                        else:
                            a1, a2 = rope_apply(qn3[:, :, 64:96], 6, 32, rpb[:, 128:160], rpb[:, 160:192], None, ["qn", rk])
                            V(lambda e: e.tensor_tensor(out=qB[:, 0:6, 64:96], in0=a1, in1=a2, op=ALU.add), r=["t1", "t2"], w=["qBq"])
                        for hf_ in range(2):
                            A(lambda e: e.activation(out=sq[:, hf_ * 192:(hf_ + 1) * 192].rearrange("p (h d) -> p h d", h=3),
                                                     in_=pK[hf_][:, 0:384].rearrange("p (h d) -> p h d", h=3)[:, :, 0:64], func=AF.Square), r=[f"pK{hf_}"], w=["sq"])
                            A(lambda e: e.copy(out=vst[:, 128 + hf_ * 192:128 + (hf_ + 1) * 192].rearrange("p (h d) -> p h d", h=3),
                                               in_=pK[hf_][:, 0:384].rearrange("p (h d) -> p h d", h=3)[:, :, 64:128]), r=[f"pK{hf_}"], w=["vst"])
                        V(lambda e: e.tensor_reduce(out=ssh[:, 0:6], in_=sq[:, 0:384].rearrange("p (h d) -> p h d", h=6), axis=AX.X, op=ALU.add), r=["sq"], w=["ssh"])
                        rstd_from_ss(ssh[:, 0:6], ssh[:, 0:6], 1.0 / 64, ["ssh"], ["ssh"])
                        kn3 = qn[:, 0:384].rearrange("p (h d) -> p h d", h=6)
                        for hf_ in range(2):
                            p3 = pK[hf_][:, 0:384].rearrange("p (h d) -> p h d", h=3)
                            V(lambda e: e.tensor_tensor(out=kn3[:, hf_ * 3:hf_ * 3 + 3, :], in0=p3[:, :, 0:64],
                                                        in1=ssh[:, hf_ * 3:hf_ * 3 + 3].unsqueeze(2).to_broadcast([128, 3, 64]), op=ALU.mult), r=[f"pK{hf_}", "ssh"], w=["qn"])
                        V(lambda e: e.tensor_tensor(out=qB[:, 6:12, 0:64], in0=kn3, in1=gBk[:, :].unsqueeze(1).to_broadcast([128, 6, 64]), op=ALU.mult), r=["qn", "gBk"], w=["qBk"])

                        S(lambda e: e.dma_start(out=vall[ti * 128:(ti + 1) * 128, :], in_=vst[:, :]), r=["vst"])
                        c0 = bi * 128
                        for i in range(4):
                            M(lambda e: e.transpose(out=pT[:, i * 128:(i + 1) * 128], in_=qA[:, 2 * i:2 * i + 2, :].rearrange("p h d -> p (h d)"), identity=ident[:, :]),
                              r=["qA", "ident"], w=["pTB"])
                        A(lambda e: e.copy(out=stA[:, :, c0:c0 + 128], in_=pT[:, 0:512].rearrange("p (i t) -> p i t", i=4)), r=["pTB"], w=["stA"])
                        for i in range(3):
                            M(lambda e: e.transpose(out=pT[:, i * 128:(i + 1) * 128], in_=qC[:, 2 * i:2 * i + 2, :].rearrange("p h d -> p (h d)"), identity=ident[:, :]),
                              r=["qC", "ident"], w=["pTB"])
                        V(lambda e: e.tensor_copy(out=stC[:, :, c0:c0 + 128], in_=pT[:, 0:384].rearrange("p (i t) -> p i t", i=3)), r=["pTB"], w=["stC"])
                        for rnd in range(2):
                            for i in range(6):
                                M(lambda e: e.transpose(out=pT[0:96, i * 128:(i + 1) * 128], in_=qB[:, rnd * 6 + i, :], identity=ident[:, :]),
                                  r=["qBq", "qBk", "ident"], w=["pTB"])
                            A(lambda e: e.copy(out=stB[0:96, rnd * 6:rnd * 6 + 6, c0:c0 + 128], in_=pT[0:96, 0:768].rearrange("p (i t) -> p i t", i=6)), r=["pTB"], w=["stB"])
                    W = len(blk) * 128
                    t0 = blk[0] * 128
                    S(lambda e: e.dma_start(out=qkT_A[:, t0:t0 + W].rearrange("(i p) t -> p i t", p=128), in_=stA[:, :, 0:W]), r=["stA"])
                    S(lambda e: e.dma_start(out=qkT_C[:, t0:t0 + W].rearrange("(i p) t -> p i t", p=128), in_=stC[:, :, 0:W]), r=["stC"])
                    S(lambda e: e.dma_start(out=qkT_B[:, :, t0:t0 + W].rearrange("j p t -> p j t"), in_=stB[0:96, :, 0:W]), r=["stB"])
            P.barrier()
            if stop_after == ("B", l):
                break

            with ExitStack() as ph:
                kt = [tsb(ph, f"kt{i}", [96, T], BF16) for i in range(2)]
                vb = [tsb(ph, f"vb{i}", [128, NT, 65], BF16) for i in range(2)]
                qt = [tsb(ph, f"qt{i}", [96, 512], BF16) for i in range(2)]
                pt = [tsb(ph, f"pt{i}", [128, 512], BF16) for i in range(3)]
                ob = [tsb(ph, f"ob{i}", [128, 4, 64], BF16) for i in range(2)]
                den = tsb(ph, "den", [128, 8], F32)
                sinkr = tsb(ph, "sinkr", [128, 4], F32)
                sinke = tsb(ph, "sinke", [128, 4], F32)
                pS = [tps(ph, f"pS{i}", [128, 512]) for i in range(3)]
                pO = [tps(ph, f"pO{i}", [128, 512]) for i in range(2)]
                den2 = [tsb(ph, f"den2_{i}", [128, 4], F32) for i in range(2)]
                L(lambda e: e.dma_start(out=sinkr[:, :], in_=c_sink[l:l + 1, :].broadcast_to([128, 4])), w=["sinkr"])
                A(lambda e: e.activation(out=sinke[:, :], in_=sinkr[:, :], func=AF.Exp), r=["sinkr"], w=["sinke"])
                for i in range(2):
                    G(lambda e: e.memset(vb[i][:, :, 64:65], 1.0), w=[f"vb{i}"])
                cnt = {"kv": 0, "q": 0, "s": 0, "p": 0, "u": 0}
                mixv = mix.rearrange("(j p) c -> p j c", p=128)
                vallv = vall.rearrange("(j p) c -> p j c", p=128)
                pending = []

                def emit_tail(st):
                    (si, pi, Wq, scale, msk, subs, u, n, nkeys, vbuf, vk, j, sink_h, out_fn) = st
                    ui = u % 2
                    A(lambda e: e.activation(out=pt[pi][:, 0:Wq], in_=pS[si][:, 0:Wq], func=AF.Exp, scale=scale), r=[f"pS{si}"], w=[f"pt{pi}"])
                    if msk is not None:
                        V(lambda e: e.tensor_tensor(out=pt[pi][:, 0:Wq].rearrange("p (s t) -> p s t", s=subs), in0=pt[pi][:, 0:Wq].rearrange("p (s t) -> p s t", s=subs),
                                                    in1=msk[:, :].unsqueeze(1).to_broadcast([128, subs, 128]), op=ALU.mult), r=[f"pt{pi}"], w=[f"pt{pi}"])
                    for s_ in range(subs):
                        M(lambda e: e.matmul(pO[ui][:, s_ * 65:(s_ + 1) * 65], lhsT=pt[pi][:, s_ * 128:(s_ + 1) * 128], rhs=vbuf[:, j, :],
                                             start=(n == 0 and s_ == 0), stop=(n == nkeys - 1), skip_group_check=True),
                          r=[f"pt{pi}", vk], w=[f"pO{ui}"])
                    if n == nkeys - 1:
                        pov = pO[ui][:, 0:subs * 65].rearrange("p (s c) -> p s c", s=subs)
                        dn = den2[ui]
                        if sink_h is not None:
                            V(lambda e: e.tensor_tensor(out=dn[:, 0:subs].unsqueeze(2), in0=pov[:, :, 64:65], in1=sinke[:, sink_h:sink_h + subs].unsqueeze(2), op=ALU.add),
                              r=[f"pO{ui}", "sinke"], w=[f"den2_{ui}"])
                            V(lambda e: e.reciprocal(out=dn[:, 0:subs], in_=dn[:, 0:subs]), r=[f"den2_{ui}"], w=[f"den2_{ui}"])
                        else:
                            V(lambda e: e.reciprocal(out=dn[:, 0:subs].unsqueeze(2), in_=pov[:, :, 64:65]), r=[f"pO{ui}"], w=[f"den2_{ui}"])
                        V(lambda e: e.tensor_tensor(out=ob[ui][:, 0:subs, :], in0=pov[:, :, 0:64], in1=dn[:, 0:subs].unsqueeze(2).to_broadcast([128, subs, 64]), op=ALU.mult),
                          r=[f"pO{ui}", f"den2_{ui}"], w=[f"ob{ui}"])
                        out_fn(ob[ui], f"ob{ui}")

                def attn_unit(qsrc_fn, dq, Wq, keys, kb, kk, vbuf, vk, scale, subs, sink_h, out_fn):
                    qi = cnt["q"] % 2; cnt["q"] += 1
                    u = cnt["u"]; cnt["u"] += 1
                    qsrc_fn(qt[qi], f"qt{qi}")
                    for n, (j, msk) in enumerate(keys):
                        si = cnt["s"] % 3; cnt["s"] += 1
                        pi = cnt["p"] % 3; cnt["p"] += 1
                        M(lambda e: e.matmul(pS[si][:, 0:Wq], lhsT=kb[0:dq, j * 128:(j + 1) * 128], rhs=qt[qi][0:dq, 0:Wq], start=True, stop=True),
                          r=[kk, f"qt{qi}"], w=[f"pS{si}"])
                        pending.append((si, pi, Wq, scale, msk, subs, u, n, len(keys), vbuf, vk, j, sink_h, out_fn))
                        if len(pending) > 1:
                            emit_tail(pending.pop(0))

                def load_kv(krows_ap, dq, voff):
                    i = cnt["kv"] % 2; cnt["kv"] += 1
                    L(lambda e: e.dma_start(out=kt[i][0:dq, :], in_=krows_ap), w=[f"kt{i}"])
                    for hh in range(2):
                        L(lambda e: e.dma_start(out=vb[i][:, hh * 17:(hh + 1) * 17, 0:64], in_=vallv[:, hh * 17:(hh + 1) * 17, voff:voff + 64]), w=[f"vb{i}"])
                    return kt[i], f"kt{i}", vb[i], f"vb{i}"

                qblocks = ([(0, NCT)] if not last else []) + [(NCT + 4 * b, 4) for b in range(SEQ // 512)]
                for mixer in ("A", "B"):
                    nkv = 2 if mixer == "A" else 6
                    for kv in range(nkv):
                        if mixer == "A":
                            kb, kk, vbuf, vk = load_kv(qkT_A[(6 + kv) * 64:(7 + kv) * 64, :], 64, kv * 64)
                            heads = [3 * kv + g for g in range(3)]; dq = 64; scale = 64 ** -0.5
                        else:
                            kb, kk, vbuf, vk = load_kv(qkT_B[6 + kv, :, :], 96, 128 + kv * 64)
                            heads = [kv]; dq = 96; scale = 96 ** -0.5
                        for h in heads:
                            for (tb, nsub) in qblocks:
                                keys = [(j, None) for j in (range(NCT) if tb < NCT else range(NT))]
                                Wq = nsub * 128
                                if mixer == "A":
                                    qsrc = lambda dst, dk, h=h, tb=tb, Wq=Wq: L(lambda e: e.dma_start(out=dst[0:64, 0:Wq], in_=qkT_A[h * 64:(h + 1) * 64, tb * 128:tb * 128 + Wq]), w=[dk])
                                    col = h * 64
                                else:
                                    qsrc = lambda dst, dk, h=h, tb=tb, Wq=Wq: L(lambda e: e.dma_start(out=dst[0:96, 0:Wq], in_=qkT_B[h, :, tb * 128:tb * 128 + Wq]), w=[dk])
                                    col = 384 + h * 64
                                outf = lambda o, ok, tb=tb, nsub=nsub, col=col: S(lambda e: e.dma_start(out=mixv[:, tb:tb + nsub, col:col + 64], in_=o[:, 0:nsub, :]), r=[ok])
                                attn_unit(qsrc, dq, Wq, keys, kb, kk, vbuf, vk, scale, nsub, None, outf)
                for kv in range(2):
                    kb, kk, vbuf, vk = load_kv(qkT_C[(4 + kv) * 64:(5 + kv) * 64, :], 64, 512 + kv * 64)
                    for ti in (range(NT) if not last else range(NCT, NT)):
                        if ti < NCT:
                            keys = [(j, None) for j in range(NCT)]
                        else:
                            keys = [(j, None) for j in range(NCT)]
                            if ti - 1 >= NCT:
                                keys.append((ti - 1, M1))
                            keys.append((ti, None))
                            if ti + 1 < NT:
                                keys.append((ti + 1, M2))

                        def qsrc(dst, dk, kv=kv, ti=ti):
                            L(lambda e: e.dma_start(out=dst[0:64, 0:256].rearrange("p (h t) -> p h t", h=2),
                                                    in_=qkT_C[2 * kv * 64:(2 * kv + 2) * 64, ti * 128:(ti + 1) * 128].rearrange("(h p) t -> p h t", p=64)), w=[dk])
                        col = 768 + 2 * kv * 64
                        outf = lambda o, ok, ti=ti, col=col: S(lambda e: e.dma_start(out=mix[ti * 128:(ti + 1) * 128, col:col + 128], in_=o[:, 0:2, :].rearrange("p s d -> p (s d)")), r=[ok])
                        attn_unit(qsrc, 64, 256, keys, kb, kk, vbuf, vk, 64 ** -0.5, 2, 2 * kv, outf)
                while pending:
                    emit_tail(pending.pop(0))
            P.barrier()
            if stop_after == ("C", l):
                break

            with ExitStack() as ph:
                wst = tsb(ph, "wstD", [128, 8, 512], F32)
                w_out_b = tsb(ph, "w_out_b", [128, 8, D], BF16)
                G1 = [tsb(ph, f"G1_{i}", [128, D], F32) for i in range(2)]
                xt = [tsb(ph, f"xtD{i}", [128, D], F32) for i in range(2)]
                mt = [tsb(ph, f"mt{i}", [128, D], BF16) for i in range(2)]
                mT = tsb(ph, "mT", [128, 8, 128], BF16)
                yt = [tsb(ph, f"yt{i}", [128, D], F32) for i in range(2)]
                pT = tps(ph, "pTD", [128, 1024], BF16)
                pY = [tps(ph, f"pY{i}", [128, 512]) for i in range(2)]
                for b in range(2):
                    for h in range(2):
                        L(lambda e: e.dma_start(out=wst[:, h * 4:(h + 1) * 4, :],
                                                in_=w_out[l, h * 512:(h + 1) * 512, b * 512:(b + 1) * 512].rearrange("(k p) n -> p k n", p=128)), w=["wstD"])
                    V(lambda e: e.tensor_copy(out=w_out_b[:, 0:4, b * 512:(b + 1) * 512], in_=wst[:, 0:4, :]), r=["wstD"], w=["w_out_b"])
                    G(lambda e: e.tensor_copy(out=w_out_b[:, 4:8, b * 512:(b + 1) * 512], in_=wst[:, 4:8, :]), r=["wstD"], w=["w_out_b"])
                for which in range(2):
                    load_bcast(G1[which][:, :], which, 2, f"G1_{which}")
                for ti in tiles_act:
                    which = 1 if ti < NCT else 0
                    i2 = ti % 2
                    L(lambda e: e.dma_start(out=xt[i2][:, :], in_=xall[ti * 128:(ti + 1) * 128, :]), w=[f"xtD{i2}"])
                    L(lambda e: e.dma_start(out=mt[i2][:, :], in_=mix[ti * 128:(ti + 1) * 128, :]), w=[f"mt{i2}"])
                    for kc in range(8):
                        M(lambda e: e.transpose(out=pT[:, kc * 128:(kc + 1) * 128], in_=mt[i2][:, kc * 128:(kc + 1) * 128], identity=ident[:, :]), r=[f"mt{i2}", "ident"], w=["pTD"])
                    A(lambda e: e.copy(out=mT[:, :, :].rearrange("p k t -> p (k t)"), in_=pT[:, :]), r=["pTD"], w=["mT"])
                    for b in range(2):
                        for kc in range(8):
                            M(lambda e: e.matmul(pY[b][:, :], lhsT=mT[:, kc, :], rhs=w_out_b[:, kc, b * 512:(b + 1) * 512], start=(kc == 0), stop=(kc == 7)), r=["mT", "w_out_b"], w=[f"pY{b}"])
                        V(lambda e: e.tensor_tensor(out=yt[i2][:, b * 512:(b + 1) * 512], in0=pY[b][:, :], in1=G1[which][:, b * 512:(b + 1) * 512], op=ALU.mult),
                          r=[f"pY{b}", f"G1_{which}"], w=[f"yt{i2}"])
                    V(lambda e: e.tensor_tensor(out=yt[i2][:, :], in0=yt[i2][:, :], in1=xt[i2][:, :], op=ALU.add), r=[f"yt{i2}", f"xtD{i2}"], w=[f"yt{i2}"])
                    S(lambda e: e.dma_start(out=xall[ti * 128:(ti + 1) * 128, :], in_=yt[i2][:, :]), r=[f"yt{i2}"])
            P.barrier()
            if stop_after == ("D", l):
                break

            with ExitStack() as ph:
                aff = tsb(ph, "aff", [128, NT, NE], F32)
                gm = tsb(ph, "gm", [128, NT, NE], F32)
                sloti = tsb(ph, "sloti", [128, NT, NE], I32)
                slotc = tsb(ph, "slotc", [128, NT, NE], I32)
                G2 = [tsb(ph, f"G2_{i}", [128, D], F32) for i in range(2)]
                p13 = ExitStack()
                affT = tsb(p13, "affT", [NE, T], F32)
                maskT = tsb(p13, "maskT", [NE, T], BF16)
                with ExitStack() as p1:
                    wr_f = tsb(p1, "wr_f", [128, 8, NE], F32)
                    wr_b = tsb(p1, "wr_b", [128, 8, NE], BF16)
                    Ar = [tsb(p1, f"Ar2_{i}", [128, D], F32) for i in range(2)]
                    Br = [tsb(p1, f"Br2_{i}", [128, D], F32) for i in range(2)]
                    xt = [tsb(p1, f"xtE{i}", [128, D], F32) for i in range(2)]
                    junk = tsb(p1, "junkE", [128, D], BF16)
                    st1 = tsb(p1, "st1E", [128, 8], F32)
                    hf = tsb(p1, "hfE", [128, D], F32)
                    hb = [tsb(p1, f"hbE{i}", [128, D], BF16) for i in range(2)]
                    hT = tsb(p1, "hTE", [128, 8, 128], BF16)
                    ex = tsb(p1, "ex", [128, NE], F32)
                    pT = tps(p1, "pTE", [128, 1024], BF16)
                    pR = tps(p1, "pR", [128, 512])
                    pAT = tps(p1, "pAT", [128, 512])
                    L(lambda e: e.dma_start(out=wr_f[:, :, :], in_=w_router[l, :, :].rearrange("(k p) n -> p k n", p=128)), w=["wr_f"])
                    V(lambda e: e.tensor_copy(out=wr_b[:, :, :], in_=wr_f[:, :, :]), r=["wr_f"], w=["wr_b"])
                    for which in range(2):
                        load_bcast(Ar[which][:, :], which, 4, f"Ar2_{which}")
                        load_bcast(Br[which][:, :], which, 3, f"Br2_{which}")
                        load_bcast(G2[which][:, :], which, 5, f"G2_{which}")
                    for ti in tiles_act:
                        which = 1 if ti < NCT else 0
                        i2 = ti % 2
                        L(lambda e: e.dma_start(out=xt[i2][:, :], in_=xall[ti * 128:(ti + 1) * 128, :]), w=[f"xtE{i2}"])
                        A(lambda e: e.activation(out=junk[:, :], in_=xt[i2][:, :], func=AF.Square, accum_out=st1[:, 0:1]), r=[f"xtE{i2}"], w=["junkE", "st1E"])
                        rstd_from_ss(st1[:, 0:1], st1[:, 1:2], 1.0 / D, ["st1E"], ["st1Eb"])
                        V(lambda e: e.scalar_tensor_tensor(out=hf[:, :], in0=xt[i2][:, :], scalar=st1[:, 1:2], in1=Ar[which][:, :], op0=ALU.mult, op1=ALU.mult),
                          r=[f"xtE{i2}", "st1Eb", f"Ar2_{which}"], w=["hfE"])
                        V(lambda e: e.tensor_tensor(out=hb[i2][:, :], in0=hf[:, :], in1=Br[which][:, :], op=ALU.add), r=["hfE", f"Br2_{which}"], w=[f"hbE{i2}"])
                        S(lambda e: e.dma_start(out=h2d[ti * 128:(ti + 1) * 128, :], in_=hb[i2][:, :]), r=[f"hbE{i2}"])
                        for kc in range(8):
                            M(lambda e: e.transpose(out=pT[:, kc * 128:(kc + 1) * 128], in_=hb[i2][:, kc * 128:(kc + 1) * 128], identity=ident[:, :]), r=[f"hbE{i2}", "ident"], w=["pTE"])
                        A(lambda e: e.copy(out=hT[:, :, :].rearrange("p k t -> p (k t)"), in_=pT[:, :]), r=["pTE"], w=["hTE"])
                        for kc in range(8):
                            M(lambda e: e.matmul(pR[:, 0:NE], lhsT=hT[:, kc, :], rhs=wr_b[:, kc, :], start=(kc == 0), stop=(kc == 7)), r=["hTE", "wr_b"], w=["pR"])
                        V(lambda e: e.tensor_reduce(out=st1[:, 2:3], in_=pR[:, 0:NE], axis=AX.X, op=ALU.max, negate=True), r=["pR"], w=["st1Ec"])
                        A(lambda e: e.activation(out=ex[:, :], in_=pR[:, 0:NE], func=AF.Exp, bias=st1[:, 2:3], accum_out=st1[:, 3:4]), r=["pR", "st1Ec"], w=["ex", "st1Ed"])
                        V(lambda e: e.reciprocal(out=st1[:, 4:5], in_=st1[:, 3:4]), r=["st1Ed"], w=["st1Ee"])
                        V(lambda e: e.tensor_scalar(out=aff[:, ti, :], in0=ex[:, :], scalar1=st1[:, 4:5], scalar2=None, op0=ALU.mult), r=["ex", "st1Ee"], w=["aff"])
                        M(lambda e: e.transpose(out=pAT[0:NE, 0:128], in_=aff[:, ti, :], identity=identf[:, :]), r=["aff", "identf"], w=["pAT"])
                        A(lambda e: e.copy(out=affT[:, ti * 128:(ti + 1) * 128], in_=pAT[0:NE, 0:128]), r=["pAT"], w=["affT"])
                P.barrier()
                with ExitStack() as p2:
                    work = tsb(p2, "work", [NE, CTX], F32)
                    mx = tsb(p2, "mx", [NE, 8], F32)
                    if not last:
                        V(lambda e: e.tensor_copy(out=work[:, :], in_=affT[:, 0:CTX]), r=["affT"], w=["work"])
                        for r_ in range(CAP_C // 8):
                            V(lambda e: e.max(out=mx[:, :], in_=work[:, :]), r=["work"], w=["mx"])
                            if r_ < CAP_C // 8 - 1:
                                V(lambda e: e.match_replace(out=work[:, :], in_to_replace=mx[:, :], in_values=work[:, :], imm_value=-1.0), r=["work", "mx"], w=["work"])
                        V(lambda e: e.tensor_scalar(out=maskT[:, 0:CTX], in0=affT[:, 0:CTX], scalar1=mx[:, 7:8], scalar2=None, op0=ALU.is_ge), r=["affT", "mx"], w=["maskT"])
                    A2 = tsb(p2, "A2", [128, 512], F32)
                    jk = tsb(p2, "jk2", [128, 512], F32)
                    mk2 = tsb(p2, "mk2", [128, 512], BF16)
                    Bsel = tsb(p2, "Bsel", [NE, 128], F32)
                    Gm = tsb(p2, "Gm", [128, 128], F32)
                    bs = tsb(p2, "bs", [128, 8], F32)
                    pG2 = tps(p2, "pGm", [128, 512])
                    pC2 = tps(p2, "pCnt", [128, 512])
                    S(lambda e: e.dma_start(out=aT_d, in_=affT[:, CTX:T]), r=["affT"], w=["aT_d"])
                    L(lambda e: e.dma_start(out=A2[:, :], in_=aT_d.rearrange("e (c t) -> (e c) t", c=8)), r=["aT_d"], w=["A2"])
                    G(lambda e: e.affine_select(out=Bsel[:, :], in_=onesf[0:NE, :], pattern=[[1, 128]], base=0, channel_multiplier=-8, compare_op=ALU.is_ge, fill=0.0), r=["onesf"], w=["Bsel"])
                    G(lambda e: e.affine_select(out=Bsel[:, :], in_=Bsel[:, :], pattern=[[-1, 128]], base=7, channel_multiplier=8, compare_op=ALU.is_ge, fill=0.0), r=["Bsel"], w=["Bsel"])
                    M(lambda e: e.matmul(pG2[:, 0:128], lhsT=Bsel[:, :], rhs=Bsel[:, :], start=True, stop=True), r=["Bsel"], w=["pGm"])
                    V(lambda e: e.tensor_copy(out=Gm[:, :], in_=pG2[:, 0:128]), r=["pGm"], w=["Gm"])
                    V(lambda e: e.memset(bs[:, 0:1], 0.0), w=["bs"])
                    V(lambda e: e.memset(bs[:, 1:2], 1.0), w=["bs"])
                    for it in range(32):
                        V(lambda e: e.tensor_scalar(out=bs[:, 2:3], in0=bs[:, 0:1], scalar1=bs[:, 1:2], scalar2=0.5, op0=ALU.add, op1=ALU.mult), r=["bs"], w=["bs"])
                        V(lambda e: e.tensor_scalar(out=jk[:, :], in0=A2[:, :], scalar1=bs[:, 2:3], scalar2=0.0, op0=ALU.is_ge, op1=ALU.add, accum_out=bs[:, 3:4]), r=["A2", "bs"], w=["jk2", "bs"])
                        M(lambda e: e.matmul(pC2[:, 0:1], lhsT=Gm[:, :], rhs=bs[:, 3:4], start=True, stop=True), r=["Gm", "bs"], w=["pCnt"])
                        V(lambda e: e.tensor_scalar(out=bs[:, 4:5], in0=pC2[:, 0:1], scalar1=float(CAP_L) - 0.5, scalar2=None, op0=ALU.is_ge), r=["pCnt"], w=["bs"])
                        V(lambda e: e.tensor_tensor(out=bs[:, 5:6], in0=bs[:, 4:5], in1=bs[:, 2:3], op=ALU.mult), r=["bs"], w=["bs"])
                        V(lambda e: e.tensor_tensor(out=bs[:, 0:1], in0=bs[:, 0:1], in1=bs[:, 5:6], op=ALU.max), r=["bs"], w=["bs"])
                        V(lambda e: e.scalar_tensor_tensor(out=bs[:, 6:7], in0=bs[:, 4:5], scalar=2.0, in1=bs[:, 2:3], op0=ALU.mult, op1=ALU.add), r=["bs"], w=["bs"])
                        V(lambda e: e.tensor_tensor(out=bs[:, 1:2], in0=bs[:, 1:2], in1=bs[:, 6:7], op=ALU.min), r=["bs"], w=["bs"])
                    V(lambda e: e.tensor_scalar(out=mk2[:, :], in0=A2[:, :], scalar1=bs[:, 0:1], scalar2=None, op0=ALU.is_ge), r=["A2", "bs"], w=["mk2"])
                    S(lambda e: e.dma_start(out=mk_d.rearrange("e (c t) -> (e c) t", c=8), in_=mk2[:, :]), r=["mk2"], w=["mk_d"])
                    L(lambda e: e.dma_start(out=maskT[:, CTX:T], in_=mk_d), r=["mk_d"], w=["maskT"])
                P.barrier()
                with ExitStack() as p3:
                    mk = tsb(p3, "mk", [128, NE], F32)
                    mkb = tsb(p3, "mkb", [128, NE], BF16)
                    Sf = tsb(p3, "Sf", [128, NE], F32)
                    Sb = tsb(p3, "Sb", [128, NE], BF16)
                    sl = tsb(p3, "sl", [128, NE], F32)
                    hb = [tsb(p3, f"hbS{i}", [128, XW], BF16) for i in range(2)]
                    tok_i = tsb(p3, "tok_i", [128, 2, NT], I32)
                    tokb = tsb(p3, "tokb", [128, NT, 2], BF16)
                    ghi = tsb(p3, "ghi", [128, NE], BF16)
                    zt = tsb(p3, "zt", [128, 2 * D], F32)
                    for hh in range(2):
                        G(lambda e: e.iota(tok_i[hh * 64:(hh + 1) * 64, 0, :], pattern=[[2, NT]], base=hh, channel_multiplier=0), w=["tok_i"])
                        G(lambda e: e.iota(tok_i[hh * 64:(hh + 1) * 64, 1, :], pattern=[[0, NT]], base=0, channel_multiplier=1), w=["tok_i"])
                    V(lambda e: e.tensor_copy(out=tokb[:, :, :].rearrange("p t j -> p j t"), in_=tok_i[:, :, :]), r=["tok_i"], w=["tokb"])
                    V(lambda e: e.memset(zt[:, :], 0.0), w=["zt"])
                    for i in range(2):
                        V(lambda e: e.memset(hb[i][:, D:XW], 0.0), w=[f"hbS{i}"])
                    for ti in range(0, NT, 2):
                        L(lambda e: e.dma_start(out=accd[ti * 128:(ti + 2) * 128, :].rearrange("(j p) c -> p j c", p=128), in_=zt[:, :].rearrange("p (j c) -> p j c", j=2)), r=["zt"])
                    pM = tps(p3, "pM", [128, 1024], BF16)
                    pPos = tps(p3, "pPos", [128, 512])
                    for ti in tiles_act:
                        isc = ti < NCT
                        i2 = ti % 2
                        if ti == 0 or ti == NCT:
                            V(lambda e: e.memset(Sf[:, :], 0.0), w=["Sf"])
                            V(lambda e: e.tensor_copy(out=Sb[:, :], in_=Sf[:, :]), r=["Sf"], w=["Sb"])
                        L(lambda e: e.dma_start(out=hb[i2][:, 0:D], in_=h2d[ti * 128:(ti + 1) * 128, :]), w=[f"hbS{i2}"])
                        M(lambda e: e.transpose(out=pM[:, 0:NE], in_=maskT[:, ti * 128:(ti + 1) * 128], identity=ident[0:NE, 0:NE]), r=["maskT", "ident"], w=["pM"])
                        V(lambda e: e.tensor_copy(out=mk[:, :], in_=pM[:, 0:NE]), r=["pM"], w=["mk"])
                        A(lambda e: e.copy(out=mkb[:, :], in_=pM[:, 0:NE]), r=["pM"], w=["mkb"])
                        M(lambda e: e.matmul(pPos[:, 0:NE], lhsT=Ub[:, :], rhs=mkb[:, :], start=True, stop=False), r=["Ub", "mkb"], w=["pPos"])
                        M(lambda e: e.matmul(pPos[:, 0:NE], lhsT=ones_b[:, :], rhs=Sb[:, :], start=False, stop=True), r=["ones_b", "Sb"], w=["pPos"])
                        V(lambda e: e.tensor_scalar(out=sl[:, :], in0=mk[:, :], scalar1=-BIG, scalar2=BIG, op0=ALU.mult, op1=ALU.add), r=["mk"], w=["sl"])
                        V(lambda e: e.tensor_tensor(out=sl[:, :], in0=pPos[:, 0:NE], in1=sl[:, :], op=ALU.add), r=["pPos", "sl"], w=["sl"])
                        V(lambda e: e.tensor_copy(out=sloti[:, ti, :], in_=sl[:, :]), r=["sl"], w=["sloti"])
                        V(lambda e: e.tensor_scalar(out=sl[:, :], in0=sl[:, :], scalar1=float((CAP_C if isc else CAP_L) - 1), scalar2=None, op0=ALU.min), r=["sl"], w=["sl"])
                        V(lambda e: e.tensor_copy(out=slotc[:, ti, :], in_=sl[:, :]), r=["sl"], w=["slotc"])
                        V(lambda e: e.tensor_tensor(out=gm[:, ti, :], in0=aff[:, ti, :], in1=mk[:, :], op=ALU.mult), r=["aff", "mk"], w=["gm"])
                        V(lambda e: e.tensor_tensor(out=Sf[:, :], in0=Sf[:, :], in1=mk[:, :], op=ALU.add), r=["Sf", "mk"], w=["Sf"])
                        V(lambda e: e.tensor_copy(out=Sb[:, :], in_=Sf[:, :]), r=["Sf"], w=["Sb"])
                        V(lambda e: e.tensor_copy(out=hb[i2][:, D:D + 2], in_=tokb[:, ti, :]), r=["tokb"], w=[f"hbS{i2}"])
                        V(lambda e: e.tensor_copy(out=ghi[:, :], in_=gm[:, ti, :]), r=["gm"], w=["ghi"])
                        V(lambda e: e.tensor_copy(out=hb[i2][:, D + 2:D + 18], in_=ghi[:, :]), r=["ghi"], w=[f"hbS{i2}"])
                        V(lambda e: e.tensor_tensor(out=hb[i2][:, D + 18:D + 34], in0=gm[:, ti, :], in1=ghi[:, :], op=ALU.subtract), r=["gm", "ghi"], w=[f"hbS{i2}"])
                        for ex_ in range(NE):
                            dst = xe_c[ex_] if isc else xe_l[ex_]
                            bnd = (CAP_C if isc else CAP_L) - 1
                            S(lambda e: e.indirect_dma_start(out=dst, out_offset=bass.IndirectOffsetOnAxis(ap=sloti[:, ti, ex_:ex_ + 1], axis=0),
                                                             in_=hb[i2][:, :], in_offset=None, bounds_check=REG[bnd], oob_is_err=False),
                              r=[f"hbS{i2}", "sloti"])
                    if "dbg_aff" in dbg:
                        S(lambda e: e.dma_start(out=dbg_aff, in_=aff[:, :, :].rearrange("p t e -> p (t e)")), r=["aff"])
                        S(lambda e: e.dma_start(out=dbg_slot, in_=sloti[:, :, :].rearrange("p t e -> p (t e)")), r=["sloti"])
                P.barrier()
                p13.close()
                if stop_after == ("E3", l):
                    break
                with ExitStack() as p4:
                    NJ = CAP_L + (CAP_C if not last else 0)
                    stg = [tsb(p4, f"stg{i}", [128, 8, D], F32) for i in range(2)]
                    wb = [tsb(p4, f"wb{i}", [128, 8, D], BF16) for i in range(4)]
                    xet = [tsb(p4, f"xet{i}", [128, XW], BF16) for i in range(2)]
                    sideb = [tsb(p4, f"sideb{i}", [128, 5, 36], BF16) for i in range(2)]
                    gatef = [tsb(p4, f"gatef{i}", [128, 5], F32) for i in range(2)]
                    idxf = [tsb(p4, f"idxf{i}", [128, 5], F32) for i in range(2)]
                    idxi = [tsb(p4, f"idxi{i}", [128, 5], I32) for i in range(2)]
                    xeT = [tsb(p4, f"xeT{i}", [128, 8, CAP_L + CAP_C], BF16) for i in range(2)]
                    hidT = tsb(p4, "hidT", [128, 8, CAP_L + CAP_C], BF16)
                    sg = [tsb(p4, f"sg{i}", [128, CAP_L + CAP_C], F32) for i in range(2)]
                    yeb = [tsb(p4, f"yeb{i}", [128, D], BF16) for i in range(2)]
                    pT = tps(p4, "pT4", [128, 1024], BF16)
                    pG = tps(p4, "pG", [128, 512]); pU = tps(p4, "pU", [128, 512])
                    pGc = tps(p4, "pGc", [128, 512])
                    pUc = tps(p4, "pUc", [128, 512])
                    pY = [tps(p4, f"pY4_{i}", [128, 512]) for i in range(2)]
                    cnt4 = {"w": 0, "s": 0, "x": 0, "g": 0, "y": 0}

                    def load_w(src):
                        si = cnt4["s"] % 2; cnt4["s"] += 1
                        wi = cnt4["w"] % 4; cnt4["w"] += 1
                        for h in range(4):
                            L(lambda e: e.dma_start(out=stg[si][:, 2 * h:2 * h + 2, :], in_=src[h * 256:(h + 1) * 256, :].rearrange("(k p) n -> p k n", p=128)), w=[f"stg{si}h{h}"])
                        V(lambda e: e.tensor_copy(out=wb[wi][:, 0:2, :], in_=stg[si][:, 0:2, :]), r=[f"stg{si}h0"], w=[f"wb{wi}a"])
                        A(lambda e: e.copy(out=wb[wi][:, 2:4, :], in_=stg[si][:, 2:4, :]), r=[f"stg{si}h1"], w=[f"wb{wi}b"])
                        V(lambda e: e.tensor_copy(out=wb[wi][:, 4:5, :], in_=stg[si][:, 4:5, :]), r=[f"stg{si}h2"], w=[f"wb{wi}c"])
                        A(lambda e: e.copy(out=wb[wi][:, 5:6, :], in_=stg[si][:, 5:6, :]), r=[f"stg{si}h2"], w=[f"wb{wi}d"])
                        G(lambda e: e.tensor_copy(out=wb[wi][:, 6:8, :], in_=stg[si][:, 6:8, :]), r=[f"stg{si}h3"], w=[f"wb{wi}e"])
                        return wb[wi], [f"wb{wi}{c}" for c in "abcde"]

                    def stage_x(ex_):
                        xT = xeT[ex_ % 2]; xk = f"xeT{ex_ % 2}"
                        jts = [(xe_l[ex_][j * 128:(j + 1) * 128, :], 128, j * 128) for j in range(4)]
                        if not last:
                            jts.append((xe_c[ex_][:, :], CAP_C, CAP_L))
                        for jt, (src, nj, c0) in enumerate(jts):
                            xi = cnt4["x"] % 2; cnt4["x"] += 1
                            L(lambda e: e.dma_start(out=xet[xi][0:nj, :], in_=src), w=[f"xet{xi}"])
                            e2 = ex_ % 2
                            V(lambda e: e.tensor_copy(out=sideb[e2][0:nj, jt, :], in_=xet[xi][0:nj, D:XW]), r=[f"xet{xi}"], w=[f"sideb{e2}"])
                            V(lambda e: e.tensor_tensor(out=gatef[e2][0:nj, jt:jt + 1], in0=sideb[e2][0:nj, jt, 2 + ex_:3 + ex_], in1=sideb[e2][0:nj, jt, 18 + ex_:19 + ex_], op=ALU.add),
                              r=[f"sideb{e2}"], w=[f"gatef{e2}"])
                            V(lambda e: e.scalar_tensor_tensor(out=idxf[e2][0:nj, jt:jt + 1], in0=sideb[e2][0:nj, jt, 0:1], scalar=64.0, in1=sideb[e2][0:nj, jt, 1:2], op0=ALU.mult, op1=ALU.add),
                              r=[f"sideb{e2}"], w=[f"idxf{e2}"])
                            V(lambda e: e.tensor_copy(out=idxi[e2][0:nj, jt:jt + 1], in_=idxf[e2][0:nj, jt:jt + 1]), r=[f"idxf{e2}"], w=[f"idxi{e2}"])
                            for kc in range(8):
                                M(lambda e: e.transpose(out=pT[:, kc * 128:kc * 128 + nj], in_=xet[xi][0:nj, kc * 128:(kc + 1) * 128], identity=ident[0:nj, 0:nj]),
                                  r=[f"xet{xi}", "ident"], w=["pT4"])
                            V(lambda e: e.tensor_copy(out=xT[:, :, c0:c0 + nj], in_=pT[:, :].rearrange("p (k t) -> p k t", k=8)[:, :, 0:nj]), r=["pT4"], w=[xk])

                    def stage_hidden(ex_, wg, wgk, wu, wuk):
                        xT = xeT[ex_ % 2]; xk = f"xeT{ex_ % 2}"
                        for fc in range(8):
                            for kc in range(8):
                                M(lambda e: e.matmul(pG[:, :], lhsT=wg[:, kc, fc * 128:(fc + 1) * 128], rhs=xT[:, kc, 0:CAP_L], start=(kc == 0), stop=(kc == 7)), r=wgk + [xk], w=["pG"])
                            for kc in range(8):
                                M(lambda e: e.matmul(pU[:, :], lhsT=wu[:, kc, fc * 128:(fc + 1) * 128], rhs=xT[:, kc, 0:CAP_L], start=(kc == 0), stop=(kc == 7)), r=wuk + [xk], w=["pU"])
                            gi = cnt4["g"] % 2; cnt4["g"] += 1
                            A(lambda e: e.activation(out=sg[gi][:, 0:CAP_L], in_=pG[:, :], func=AF.Silu), r=["pG"], w=[f"sg{gi}"])
                            V(lambda e: e.tensor_tensor(out=hidT[:, fc, 0:CAP_L], in0=pU[:, :], in1=sg[gi][:, 0:CAP_L], op=ALU.mult), r=["pU", f"sg{gi}"], w=["hidT"])
                            if not last:
                                for kc in range(8):
                                    M(lambda e: e.matmul(pGc[:, 0:CAP_C], lhsT=wg[:, kc, fc * 128:(fc + 1) * 128], rhs=xT[:, kc, CAP_L:NJ], start=(kc == 0), stop=(kc == 7)), r=wgk + [xk], w=["pGc0"])
                                for kc in range(8):
                                    M(lambda e: e.matmul(pUc[:, 0:CAP_C], lhsT=wu[:, kc, fc * 128:(fc + 1) * 128], rhs=xT[:, kc, CAP_L:NJ], start=(kc == 0), stop=(kc == 7)), r=wuk + [xk], w=["pGc1"])
                                A(lambda e: e.activation(out=sg[gi][:, CAP_L:NJ], in_=pGc[:, 0:CAP_C], func=AF.Silu), r=["pGc0"], w=[f"sg{gi}c"])
                                V(lambda e: e.tensor_tensor(out=hidT[:, fc, CAP_L:NJ], in0=pUc[:, 0:CAP_C], in1=sg[gi][:, CAP_L:NJ], op=ALU.mult), r=["pGc1", f"sg{gi}c"], w=["hidTc"])

                    def stage_down(ex_, wd, wdk):
                        outs = [(None, 128, j * 128) for j in range(4)]
                        if not last:
                            outs.append((None, CAP_C, CAP_L))
                        e2 = ex_ % 2
                        for jt, (dst, nj, c0) in enumerate(outs):
                            gate_ap = gatef[e2][0:nj, jt:jt + 1]
                            idx_ap = idxi[e2][0:nj, jt:jt + 1]
                            yi = cnt4["y"] % 2; cnt4["y"] += 1
                            for dh in range(2):
                                for fc in range(8):
                                    M(lambda e: e.matmul(pY[dh][0:nj, :], lhsT=hidT[:, fc, c0:c0 + nj], rhs=wd[:, fc, dh * 512:(dh + 1) * 512], start=(fc == 0), stop=(fc == 7)),
                                      r=["hidT", "hidTc"] + wdk, w=[f"pY4_{dh}"])
                            A(lambda e: e.activation(out=yeb[yi][0:nj, 0:512], in_=pY[0][0:nj, :], func=AF.Identity, scale=gate_ap), r=["pY4_0", f"gatef{e2}"], w=[f"yeb{yi}a"])
                            V(lambda e: e.tensor_scalar(out=yeb[yi][0:nj, 512:1024], in0=pY[1][0:nj, :], scalar1=gate_ap, scalar2=None, op0=ALU.mult), r=["pY4_1", f"gatef{e2}"], w=[f"yeb{yi}b"])
                            S(lambda e: e.indirect_dma_start(out=accd[:, :], out_offset=bass.IndirectOffsetOnAxis(ap=idx_ap, axis=0),
                                                             in_=yeb[yi][0:nj, :], in_offset=None, compute_op=ALU.add),
                              r=[f"yeb{yi}a", f"yeb{yi}b", f"idxi{e2}"], w=["accd"])

                    wg, wgk = load_w(w_e_gate[l, 0])
                    wu, wuk = load_w(w_e_up[l, 0])
                    stage_x(0)
                    for ex_ in range(NE):
                        wd, wdk = load_w(w_e_down[l, ex_])
                        stage_hidden(ex_, wg, wgk, wu, wuk)
                        if ex_ + 1 < NE:
                            wg, wgk = load_w(w_e_gate[l, ex_ + 1])
                            wu, wuk = load_w(w_e_up[l, ex_ + 1])
                            stage_x(ex_ + 1)
                        stage_down(ex_, wd, wdk)
                P.barrier()
                if stop_after == ("E4", l):
                    break
                with ExitStack() as p5:
                    xt = [tsb(p5, f"xt5_{i}", [128, D], F32) for i in range(3)]
                    acc = [tsb(p5, f"acc{i}", [128, D], F32) for i in range(3)]
                    for n_, ti in enumerate(tiles_act):
                        which = 1 if ti < NCT else 0
                        i2 = n_ % 3
                        L(lambda e: e.dma_start(out=xt[i2][:, :], in_=xall[ti * 128:(ti + 1) * 128, :]), w=[f"xt5_{i2}"])
                        L(lambda e: e.dma_start(out=acc[i2][:, :], in_=accd[ti * 128:(ti + 1) * 128, :]), w=[f"acc{i2}"])
                        V(lambda e: e.tensor_tensor(out=acc[i2][:, :], in0=acc[i2][:, :], in1=G2[which][:, :], op=ALU.mult), r=[f"acc{i2}", f"G2_{which}"], w=[f"acc{i2}"])
                        V(lambda e: e.tensor_tensor(out=acc[i2][:, :], in0=acc[i2][:, :], in1=xt[i2][:, :], op=ALU.add), r=[f"acc{i2}", f"xt5_{i2}"], w=[f"acc{i2}"])
                        if last:
                            S(lambda e: e.dma_start(out=y_out[(ti - NCT) * 128:(ti - NCT + 1) * 128, :], in_=acc[i2][:, :]), r=[f"acc{i2}"])
                        else:
                            S(lambda e: e.dma_start(out=xall[ti * 128:(ti + 1) * 128, :], in_=acc[i2][:, :]), r=[f"acc{i2}"])
            P.barrier()
            if stop_after == ("L", l):
                break
        P.barrier()
        print("instr counts", P.ninstr, "total", P.total, flush=True)
    return nc


def _rope_table():
    t = np.arange(SEQ)
    row = (t // 64).astype(np.float64)
    col = (t % 64).astype(np.float64)
    out = np.zeros((SEQ, 192), np.float64)

    def fill(off, dim):
        ad = dim // 2
        inv = 10000.0 ** (-np.arange(0, ad, 2, dtype=np.float64) / ad)
        ar = row[:, None] * inv[None, :]
        ac = col[:, None] * inv[None, :]
        C = np.concatenate([np.cos(ar), np.cos(ar), np.cos(ac), np.cos(ac)], axis=1)
        S_ = np.concatenate([-np.sin(ar), np.sin(ar), -np.sin(ac), np.sin(ac)], axis=1)
        out[:, off:off + dim] = C
        out[:, off + dim:off + 2 * dim] = S_
    fill(0, 64)
    fill(128, 32)
    return out.astype(np.float32)


_NC_CACHE = {}


def kernel(**inputs):
    key = "full"
    if key not in _NC_CACHE:
        _NC_CACHE[key] = build()
    nc = _NC_CACHE[key]
    rope = _rope_table()
    shared = {k: np.ascontiguousarray(np.asarray(v, dtype=np.float32)) for k, v in inputs.items() if k not in ("x", "c", "ctx")}
    x = np.asarray(inputs["x"], dtype=np.float32); c = np.asarray(inputs["c"], dtype=np.float32); ctx = np.asarray(inputs["ctx"], dtype=np.float32)
    in_maps = []
    for b in range(8):
        m = dict(shared)
        m["x"] = np.ascontiguousarray(x[b]); m["ctx"] = np.ascontiguousarray(ctx[b]); m["c"] = np.ascontiguousarray(c[b])
        m["rope"] = rope
        in_maps.append(m)
    res = run_bass_kernel_spmd(nc, in_maps, core_ids=list(range(8)))
    return np.stack([np.asarray(r["y"], dtype=np.float32) for r in res.results], axis=0)
```

```python
import numpy as np
from contextlib import ExitStack
import threading
import concourse.bass as bass
import concourse.mybir as mybir
from concourse.bass_utils import run_bass_kernel_spmd

F32 = mybir.dt.float32
BF16 = mybir.dt.bfloat16
I32 = mybir.dt.int32
AF = mybir.ActivationFunctionType
ALU = mybir.AluOpType
AX = mybir.AxisListType

D = 1024
SEQ = 4096
CTX = 256
T = SEQ + CTX
NT = T // 128
NCT = CTX // 128
DEPTH = 2
INW = 1504
NE = 16
CAP_L = 512
CAP_C = 32
XW = D + 36
EPS = 1e-6
BIG = 8192.0


def _is_psum(k):
    return k.startswith("pmod") or (len(k) > 1 and k[0] == "p" and k[1].isupper())


class Prog:
    ENG = ("pe", "dve", "act", "pool", "sp")

    def __init__(self, nc, stack, same_engine_sync=True, ring=(("sp", 16), ("pool", 16))):
        self.nc = nc
        self.stack = stack
        self.e = {"pe": nc.tensor, "dve": nc.vector, "act": nc.scalar, "pool": nc.gpsimd, "sp": nc.sync}
        self.same = same_engine_sync
        self.sems = {}
        self.semval = {}
        self.cur = {}
        self.epoch = {n: 0 for n in self.ENG}
        for n in self.ENG:
            self.cur[n] = self._mk("E_" + n + "_0")
        self.ring = {}
        self.ringpos = {}
        for q, k in ring:
            self.ring[q] = [self._mk(f"D_{q}{i}") for i in range(k)]
            self.ringpos[q] = 0
        self.waited = {n: {} for n in self.ENG}
        self.lastw = {}
        self.readers = {}
        self.ninstr = {n: 0 for n in self.ENG}

    def _mk(self, name):
        s = self.stack.enter_context(self.nc.semaphore(name))
        self.sems[name] = s
        self.semval[name] = 0
        return name

    def _wait(self, eng, ev):
        sem, val = ev
        if val <= 0 or self.waited[eng].get(sem, 0) >= val:
            return
        if sem == self.cur[eng] and (eng == "pe" or not self.same):
            return
        self.e[eng].wait_ge(self.sems[sem], val)
        self.waited[eng][sem] = val

    def _deps(self, eng, r, w):
        for res in list(r) + list(w):
            ev = self.lastw.get(res)
            if ev is not None:
                self._wait(eng, ev)
        for res in w:
            for sem, val in self.readers.get(res, {}).items():
                self._wait(eng, (sem, val))

    def _record(self, ev, r, w):
        for res in w:
            self.lastw[res] = ev
            self.readers[res] = {}
        for res in r:
            d = self.readers.setdefault(res, {})
            if d.get(ev[0], 0) < ev[1]:
                d[ev[0]] = ev[1]

    LIMIT = None
    total = 0

    def op(self, eng, fn, r=(), w=()):
        self.total += 1
        if self.LIMIT is not None and self.total > self.LIMIT:
            return
        w = list(w) + [k for k in r if _is_psum(k)]
        r = [k for k in r if not _is_psum(k)]
        self._deps(eng, r, w)
        ins = fn(self.e[eng])
        sem = self.cur[eng]
        self.semval[sem] += 1
        ins.then_inc(self.sems[sem], 1)
        self.ninstr[eng] += 1
        self._record((sem, self.semval[sem]), r, w)

    def dma(self, q, fn, r=(), w=()):
        self.total += 1
        if self.LIMIT is not None and self.total > self.LIMIT:
            return
        self._deps(q, r, w)
        k = self.ringpos[q]
        sem = self.ring[q][k]
        self.ringpos[q] = (k + 1) % len(self.ring[q])
        self._wait(q, (sem, self.semval[sem]))
        ins = fn(self.e[q])
        self.semval[sem] += 16
        ins.then_inc(self.sems[sem], 16)
        self.ninstr[q] += 1
        self._record((sem, self.semval[sem]), r, w)

    def barrier(self):
        for eng in self.ENG:
            for sem, val in self.semval.items():
                self._wait(eng, (sem, val))
        self.lastw.clear()
        self.readers.clear()
        for eng in self.ENG:
            if self.semval[self.cur[eng]] > 15000:
                self.epoch[eng] += 1
                self.cur[eng] = self._mk(f"E_{eng}_{self.epoch[eng]}")


def build(stop_after=None, dbg=(), limit=None, light=False):
    nc = bass.Bass("TRN2", target_bir_lowering=False)
    Prog.LIMIT = limit

    def din(name, shape, dt=F32):
        return nc.dram_tensor(name, list(shape), dt, kind="ExternalInput").ap()

    x_in = din("x", [SEQ, D]); ctx_in = din("ctx", [CTX, D]); c_in = din("c", [D]); cctx_in = din("c_ctx", [D])
    w_ada = din("w_ada", [DEPTH, D, 6 * D]); b_ada = din("b_ada", [DEPTH, 6 * D])
    norm1_g = din("norm1_g", [DEPTH, D]); norm2_g = din("norm2_g", [DEPTH, D])
    w_in = din("w_in", [DEPTH, D, INW])
    a_q_norm = din("a_q_norm", [DEPTH, 64]); a_k_norm = din("a_k_norm", [DEPTH, 64])
    b_cq_norm = din("b_cq_norm", [DEPTH, 192]); b_ckv_norm = din("b_ckv_norm", [DEPTH, 128])
    w_uq = din("w_uq", [DEPTH, 192, 576]); w_ukv = din("w_ukv", [DEPTH, 128, 768])
    b_qn_norm = din("b_qn_norm", [DEPTH, 64]); b_kn_norm = din("b_kn_norm", [DEPTH, 64])
    b_qr_norm = din("b_qr_norm", [DEPTH, 32]); b_kr_norm = din("b_kr_norm", [DEPTH, 32])
    c_q_norm = din("c_q_norm", [DEPTH, 64]); c_k_norm = din("c_k_norm", [DEPTH, 64])
    c_sink = din("c_sink", [DEPTH, 4])
    w_out = din("w_out", [DEPTH, D, D]); w_router = din("w_router", [DEPTH, D, NE])
    if not light:
        w_e_gate = din("w_e_gate", [DEPTH, NE, D, D]); w_e_up = din("w_e_up", [DEPTH, NE, D, D])
        w_e_down = din("w_e_down", [DEPTH, NE, D, D])
    rope = din("rope", [SEQ, 192])
    y_out = nc.dram_tensor("y", [SEQ, D], F32, kind="ExternalOutput").ap()

    def dscr(name, shape, dt):
        kind = "ExternalOutput" if name in dbg else "Internal"
        return nc.dram_tensor(name, list(shape), dt, kind=kind).ap()

    xall = dscr("xall", [T, D], F32)
    modv = dscr("modv", [2, 6 * D], F32)
    qkT_A = dscr("qkT_A", [512, T], BF16)
    qkT_B = dscr("qkT_B", [12, 96, T], BF16)
    qkT_C = dscr("qkT_C", [384, T], BF16)
    vall = dscr("vall", [T, 640], BF16)
    mix = dscr("mix", [T, D], BF16)
    h2d = dscr("h2d", [T, D], BF16)
    xe_l = [dscr(f"xe_l{i}", [CAP_L, XW], BF16) for i in range(NE)]
    xe_c = [dscr(f"xe_c{i}", [CAP_C, XW], BF16) for i in range(NE)]
    accd = dscr("accd", [T, D], F32)
    aT_d = dscr("aT_d", [NE, SEQ], F32)
    mk_d = dscr("mk_d", [NE, SEQ], BF16)
    ye_l = [dscr(f"ye_l{i}", [CAP_L, D], BF16) for i in range(NE)]
    ye_c = [dscr(f"ye_c{i}", [CAP_C, D], BF16) for i in range(NE)]
    dbg_aff = dscr("dbg_aff", [128, NT * NE], F32)
    dbg_slot = dscr("dbg_slot", [128, NT * NE], I32)

    with ExitStack() as top:
        P = Prog(nc, top)
        V = lambda fn, r=(), w=(): P.op("dve", fn, r, w)
        A = lambda fn, r=(), w=(): P.op("act", fn, r, w)
        G = lambda fn, r=(), w=(): P.op("pool", fn, r, w)
        M = lambda fn, r=(), w=(): P.op("pe", fn, r, w)
        L = lambda fn, r=(), w=(): P.dma("sp", fn, r, w)
        S = lambda fn, r=(), w=(): P.dma("pool", fn, r, w)

        LAYER = [-1]

        def tsb(stk, name, shape, dt):
            return stk.enter_context(nc.sbuf_tensor(f"{name}_L{LAYER[0]}", list(shape), dt))

        def tps(stk, name, shape, dt=F32):
            return stk.enter_context(nc.psum_tensor(f"{name}_L{LAYER[0]}", list(shape), dt))

        REG = {CAP_L - 1: nc.gpsimd.to_reg(CAP_L - 1), CAP_C - 1: nc.gpsimd.to_reg(CAP_C - 1)}
        identf = tsb(top, "identf", [128, 128], F32)
        ident = tsb(top, "ident", [128, 128], BF16)
        onesf = tsb(top, "onesf", [128, 128], F32)
        ones_b = tsb(top, "ones_b", [128, 128], BF16)
        Ub = tsb(top, "Ub", [128, 128], BF16)
        M1 = tsb(top, "M1", [128, 128], BF16)
        M2 = tsb(top, "M2", [128, 128], BF16)
        ctmp = tsb(top, "ctmp", [128, 128], F32)
        cs = tsb(top, "cs", [128, 8, 2], F32)
        craw = tsb(top, "craw", [128, 2, 8], F32)

        G(lambda e: e.memset(identf[:], 0.0), w=["identf"])
        G(lambda e: e.affine_select(out=identf[:], in_=identf[:], pattern=[[-1, 128]], base=0, channel_multiplier=1,
                                    compare_op=ALU.not_equal, fill=1.0), r=["identf"], w=["identf"])
        V(lambda e: e.tensor_copy(out=ident[:], in_=identf[:]), r=["identf"], w=["ident"])
        G(lambda e: e.memset(onesf[:], 1.0), w=["onesf"])
        V(lambda e: e.tensor_copy(out=ones_b[:], in_=onesf[:]), r=["onesf"], w=["ones_b"])
        G(lambda e: e.affine_select(out=ctmp[:], in_=onesf[:], pattern=[[-1, 128]], base=0, channel_multiplier=1,
                                    compare_op=ALU.is_ge, fill=0.0), r=["onesf"], w=["ctmp"])
        V(lambda e: e.tensor_copy(out=M1[:], in_=ctmp[:]), r=["ctmp"], w=["M1"])
        G(lambda e: e.affine_select(out=ctmp[:], in_=onesf[:], pattern=[[1, 128]], base=0, channel_multiplier=-1,
                                    compare_op=ALU.is_ge, fill=0.0), r=["onesf"], w=["ctmp"])
        V(lambda e: e.tensor_copy(out=M2[:], in_=ctmp[:]), r=["ctmp"], w=["M2"])
        G(lambda e: e.affine_select(out=ctmp[:], in_=onesf[:], pattern=[[1, 128]], base=-1, channel_multiplier=-1,
                                    compare_op=ALU.is_ge, fill=0.0), r=["onesf"], w=["ctmp"])
        V(lambda e: e.tensor_copy(out=Ub[:], in_=ctmp[:]), r=["ctmp"], w=["Ub"])

        L(lambda e: e.dma_start(out=craw[:, 0, :], in_=c_in.rearrange("(k p) -> p k", p=128), allow_slow_non_contiguous=True), w=["craw"])
        L(lambda e: e.dma_start(out=craw[:, 1, :], in_=cctx_in.rearrange("(k p) -> p k", p=128), allow_slow_non_contiguous=True), w=["craw"])
        A(lambda e: e.activation(out=cs[:, :, :].rearrange("p k j -> p j k"), in_=craw[:, :, :], func=AF.Silu), r=["craw"], w=["cs"])

        L(lambda e: e.dma_start(out=xall[0:CTX, :], in_=ctx_in))
        for i in range(8):
            L(lambda e: e.dma_start(out=xall[CTX + i * 512:CTX + (i + 1) * 512, :], in_=x_in[i * 512:(i + 1) * 512, :]))
        P.barrier()

        def rstd_from_ss(ss_ap, out_ap, scale, r, w, Af=None, Vf=None):
            Af = Af or A; Vf = Vf or V
            Af(lambda e: e.activation(out=out_ap, in_=ss_ap, func=AF.Sqrt, scale=scale, bias=EPS), r=r, w=w)
            Vf(lambda e: e.reciprocal(out=out_ap, in_=out_ap), r=w, w=w)

        def interleave(fns):
            n = len(fns)
            cond = threading.Condition()
            st = {"turn": 0, "alive": [True] * n, "err": None}

            def advance():
                for k in range(1, n + 1):
                    j = (st["turn"] + k) % n
                    if st["alive"][j]:
                        st["turn"] = j
                        break
                cond.notify_all()

            def make_yp(i):
                def yp():
                    with cond:
                        advance()
                        while st["turn"] != i:
                            cond.wait()
                return yp

            def runner(i):
                with cond:
                    while st["turn"] != i:
                        cond.wait()
                try:
                    fns[i](make_yp(i))
                except BaseException as ex:
                    st["err"] = ex
                finally:
                    with cond:
                        st["alive"][i] = False
                        if any(st["alive"]):
                            advance()
                        else:
                            st["turn"] = -1
                            cond.notify_all()

            ths = [threading.Thread(target=runner, args=(i,)) for i in range(n)]
            for t_ in ths:
                t_.start()
            for t_ in ths:
                t_.join()
            if st["err"] is not None:
                raise st["err"]

        for l in range(DEPTH):
            last = l == DEPTH - 1
            LAYER[0] = l
            tiles_act = list(range(NT)) if not last else list(range(NCT, NT))

            with ExitStack() as ph:
                wst = [tsb(ph, f"wstA{i}", [128, 8, 512], F32) for i in range(2)]
                modrow = tsb(ph, "modrow", [2, 6 * D], F32)
                bada = tsb(ph, "bada", [2, 6 * D], F32)
                gn = tsb(ph, "gn", [2, 2, D], F32)
                pmod = [tps(ph, f"pmod{i}", [128, 512]) for i in range(2)]
                L(lambda e: e.dma_start(out=bada[:, :], in_=b_ada[l:l + 1, :].broadcast_to([2, 6 * D])), w=["bada"])
                L(lambda e: e.dma_start(out=gn[:, 0, :], in_=norm1_g[l:l + 1, :].broadcast_to([2, D])), w=["gn"])
                L(lambda e: e.dma_start(out=gn[:, 1, :], in_=norm2_g[l:l + 1, :].broadcast_to([2, D])), w=["gn"])
                for nb in range(12):
                    wb = wst[nb % 2]; pm = pmod[nb % 2]
                    for h in range(2):
                        L(lambda e: e.dma_start(out=wb[:, h * 4:(h + 1) * 4, :],
                                                in_=w_ada[l, h * 512:(h + 1) * 512, nb * 512:(nb + 1) * 512].rearrange("(k p) n -> p k n", p=128)),
                          w=[f"wstA{nb % 2}"])
                    for kc in range(8):
                        M(lambda e: e.matmul(pm[0:2, :], lhsT=cs[:, kc, :], rhs=wb[:, kc, :], start=(kc == 0), stop=(kc == 7)),
                          r=[f"wstA{nb % 2}", "cs"], w=[f"pmod{nb % 2}"])
                    V(lambda e: e.tensor_tensor(out=modrow[:, nb * 512:(nb + 1) * 512], in0=pm[0:2, :], in1=bada[:, nb * 512:(nb + 1) * 512], op=ALU.add),
                      r=[f"pmod{nb % 2}", "bada"], w=["modrow"])
                for j, off in ((0, 1 * D), (1, 4 * D)):
                    V(lambda e: e.scalar_tensor_tensor(out=modrow[:, off:off + D], in0=modrow[:, off:off + D], scalar=1.0, in1=gn[:, j, :],
                                                       op0=ALU.add, op1=ALU.mult), r=["modrow", "gn"], w=["modrow"])
                S(lambda e: e.dma_start(out=modv, in_=modrow[:, :]), r=["modrow"])
            P.barrier()
            if stop_after == ("A", l):
                break

            def load_bcast(dst, which, j, key):
                L(lambda e: e.dma_start(out=dst, in_=modv[which:which + 1, j * D:(j + 1) * D].broadcast_to([128, D])), w=[key])

            with ExitStack() as ph:
                wst = tsb(ph, "wstB", [128, 8, 512], F32)
                w_in_b = tsb(ph, "w_in_b", [128, 8, INW], BF16)
                wuq_f = tsb(ph, "wuq_f", [128, 2, 576], F32)
                wuq_b = tsb(ph, "wuq_b", [128, 2, 576], BF16)
                wukv_f = tsb(ph, "wukv_f", [128, 768], F32)
                wukv_b = tsb(ph, "wukv_b", [128, 768], BF16)
                gcq = tsb(ph, "gcq", [128, 2], F32)
                gckv = tsb(ph, "gckv", [128, 1], F32)
                gA = tsb(ph, "gA", [128, 8, 64], F32)
                gC = tsb(ph, "gC", [128, 6, 64], F32)
                gBq = tsb(ph, "gBq", [128, 96], F32)
                gBk = tsb(ph, "gBk", [128, 64], F32)
                gBkr = tsb(ph, "gBkr", [128, 32], F32)
                invB = tsb(ph, "invB", [128, 3], F32)
                invQ = tsb(ph, "invQ", [128, 12], F32)
                Ar = [tsb(ph, f"Ar{i}", [128, D], F32) for i in range(2)]
                Br = [tsb(ph, f"Br{i}", [128, D], F32) for i in range(2)]
                xt = [tsb(ph, f"xtB{i}", [128, D], F32) for i in range(2)]
                rp = [tsb(ph, f"rp{i}", [128, 192], F32) for i in range(2)]
                junk = tsb(ph, "junkB", [128, D], BF16)
                st1 = tsb(ph, "st1", [128, 4], F32)
                hf = tsb(ph, "hf", [128, D], F32)
                hb = tsb(ph, "hb", [128, D], BF16)
                hT = tsb(ph, "hT", [128, 8, 128], BF16)
                sqA = tsb(ph, "sqA", [128, 512], F32); sqC = tsb(ph, "sqC", [128, 384], F32); sqB = tsb(ph, "sqB", [128, 576], F32)
                sshA = tsb(ph, "sshA", [128, 8], F32); sshC = tsb(ph, "sshC", [128, 8], F32); sshB = tsb(ph, "sshB", [128, 16], F32)
                qnA = tsb(ph, "qnA", [128, 512], F32); qnC = tsb(ph, "qnC", [128, 384], F32); qnB = tsb(ph, "qnB", [128, 576], F32)
                t1A = tsb(ph, "t1A", [128, 512], F32); t1C = tsb(ph, "t1C", [128, 384], F32); t1B = tsb(ph, "t1B", [128, 192], F32)
                t2A = tsb(ph, "t2A", [128, 512], F32); t2C = tsb(ph, "t2C", [128, 384], F32); t2B = tsb(ph, "t2B", [128, 192], F32)
                qA = tsb(ph, "qA", [128, 8, 64], BF16)
                qC = tsb(ph, "qC", [128, 6, 64], BF16)
                qB = tsb(ph, "qB", [128, 12, 96], BF16)
                cqn = tsb(ph, "cqn", [128, 320], BF16)
                cT = tsb(ph, "cT", [128, 3, 128], BF16)
                kpe = tsb(ph, "kpe", [128, 32], F32)
                kpe2 = tsb(ph, "kpe2", [128, 32], F32)
                vst = tsb(ph, "vst", [128, 640], BF16)
                stA = tsb(ph, "stA", [128, 4, 512], BF16)
                stB = tsb(ph, "stB", [128, 12, 512], BF16)
                stC = tsb(ph, "stC", [128, 3, 512], BF16)
                pT = tps(ph, "pTB", [128, 1024], BF16)
                pP = [tps(ph, f"pP{i}", [128, 512]) for i in range(3)]
                pQ = [tps(ph, f"pQ{i}", [128, 512]) for i in range(2)]
                pK = [tps(ph, f"pK{i}", [128, 512]) for i in range(2)]

                for b, (c0, c1) in enumerate(((0, 512), (512, 1024), (1024, INW))):
                    n = c1 - c0
                    for h in range(2):
                        L(lambda e: e.dma_start(out=wst[:, h * 4:(h + 1) * 4, 0:n],
                                                in_=w_in[l, h * 512:(h + 1) * 512, c0:c1].rearrange("(k p) n -> p k n", p=128)), w=["wstB"])
                    V(lambda e: e.tensor_copy(out=w_in_b[:, 0:4, c0:c1], in_=wst[:, 0:4, 0:n]), r=["wstB"], w=["w_in_b"])
                    G(lambda e: e.tensor_copy(out=w_in_b[:, 4:8, c0:c1], in_=wst[:, 4:8, 0:n]), r=["wstB"], w=["w_in_b"])
                L(lambda e: e.dma_start(out=wuq_f[:, 0, :], in_=w_uq[l, 0:128, :]), w=["wuq_f"])
                L(lambda e: e.dma_start(out=wuq_f[0:64, 1, :], in_=w_uq[l, 128:192, :]), w=["wuq_f"])
                L(lambda e: e.dma_start(out=wukv_f[:, :], in_=w_ukv[l, :, :]), w=["wukv_f"])
                L(lambda e: e.dma_start(out=gcq[:, 0:1], in_=b_cq_norm[l, 0:128].rearrange("(p o) -> p o", o=1)), w=["gcq"])
                L(lambda e: e.dma_start(out=gcq[0:64, 1:2], in_=b_cq_norm[l, 128:192].rearrange("(p o) -> p o", o=1)), w=["gcq"])
                L(lambda e: e.dma_start(out=gckv[:, 0:1], in_=b_ckv_norm[l, :].rearrange("(p o) -> p o", o=1)), w=["gckv"])
                V(lambda e: e.tensor_scalar(out=wuq_b[:, 0, :], in0=wuq_f[:, 0, :], scalar1=gcq[:, 0:1], scalar2=None, op0=ALU.mult), r=["wuq_f", "gcq"], w=["wuq_b"])
                V(lambda e: e.tensor_scalar(out=wuq_b[0:64, 1, :], in0=wuq_f[0:64, 1, :], scalar1=gcq[0:64, 1:2], scalar2=None, op0=ALU.mult), r=["wuq_f", "gcq"], w=["wuq_b"])
                V(lambda e: e.tensor_scalar(out=wukv_b[:, :], in0=wukv_f[:, :], scalar1=gckv[:, 0:1], scalar2=None, op0=ALU.mult), r=["wukv_f", "gckv"], w=["wukv_b"])
                for hh in range(8):
                    src = a_q_norm if hh < 6 else a_k_norm
                    L(lambda e: e.dma_start(out=gA[:, hh, :], in_=src[l:l + 1, :].broadcast_to([128, 64])), w=["gA"])
                for hh in range(6):
                    src = c_q_norm if hh < 4 else c_k_norm
                    L(lambda e: e.dma_start(out=gC[:, hh, :], in_=src[l:l + 1, :].broadcast_to([128, 64])), w=["gC"])
                L(lambda e: e.dma_start(out=gBq[:, 0:64], in_=b_qn_norm[l:l + 1, :].broadcast_to([128, 64])), w=["gBq"])
                L(lambda e: e.dma_start(out=gBq[:, 64:96], in_=b_qr_norm[l:l + 1, :].broadcast_to([128, 32])), w=["gBq"])
                L(lambda e: e.dma_start(out=gBk[:, :], in_=b_kn_norm[l:l + 1, :].broadcast_to([128, 64])), w=["gBk"])
                L(lambda e: e.dma_start(out=gBkr[:, :], in_=b_kr_norm[l:l + 1, :].broadcast_to([128, 32])), w=["gBkr"])
                for j, v in enumerate((1.0 / 192, 1.0 / 128, 1.0 / 32)):
                    G(lambda e: e.memset(invB[:, j:j + 1], v), w=["invB"])
                G(lambda e: e.memset(invQ[:, 0:6], 1.0 / 64), w=["invQ"])
                G(lambda e: e.memset(invQ[:, 6:12], 1.0 / 32), w=["invQ"])
                for which in range(2):
                    load_bcast(Ar[which][:, :], which, 1, f"Ar{which}")
                    load_bcast(Br[which][:, :], which, 0, f"Br{which}")

                def rope_apply(src_ap, H, Dh, C_ap, S_ap, out_ap, rs, t1b, t2b, sfx, Vf):
                    c4 = Dh // 4
                    t1v = t1b[:, 0:H * Dh].rearrange("p (h d) -> p h d", h=H)
                    t2v = t2b[:, 0:H * Dh].rearrange("p (h a b c) -> p h a b c", h=H, a=2, b=2)
                    s5 = src_ap.rearrange("p h (a b c) -> p h a b c", a=2, b=2)
                    S4 = S_ap.rearrange("p (a b c) -> p a b c", a=2, b=2)
                    Vf(lambda e: e.tensor_tensor(out=t1v, in0=src_ap, in1=C_ap.unsqueeze(1).to_broadcast([128, H, Dh]), op=ALU.mult), r=rs, w=["t1" + sfx])
                    Vf(lambda e: e.tensor_tensor(out=t2v[:, :, :, 0, :], in0=s5[:, :, :, 1, :],
                                                 in1=S4[:, :, 0, :].unsqueeze(1).to_broadcast([128, H, 2, c4]), op=ALU.mult), r=rs, w=["t2" + sfx])
                    Vf(lambda e: e.tensor_tensor(out=t2v[:, :, :, 1, :], in0=s5[:, :, :, 0, :],
                                                 in1=S4[:, :, 1, :].unsqueeze(1).to_broadcast([128, H, 2, c4]), op=ALU.mult), r=rs, w=["t2" + sfx])
                    return t1v, t2b[:, 0:H * Dh].rearrange("p (h d) -> p h d", h=H)

                V0, A0, M0 = V, A, M
                blocks = [list(range(0, NCT))] + [list(range(NCT + 4 * b, NCT + 4 * b + 4)) for b in range(SEQ // 512)]
                for blk in blocks:
                    for bi, ti in enumerate(blk):
                        isc = ti < NCT
                        which = 1 if isc else 0
                        li = ti - NCT
                        xb = xt[ti % 2]; xk = f"xtB{ti % 2}"
                        rpb = rp[ti % 2]; rk = f"rp{ti % 2}"
                        L(lambda e: e.dma_start(out=xb[:, :], in_=xall[ti * 128:(ti + 1) * 128, :]), w=[xk])
                        if not isc:
                            L(lambda e: e.dma_start(out=rpb[:, :], in_=rope[li * 128:(li + 1) * 128, :]), w=[rk])
                        A(lambda e: e.activation(out=junk[:, :], in_=xb[:, :], func=AF.Square, accum_out=st1[:, 0:1]), r=[xk], w=["junkB", "st1"])
                        rstd_from_ss(st1[:, 0:1], st1[:, 1:2], 1.0 / D, ["st1"], ["st1b"])
                        V(lambda e: e.scalar_tensor_tensor(out=hf[:, :], in0=xb[:, :], scalar=st1[:, 1:2], in1=Ar[which][:, :], op0=ALU.mult, op1=ALU.mult),
                          r=[xk, "st1b", f"Ar{which}"], w=["hf"])
                        V(lambda e: e.tensor_tensor(out=hb[:, :], in0=hf[:, :], in1=Br[which][:, :], op=ALU.add), r=["hf", f"Br{which}"], w=["hb"])
                        for kc in range(8):
                            M(lambda e: e.transpose(out=pT[:, kc * 128:(kc + 1) * 128], in_=hb[:, kc * 128:(kc + 1) * 128], identity=ident[:, :]),
                              r=["hb", "ident"], w=["pTB"])
                        A(lambda e: e.copy(out=hT[:, :, :].rearrange("p k t -> p (k t)"), in_=pT[:, :]), r=["pTB"], w=["hT"])
                        for b, (c0, c1) in enumerate(((0, 512), (512, 992), (992, INW))):
                            for kc in range(8):
                                M(lambda e: e.matmul(pP[b][:, 0:c1 - c0], lhsT=hT[:, kc, :], rhs=w_in_b[:, kc, c0:c1], start=(kc == 0), stop=(kc == 7)),
                                  r=["hT", "w_in_b"], w=[f"pP{b}"])

                        def gqa_stream(yp, pp, pk, H, gain, gk, dst, dk, sqb, sshb, qnb, t1b, t2b, sfx):
                            Vg = lambda fn, r=(), w=(): (V(fn, r, w), yp())
                            Ag = lambda fn, r=(), w=(): (A(fn, r, w), yp())
                            n = H * 64
                            src3 = pp[:, 0:n].rearrange("p (h d) -> p h d", h=H)
                            Ag(lambda e: e.activation(out=sqb[:, 0:n], in_=pp[:, 0:n], func=AF.Square), r=[pk], w=["sq" + sfx])
                            Vg(lambda e: e.tensor_reduce(out=sshb[:, 0:H], in_=sqb[:, 0:n].rearrange("p (h d) -> p h d", h=H), axis=AX.X, op=ALU.add), r=["sq" + sfx], w=["ssh" + sfx])
                            rstd_from_ss(sshb[:, 0:H], sshb[:, 0:H], 1.0 / 64, ["ssh" + sfx], ["ssh" + sfx], Ag, Vg)
                            qn3 = qnb[:, 0:n].rearrange("p (h d) -> p h d", h=H)
                            Vg(lambda e: e.tensor_tensor(out=qn3, in0=src3, in1=sshb[:, 0:H].unsqueeze(2).to_broadcast([128, H, 64]), op=ALU.mult), r=[pk, "ssh" + sfx], w=["qn" + sfx])
                            if isc:
                                Vg(lambda e: e.tensor_tensor(out=dst[:, :, :], in0=qn3, in1=gain[:, :, :], op=ALU.mult), r=["qn" + sfx, gk], w=[dk])
                            else:
                                Vg(lambda e: e.tensor_tensor(out=qn3, in0=qn3, in1=gain[:, :, :], op=ALU.mult), r=["qn" + sfx, gk], w=["qn" + sfx])
                                a1, a2 = rope_apply(qn3, H, 64, rpb[:, 0:64], rpb[:, 64:128], None, ["qn" + sfx, rk], t1b, t2b, sfx, Vg)
                                Vg(lambda e: e.tensor_tensor(out=dst[:, :, :], in0=a1, in1=a2, op=ALU.add), r=["t1" + sfx, "t2" + sfx], w=[dk])

                        def mla_stream(yp):
                            V = lambda fn, r=(), w=(): (V0(fn, r, w), yp())
                            A = lambda fn, r=(), w=(): (A0(fn, r, w), yp())
                            M = lambda fn, r=(), w=(): (M0(fn, r, w), yp())
                            sq, ssh, qn, sfx = sqB, sshB, qnB, "B"
                            A(lambda e: e.copy(out=vst[:, 0:128], in_=pP[1][:, 0:128]), r=["pP1"], w=["vst"])
                            A(lambda e: e.copy(out=vst[:, 512:640], in_=pP[2][:, 384:512]), r=["pP2"], w=["vst"])

                            A(lambda e: e.activation(out=sq[:, 0:352], in_=pP[1][:, 128:480], func=AF.Square), r=["pP1"], w=["sqB"])
                            V(lambda e: e.tensor_reduce(out=ssh[:, 0:1], in_=sq[:, 0:192], axis=AX.X, op=ALU.add), r=["sqB"], w=["sshB"])
                            V(lambda e: e.tensor_reduce(out=ssh[:, 1:2], in_=sq[:, 192:320], axis=AX.X, op=ALU.add), r=["sqB"], w=["sshB"])
                            V(lambda e: e.tensor_reduce(out=ssh[:, 2:3], in_=sq[:, 320:352], axis=AX.X, op=ALU.add), r=["sqB"], w=["sshB"])
                            V(lambda e: e.tensor_tensor(out=ssh[:, 0:3], in0=ssh[:, 0:3], in1=invB[:, :], op=ALU.mult), r=["sshB", "invB"], w=["sshB"])
                            rstd_from_ss(ssh[:, 0:3], ssh[:, 0:3], 1.0, ["sshB"], ["sshB"], A, V)
                            V(lambda e: e.tensor_scalar(out=cqn[:, 0:192], in0=pP[1][:, 128:320], scalar1=ssh[:, 0:1], scalar2=None, op0=ALU.mult), r=["pP1", "sshB"], w=["cqn"])
                            V(lambda e: e.tensor_scalar(out=cqn[:, 192:320], in0=pP[1][:, 320:448], scalar1=ssh[:, 1:2], scalar2=None, op0=ALU.mult), r=["pP1", "sshB"], w=["cqn"])
                            V(lambda e: e.scalar_tensor_tensor(out=kpe[:, :], in0=pP[1][:, 448:480], scalar=ssh[:, 2:3], in1=gBkr[:, :], op0=ALU.mult, op1=ALU.mult),
                              r=["pP1", "sshB", "gBkr"], w=["kpe"])
                            if not isc:
                                a1, a2 = rope_apply(kpe[:, :].unsqueeze(1), 1, 32, rpb[:, 128:160], rpb[:, 160:192], None, ["kpe", rk], t1B, t2B, "B", V)
                                V(lambda e: e.tensor_tensor(out=kpe2[:, :].unsqueeze(1), in0=a1, in1=a2, op=ALU.add), r=["t1B", "t2B"], w=["kpe2"])
                                kp = kpe2
                            else:
                                kp = kpe
                            V(lambda e: e.tensor_copy(out=qB[:, 6:12, 64:96], in_=kp[:, :].unsqueeze(1).to_broadcast([128, 6, 32])), r=["kpe", "kpe2"], w=["qBk"])
                            M(lambda e: e.transpose(out=pT[:, 0:128], in_=cqn[:, 0:128], identity=ident[:, :]), r=["cqn", "ident"], w=["pTB"])
                            M(lambda e: e.transpose(out=pT[0:64, 128:256], in_=cqn[:, 128:192], identity=ident[:, :]), r=["cqn", "ident"], w=["pTB"])
                            M(lambda e: e.transpose(out=pT[:, 256:384], in_=cqn[:, 192:320], identity=ident[:, :]), r=["cqn", "ident"], w=["pTB"])
                            A(lambda e: e.copy(out=cT[:, 0, :], in_=pT[:, 0:128]), r=["pTB"], w=["cT"])
                            A(lambda e: e.copy(out=cT[0:64, 1, :], in_=pT[0:64, 128:256]), r=["pTB"], w=["cT"])
                            A(lambda e: e.copy(out=cT[:, 2, :], in_=pT[:, 256:384]), r=["pTB"], w=["cT"])
                            for hf_ in range(2):
                                M(lambda e: e.matmul(pQ[hf_][:, 0:288], lhsT=cT[:, 0, :], rhs=wuq_b[:, 0, hf_ * 288:(hf_ + 1) * 288], start=True, stop=False),
                                  r=["cT", "wuq_b"], w=[f"pQ{hf_}"])
                                M(lambda e: e.matmul(pQ[hf_][:, 0:288], lhsT=cT[0:64, 1, :], rhs=wuq_b[0:64, 1, hf_ * 288:(hf_ + 1) * 288], start=False, stop=True),
                                  r=["cT", "wuq_b"], w=[f"pQ{hf_}"])
                                M(lambda e: e.matmul(pK[hf_][:, 0:384], lhsT=cT[:, 2, :], rhs=wukv_b[:, hf_ * 384:(hf_ + 1) * 384], start=True, stop=True),
                                  r=["cT", "wukv_b"], w=[f"pK{hf_}"])
                            for hf_ in range(2):
                                A(lambda e: e.activation(out=sq[:, hf_ * 288:(hf_ + 1) * 288], in_=pQ[hf_][:, 0:288], func=AF.Square), r=[f"pQ{hf_}"], w=["sqB"])
                            sq3 = sq[:, 0:576].rearrange("p (h d) -> p h d", h=6)
                            V(lambda e: e.tensor_reduce(out=ssh[:, 0:6], in_=sq3[:, :, 0:64], axis=AX.X, op=ALU.add), r=["sqB"], w=["sshB"])
                            V(lambda e: e.tensor_reduce(out=ssh[:, 6:12], in_=sq3[:, :, 64:96], axis=AX.X, op=ALU.add), r=["sqB"], w=["sshB"])
                            V(lambda e: e.tensor_tensor(out=ssh[:, 0:12], in0=ssh[:, 0:12], in1=invQ[:, :], op=ALU.mult), r=["sshB", "invQ"], w=["sshB"])
                            rstd_from_ss(ssh[:, 0:12], ssh[:, 0:12], 1.0, ["sshB"], ["sshB"], A, V)
                            qn3 = qn[:, 0:576].rearrange("p (h d) -> p h d", h=6)
                            for hf_ in range(2):
                                p3 = pQ[hf_][:, 0:288].rearrange("p (h d) -> p h d", h=3)
                                hs = slice(hf_ * 3, hf_ * 3 + 3)
                                V(lambda e: e.tensor_tensor(out=qn3[:, hs, 0:64], in0=p3[:, :, 0:64],
                                                            in1=ssh[:, hf_ * 3:hf_ * 3 + 3].unsqueeze(2).to_broadcast([128, 3, 64]), op=ALU.mult), r=[f"pQ{hf_}", "sshB"], w=["qnB"])
                                V(lambda e: e.tensor_tensor(out=qn3[:, hs, 64:96], in0=p3[:, :, 64:96],
                                                            in1=ssh[:, 6 + hf_ * 3:6 + hf_ * 3 + 3].unsqueeze(2).to_broadcast([128, 3, 32]), op=ALU.mult), r=[f"pQ{hf_}", "sshB"], w=["qnB"])
                            V(lambda e: e.tensor_tensor(out=qn3, in0=qn3, in1=gBq[:, :].unsqueeze(1).to_broadcast([128, 6, 96]), op=ALU.mult), r=["qnB", "gBq"], w=["qnB"])
                            V(lambda e: e.tensor_copy(out=qB[:, 0:6, 0:64], in_=qn3[:, :, 0:64]), r=["qnB"], w=["qBq"])
                            if isc:
                                V(lambda e: e.tensor_copy(out=qB[:, 0:6, 64:96], in_=qn3[:, :, 64:96]), r=["qnB"], w=["qBq"])
                            else:
                                a1, a2 = rope_apply(qn3[:, :, 64:96], 6, 32, rpb[:, 128:160], rpb[:, 160:192], None, ["qnB", rk], t1B, t2B, "B", V)
                                V(lambda e: e.tensor_tensor(out=qB[:, 0:6, 64:96], in0=a1, in1=a2, op=ALU.add), r=["t1B", "t2B"], w=["qBq"])
                            for hf_ in range(2):
                                A(lambda e: e.activation(out=sq[:, hf_ * 192:(hf_ + 1) * 192].rearrange("p (h d) -> p h d", h=3),
                                                         in_=pK[hf_][:, 0:384].rearrange("p (h d) -> p h d", h=3)[:, :, 0:64], func=AF.Square), r=[f"pK{hf_}"], w=["sqB"])
                                A(lambda e: e.copy(out=vst[:, 128 + hf_ * 192:128 + (hf_ + 1) * 192].rearrange("p (h d) -> p h d", h=3),
                                                   in_=pK[hf_][:, 0:384].rearrange("p (h d) -> p h d", h=3)[:, :, 64:128]), r=[f"pK{hf_}"], w=["vst"])
                            V(lambda e: e.tensor_reduce(out=ssh[:, 0:6], in_=sq[:, 0:384].rearrange("p (h d) -> p h d", h=6), axis=AX.X, op=ALU.add), r=["sqB"], w=["sshB"])
                            rstd_from_ss(ssh[:, 0:6], ssh[:, 0:6], 1.0 / 64, ["sshB"], ["sshB"], A, V)
                            kn3 = qn[:, 0:384].rearrange("p (h d) -> p h d", h=6)
                            for hf_ in range(2):
                                p3 = pK[hf_][:, 0:384].rearrange("p (h d) -> p h d", h=3)
                                V(lambda e: e.tensor_tensor(out=kn3[:, hf_ * 3:hf_ * 3 + 3, :], in0=p3[:, :, 0:64],
                                                            in1=ssh[:, hf_ * 3:hf_ * 3 + 3].unsqueeze(2).to_broadcast([128, 3, 64]), op=ALU.mult), r=[f"pK{hf_}", "sshB"], w=["qnB"])
                            V(lambda e: e.tensor_tensor(out=qB[:, 6:12, 0:64], in0=kn3, in1=gBk[:, :].unsqueeze(1).to_broadcast([128, 6, 64]), op=ALU.mult), r=["qnB", "gBk"], w=["qBk"])


                        interleave([lambda yp: gqa_stream(yp, pP[0], "pP0", 8, gA, "gA", qA, "qA", sqA, sshA, qnA, t1A, t2A, "A"),
                                    lambda yp: gqa_stream(yp, pP[2], "pP2", 6, gC, "gC", qC, "qC", sqC, sshC, qnC, t1C, t2C, "C"),
                                    mla_stream])

                        S(lambda e: e.dma_start(out=vall[ti * 128:(ti + 1) * 128, :], in_=vst[:, :]), r=["vst"])
                        c0 = bi * 128
                        for i in range(4):
                            M(lambda e: e.transpose(out=pT[:, i * 128:(i + 1) * 128], in_=qA[:, 2 * i:2 * i + 2, :].rearrange("p h d -> p (h d)"), identity=ident[:, :]),
                              r=["qA", "ident"], w=["pTB"])
                        A(lambda e: e.copy(out=stA[:, :, c0:c0 + 128], in_=pT[:, 0:512].rearrange("p (i t) -> p i t", i=4)), r=["pTB"], w=["stA"])
                        for i in range(3):
                            M(lambda e: e.transpose(out=pT[:, i * 128:(i + 1) * 128], in_=qC[:, 2 * i:2 * i + 2, :].rearrange("p h d -> p (h d)"), identity=ident[:, :]),
                              r=["qC", "ident"], w=["pTB"])
                        V(lambda e: e.tensor_copy(out=stC[:, :, c0:c0 + 128], in_=pT[:, 0:384].rearrange("p (i t) -> p i t", i=3)), r=["pTB"], w=["stC"])
                        for rnd in range(2):
                            for i in range(6):
                                M(lambda e: e.transpose(out=pT[0:96, i * 128:(i + 1) * 128], in_=qB[:, rnd * 6 + i, :], identity=ident[:, :]),
                                  r=["qBq", "qBk", "ident"], w=["pTB"])
                            A(lambda e: e.copy(out=stB[0:96, rnd * 6:rnd * 6 + 6, c0:c0 + 128], in_=pT[0:96, 0:768].rearrange("p (i t) -> p i t", i=6)), r=["pTB"], w=["stB"])
                    W = len(blk) * 128
                    t0 = blk[0] * 128
                    S(lambda e: e.dma_start(out=qkT_A[:, t0:t0 + W].rearrange("(i p) t -> p i t", p=128), in_=stA[:, :, 0:W]), r=["stA"])
                    S(lambda e: e.dma_start(out=qkT_C[:, t0:t0 + W].rearrange("(i p) t -> p i t", p=128), in_=stC[:, :, 0:W]), r=["stC"])
                    S(lambda e: e.dma_start(out=qkT_B[:, :, t0:t0 + W].rearrange("j p t -> p j t"), in_=stB[0:96, :, 0:W]), r=["stB"])
            P.barrier()
            if stop_after == ("B", l):
                break

            with ExitStack() as ph:
                kt = [tsb(ph, f"kt{i}", [96, T], BF16) for i in range(2)]
                vb = [tsb(ph, f"vb{i}", [128, NT, 65], BF16) for i in range(2)]
                qt = [tsb(ph, f"qt{i}", [96, 512], BF16) for i in range(2)]
                pt = [tsb(ph, f"pt{i}", [128, 512], BF16) for i in range(3)]
                ob = [tsb(ph, f"ob{i}", [128, 4, 64], BF16) for i in range(2)]
                den = tsb(ph, "den", [128, 8], F32)
                sinkr = tsb(ph, "sinkr", [128, 4], F32)
                sinke = tsb(ph, "sinke", [128, 4], F32)
                pS = [tps(ph, f"pS{i}", [128, 512]) for i in range(3)]
                pO = [tps(ph, f"pO{i}", [128, 512]) for i in range(2)]
                den2 = [tsb(ph, f"den2_{i}", [128, 4], F32) for i in range(2)]
                L(lambda e: e.dma_start(out=sinkr[:, :], in_=c_sink[l:l + 1, :].broadcast_to([128, 4])), w=["sinkr"])
                A(lambda e: e.activation(out=sinke[:, :], in_=sinkr[:, :], func=AF.Exp), r=["sinkr"], w=["sinke"])
                for i in range(2):
                    G(lambda e: e.memset(vb[i][:, :, 64:65], 1.0), w=[f"vb{i}"])
                cnt = {"kv": 0, "q": 0, "s": 0, "p": 0, "u": 0}
                mixv = mix.rearrange("(j p) c -> p j c", p=128)
                vallv = vall.rearrange("(j p) c -> p j c", p=128)
                pending = []

                def emit_tail(st):
                    (si, pi, Wq, scale, msk, subs, u, n, nkeys, vbuf, vk, j, sink_h, out_fn) = st
                    ui = u % 2
                    A(lambda e: e.activation(out=pt[pi][:, 0:Wq], in_=pS[si][:, 0:Wq], func=AF.Exp, scale=scale), r=[f"pS{si}"], w=[f"pt{pi}"])
                    if msk is not None:
                        V(lambda e: e.tensor_tensor(out=pt[pi][:, 0:Wq].rearrange("p (s t) -> p s t", s=subs), in0=pt[pi][:, 0:Wq].rearrange("p (s t) -> p s t", s=subs),
                                                    in1=msk[:, :].unsqueeze(1).to_broadcast([128, subs, 128]), op=ALU.mult), r=[f"pt{pi}"], w=[f"pt{pi}"])
                    for s_ in range(subs):
                        M(lambda e: e.matmul(pO[ui][:, s_ * 65:(s_ + 1) * 65], lhsT=pt[pi][:, s_ * 128:(s_ + 1) * 128], rhs=vbuf[:, j, :],
                                             start=(n == 0 and s_ == 0), stop=(n == nkeys - 1), skip_group_check=True),
                          r=[f"pt{pi}", vk], w=[f"pO{ui}"])
                    if n == nkeys - 1:
                        pov = pO[ui][:, 0:subs * 65].rearrange("p (s c) -> p s c", s=subs)
                        dn = den2[ui]
                        if sink_h is not None:
                            V(lambda e: e.tensor_tensor(out=dn[:, 0:subs].unsqueeze(2), in0=pov[:, :, 64:65], in1=sinke[:, sink_h:sink_h + subs].unsqueeze(2), op=ALU.add),
                              r=[f"pO{ui}", "sinke"], w=[f"den2_{ui}"])
                            V(lambda e: e.reciprocal(out=dn[:, 0:subs], in_=dn[:, 0:subs]), r=[f"den2_{ui}"], w=[f"den2_{ui}"])
                        else:
                            V(lambda e: e.reciprocal(out=dn[:, 0:subs].unsqueeze(2), in_=pov[:, :, 64:65]), r=[f"pO{ui}"], w=[f"den2_{ui}"])
                        V(lambda e: e.tensor_tensor(out=ob[ui][:, 0:subs, :], in0=pov[:, :, 0:64], in1=dn[:, 0:subs].unsqueeze(2).to_broadcast([128, subs, 64]), op=ALU.mult),
                          r=[f"pO{ui}", f"den2_{ui}"], w=[f"ob{ui}"])
                        out_fn(ob[ui], f"ob{ui}")

                def attn_unit(qsrc_fn, dq, Wq, keys, kb, kk, vbuf, vk, scale, subs, sink_h, out_fn):
                    qi = cnt["q"] % 2; cnt["q"] += 1
                    u = cnt["u"]; cnt["u"] += 1
                    qsrc_fn(qt[qi], f"qt{qi}")
                    for n, (j, msk) in enumerate(keys):
                        si = cnt["s"] % 3; cnt["s"] += 1
                        pi = cnt["p"] % 3; cnt["p"] += 1
                        M(lambda e: e.matmul(pS[si][:, 0:Wq], lhsT=kb[0:dq, j * 128:(j + 1) * 128], rhs=qt[qi][0:dq, 0:Wq], start=True, stop=True),
                          r=[kk, f"qt{qi}"], w=[f"pS{si}"])
                        pending.append((si, pi, Wq, scale, msk, subs, u, n, len(keys), vbuf, vk, j, sink_h, out_fn))
                        if len(pending) > 1:
                            emit_tail(pending.pop(0))

                def load_kv(krows_ap, dq, voff):
                    i = cnt["kv"] % 2; cnt["kv"] += 1
                    L(lambda e: e.dma_start(out=kt[i][0:dq, :], in_=krows_ap), w=[f"kt{i}"])
                    for hh in range(2):
                        L(lambda e: e.dma_start(out=vb[i][:, hh * 17:(hh + 1) * 17, 0:64], in_=vallv[:, hh * 17:(hh + 1) * 17, voff:voff + 64]), w=[f"vb{i}"])
                    return kt[i], f"kt{i}", vb[i], f"vb{i}"

                qblocks = ([(0, NCT)] if not last else []) + [(NCT + 4 * b, 4) for b in range(SEQ // 512)]
                for mixer in ("A", "B"):
                    nkv = 2 if mixer == "A" else 6
                    for kv in range(nkv):
                        if mixer == "A":
                            kb, kk, vbuf, vk = load_kv(qkT_A[(6 + kv) * 64:(7 + kv) * 64, :], 64, kv * 64)
                            heads = [3 * kv + g for g in range(3)]; dq = 64; scale = 64 ** -0.5
                        else:
                            kb, kk, vbuf, vk = load_kv(qkT_B[6 + kv, :, :], 96, 128 + kv * 64)
                            heads = [kv]; dq = 96; scale = 96 ** -0.5
                        for h in heads:
                            for (tb, nsub) in qblocks:
                                keys = [(j, None) for j in (range(NCT) if tb < NCT else range(NT))]
                                Wq = nsub * 128
                                if mixer == "A":
                                    qsrc = lambda dst, dk, h=h, tb=tb, Wq=Wq: L(lambda e: e.dma_start(out=dst[0:64, 0:Wq], in_=qkT_A[h * 64:(h + 1) * 64, tb * 128:tb * 128 + Wq]), w=[dk])
                                    col = h * 64
                                else:
                                    qsrc = lambda dst, dk, h=h, tb=tb, Wq=Wq: L(lambda e: e.dma_start(out=dst[0:96, 0:Wq], in_=qkT_B[h, :, tb * 128:tb * 128 + Wq]), w=[dk])
                                    col = 384 + h * 64
                                outf = lambda o, ok, tb=tb, nsub=nsub, col=col: S(lambda e: e.dma_start(out=mixv[:, tb:tb + nsub, col:col + 64], in_=o[:, 0:nsub, :]), r=[ok])
                                attn_unit(qsrc, dq, Wq, keys, kb, kk, vbuf, vk, scale, nsub, None, outf)
                for kv in range(2):
                    kb, kk, vbuf, vk = load_kv(qkT_C[(4 + kv) * 64:(5 + kv) * 64, :], 64, 512 + kv * 64)
                    for ti in (range(NT) if not last else range(NCT, NT)):
                        if ti < NCT:
                            keys = [(j, None) for j in range(NCT)]
                        else:
                            keys = [(j, None) for j in range(NCT)]
                            if ti - 1 >= NCT:
                                keys.append((ti - 1, M1))
                            keys.append((ti, None))
                            if ti + 1 < NT:
                                keys.append((ti + 1, M2))

                        def qsrc(dst, dk, kv=kv, ti=ti):
                            L(lambda e: e.dma_start(out=dst[0:64, 0:256].rearrange("p (h t) -> p h t", h=2),
                                                    in_=qkT_C[2 * kv * 64:(2 * kv + 2) * 64, ti * 128:(ti + 1) * 128].rearrange("(h p) t -> p h t", p=64)), w=[dk])
                        col = 768 + 2 * kv * 64
                        outf = lambda o, ok, ti=ti, col=col: S(lambda e: e.dma_start(out=mix[ti * 128:(ti + 1) * 128, col:col + 128], in_=o[:, 0:2, :].rearrange("p s d -> p (s d)")), r=[ok])
                        attn_unit(qsrc, 64, 256, keys, kb, kk, vbuf, vk, 64 ** -0.5, 2, 2 * kv, outf)
                while pending:
                    emit_tail(pending.pop(0))
            P.barrier()
            if stop_after == ("C", l):
                break

            with ExitStack() as ph:
                wst = tsb(ph, "wstD", [128, 8, 512], F32)
                w_out_b = tsb(ph, "w_out_b", [128, 8, D], BF16)
                G1 = [tsb(ph, f"G1_{i}", [128, D], F32) for i in range(2)]
                xt = [tsb(ph, f"xtD{i}", [128, D], F32) for i in range(2)]
                mt = [tsb(ph, f"mt{i}", [128, D], BF16) for i in range(2)]
                mT = tsb(ph, "mT", [128, 8, 128], BF16)
                yt = [tsb(ph, f"yt{i}", [128, D], F32) for i in range(2)]
                pT = tps(ph, "pTD", [128, 1024], BF16)
                pY = [tps(ph, f"pY{i}", [128, 512]) for i in range(2)]
                for b in range(2):
                    for h in range(2):
                        L(lambda e: e.dma_start(out=wst[:, h * 4:(h + 1) * 4, :],
                                                in_=w_out[l, h * 512:(h + 1) * 512, b * 512:(b + 1) * 512].rearrange("(k p) n -> p k n", p=128)), w=["wstD"])
                    V(lambda e: e.tensor_copy(out=w_out_b[:, 0:4, b * 512:(b + 1) * 512], in_=wst[:, 0:4, :]), r=["wstD"], w=["w_out_b"])
                    G(lambda e: e.tensor_copy(out=w_out_b[:, 4:8, b * 512:(b + 1) * 512], in_=wst[:, 4:8, :]), r=["wstD"], w=["w_out_b"])
                for which in range(2):
                    load_bcast(G1[which][:, :], which, 2, f"G1_{which}")
                for ti in tiles_act:
                    which = 1 if ti < NCT else 0
                    i2 = ti % 2
                    L(lambda e: e.dma_start(out=xt[i2][:, :], in_=xall[ti * 128:(ti + 1) * 128, :]), w=[f"xtD{i2}"])
                    L(lambda e: e.dma_start(out=mt[i2][:, :], in_=mix[ti * 128:(ti + 1) * 128, :]), w=[f"mt{i2}"])
                    for kc in range(8):
                        M(lambda e: e.transpose(out=pT[:, kc * 128:(kc + 1) * 128], in_=mt[i2][:, kc * 128:(kc + 1) * 128], identity=ident[:, :]), r=[f"mt{i2}", "ident"], w=["pTD"])
                    A(lambda e: e.copy(out=mT[:, :, :].rearrange("p k t -> p (k t)"), in_=pT[:, :]), r=["pTD"], w=["mT"])
                    for b in range(2):
                        for kc in range(8):
                            M(lambda e: e.matmul(pY[b][:, :], lhsT=mT[:, kc, :], rhs=w_out_b[:, kc, b * 512:(b + 1) * 512], start=(kc == 0), stop=(kc == 7)), r=["mT", "w_out_b"], w=[f"pY{b}"])
                        V(lambda e: e.tensor_tensor(out=yt[i2][:, b * 512:(b + 1) * 512], in0=pY[b][:, :], in1=G1[which][:, b * 512:(b + 1) * 512], op=ALU.mult),
                          r=[f"pY{b}", f"G1_{which}"], w=[f"yt{i2}"])
                    V(lambda e: e.tensor_tensor(out=yt[i2][:, :], in0=yt[i2][:, :], in1=xt[i2][:, :], op=ALU.add), r=[f"yt{i2}", f"xtD{i2}"], w=[f"yt{i2}"])
                    S(lambda e: e.dma_start(out=xall[ti * 128:(ti + 1) * 128, :], in_=yt[i2][:, :]), r=[f"yt{i2}"])
            P.barrier()
            if stop_after == ("D", l):
                break

            with ExitStack() as ph:
                aff = tsb(ph, "aff", [128, NT, NE], F32)
                gm = tsb(ph, "gm", [128, NT, NE], F32)
                sloti = tsb(ph, "sloti", [128, NT, NE], I32)
                slotc = tsb(ph, "slotc", [128, NT, NE], I32)
                G2 = [tsb(ph, f"G2_{i}", [128, D], F32) for i in range(2)]
                p13 = ExitStack()
                affT = tsb(p13, "affT", [NE, T], F32)
                maskT = tsb(p13, "maskT", [NE, T], BF16)
                with ExitStack() as p1:
                    wr_f = tsb(p1, "wr_f", [128, 8, NE], F32)
                    wr_b = tsb(p1, "wr_b", [128, 8, NE], BF16)
                    Ar = [tsb(p1, f"Ar2_{i}", [128, D], F32) for i in range(2)]
                    Br = [tsb(p1, f"Br2_{i}", [128, D], F32) for i in range(2)]
                    xt = [tsb(p1, f"xtE{i}", [128, D], F32) for i in range(2)]
                    junk = tsb(p1, "junkE", [128, D], BF16)
                    st1 = tsb(p1, "st1E", [128, 8], F32)
                    hf = tsb(p1, "hfE", [128, D], F32)
                    hb = [tsb(p1, f"hbE{i}", [128, D], BF16) for i in range(2)]
                    hT = tsb(p1, "hTE", [128, 8, 128], BF16)
                    ex = tsb(p1, "ex", [128, NE], F32)
                    pT = tps(p1, "pTE", [128, 1024], BF16)
                    pR = tps(p1, "pR", [128, 512])
                    pAT = tps(p1, "pAT", [128, 512])
                    L(lambda e: e.dma_start(out=wr_f[:, :, :], in_=w_router[l, :, :].rearrange("(k p) n -> p k n", p=128)), w=["wr_f"])
                    V(lambda e: e.tensor_copy(out=wr_b[:, :, :], in_=wr_f[:, :, :]), r=["wr_f"], w=["wr_b"])
                    for which in range(2):
                        load_bcast(Ar[which][:, :], which, 4, f"Ar2_{which}")
                        load_bcast(Br[which][:, :], which, 3, f"Br2_{which}")
                        load_bcast(G2[which][:, :], which, 5, f"G2_{which}")
                    for ti in tiles_act:
                        which = 1 if ti < NCT else 0
                        i2 = ti % 2
                        L(lambda e: e.dma_start(out=xt[i2][:, :], in_=xall[ti * 128:(ti + 1) * 128, :]), w=[f"xtE{i2}"])
                        A(lambda e: e.activation(out=junk[:, :], in_=xt[i2][:, :], func=AF.Square, accum_out=st1[:, 0:1]), r=[f"xtE{i2}"], w=["junkE", "st1E"])
                        rstd_from_ss(st1[:, 0:1], st1[:, 1:2], 1.0 / D, ["st1E"], ["st1Eb"])
                        V(lambda e: e.scalar_tensor_tensor(out=hf[:, :], in0=xt[i2][:, :], scalar=st1[:, 1:2], in1=Ar[which][:, :], op0=ALU.mult, op1=ALU.mult),
                          r=[f"xtE{i2}", "st1Eb", f"Ar2_{which}"], w=["hfE"])
                        V(lambda e: e.tensor_tensor(out=hb[i2][:, :], in0=hf[:, :], in1=Br[which][:, :], op=ALU.add), r=["hfE", f"Br2_{which}"], w=[f"hbE{i2}"])
                        S(lambda e: e.dma_start(out=h2d[ti * 128:(ti + 1) * 128, :], in_=hb[i2][:, :]), r=[f"hbE{i2}"])
                        for kc in range(8):
                            M(lambda e: e.transpose(out=pT[:, kc * 128:(kc + 1) * 128], in_=hb[i2][:, kc * 128:(kc + 1) * 128], identity=ident[:, :]), r=[f"hbE{i2}", "ident"], w=["pTE"])
                        A(lambda e: e.copy(out=hT[:, :, :].rearrange("p k t -> p (k t)"), in_=pT[:, :]), r=["pTE"], w=["hTE"])
                        for kc in range(8):
                            M(lambda e: e.matmul(pR[:, 0:NE], lhsT=hT[:, kc, :], rhs=wr_b[:, kc, :], start=(kc == 0), stop=(kc == 7)), r=["hTE", "wr_b"], w=["pR"])
                        V(lambda e: e.tensor_reduce(out=st1[:, 2:3], in_=pR[:, 0:NE], axis=AX.X, op=ALU.max, negate=True), r=["pR"], w=["st1Ec"])
                        A(lambda e: e.activation(out=ex[:, :], in_=pR[:, 0:NE], func=AF.Exp, bias=st1[:, 2:3], accum_out=st1[:, 3:4]), r=["pR", "st1Ec"], w=["ex", "st1Ed"])
                        V(lambda e: e.reciprocal(out=st1[:, 4:5], in_=st1[:, 3:4]), r=["st1Ed"], w=["st1Ee"])
                        V(lambda e: e.tensor_scalar(out=aff[:, ti, :], in0=ex[:, :], scalar1=st1[:, 4:5], scalar2=None, op0=ALU.mult), r=["ex", "st1Ee"], w=["aff"])
                        M(lambda e: e.transpose(out=pAT[0:NE, 0:128], in_=aff[:, ti, :], identity=identf[:, :]), r=["aff", "identf"], w=["pAT"])
                        A(lambda e: e.copy(out=affT[:, ti * 128:(ti + 1) * 128], in_=pAT[0:NE, 0:128]), r=["pAT"], w=["affT"])
                P.barrier()
                with ExitStack() as p2:
                    work = tsb(p2, "work", [NE, CTX], F32)
                    mx = tsb(p2, "mx", [NE, 8], F32)
                    if not last:
                        V(lambda e: e.tensor_copy(out=work[:, :], in_=affT[:, 0:CTX]), r=["affT"], w=["work"])
                        for r_ in range(CAP_C // 8):
                            V(lambda e: e.max(out=mx[:, :], in_=work[:, :]), r=["work"], w=["mx"])
                            if r_ < CAP_C // 8 - 1:
                                V(lambda e: e.match_replace(out=work[:, :], in_to_replace=mx[:, :], in_values=work[:, :], imm_value=-1.0), r=["work", "mx"], w=["work"])
                        V(lambda e: e.tensor_scalar(out=maskT[:, 0:CTX], in0=affT[:, 0:CTX], scalar1=mx[:, 7:8], scalar2=None, op0=ALU.is_ge), r=["affT", "mx"], w=["maskT"])
                    A2 = tsb(p2, "A2", [128, 512], F32)
                    jk = tsb(p2, "jk2", [128, 512], F32)
                    mk2 = tsb(p2, "mk2", [128, 512], BF16)
                    Bsel = tsb(p2, "Bsel", [NE, 128], F32)
                    Gm = tsb(p2, "Gm", [128, 128], F32)
                    bs = tsb(p2, "bs", [128, 8], F32)
                    pG2 = tps(p2, "pGm", [128, 512])
                    pC2 = tps(p2, "pCnt", [128, 512])
                    S(lambda e: e.dma_start(out=aT_d, in_=affT[:, CTX:T]), r=["affT"], w=["aT_d"])
                    L(lambda e: e.dma_start(out=A2[:, :], in_=aT_d.rearrange("e (c t) -> (e c) t", c=8)), r=["aT_d"], w=["A2"])
                    G(lambda e: e.affine_select(out=Bsel[:, :], in_=onesf[0:NE, :], pattern=[[1, 128]], base=0, channel_multiplier=-8, compare_op=ALU.is_ge, fill=0.0), r=["onesf"], w=["Bsel"])
                    G(lambda e: e.affine_select(out=Bsel[:, :], in_=Bsel[:, :], pattern=[[-1, 128]], base=7, channel_multiplier=8, compare_op=ALU.is_ge, fill=0.0), r=["Bsel"], w=["Bsel"])
                    M(lambda e: e.matmul(pG2[:, 0:128], lhsT=Bsel[:, :], rhs=Bsel[:, :], start=True, stop=True), r=["Bsel"], w=["pGm"])
                    V(lambda e: e.tensor_copy(out=Gm[:, :], in_=pG2[:, 0:128]), r=["pGm"], w=["Gm"])
                    V(lambda e: e.memset(bs[:, 0:1], 0.0), w=["bs"])
                    V(lambda e: e.memset(bs[:, 1:2], 1.0), w=["bs"])
                    for it in range(32):
                        V(lambda e: e.tensor_scalar(out=bs[:, 2:3], in0=bs[:, 0:1], scalar1=bs[:, 1:2], scalar2=0.5, op0=ALU.add, op1=ALU.mult), r=["bs"], w=["bs"])
                        V(lambda e: e.tensor_scalar(out=jk[:, :], in0=A2[:, :], scalar1=bs[:, 2:3], scalar2=0.0, op0=ALU.is_ge, op1=ALU.add, accum_out=bs[:, 3:4]), r=["A2", "bs"], w=["jk2", "bs"])
                        M(lambda e: e.matmul(pC2[:, 0:1], lhsT=Gm[:, :], rhs=bs[:, 3:4], start=True, stop=True), r=["Gm", "bs"], w=["pCnt"])
                        V(lambda e: e.tensor_scalar(out=bs[:, 4:5], in0=pC2[:, 0:1], scalar1=float(CAP_L) - 0.5, scalar2=None, op0=ALU.is_ge), r=["pCnt"], w=["bs"])
                        V(lambda e: e.tensor_tensor(out=bs[:, 5:6], in0=bs[:, 4:5], in1=bs[:, 2:3], op=ALU.mult), r=["bs"], w=["bs"])
                        V(lambda e: e.tensor_tensor(out=bs[:, 0:1], in0=bs[:, 0:1], in1=bs[:, 5:6], op=ALU.max), r=["bs"], w=["bs"])
                        V(lambda e: e.scalar_tensor_tensor(out=bs[:, 6:7], in0=bs[:, 4:5], scalar=2.0, in1=bs[:, 2:3], op0=ALU.mult, op1=ALU.add), r=["bs"], w=["bs"])
                        V(lambda e: e.tensor_tensor(out=bs[:, 1:2], in0=bs[:, 1:2], in1=bs[:, 6:7], op=ALU.min), r=["bs"], w=["bs"])
                    V(lambda e: e.tensor_scalar(out=mk2[:, :], in0=A2[:, :], scalar1=bs[:, 0:1], scalar2=None, op0=ALU.is_ge), r=["A2", "bs"], w=["mk2"])
                    S(lambda e: e.dma_start(out=mk_d.rearrange("e (c t) -> (e c) t", c=8), in_=mk2[:, :]), r=["mk2"], w=["mk_d"])
                    L(lambda e: e.dma_start(out=maskT[:, CTX:T], in_=mk_d), r=["mk_d"], w=["maskT"])
                P.barrier()
                with ExitStack() as p3:
                    mk = tsb(p3, "mk", [128, NE], F32)
                    mkb = tsb(p3, "mkb", [128, NE], BF16)
                    Sf = tsb(p3, "Sf", [128, NE], F32)
                    Sb = tsb(p3, "Sb", [128, NE], BF16)
                    sl = tsb(p3, "sl", [128, NE], F32)
                    hb = [tsb(p3, f"hbS{i}", [128, XW], BF16) for i in range(2)]
                    tok_i = tsb(p3, "tok_i", [128, 2, NT], I32)
                    tokb = tsb(p3, "tokb", [128, NT, 2], BF16)
                    ghi = tsb(p3, "ghi", [128, NE], BF16)
                    zt = tsb(p3, "zt", [128, 2 * D], F32)
                    for hh in range(2):
                        G(lambda e: e.iota(tok_i[hh * 64:(hh + 1) * 64, 0, :], pattern=[[2, NT]], base=hh, channel_multiplier=0), w=["tok_i"])
                        G(lambda e: e.iota(tok_i[hh * 64:(hh + 1) * 64, 1, :], pattern=[[0, NT]], base=0, channel_multiplier=1), w=["tok_i"])
                    V(lambda e: e.tensor_copy(out=tokb[:, :, :].rearrange("p t j -> p j t"), in_=tok_i[:, :, :]), r=["tok_i"], w=["tokb"])
                    V(lambda e: e.memset(zt[:, :], 0.0), w=["zt"])
                    for i in range(2):
                        V(lambda e: e.memset(hb[i][:, D:XW], 0.0), w=[f"hbS{i}"])
                    for ti in range(0, NT, 2):
                        L(lambda e: e.dma_start(out=accd[ti * 128:(ti + 2) * 128, :].rearrange("(j p) c -> p j c", p=128), in_=zt[:, :].rearrange("p (j c) -> p j c", j=2)), r=["zt"])
                    pM = tps(p3, "pM", [128, 1024], BF16)
                    pPos = tps(p3, "pPos", [128, 512])
                    for ti in tiles_act:
                        isc = ti < NCT
                        i2 = ti % 2
                        if ti == 0 or ti == NCT:
                            V(lambda e: e.memset(Sf[:, :], 0.0), w=["Sf"])
                            V(lambda e: e.tensor_copy(out=Sb[:, :], in_=Sf[:, :]), r=["Sf"], w=["Sb"])
                        L(lambda e: e.dma_start(out=hb[i2][:, 0:D], in_=h2d[ti * 128:(ti + 1) * 128, :]), w=[f"hbS{i2}"])
                        M(lambda e: e.transpose(out=pM[:, 0:NE], in_=maskT[:, ti * 128:(ti + 1) * 128], identity=ident[0:NE, 0:NE]), r=["maskT", "ident"], w=["pM"])
                        V(lambda e: e.tensor_copy(out=mk[:, :], in_=pM[:, 0:NE]), r=["pM"], w=["mk"])
                        A(lambda e: e.copy(out=mkb[:, :], in_=pM[:, 0:NE]), r=["pM"], w=["mkb"])
                        M(lambda e: e.matmul(pPos[:, 0:NE], lhsT=Ub[:, :], rhs=mkb[:, :], start=True, stop=False), r=["Ub", "mkb"], w=["pPos"])
                        M(lambda e: e.matmul(pPos[:, 0:NE], lhsT=ones_b[:, :], rhs=Sb[:, :], start=False, stop=True), r=["ones_b", "Sb"], w=["pPos"])
                        V(lambda e: e.tensor_scalar(out=sl[:, :], in0=mk[:, :], scalar1=-BIG, scalar2=BIG, op0=ALU.mult, op1=ALU.add), r=["mk"], w=["sl"])
                        V(lambda e: e.tensor_tensor(out=sl[:, :], in0=pPos[:, 0:NE], in1=sl[:, :], op=ALU.add), r=["pPos", "sl"], w=["sl"])
                        V(lambda e: e.tensor_copy(out=sloti[:, ti, :], in_=sl[:, :]), r=["sl"], w=["sloti"])
                        V(lambda e: e.tensor_scalar(out=sl[:, :], in0=sl[:, :], scalar1=float((CAP_C if isc else CAP_L) - 1), scalar2=None, op0=ALU.min), r=["sl"], w=["sl"])
                        V(lambda e: e.tensor_copy(out=slotc[:, ti, :], in_=sl[:, :]), r=["sl"], w=["slotc"])
                        V(lambda e: e.tensor_tensor(out=gm[:, ti, :], in0=aff[:, ti, :], in1=mk[:, :], op=ALU.mult), r=["aff", "mk"], w=["gm"])
                        V(lambda e: e.tensor_tensor(out=Sf[:, :], in0=Sf[:, :], in1=mk[:, :], op=ALU.add), r=["Sf", "mk"], w=["Sf"])
                        V(lambda e: e.tensor_copy(out=Sb[:, :], in_=Sf[:, :]), r=["Sf"], w=["Sb"])
                        V(lambda e: e.tensor_copy(out=hb[i2][:, D:D + 2], in_=tokb[:, ti, :]), r=["tokb"], w=[f"hbS{i2}"])
                        V(lambda e: e.tensor_copy(out=ghi[:, :], in_=gm[:, ti, :]), r=["gm"], w=["ghi"])
                        V(lambda e: e.tensor_copy(out=hb[i2][:, D + 2:D + 18], in_=ghi[:, :]), r=["ghi"], w=[f"hbS{i2}"])
                        V(lambda e: e.tensor_tensor(out=hb[i2][:, D + 18:D + 34], in0=gm[:, ti, :], in1=ghi[:, :], op=ALU.subtract), r=["gm", "ghi"], w=[f"hbS{i2}"])
                        for ex_ in range(NE):
                            dst = xe_c[ex_] if isc else xe_l[ex_]
                            bnd = (CAP_C if isc else CAP_L) - 1
                            S(lambda e: e.indirect_dma_start(out=dst, out_offset=bass.IndirectOffsetOnAxis(ap=sloti[:, ti, ex_:ex_ + 1], axis=0),
                                                             in_=hb[i2][:, :], in_offset=None, bounds_check=REG[bnd], oob_is_err=False),
                              r=[f"hbS{i2}", "sloti"])
                    if "dbg_aff" in dbg:
                        S(lambda e: e.dma_start(out=dbg_aff, in_=aff[:, :, :].rearrange("p t e -> p (t e)")), r=["aff"])
                        S(lambda e: e.dma_start(out=dbg_slot, in_=sloti[:, :, :].rearrange("p t e -> p (t e)")), r=["sloti"])
                P.barrier()
                p13.close()
                if stop_after == ("E3", l):
                    break
                with ExitStack() as p4:
                    NJ = CAP_L + (CAP_C if not last else 0)
                    stg = [tsb(p4, f"stg{i}", [128, 8, D], F32) for i in range(2)]
                    wb = [tsb(p4, f"wb{i}", [128, 8, D], BF16) for i in range(4)]
                    xet = [tsb(p4, f"xet{i}", [128, XW], BF16) for i in range(2)]
                    sideb = [tsb(p4, f"sideb{i}", [128, 5, 36], BF16) for i in range(2)]
                    gatef = [tsb(p4, f"gatef{i}", [128, 5], F32) for i in range(2)]
                    idxf = [tsb(p4, f"idxf{i}", [128, 5], F32) for i in range(2)]
                    idxi = [tsb(p4, f"idxi{i}", [128, 5], I32) for i in range(2)]
                    xeT = [tsb(p4, f"xeT{i}", [128, 8, CAP_L + CAP_C], BF16) for i in range(2)]
                    hidT = tsb(p4, "hidT", [128, 8, CAP_L + CAP_C], BF16)
                    sg = [tsb(p4, f"sg{i}", [128, CAP_L + CAP_C], F32) for i in range(2)]
                    yeb = [tsb(p4, f"yeb{i}", [128, D], BF16) for i in range(2)]
                    pT = tps(p4, "pT4", [128, 1024], BF16)
                    pG = tps(p4, "pG", [128, 512]); pU = tps(p4, "pU", [128, 512])
                    pGc = tps(p4, "pGc", [128, 512])
                    pUc = tps(p4, "pUc", [128, 512])
                    pY = [tps(p4, f"pY4_{i}", [128, 512]) for i in range(2)]
                    cnt4 = {"w": 0, "s": 0, "x": 0, "g": 0, "y": 0}

                    def load_w(src):
                        si = cnt4["s"] % 2; cnt4["s"] += 1
                        wi = cnt4["w"] % 4; cnt4["w"] += 1
                        for h in range(4):
                            L(lambda e: e.dma_start(out=stg[si][:, 2 * h:2 * h + 2, :], in_=src[h * 256:(h + 1) * 256, :].rearrange("(k p) n -> p k n", p=128)), w=[f"stg{si}h{h}"])
                        V(lambda e: e.tensor_copy(out=wb[wi][:, 0:2, :], in_=stg[si][:, 0:2, :]), r=[f"stg{si}h0"], w=[f"wb{wi}a"])
                        A(lambda e: e.copy(out=wb[wi][:, 2:4, :], in_=stg[si][:, 2:4, :]), r=[f"stg{si}h1"], w=[f"wb{wi}b"])
                        V(lambda e: e.tensor_copy(out=wb[wi][:, 4:5, :], in_=stg[si][:, 4:5, :]), r=[f"stg{si}h2"], w=[f"wb{wi}c"])
                        A(lambda e: e.copy(out=wb[wi][:, 5:6, :], in_=stg[si][:, 5:6, :]), r=[f"stg{si}h2"], w=[f"wb{wi}d"])
                        G(lambda e: e.tensor_copy(out=wb[wi][:, 6:8, :], in_=stg[si][:, 6:8, :]), r=[f"stg{si}h3"], w=[f"wb{wi}e"])
                        return wb[wi], [f"wb{wi}{c}" for c in "abcde"]

                    def stage_x(ex_):
                        xT = xeT[ex_ % 2]; xk = f"xeT{ex_ % 2}"
                        jts = [(xe_l[ex_][j * 128:(j + 1) * 128, :], 128, j * 128) for j in range(4)]
                        if not last:
                            jts.append((xe_c[ex_][:, :], CAP_C, CAP_L))
                        for jt, (src, nj, c0) in enumerate(jts):
                            xi = cnt4["x"] % 2; cnt4["x"] += 1
                            L(lambda e: e.dma_start(out=xet[xi][0:nj, :], in_=src), w=[f"xet{xi}"])
                            e2 = ex_ % 2
                            V(lambda e: e.tensor_copy(out=sideb[e2][0:nj, jt, :], in_=xet[xi][0:nj, D:XW]), r=[f"xet{xi}"], w=[f"sideb{e2}"])
                            V(lambda e: e.tensor_tensor(out=gatef[e2][0:nj, jt:jt + 1], in0=sideb[e2][0:nj, jt, 2 + ex_:3 + ex_], in1=sideb[e2][0:nj, jt, 18 + ex_:19 + ex_], op=ALU.add),
                              r=[f"sideb{e2}"], w=[f"gatef{e2}"])
                            V(lambda e: e.scalar_tensor_tensor(out=idxf[e2][0:nj, jt:jt + 1], in0=sideb[e2][0:nj, jt, 0:1], scalar=64.0, in1=sideb[e2][0:nj, jt, 1:2], op0=ALU.mult, op1=ALU.add),
                              r=[f"sideb{e2}"], w=[f"idxf{e2}"])
                            V(lambda e: e.tensor_copy(out=idxi[e2][0:nj, jt:jt + 1], in_=idxf[e2][0:nj, jt:jt + 1]), r=[f"idxf{e2}"], w=[f"idxi{e2}"])
                            for kc in range(8):
                                M(lambda e: e.transpose(out=pT[:, kc * 128:kc * 128 + nj], in_=xet[xi][0:nj, kc * 128:(kc + 1) * 128], identity=ident[0:nj, 0:nj]),
                                  r=[f"xet{xi}", "ident"], w=["pT4"])
                            V(lambda e: e.tensor_copy(out=xT[:, :, c0:c0 + nj], in_=pT[:, :].rearrange("p (k t) -> p k t", k=8)[:, :, 0:nj]), r=["pT4"], w=[xk])

                    def stage_hidden(ex_, wg, wgk, wu, wuk):
                        xT = xeT[ex_ % 2]; xk = f"xeT{ex_ % 2}"
                        for fc in range(8):
                            for kc in range(8):
                                M(lambda e: e.matmul(pG[:, :], lhsT=wg[:, kc, fc * 128:(fc + 1) * 128], rhs=xT[:, kc, 0:CAP_L], start=(kc == 0), stop=(kc == 7)), r=wgk + [xk], w=["pG"])
                            for kc in range(8):
                                M(lambda e: e.matmul(pU[:, :], lhsT=wu[:, kc, fc * 128:(fc + 1) * 128], rhs=xT[:, kc, 0:CAP_L], start=(kc == 0), stop=(kc == 7)), r=wuk + [xk], w=["pU"])
                            gi = cnt4["g"] % 2; cnt4["g"] += 1
                            A(lambda e: e.activation(out=sg[gi][:, 0:CAP_L], in_=pG[:, :], func=AF.Silu), r=["pG"], w=[f"sg{gi}"])
                            V(lambda e: e.tensor_tensor(out=hidT[:, fc, 0:CAP_L], in0=pU[:, :], in1=sg[gi][:, 0:CAP_L], op=ALU.mult), r=["pU", f"sg{gi}"], w=["hidT"])
                            if not last:
                                for kc in range(8):
                                    M(lambda e: e.matmul(pGc[:, 0:CAP_C], lhsT=wg[:, kc, fc * 128:(fc + 1) * 128], rhs=xT[:, kc, CAP_L:NJ], start=(kc == 0), stop=(kc == 7)), r=wgk + [xk], w=["pGc0"])
                                for kc in range(8):
                                    M(lambda e: e.matmul(pUc[:, 0:CAP_C], lhsT=wu[:, kc, fc * 128:(fc + 1) * 128], rhs=xT[:, kc, CAP_L:NJ], start=(kc == 0), stop=(kc == 7)), r=wuk + [xk], w=["pGc1"])
                                A(lambda e: e.activation(out=sg[gi][:, CAP_L:NJ], in_=pGc[:, 0:CAP_C], func=AF.Silu), r=["pGc0"], w=[f"sg{gi}c"])
                                V(lambda e: e.tensor_tensor(out=hidT[:, fc, CAP_L:NJ], in0=pUc[:, 0:CAP_C], in1=sg[gi][:, CAP_L:NJ], op=ALU.mult), r=["pGc1", f"sg{gi}c"], w=["hidTc"])

                    def stage_down(ex_, wd, wdk):
                        outs = [(None, 128, j * 128) for j in range(4)]
                        if not last:
                            outs.append((None, CAP_C, CAP_L))
                        e2 = ex_ % 2
                        for jt, (dst, nj, c0) in enumerate(outs):
                            gate_ap = gatef[e2][0:nj, jt:jt + 1]
                            idx_ap = idxi[e2][0:nj, jt:jt + 1]
                            yi = cnt4["y"] % 2; cnt4["y"] += 1
                            for dh in range(2):
                                for fc in range(8):
                                    M(lambda e: e.matmul(pY[dh][0:nj, :], lhsT=hidT[:, fc, c0:c0 + nj], rhs=wd[:, fc, dh * 512:(dh + 1) * 512], start=(fc == 0), stop=(fc == 7)),
                                      r=["hidT", "hidTc"] + wdk, w=[f"pY4_{dh}"])
                            A(lambda e: e.activation(out=yeb[yi][0:nj, 0:512], in_=pY[0][0:nj, :], func=AF.Identity, scale=gate_ap), r=["pY4_0", f"gatef{e2}"], w=[f"yeb{yi}a"])
                            V(lambda e: e.tensor_scalar(out=yeb[yi][0:nj, 512:1024], in0=pY[1][0:nj, :], scalar1=gate_ap, scalar2=None, op0=ALU.mult), r=["pY4_1", f"gatef{e2}"], w=[f"yeb{yi}b"])
                            S(lambda e: e.indirect_dma_start(out=accd[:, :], out_offset=bass.IndirectOffsetOnAxis(ap=idx_ap, axis=0),
                                                             in_=yeb[yi][0:nj, :], in_offset=None, compute_op=ALU.add),
                              r=[f"yeb{yi}a", f"yeb{yi}b", f"idxi{e2}"], w=["accd"])

                    wg, wgk = load_w(w_e_gate[l, 0])
                    wu, wuk = load_w(w_e_up[l, 0])
                    stage_x(0)
                    for ex_ in range(NE):
                        wd, wdk = load_w(w_e_down[l, ex_])
                        stage_hidden(ex_, wg, wgk, wu, wuk)
                        if ex_ + 1 < NE:
                            wg, wgk = load_w(w_e_gate[l, ex_ + 1])
                            wu, wuk = load_w(w_e_up[l, ex_ + 1])
                            stage_x(ex_ + 1)
                        stage_down(ex_, wd, wdk)
                P.barrier()
                if stop_after == ("E4", l):
                    break
                with ExitStack() as p5:
                    xt = [tsb(p5, f"xt5_{i}", [128, D], F32) for i in range(3)]
                    acc = [tsb(p5, f"acc{i}", [128, D], F32) for i in range(3)]
                    for n_, ti in enumerate(tiles_act):
                        which = 1 if ti < NCT else 0
                        i2 = n_ % 3
                        L(lambda e: e.dma_start(out=xt[i2][:, :], in_=xall[ti * 128:(ti + 1) * 128, :]), w=[f"xt5_{i2}"])
                        L(lambda e: e.dma_start(out=acc[i2][:, :], in_=accd[ti * 128:(ti + 1) * 128, :]), w=[f"acc{i2}"])
                        V(lambda e: e.tensor_tensor(out=acc[i2][:, :], in0=acc[i2][:, :], in1=G2[which][:, :], op=ALU.mult), r=[f"acc{i2}", f"G2_{which}"], w=[f"acc{i2}"])
                        V(lambda e: e.tensor_tensor(out=acc[i2][:, :], in0=acc[i2][:, :], in1=xt[i2][:, :], op=ALU.add), r=[f"acc{i2}", f"xt5_{i2}"], w=[f"acc{i2}"])
                        if last:
                            S(lambda e: e.dma_start(out=y_out[(ti - NCT) * 128:(ti - NCT + 1) * 128, :], in_=acc[i2][:, :]), r=[f"acc{i2}"])
                        else:
                            S(lambda e: e.dma_start(out=xall[ti * 128:(ti + 1) * 128, :], in_=acc[i2][:, :]), r=[f"acc{i2}"])
            P.barrier()
            if stop_after == ("L", l):
                break
        P.barrier()
        print("instr counts", P.ninstr, "total", P.total, flush=True)
    return nc


def _rope_table():
    t = np.arange(SEQ)
    row = (t // 64).astype(np.float64)
    col = (t % 64).astype(np.float64)
    out = np.zeros((SEQ, 192), np.float64)

    def fill(off, dim):
        ad = dim // 2
        inv = 10000.0 ** (-np.arange(0, ad, 2, dtype=np.float64) / ad)
        ar = row[:, None] * inv[None, :]
        ac = col[:, None] * inv[None, :]
        C = np.concatenate([np.cos(ar), np.cos(ar), np.cos(ac), np.cos(ac)], axis=1)
        S_ = np.concatenate([-np.sin(ar), np.sin(ar), -np.sin(ac), np.sin(ac)], axis=1)
        out[:, off:off + dim] = C
        out[:, off + dim:off + 2 * dim] = S_
    fill(0, 64)
    fill(128, 32)
    return out.astype(np.float32)


_NC_CACHE = {}


def kernel(**inputs):
    key = "full"
    if key not in _NC_CACHE:
        _NC_CACHE[key] = build()
    nc = _NC_CACHE[key]
    rope = _rope_table()
    shared = {k: np.ascontiguousarray(np.asarray(v, dtype=np.float32)) for k, v in inputs.items() if k not in ("x", "c", "ctx")}
    x = np.asarray(inputs["x"], dtype=np.float32); c = np.asarray(inputs["c"], dtype=np.float32); ctx = np.asarray(inputs["ctx"], dtype=np.float32)
    in_maps = []
    for b in range(8):
        m = dict(shared)
        m["x"] = np.ascontiguousarray(x[b]); m["ctx"] = np.ascontiguousarray(ctx[b]); m["c"] = np.ascontiguousarray(c[b])
        m["rope"] = rope
        in_maps.append(m)
    res = run_bass_kernel_spmd(nc, in_maps, core_ids=list(range(8)))
    return np.stack([np.asarray(r["y"], dtype=np.float32) for r in res.results], axis=0)
```

```python
import numpy as np
from contextlib import ExitStack
import threading
import concourse.bass as bass
import concourse.mybir as mybir
from concourse.bass_utils import run_bass_kernel_spmd

F32 = mybir.dt.float32
BF16 = mybir.dt.bfloat16
I32 = mybir.dt.int32
AF = mybir.ActivationFunctionType
ALU = mybir.AluOpType
AX = mybir.AxisListType

D = 1024
SEQ = 4096
CTX = 256
T = SEQ + CTX
NT = T // 128
NCT = CTX // 128
DEPTH = 2
INW = 1504
NE = 16
CAP_L = 512
CAP_C = 32
XW = D + 36
EPS = 1e-6
BIG = 8192.0


def _is_psum(k):
    return k.startswith("pmod") or (len(k) > 1 and k[0] == "p" and k[1].isupper())


class Prog:
    ENG = ("pe", "dve", "act", "pool", "sp")

    def __init__(self, nc, stack, same_engine_sync=True, ring=(("sp", 16), ("pool", 16))):
        self.nc = nc
        self.stack = stack
        self.e = {"pe": nc.tensor, "dve": nc.vector, "act": nc.scalar, "pool": nc.gpsimd, "sp": nc.sync}
        self.same = same_engine_sync
        self.sems = {}
        self.semval = {}
        self.cur = {}
        self.epoch = {n: 0 for n in self.ENG}
        for n in self.ENG:
            self.cur[n] = self._mk("E_" + n + "_0")
        self.ring = {}
        self.ringpos = {}
        for q, k in ring:
            self.ring[q] = [self._mk(f"D_{q}{i}") for i in range(k)]
            self.ringpos[q] = 0
        self.waited = {n: {} for n in self.ENG}
        self.lastw = {}
        self.readers = {}
        self.ninstr = {n: 0 for n in self.ENG}

    def _mk(self, name):
        s = self.stack.enter_context(self.nc.semaphore(name))
        self.sems[name] = s
        self.semval[name] = 0
        return name

    def _wait(self, eng, ev):
        sem, val = ev
        if val <= 0 or self.waited[eng].get(sem, 0) >= val:
            return
        if sem == self.cur[eng] and (eng == "pe" or not self.same):
            return
        self.e[eng].wait_ge(self.sems[sem], val)
        self.waited[eng][sem] = val

    def _deps(self, eng, r, w):
        for res in list(r) + list(w):
            ev = self.lastw.get(res)
            if ev is not None:
                self._wait(eng, ev)
        for res in w:
            for sem, val in self.readers.get(res, {}).items():
                self._wait(eng, (sem, val))

    def _record(self, ev, r, w):
        for res in w:
            self.lastw[res] = ev
            self.readers[res] = {}
        for res in r:
            d = self.readers.setdefault(res, {})
            if d.get(ev[0], 0) < ev[1]:
                d[ev[0]] = ev[1]

    LIMIT = None
    total = 0

    def op(self, eng, fn, r=(), w=()):
        self.total += 1
        if self.LIMIT is not None and self.total > self.LIMIT:
            return
        w = list(w) + [k for k in r if _is_psum(k)]
        r = [k for k in r if not _is_psum(k)]
        self._deps(eng, r, w)
        ins = fn(self.e[eng])
        sem = self.cur[eng]
        self.semval[sem] += 1
        ins.then_inc(self.sems[sem], 1)
        self.ninstr[eng] += 1
        self._record((sem, self.semval[sem]), r, w)

    def dma(self, q, fn, r=(), w=()):
        self.total += 1
        if self.LIMIT is not None and self.total > self.LIMIT:
            return
        self._deps(q, r, w)
        k = self.ringpos[q]
        sem = self.ring[q][k]
        self.ringpos[q] = (k + 1) % len(self.ring[q])
        self._wait(q, (sem, self.semval[sem]))
        ins = fn(self.e[q])
        self.semval[sem] += 16
        ins.then_inc(self.sems[sem], 16)
        self.ninstr[q] += 1
        self._record((sem, self.semval[sem]), r, w)

    def barrier(self):
        for eng in self.ENG:
            for sem, val in self.semval.items():
                self._wait(eng, (sem, val))
        self.lastw.clear()
        self.readers.clear()
        for eng in self.ENG:
            if self.semval[self.cur[eng]] > 15000:
                self.epoch[eng] += 1
                self.cur[eng] = self._mk(f"E_{eng}_{self.epoch[eng]}")


def build(stop_after=None, dbg=(), limit=None, light=False):
    nc = bass.Bass("TRN2", target_bir_lowering=False)
    Prog.LIMIT = limit

    def din(name, shape, dt=F32):
        return nc.dram_tensor(name, list(shape), dt, kind="ExternalInput").ap()

    x_in = din("x", [SEQ, D]); ctx_in = din("ctx", [CTX, D]); c_in = din("c", [D]); cctx_in = din("c_ctx", [D])
    w_ada = din("w_ada", [DEPTH, D, 6 * D]); b_ada = din("b_ada", [DEPTH, 6 * D])
    norm1_g = din("norm1_g", [DEPTH, D]); norm2_g = din("norm2_g", [DEPTH, D])
    w_in = din("w_in", [DEPTH, D, INW])
    a_q_norm = din("a_q_norm", [DEPTH, 64]); a_k_norm = din("a_k_norm", [DEPTH, 64])
    b_cq_norm = din("b_cq_norm", [DEPTH, 192]); b_ckv_norm = din("b_ckv_norm", [DEPTH, 128])
    w_uq = din("w_uq", [DEPTH, 192, 576]); w_ukv = din("w_ukv", [DEPTH, 128, 768])
    b_qn_norm = din("b_qn_norm", [DEPTH, 64]); b_kn_norm = din("b_kn_norm", [DEPTH, 64])
    b_qr_norm = din("b_qr_norm", [DEPTH, 32]); b_kr_norm = din("b_kr_norm", [DEPTH, 32])
    c_q_norm = din("c_q_norm", [DEPTH, 64]); c_k_norm = din("c_k_norm", [DEPTH, 64])
    c_sink = din("c_sink", [DEPTH, 4])
    w_out = din("w_out", [DEPTH, D, D]); w_router = din("w_router", [DEPTH, D, NE])
    if not light:
        w_e_gate = din("w_e_gate", [DEPTH, NE, D, D]); w_e_up = din("w_e_up", [DEPTH, NE, D, D])
        w_e_down = din("w_e_down", [DEPTH, NE, D, D])
    rope = din("rope", [SEQ, 192])
    y_out = nc.dram_tensor("y", [SEQ, D], F32, kind="ExternalOutput").ap()

    def dscr(name, shape, dt):
        kind = "ExternalOutput" if name in dbg else "Internal"
        return nc.dram_tensor(name, list(shape), dt, kind=kind).ap()

    xall = dscr("xall", [T, D], F32)
    modv = dscr("modv", [2, 6 * D], F32)
    qkT_A = dscr("qkT_A", [512, T], BF16)
    qkT_B = dscr("qkT_B", [12, 96, T], BF16)
    qkT_C = dscr("qkT_C", [384, T], BF16)
    vall = dscr("vall", [T, 640], BF16)
    mix = dscr("mix", [T, D], BF16)
    h2d = dscr("h2d", [T, D], BF16)
    xe_l = [dscr(f"xe_l{i}", [CAP_L, XW], BF16) for i in range(NE)]
    xe_c = [dscr(f"xe_c{i}", [CAP_C, XW], BF16) for i in range(NE)]
    accd = dscr("accd", [T, D], F32)
    aT_d = dscr("aT_d", [NE, SEQ], F32)
    mk_d = dscr("mk_d", [NE, SEQ], BF16)
    ye_l = [dscr(f"ye_l{i}", [CAP_L, D], BF16) for i in range(NE)]
    ye_c = [dscr(f"ye_c{i}", [CAP_C, D], BF16) for i in range(NE)]
    dbg_aff = dscr("dbg_aff", [128, NT * NE], F32)
    dbg_slot = dscr("dbg_slot", [128, NT * NE], I32)

    with ExitStack() as top:
        P = Prog(nc, top)
        V = lambda fn, r=(), w=(): P.op("dve", fn, r, w)
        A = lambda fn, r=(), w=(): P.op("act", fn, r, w)
        G = lambda fn, r=(), w=(): P.op("pool", fn, r, w)
        M = lambda fn, r=(), w=(): P.op("pe", fn, r, w)
        L = lambda fn, r=(), w=(): P.dma("sp", fn, r, w)
        S = lambda fn, r=(), w=(): P.dma("pool", fn, r, w)

        LAYER = [-1]

        def tsb(stk, name, shape, dt):
            return stk.enter_context(nc.sbuf_tensor(f"{name}_L{LAYER[0]}", list(shape), dt))

        def tps(stk, name, shape, dt=F32):
            return stk.enter_context(nc.psum_tensor(f"{name}_L{LAYER[0]}", list(shape), dt))

        REG = {CAP_L - 1: nc.gpsimd.to_reg(CAP_L - 1), CAP_C - 1: nc.gpsimd.to_reg(CAP_C - 1)}
        identf = tsb(top, "identf", [128, 128], F32)
        ident = tsb(top, "ident", [128, 128], BF16)
        onesf = tsb(top, "onesf", [128, 128], F32)
        ones_b = tsb(top, "ones_b", [128, 128], BF16)
        Ub = tsb(top, "Ub", [128, 128], BF16)
        M1 = tsb(top, "M1", [128, 128], BF16)
        M2 = tsb(top, "M2", [128, 128], BF16)
        ctmp = tsb(top, "ctmp", [128, 128], F32)
        cs = tsb(top, "cs", [128, 8, 2], F32)
        craw = tsb(top, "craw", [128, 2, 8], F32)

        G(lambda e: e.memset(identf[:], 0.0), w=["identf"])
        G(lambda e: e.affine_select(out=identf[:], in_=identf[:], pattern=[[-1, 128]], base=0, channel_multiplier=1,
                                    compare_op=ALU.not_equal, fill=1.0), r=["identf"], w=["identf"])
        V(lambda e: e.tensor_copy(out=ident[:], in_=identf[:]), r=["identf"], w=["ident"])
        G(lambda e: e.memset(onesf[:], 1.0), w=["onesf"])
        V(lambda e: e.tensor_copy(out=ones_b[:], in_=onesf[:]), r=["onesf"], w=["ones_b"])
        G(lambda e: e.affine_select(out=ctmp[:], in_=onesf[:], pattern=[[-1, 128]], base=0, channel_multiplier=1,
                                    compare_op=ALU.is_ge, fill=0.0), r=["onesf"], w=["ctmp"])
        V(lambda e: e.tensor_copy(out=M1[:], in_=ctmp[:]), r=["ctmp"], w=["M1"])
        G(lambda e: e.affine_select(out=ctmp[:], in_=onesf[:], pattern=[[1, 128]], base=0, channel_multiplier=-1,
                                    compare_op=ALU.is_ge, fill=0.0), r=["onesf"], w=["ctmp"])
        V(lambda e: e.tensor_copy(out=M2[:], in_=ctmp[:]), r=["ctmp"], w=["M2"])
        G(lambda e: e.affine_select(out=ctmp[:], in_=onesf[:], pattern=[[1, 128]], base=-1, channel_multiplier=-1,
                                    compare_op=ALU.is_ge, fill=0.0), r=["onesf"], w=["ctmp"])
        V(lambda e: e.tensor_copy(out=Ub[:], in_=ctmp[:]), r=["ctmp"], w=["Ub"])

        L(lambda e: e.dma_start(out=craw[:, 0, :], in_=c_in.rearrange("(k p) -> p k", p=128), allow_slow_non_contiguous=True), w=["craw"])
        L(lambda e: e.dma_start(out=craw[:, 1, :], in_=cctx_in.rearrange("(k p) -> p k", p=128), allow_slow_non_contiguous=True), w=["craw"])
        A(lambda e: e.activation(out=cs[:, :, :].rearrange("p k j -> p j k"), in_=craw[:, :, :], func=AF.Silu), r=["craw"], w=["cs"])

        L(lambda e: e.dma_start(out=xall[0:CTX, :], in_=ctx_in))
        for i in range(8):
            L(lambda e: e.dma_start(out=xall[CTX + i * 512:CTX + (i + 1) * 512, :], in_=x_in[i * 512:(i + 1) * 512, :]))
        P.barrier()

        def rstd_from_ss(ss_ap, out_ap, scale, r, w, Af=None, Vf=None):
            Af = Af or A; Vf = Vf or V
            Af(lambda e: e.activation(out=out_ap, in_=ss_ap, func=AF.Sqrt, scale=scale, bias=EPS), r=r, w=w)
            Vf(lambda e: e.reciprocal(out=out_ap, in_=out_ap), r=w, w=w)

        def interleave(fns):
            n = len(fns)
            cond = threading.Condition()
            st = {"turn": 0, "alive": [True] * n, "err": None}

            def advance():
                for k in range(1, n + 1):
                    j = (st["turn"] + k) % n
                    if st["alive"][j]:
                        st["turn"] = j
                        break
                cond.notify_all()

            def make_yp(i):
                def yp():
                    with cond:
                        advance()
                        while st["turn"] != i:
                            cond.wait()
                return yp

            def runner(i):
                with cond:
                    while st["turn"] != i:
                        cond.wait()
                try:
                    fns[i](make_yp(i))
                except BaseException as ex:
                    st["err"] = ex
                finally:
                    with cond:
                        st["alive"][i] = False
                        if any(st["alive"]):
                            advance()
                        else:
                            st["turn"] = -1
                            cond.notify_all()

            ths = [threading.Thread(target=runner, args=(i,)) for i in range(n)]
            for t_ in ths:
                t_.start()
            for t_ in ths:
                t_.join()
            if st["err"] is not None:
                raise st["err"]

        for l in range(DEPTH):
            last = l == DEPTH - 1
            LAYER[0] = l
            tiles_act = list(range(NT)) if not last else list(range(NCT, NT))

            with ExitStack() as ph:
                wst = [tsb(ph, f"wstA{i}", [128, 8, 512], F32) for i in range(2)]
                modrow = tsb(ph, "modrow", [2, 6 * D], F32)
                bada = tsb(ph, "bada", [2, 6 * D], F32)
                gn = tsb(ph, "gn", [2, 2, D], F32)
                pmod = [tps(ph, f"pmod{i}", [128, 512]) for i in range(2)]
                L(lambda e: e.dma_start(out=bada[:, :], in_=b_ada[l:l + 1, :].broadcast_to([2, 6 * D])), w=["bada"])
                L(lambda e: e.dma_start(out=gn[:, 0, :], in_=norm1_g[l:l + 1, :].broadcast_to([2, D])), w=["gn"])
                L(lambda e: e.dma_start(out=gn[:, 1, :], in_=norm2_g[l:l + 1, :].broadcast_to([2, D])), w=["gn"])
                for nb in range(12):
                    wb = wst[nb % 2]; pm = pmod[nb % 2]
                    for h in range(2):
                        L(lambda e: e.dma_start(out=wb[:, h * 4:(h + 1) * 4, :],
                                                in_=w_ada[l, h * 512:(h + 1) * 512, nb * 512:(nb + 1) * 512].rearrange("(k p) n -> p k n", p=128)),
                          w=[f"wstA{nb % 2}"])
                    for kc in range(8):
                        M(lambda e: e.matmul(pm[0:2, :], lhsT=cs[:, kc, :], rhs=wb[:, kc, :], start=(kc == 0), stop=(kc == 7)),
                          r=[f"wstA{nb % 2}", "cs"], w=[f"pmod{nb % 2}"])
                    V(lambda e: e.tensor_tensor(out=modrow[:, nb * 512:(nb + 1) * 512], in0=pm[0:2, :], in1=bada[:, nb * 512:(nb + 1) * 512], op=ALU.add),
                      r=[f"pmod{nb % 2}", "bada"], w=["modrow"])
                for j, off in ((0, 1 * D), (1, 4 * D)):
                    V(lambda e: e.scalar_tensor_tensor(out=modrow[:, off:off + D], in0=modrow[:, off:off + D], scalar=1.0, in1=gn[:, j, :],
                                                       op0=ALU.add, op1=ALU.mult), r=["modrow", "gn"], w=["modrow"])
                S(lambda e: e.dma_start(out=modv, in_=modrow[:, :]), r=["modrow"])
            P.barrier()
            if stop_after == ("A", l):
                break

            def load_bcast(dst, which, j, key):
                L(lambda e: e.dma_start(out=dst, in_=modv[which:which + 1, j * D:(j + 1) * D].broadcast_to([128, D])), w=[key])

            with ExitStack() as ph:
                wst = tsb(ph, "wstB", [128, 8, 512], F32)
                w_in_b = tsb(ph, "w_in_b", [128, 8, INW], BF16)
                wuq_f = tsb(ph, "wuq_f", [128, 2, 576], F32)
                wuq_b = tsb(ph, "wuq_b", [128, 2, 576], BF16)
                wukv_f = tsb(ph, "wukv_f", [128, 768], F32)
                wukv_b = tsb(ph, "wukv_b", [128, 768], BF16)
                gcq = tsb(ph, "gcq", [128, 2], F32)
                gckv = tsb(ph, "gckv", [128, 1], F32)
                gA = tsb(ph, "gA", [128, 8, 64], F32)
                gC = tsb(ph, "gC", [128, 6, 64], F32)
                gBq = tsb(ph, "gBq", [128, 96], F32)
                gBk = tsb(ph, "gBk", [128, 64], F32)
                gBkr = tsb(ph, "gBkr", [128, 32], F32)
                invB = tsb(ph, "invB", [128, 3], F32)
                invQ = tsb(ph, "invQ", [128, 12], F32)
                Ar = [tsb(ph, f"Ar{i}", [128, D], F32) for i in range(2)]
                Br = [tsb(ph, f"Br{i}", [128, D], F32) for i in range(2)]
                xt = [tsb(ph, f"xtB{i}", [128, D], F32) for i in range(2)]
                rp = [tsb(ph, f"rp{i}", [128, 192], F32) for i in range(2)]
                junk = tsb(ph, "junkB", [128, D], BF16)
                st1 = tsb(ph, "st1", [128, 4], F32)
                hf = tsb(ph, "hf", [128, D], F32)
                hb = tsb(ph, "hb", [128, D], BF16)
                hT = tsb(ph, "hT", [128, 8, 128], BF16)
                sqA = tsb(ph, "sqA", [128, 512], F32); sqC = tsb(ph, "sqC", [128, 384], F32); sqB = tsb(ph, "sqB", [128, 576], F32)
                sshA = tsb(ph, "sshA", [128, 8], F32); sshC = tsb(ph, "sshC", [128, 8], F32); sshB = tsb(ph, "sshB", [128, 16], F32)
                qnA = tsb(ph, "qnA", [128, 512], F32); qnC = tsb(ph, "qnC", [128, 384], F32); qnB = tsb(ph, "qnB", [128, 576], F32)
                t1A = tsb(ph, "t1A", [128, 512], F32); t1C = tsb(ph, "t1C", [128, 384], F32); t1B = tsb(ph, "t1B", [128, 192], F32)
                t2A = tsb(ph, "t2A", [128, 512], F32); t2C = tsb(ph, "t2C", [128, 384], F32); t2B = tsb(ph, "t2B", [128, 192], F32)
                qA = tsb(ph, "qA", [128, 8, 64], BF16)
                qC = tsb(ph, "qC", [128, 6, 64], BF16)
                qB = tsb(ph, "qB", [128, 12, 96], BF16)
                cqn = tsb(ph, "cqn", [128, 320], BF16)
                cT = tsb(ph, "cT", [128, 3, 128], BF16)
                kpe = tsb(ph, "kpe", [128, 32], F32)
                kpe2 = tsb(ph, "kpe2", [128, 32], F32)
                vst = tsb(ph, "vst", [128, 640], BF16)
                stA = tsb(ph, "stA", [128, 4, 512], BF16)
                stB = tsb(ph, "stB", [128, 12, 512], BF16)
                stC = tsb(ph, "stC", [128, 3, 512], BF16)
                pT = tps(ph, "pTB", [128, 1024], BF16)
                pP = [tps(ph, f"pP{i}", [128, 512]) for i in range(3)]
                pQ = [tps(ph, f"pQ{i}", [128, 512]) for i in range(2)]
                pK = [tps(ph, f"pK{i}", [128, 512]) for i in range(2)]

                for b, (c0, c1) in enumerate(((0, 512), (512, 1024), (1024, INW))):
                    n = c1 - c0
                    for h in range(2):
                        L(lambda e: e.dma_start(out=wst[:, h * 4:(h + 1) * 4, 0:n],
                                                in_=w_in[l, h * 512:(h + 1) * 512, c0:c1].rearrange("(k p) n -> p k n", p=128)), w=["wstB"])
                    V(lambda e: e.tensor_copy(out=w_in_b[:, 0:4, c0:c1], in_=wst[:, 0:4, 0:n]), r=["wstB"], w=["w_in_b"])
                    G(lambda e: e.tensor_copy(out=w_in_b[:, 4:8, c0:c1], in_=wst[:, 4:8, 0:n]), r=["wstB"], w=["w_in_b"])
                L(lambda e: e.dma_start(out=wuq_f[:, 0, :], in_=w_uq[l, 0:128, :]), w=["wuq_f"])
                L(lambda e: e.dma_start(out=wuq_f[0:64, 1, :], in_=w_uq[l, 128:192, :]), w=["wuq_f"])
                L(lambda e: e.dma_start(out=wukv_f[:, :], in_=w_ukv[l, :, :]), w=["wukv_f"])
                L(lambda e: e.dma_start(out=gcq[:, 0:1], in_=b_cq_norm[l, 0:128].rearrange("(p o) -> p o", o=1)), w=["gcq"])
                L(lambda e: e.dma_start(out=gcq[0:64, 1:2], in_=b_cq_norm[l, 128:192].rearrange("(p o) -> p o", o=1)), w=["gcq"])
                L(lambda e: e.dma_start(out=gckv[:, 0:1], in_=b_ckv_norm[l, :].rearrange("(p o) -> p o", o=1)), w=["gckv"])
                V(lambda e: e.tensor_scalar(out=wuq_b[:, 0, :], in0=wuq_f[:, 0, :], scalar1=gcq[:, 0:1], scalar2=None, op0=ALU.mult), r=["wuq_f", "gcq"], w=["wuq_b"])
                V(lambda e: e.tensor_scalar(out=wuq_b[0:64, 1, :], in0=wuq_f[0:64, 1, :], scalar1=gcq[0:64, 1:2], scalar2=None, op0=ALU.mult), r=["wuq_f", "gcq"], w=["wuq_b"])
                V(lambda e: e.tensor_scalar(out=wukv_b[:, :], in0=wukv_f[:, :], scalar1=gckv[:, 0:1], scalar2=None, op0=ALU.mult), r=["wukv_f", "gckv"], w=["wukv_b"])
                for hh in range(8):
                    src = a_q_norm if hh < 6 else a_k_norm
                    L(lambda e: e.dma_start(out=gA[:, hh, :], in_=src[l:l + 1, :].broadcast_to([128, 64])), w=["gA"])
                for hh in range(6):
                    src = c_q_norm if hh < 4 else c_k_norm
                    L(lambda e: e.dma_start(out=gC[:, hh, :], in_=src[l:l + 1, :].broadcast_to([128, 64])), w=["gC"])
                L(lambda e: e.dma_start(out=gBq[:, 0:64], in_=b_qn_norm[l:l + 1, :].broadcast_to([128, 64])), w=["gBq"])
                L(lambda e: e.dma_start(out=gBq[:, 64:96], in_=b_qr_norm[l:l + 1, :].broadcast_to([128, 32])), w=["gBq"])
                L(lambda e: e.dma_start(out=gBk[:, :], in_=b_kn_norm[l:l + 1, :].broadcast_to([128, 64])), w=["gBk"])
                L(lambda e: e.dma_start(out=gBkr[:, :], in_=b_kr_norm[l:l + 1, :].broadcast_to([128, 32])), w=["gBkr"])
                for j, v in enumerate((1.0 / 192, 1.0 / 128, 1.0 / 32)):
                    G(lambda e: e.memset(invB[:, j:j + 1], v), w=["invB"])
                G(lambda e: e.memset(invQ[:, 0:6], 1.0 / 64), w=["invQ"])
                G(lambda e: e.memset(invQ[:, 6:12], 1.0 / 32), w=["invQ"])
                for which in range(2):
                    load_bcast(Ar[which][:, :], which, 1, f"Ar{which}")
                    load_bcast(Br[which][:, :], which, 0, f"Br{which}")

                def rope_apply(src_ap, H, Dh, C_ap, S_ap, out_ap, rs, t1b, t2b, sfx, Vf):
                    c4 = Dh // 4
                    t1v = t1b[:, 0:H * Dh].rearrange("p (h d) -> p h d", h=H)
                    t2v = t2b[:, 0:H * Dh].rearrange("p (h a b c) -> p h a b c", h=H, a=2, b=2)
                    s5 = src_ap.rearrange("p h (a b c) -> p h a b c", a=2, b=2)
                    S4 = S_ap.rearrange("p (a b c) -> p a b c", a=2, b=2)
                    Vf(lambda e: e.tensor_tensor(out=t1v, in0=src_ap, in1=C_ap.unsqueeze(1).to_broadcast([128, H, Dh]), op=ALU.mult), r=rs, w=["t1" + sfx])
                    Vf(lambda e: e.tensor_tensor(out=t2v[:, :, :, 0, :], in0=s5[:, :, :, 1, :],
                                                 in1=S4[:, :, 0, :].unsqueeze(1).to_broadcast([128, H, 2, c4]), op=ALU.mult), r=rs, w=["t2" + sfx])
                    Vf(lambda e: e.tensor_tensor(out=t2v[:, :, :, 1, :], in0=s5[:, :, :, 0, :],
                                                 in1=S4[:, :, 1, :].unsqueeze(1).to_broadcast([128, H, 2, c4]), op=ALU.mult), r=rs, w=["t2" + sfx])
                    return t1v, t2b[:, 0:H * Dh].rearrange("p (h d) -> p h d", h=H)

                V0, A0, M0 = V, A, M
                blocks = [list(range(0, NCT))] + [list(range(NCT + 4 * b, NCT + 4 * b + 4)) for b in range(SEQ // 512)]
                for blk in blocks:
                    for bi, ti in enumerate(blk):
                        isc = ti < NCT
                        which = 1 if isc else 0
                        li = ti - NCT
                        xb = xt[ti % 2]; xk = f"xtB{ti % 2}"
                        rpb = rp[ti % 2]; rk = f"rp{ti % 2}"
                        L(lambda e: e.dma_start(out=xb[:, :], in_=xall[ti * 128:(ti + 1) * 128, :]), w=[xk])
                        if not isc:
                            L(lambda e: e.dma_start(out=rpb[:, :], in_=rope[li * 128:(li + 1) * 128, :]), w=[rk])
                        A(lambda e: e.activation(out=junk[:, :], in_=xb[:, :], func=AF.Square, accum_out=st1[:, 0:1]), r=[xk], w=["junkB", "st1"])
                        rstd_from_ss(st1[:, 0:1], st1[:, 1:2], 1.0 / D, ["st1"], ["st1b"])
                        V(lambda e: e.scalar_tensor_tensor(out=hf[:, :], in0=xb[:, :], scalar=st1[:, 1:2], in1=Ar[which][:, :], op0=ALU.mult, op1=ALU.mult),
                          r=[xk, "st1b", f"Ar{which}"], w=["hf"])
                        V(lambda e: e.tensor_tensor(out=hb[:, :], in0=hf[:, :], in1=Br[which][:, :], op=ALU.add), r=["hf", f"Br{which}"], w=["hb"])
                        for kc in range(8):
                            M(lambda e: e.transpose(out=pT[:, kc * 128:(kc + 1) * 128], in_=hb[:, kc * 128:(kc + 1) * 128], identity=ident[:, :]),
                              r=["hb", "ident"], w=["pTB"])
                        A(lambda e: e.copy(out=hT[:, :, :].rearrange("p k t -> p (k t)"), in_=pT[:, :]), r=["pTB"], w=["hT"])
                        for b, (c0, c1) in enumerate(((0, 512), (512, 992), (992, INW))):
                            for kc in range(8):
                                M(lambda e: e.matmul(pP[b][:, 0:c1 - c0], lhsT=hT[:, kc, :], rhs=w_in_b[:, kc, c0:c1], start=(kc == 0), stop=(kc == 7)),
                                  r=["hT", "w_in_b"], w=[f"pP{b}"])

                        def gqa_stream(yp, pp, pk, H, gain, gk, dst, dk, sqb, sshb, qnb, t1b, t2b, sfx):
                            Vg = lambda fn, r=(), w=(): (V(fn, r, w), yp())
                            Ag = lambda fn, r=(), w=(): (A(fn, r, w), yp())
                            n = H * 64
                            src3 = pp[:, 0:n].rearrange("p (h d) -> p h d", h=H)
                            Ag(lambda e: e.activation(out=sqb[:, 0:n], in_=pp[:, 0:n], func=AF.Square), r=[pk], w=["sq" + sfx])
                            Vg(lambda e: e.tensor_reduce(out=sshb[:, 0:H], in_=sqb[:, 0:n].rearrange("p (h d) -> p h d", h=H), axis=AX.X, op=ALU.add), r=["sq" + sfx], w=["ssh" + sfx])
                            rstd_from_ss(sshb[:, 0:H], sshb[:, 0:H], 1.0 / 64, ["ssh" + sfx], ["ssh" + sfx], Ag, Vg)
                            qn3 = qnb[:, 0:n].rearrange("p (h d) -> p h d", h=H)
                            Vg(lambda e: e.tensor_tensor(out=qn3, in0=src3, in1=sshb[:, 0:H].unsqueeze(2).to_broadcast([128, H, 64]), op=ALU.mult), r=[pk, "ssh" + sfx], w=["qn" + sfx])
                            if isc:
                                Vg(lambda e: e.tensor_tensor(out=dst[:, :, :], in0=qn3, in1=gain[:, :, :], op=ALU.mult), r=["qn" + sfx, gk], w=[dk])
                            else:
                                Vg(lambda e: e.tensor_tensor(out=qn3, in0=qn3, in1=gain[:, :, :], op=ALU.mult), r=["qn" + sfx, gk], w=["qn" + sfx])
                                a1, a2 = rope_apply(qn3, H, 64, rpb[:, 0:64], rpb[:, 64:128], None, ["qn" + sfx, rk], t1b, t2b, sfx, Vg)
                                Vg(lambda e: e.tensor_tensor(out=dst[:, :, :], in0=a1, in1=a2, op=ALU.add), r=["t1" + sfx, "t2" + sfx], w=[dk])

                        def mla_stream(yp):
                            V = lambda fn, r=(), w=(): (V0(fn, r, w), yp())
                            A = lambda fn, r=(), w=(): (A0(fn, r, w), yp())
                            M = lambda fn, r=(), w=(): (M0(fn, r, w), yp())
                            sq, ssh, qn, sfx = sqB, sshB, qnB, "B"
                            A(lambda e: e.copy(out=vst[:, 0:128], in_=pP[1][:, 0:128]), r=["pP1"], w=["vst"])
                            A(lambda e: e.copy(out=vst[:, 512:640], in_=pP[2][:, 384:512]), r=["pP2"], w=["vst"])

                            A(lambda e: e.activation(out=sq[:, 0:352], in_=pP[1][:, 128:480], func=AF.Square), r=["pP1"], w=["sqB"])
                            V(lambda e: e.tensor_reduce(out=ssh[:, 0:1], in_=sq[:, 0:192], axis=AX.X, op=ALU.add), r=["sqB"], w=["sshB"])
                            V(lambda e: e.tensor_reduce(out=ssh[:, 1:2], in_=sq[:, 192:320], axis=AX.X, op=ALU.add), r=["sqB"], w=["sshB"])
                            V(lambda e: e.tensor_reduce(out=ssh[:, 2:3], in_=sq[:, 320:352], axis=AX.X, op=ALU.add), r=["sqB"], w=["sshB"])
                            V(lambda e: e.tensor_tensor(out=ssh[:, 0:3], in0=ssh[:, 0:3], in1=invB[:, :], op=ALU.mult), r=["sshB", "invB"], w=["sshB"])
                            rstd_from_ss(ssh[:, 0:3], ssh[:, 0:3], 1.0, ["sshB"], ["sshB"], A, V)
                            V(lambda e: e.tensor_scalar(out=cqn[:, 0:192], in0=pP[1][:, 128:320], scalar1=ssh[:, 0:1], scalar2=None, op0=ALU.mult), r=["pP1", "sshB"], w=["cqn"])
                            V(lambda e: e.tensor_scalar(out=cqn[:, 192:320], in0=pP[1][:, 320:448], scalar1=ssh[:, 1:2], scalar2=None, op0=ALU.mult), r=["pP1", "sshB"], w=["cqn"])
                            V(lambda e: e.scalar_tensor_tensor(out=kpe[:, :], in0=pP[1][:, 448:480], scalar=ssh[:, 2:3], in1=gBkr[:, :], op0=ALU.mult, op1=ALU.mult),
                              r=["pP1", "sshB", "gBkr"], w=["kpe"])
                            if not isc:
                                a1, a2 = rope_apply(kpe[:, :].unsqueeze(1), 1, 32, rpb[:, 128:160], rpb[:, 160:192], None, ["kpe", rk], t1B, t2B, "B", V)
                                V(lambda e: e.tensor_tensor(out=kpe2[:, :].unsqueeze(1), in0=a1, in1=a2, op=ALU.add), r=["t1B", "t2B"], w=["kpe2"])
                                kp = kpe2
                            else:
                                kp = kpe
                            V(lambda e: e.tensor_copy(out=qB[:, 6:12, 64:96], in_=kp[:, :].unsqueeze(1).to_broadcast([128, 6, 32])), r=["kpe", "kpe2"], w=["qBk"])
                            M(lambda e: e.transpose(out=pT[:, 0:128], in_=cqn[:, 0:128], identity=ident[:, :]), r=["cqn", "ident"], w=["pTB"])
                            M(lambda e: e.transpose(out=pT[0:64, 128:256], in_=cqn[:, 128:192], identity=ident[:, :]), r=["cqn", "ident"], w=["pTB"])
                            M(lambda e: e.transpose(out=pT[:, 256:384], in_=cqn[:, 192:320], identity=ident[:, :]), r=["cqn", "ident"], w=["pTB"])
                            A(lambda e: e.copy(out=cT[:, 0, :], in_=pT[:, 0:128]), r=["pTB"], w=["cT"])
                            A(lambda e: e.copy(out=cT[0:64, 1, :], in_=pT[0:64, 128:256]), r=["pTB"], w=["cT"])
                            A(lambda e: e.copy(out=cT[:, 2, :], in_=pT[:, 256:384]), r=["pTB"], w=["cT"])
                            for hf_ in range(2):
                                M(lambda e: e.matmul(pQ[hf_][:, 0:288], lhsT=cT[:, 0, :], rhs=wuq_b[:, 0, hf_ * 288:(hf_ + 1) * 288], start=True, stop=False),
                                  r=["cT", "wuq_b"], w=[f"pQ{hf_}"])
                                M(lambda e: e.matmul(pQ[hf_][:, 0:288], lhsT=cT[0:64, 1, :], rhs=wuq_b[0:64, 1, hf_ * 288:(hf_ + 1) * 288], start=False, stop=True),
                                  r=["cT", "wuq_b"], w=[f"pQ{hf_}"])
                                M(lambda e: e.matmul(pK[hf_][:, 0:384], lhsT=cT[:, 2, :], rhs=wukv_b[:, hf_ * 384:(hf_ + 1) * 384], start=True, stop=True),
                                  r=["cT", "wukv_b"], w=[f"pK{hf_}"])
                            for hf_ in range(2):
                                A(lambda e: e.activation(out=sq[:, hf_ * 288:(hf_ + 1) * 288], in_=pQ[hf_][:, 0:288], func=AF.Square), r=[f"pQ{hf_}"], w=["sqB"])
                            sq3 = sq[:, 0:576].rearrange("p (h d) -> p h d", h=6)
                            V(lambda e: e.tensor_reduce(out=ssh[:, 0:6], in_=sq3[:, :, 0:64], axis=AX.X, op=ALU.add), r=["sqB"], w=["sshB"])
                            V(lambda e: e.tensor_reduce(out=ssh[:, 6:12], in_=sq3[:, :, 64:96], axis=AX.X, op=ALU.add), r=["sqB"], w=["sshB"])
                            V(lambda e: e.tensor_tensor(out=ssh[:, 0:12], in0=ssh[:, 0:12], in1=invQ[:, :], op=ALU.mult), r=["sshB", "invQ"], w=["sshB"])
                            rstd_from_ss(ssh[:, 0:12], ssh[:, 0:12], 1.0, ["sshB"], ["sshB"], A, V)
                            qn3 = qn[:, 0:576].rearrange("p (h d) -> p h d", h=6)
                            for hf_ in range(2):
                                p3 = pQ[hf_][:, 0:288].rearrange("p (h d) -> p h d", h=3)
                                hs = slice(hf_ * 3, hf_ * 3 + 3)
                                V(lambda e: e.tensor_tensor(out=qn3[:, hs, 0:64], in0=p3[:, :, 0:64],
                                                            in1=ssh[:, hf_ * 3:hf_ * 3 + 3].unsqueeze(2).to_broadcast([128, 3, 64]), op=ALU.mult), r=[f"pQ{hf_}", "sshB"], w=["qnB"])
                                V(lambda e: e.tensor_tensor(out=qn3[:, hs, 64:96], in0=p3[:, :, 64:96],
                                                            in1=ssh[:, 6 + hf_ * 3:6 + hf_ * 3 + 3].unsqueeze(2).to_broadcast([128, 3, 32]), op=ALU.mult), r=[f"pQ{hf_}", "sshB"], w=["qnB"])
                            V(lambda e: e.tensor_tensor(out=qn3, in0=qn3, in1=gBq[:, :].unsqueeze(1).to_broadcast([128, 6, 96]), op=ALU.mult), r=["qnB", "gBq"], w=["qnB"])
                            V(lambda e: e.tensor_copy(out=qB[:, 0:6, 0:64], in_=qn3[:, :, 0:64]), r=["qnB"], w=["qBq"])
                            if isc:
                                V(lambda e: e.tensor_copy(out=qB[:, 0:6, 64:96], in_=qn3[:, :, 64:96]), r=["qnB"], w=["qBq"])
                            else:
                                a1, a2 = rope_apply(qn3[:, :, 64:96], 6, 32, rpb[:, 128:160], rpb[:, 160:192], None, ["qnB", rk], t1B, t2B, "B", V)
                                V(lambda e: e.tensor_tensor(out=qB[:, 0:6, 64:96], in0=a1, in1=a2, op=ALU.add), r=["t1B", "t2B"], w=["qBq"])
                            for hf_ in range(2):
                                A(lambda e: e.activation(out=sq[:, hf_ * 192:(hf_ + 1) * 192].rearrange("p (h d) -> p h d", h=3),
                                                         in_=pK[hf_][:, 0:384].rearrange("p (h d) -> p h d", h=3)[:, :, 0:64], func=AF.Square), r=[f"pK{hf_}"], w=["sqB"])
                                A(lambda e: e.copy(out=vst[:, 128 + hf_ * 192:128 + (hf_ + 1) * 192].rearrange("p (h d) -> p h d", h=3),
                                                   in_=pK[hf_][:, 0:384].rearrange("p (h d) -> p h d", h=3)[:, :, 64:128]), r=[f"pK{hf_}"], w=["vst"])
                            V(lambda e: e.tensor_reduce(out=ssh[:, 0:6], in_=sq[:, 0:384].rearrange("p (h d) -> p h d", h=6), axis=AX.X, op=ALU.add), r=["sqB"], w=["sshB"])
                            rstd_from_ss(ssh[:, 0:6], ssh[:, 0:6], 1.0 / 64, ["sshB"], ["sshB"], A, V)
                            kn3 = qn[:, 0:384].rearrange("p (h d) -> p h d", h=6)
                            for hf_ in range(2):
                                p3 = pK[hf_][:, 0:384].rearrange("p (h d) -> p h d", h=3)
                                V(lambda e: e.tensor_tensor(out=kn3[:, hf_ * 3:hf_ * 3 + 3, :], in0=p3[:, :, 0:64],
                                                            in1=ssh[:, hf_ * 3:hf_ * 3 + 3].unsqueeze(2).to_broadcast([128, 3, 64]), op=ALU.mult), r=[f"pK{hf_}", "sshB"], w=["qnB"])
                            V(lambda e: e.tensor_tensor(out=qB[:, 6:12, 0:64], in0=kn3, in1=gBk[:, :].unsqueeze(1).to_broadcast([128, 6, 64]), op=ALU.mult), r=["qnB", "gBk"], w=["qBk"])


                        interleave([lambda yp: gqa_stream(yp, pP[0], "pP0", 8, gA, "gA", qA, "qA", sqA, sshA, qnA, t1A, t2A, "A"),
                                    lambda yp: gqa_stream(yp, pP[2], "pP2", 6, gC, "gC", qC, "qC", sqC, sshC, qnC, t1C, t2C, "C"),
                                    mla_stream])

                        S(lambda e: e.dma_start(out=vall[ti * 128:(ti + 1) * 128, :], in_=vst[:, :]), r=["vst"])
                        c0 = bi * 128
                        for i in range(4):
                            M(lambda e: e.transpose(out=pT[:, i * 128:(i + 1) * 128], in_=qA[:, 2 * i:2 * i + 2, :].rearrange("p h d -> p (h d)"), identity=ident[:, :]),
                              r=["qA", "ident"], w=["pTB"])
                        A(lambda e: e.copy(out=stA[:, :, c0:c0 + 128], in_=pT[:, 0:512].rearrange("p (i t) -> p i t", i=4)), r=["pTB"], w=["stA"])
                        for i in range(3):
                            M(lambda e: e.transpose(out=pT[:, i * 128:(i + 1) * 128], in_=qC[:, 2 * i:2 * i + 2, :].rearrange("p h d -> p (h d)"), identity=ident[:, :]),
                              r=["qC", "ident"], w=["pTB"])
                        V(lambda e: e.tensor_copy(out=stC[:, :, c0:c0 + 128], in_=pT[:, 0:384].rearrange("p (i t) -> p i t", i=3)), r=["pTB"], w=["stC"])
                        for rnd in range(2):
                            for i in range(6):
                                M(lambda e: e.transpose(out=pT[0:96, i * 128:(i + 1) * 128], in_=qB[:, rnd * 6 + i, :], identity=ident[:, :]),
                                  r=["qBq", "qBk", "ident"], w=["pTB"])
                            A(lambda e: e.copy(out=stB[0:96, rnd * 6:rnd * 6 + 6, c0:c0 + 128], in_=pT[0:96, 0:768].rearrange("p (i t) -> p i t", i=6)), r=["pTB"], w=["stB"])
                    W = len(blk) * 128
                    t0 = blk[0] * 128
                    S(lambda e: e.dma_start(out=qkT_A[:, t0:t0 + W].rearrange("(i p) t -> p i t", p=128), in_=stA[:, :, 0:W]), r=["stA"])
                    S(lambda e: e.dma_start(out=qkT_C[:, t0:t0 + W].rearrange("(i p) t -> p i t", p=128), in_=stC[:, :, 0:W]), r=["stC"])
                    S(lambda e: e.dma_start(out=qkT_B[:, :, t0:t0 + W].rearrange("j p t -> p j t"), in_=stB[0:96, :, 0:W]), r=["stB"])
            P.barrier()
            if stop_after == ("B", l):
                break

            with ExitStack() as ph:
                kt = [tsb(ph, f"kt{i}", [96, T], BF16) for i in range(2)]
                vb = [tsb(ph, f"vb{i}", [128, NT, 65], BF16) for i in range(2)]
                qt = [tsb(ph, f"qt{i}", [96, 512], BF16) for i in range(2)]
                pt = [tsb(ph, f"pt{i}", [128, 512], BF16) for i in range(3)]
                ob = [tsb(ph, f"ob{i}", [128, 4, 64], BF16) for i in range(2)]
                den = tsb(ph, "den", [128, 8], F32)
                sinkr = tsb(ph, "sinkr", [128, 4], F32)
                sinke = tsb(ph, "sinke", [128, 4], F32)
                pS = [tps(ph, f"pS{i}", [128, 512]) for i in range(3)]
                pO = [tps(ph, f"pO{i}", [128, 512]) for i in range(2)]
                den2 = [tsb(ph, f"den2_{i}", [128, 4], F32) for i in range(2)]
                L(lambda e: e.dma_start(out=sinkr[:, :], in_=c_sink[l:l + 1, :].broadcast_to([128, 4])), w=["sinkr"])
                A(lambda e: e.activation(out=sinke[:, :], in_=sinkr[:, :], func=AF.Exp), r=["sinkr"], w=["sinke"])
                for i in range(2):
                    G(lambda e: e.memset(vb[i][:, :, 64:65], 1.0), w=[f"vb{i}"])
                cnt = {"kv": 0, "q": 0, "s": 0, "p": 0, "u": 0}
                mixv = mix.rearrange("(j p) c -> p j c", p=128)
                vallv = vall.rearrange("(j p) c -> p j c", p=128)
                pending = []

                def emit_tail(st):
                    (si, pi, Wq, scale, msk, subs, u, n, nkeys, vbuf, vk, j, sink_h, out_fn) = st
                    ui = u % 2
                    A(lambda e: e.activation(out=pt[pi][:, 0:Wq], in_=pS[si][:, 0:Wq], func=AF.Exp, scale=scale), r=[f"pS{si}"], w=[f"pt{pi}"])
                    if msk is not None:
                        V(lambda e: e.tensor_tensor(out=pt[pi][:, 0:Wq].rearrange("p (s t) -> p s t", s=subs), in0=pt[pi][:, 0:Wq].rearrange("p (s t) -> p s t", s=subs),
                                                    in1=msk[:, :].unsqueeze(1).to_broadcast([128, subs, 128]), op=ALU.mult), r=[f"pt{pi}"], w=[f"pt{pi}"])
                    for s_ in range(subs):
                        M(lambda e: e.matmul(pO[ui][:, s_ * 65:(s_ + 1) * 65], lhsT=pt[pi][:, s_ * 128:(s_ + 1) * 128], rhs=vbuf[:, j, :],
                                             start=(n == 0 and s_ == 0), stop=(n == nkeys - 1), skip_group_check=True),
                          r=[f"pt{pi}", vk], w=[f"pO{ui}"])
                    if n == nkeys - 1:
                        pov = pO[ui][:, 0:subs * 65].rearrange("p (s c) -> p s c", s=subs)
                        dn = den2[ui]
                        if sink_h is not None:
                            V(lambda e: e.tensor_tensor(out=dn[:, 0:subs].unsqueeze(2), in0=pov[:, :, 64:65], in1=sinke[:, sink_h:sink_h + subs].unsqueeze(2), op=ALU.add),
                              r=[f"pO{ui}", "sinke"], w=[f"den2_{ui}"])
                            V(lambda e: e.reciprocal(out=dn[:, 0:subs], in_=dn[:, 0:subs]), r=[f"den2_{ui}"], w=[f"den2_{ui}"])
                        else:
                            V(lambda e: e.reciprocal(out=dn[:, 0:subs].unsqueeze(2), in_=pov[:, :, 64:65]), r=[f"pO{ui}"], w=[f"den2_{ui}"])
                        V(lambda e: e.tensor_tensor(out=ob[ui][:, 0:subs, :], in0=pov[:, :, 0:64], in1=dn[:, 0:subs].unsqueeze(2).to_broadcast([128, subs, 64]), op=ALU.mult),
                          r=[f"pO{ui}", f"den2_{ui}"], w=[f"ob{ui}"])
                        out_fn(ob[ui], f"ob{ui}")

                def attn_unit(qsrc_fn, dq, Wq, keys, kb, kk, vbuf, vk, scale, subs, sink_h, out_fn):
                    qi = cnt["q"] % 2; cnt["q"] += 1
                    u = cnt["u"]; cnt["u"] += 1
                    qsrc_fn(qt[qi], f"qt{qi}")
                    for n, (j, msk) in enumerate(keys):
                        si = cnt["s"] % 3; cnt["s"] += 1
                        pi = cnt["p"] % 3; cnt["p"] += 1
                        M(lambda e: e.matmul(pS[si][:, 0:Wq], lhsT=kb[0:dq, j * 128:(j + 1) * 128], rhs=qt[qi][0:dq, 0:Wq], start=True, stop=True),
                          r=[kk, f"qt{qi}"], w=[f"pS{si}"])
                        pending.append((si, pi, Wq, scale, msk, subs, u, n, len(keys), vbuf, vk, j, sink_h, out_fn))
                        if len(pending) > 1:
                            emit_tail(pending.pop(0))

                def load_kv(krows_ap, dq, voff):
                    i = cnt["kv"] % 2; cnt["kv"] += 1
                    L(lambda e: e.dma_start(out=kt[i][0:dq, :], in_=krows_ap), w=[f"kt{i}"])
                    for hh in range(2):
                        L(lambda e: e.dma_start(out=vb[i][:, hh * 17:(hh + 1) * 17, 0:64], in_=vallv[:, hh * 17:(hh + 1) * 17, voff:voff + 64]), w=[f"vb{i}"])
                    return kt[i], f"kt{i}", vb[i], f"vb{i}"

                qblocks = ([(0, NCT)] if not last else []) + [(NCT + 4 * b, 4) for b in range(SEQ // 512)]
                for mixer in ("A", "B"):
                    nkv = 2 if mixer == "A" else 6
                    for kv in range(nkv):
                        if mixer == "A":
                            kb, kk, vbuf, vk = load_kv(qkT_A[(6 + kv) * 64:(7 + kv) * 64, :], 64, kv * 64)
                            heads = [3 * kv + g for g in range(3)]; dq = 64; scale = 64 ** -0.5
                        else:
                            kb, kk, vbuf, vk = load_kv(qkT_B[6 + kv, :, :], 96, 128 + kv * 64)
                            heads = [kv]; dq = 96; scale = 96 ** -0.5
                        for h in heads:
                            for (tb, nsub) in qblocks:
                                keys = [(j, None) for j in (range(NCT) if tb < NCT else range(NT))]
                                Wq = nsub * 128
                                if mixer == "A":
                                    qsrc = lambda dst, dk, h=h, tb=tb, Wq=Wq: L(lambda e: e.dma_start(out=dst[0:64, 0:Wq], in_=qkT_A[h * 64:(h + 1) * 64, tb * 128:tb * 128 + Wq]), w=[dk])
                                    col = h * 64
                                else:
                                    qsrc = lambda dst, dk, h=h, tb=tb, Wq=Wq: L(lambda e: e.dma_start(out=dst[0:96, 0:Wq], in_=qkT_B[h, :, tb * 128:tb * 128 + Wq]), w=[dk])
                                    col = 384 + h * 64
                                outf = lambda o, ok, tb=tb, nsub=nsub, col=col: S(lambda e: e.dma_start(out=mixv[:, tb:tb + nsub, col:col + 64], in_=o[:, 0:nsub, :]), r=[ok])
                                attn_unit(qsrc, dq, Wq, keys, kb, kk, vbuf, vk, scale, nsub, None, outf)
                for kv in range(2):
                    kb, kk, vbuf, vk = load_kv(qkT_C[(4 + kv) * 64:(5 + kv) * 64, :], 64, 512 + kv * 64)
                    for ti in (range(NT) if not last else range(NCT, NT)):
                        if ti < NCT:
                            keys = [(j, None) for j in range(NCT)]
                        else:
                            keys = [(j, None) for j in range(NCT)]
                            if ti - 1 >= NCT:
                                keys.append((ti - 1, M1))
                            keys.append((ti, None))
                            if ti + 1 < NT:
                                keys.append((ti + 1, M2))

                        def qsrc(dst, dk, kv=kv, ti=ti):
                            L(lambda e: e.dma_start(out=dst[0:64, 0:256].rearrange("p (h t) -> p h t", h=2),
                                                    in_=qkT_C[2 * kv * 64:(2 * kv + 2) * 64, ti * 128:(ti + 1) * 128].rearrange("(h p) t -> p h t", p=64)), w=[dk])
                        col = 768 + 2 * kv * 64
                        outf = lambda o, ok, ti=ti, col=col: S(lambda e: e.dma_start(out=mix[ti * 128:(ti + 1) * 128, col:col + 128], in_=o[:, 0:2, :].rearrange("p s d -> p (s d)")), r=[ok])
                        attn_unit(qsrc, 64, 256, keys, kb, kk, vbuf, vk, 64 ** -0.5, 2, 2 * kv, outf)
                while pending:
                    emit_tail(pending.pop(0))
            P.barrier()
            if stop_after == ("C", l):
                break

            with ExitStack() as ph:
                wst = tsb(ph, "wstD", [128, 8, 512], F32)
                w_out_b = tsb(ph, "w_out_b", [128, 8, D], BF16)
                G1 = [tsb(ph, f"G1_{i}", [128, D], F32) for i in range(2)]
                xt = [tsb(ph, f"xtD{i}", [128, D], F32) for i in range(2)]
                mt = [tsb(ph, f"mt{i}", [128, D], BF16) for i in range(2)]
                mT = tsb(ph, "mT", [128, 8, 128], BF16)
                yt = [tsb(ph, f"yt{i}", [128, D], F32) for i in range(2)]
                pT = tps(ph, "pTD", [128, 1024], BF16)
                pY = [tps(ph, f"pY{i}", [128, 512]) for i in range(2)]
                for b in range(2):
                    for h in range(2):
                        L(lambda e: e.dma_start(out=wst[:, h * 4:(h + 1) * 4, :],
                                                in_=w_out[l, h * 512:(h + 1) * 512, b * 512:(b + 1) * 512].rearrange("(k p) n -> p k n", p=128)), w=["wstD"])
                    V(lambda e: e.tensor_copy(out=w_out_b[:, 0:4, b * 512:(b + 1) * 512], in_=wst[:, 0:4, :]), r=["wstD"], w=["w_out_b"])
                    G(lambda e: e.tensor_copy(out=w_out_b[:, 4:8, b * 512:(b + 1) * 512], in_=wst[:, 4:8, :]), r=["wstD"], w=["w_out_b"])
                for which in range(2):
                    load_bcast(G1[which][:, :], which, 2, f"G1_{which}")
                for ti in tiles_act:
                    which = 1 if ti < NCT else 0
                    i2 = ti % 2
                    L(lambda e: e.dma_start(out=xt[i2][:, :], in_=xall[ti * 128:(ti + 1) * 128, :]), w=[f"xtD{i2}"])
                    L(lambda e: e.dma_start(out=mt[i2][:, :], in_=mix[ti * 128:(ti + 1) * 128, :]), w=[f"mt{i2}"])
                    for kc in range(8):
                        M(lambda e: e.transpose(out=pT[:, kc * 128:(kc + 1) * 128], in_=mt[i2][:, kc * 128:(kc + 1) * 128], identity=ident[:, :]), r=[f"mt{i2}", "ident"], w=["pTD"])
                    A(lambda e: e.copy(out=mT[:, :, :].rearrange("p k t -> p (k t)"), in_=pT[:, :]), r=["pTD"], w=["mT"])
                    for b in range(2):
                        for kc in range(8):
                            M(lambda e: e.matmul(pY[b][:, :], lhsT=mT[:, kc, :], rhs=w_out_b[:, kc, b * 512:(b + 1) * 512], start=(kc == 0), stop=(kc == 7)), r=["mT", "w_out_b"], w=[f"pY{b}"])
                        V(lambda e: e.tensor_tensor(out=yt[i2][:, b * 512:(b + 1) * 512], in0=pY[b][:, :], in1=G1[which][:, b * 512:(b + 1) * 512], op=ALU.mult),
                          r=[f"pY{b}", f"G1_{which}"], w=[f"yt{i2}"])
                    V(lambda e: e.tensor_tensor(out=yt[i2][:, :], in0=yt[i2][:, :], in1=xt[i2][:, :], op=ALU.add), r=[f"yt{i2}", f"xtD{i2}"], w=[f"yt{i2}"])
                    S(lambda e: e.dma_start(out=xall[ti * 128:(ti + 1) * 128, :], in_=yt[i2][:, :]), r=[f"yt{i2}"])
            P.barrier()
            if stop_after == ("D", l):
                break

            with ExitStack() as ph:
                aff = tsb(ph, "aff", [128, NT, NE], F32)
                gm = tsb(ph, "gm", [128, NT, NE], F32)
                sloti = tsb(ph, "sloti", [128, NT, NE], I32)
                slotc = tsb(ph, "slotc", [128, NT, NE], I32)
                G2 = [tsb(ph, f"G2_{i}", [128, D], F32) for i in range(2)]
                p13 = ExitStack()
                affT = tsb(p13, "affT", [NE, T], F32)
                maskT = tsb(p13, "maskT", [NE, T], BF16)
                with ExitStack() as p1:
                    wr_f = tsb(p1, "wr_f", [128, 8, NE], F32)
                    wr_b = tsb(p1, "wr_b", [128, 8, NE], BF16)
                    Ar = [tsb(p1, f"Ar2_{i}", [128, D], F32) for i in range(2)]
                    Br = [tsb(p1, f"Br2_{i}", [128, D], F32) for i in range(2)]
                    xt = [tsb(p1, f"xtE{i}", [128, D], F32) for i in range(2)]
                    junk = tsb(p1, "junkE", [128, D], BF16)
                    st1 = tsb(p1, "st1E", [128, 8], F32)
                    hf = tsb(p1, "hfE", [128, D], F32)
                    hb = [tsb(p1, f"hbE{i}", [128, D], BF16) for i in range(2)]
                    hT = tsb(p1, "hTE", [128, 8, 128], BF16)
                    ex = tsb(p1, "ex", [128, NE], F32)
                    pT = tps(p1, "pTE", [128, 1024], BF16)
                    pR = tps(p1, "pR", [128, 512])
                    pAT = tps(p1, "pAT", [128, 512])
                    L(lambda e: e.dma_start(out=wr_f[:, :, :], in_=w_router[l, :, :].rearrange("(k p) n -> p k n", p=128)), w=["wr_f"])
                    V(lambda e: e.tensor_copy(out=wr_b[:, :, :], in_=wr_f[:, :, :]), r=["wr_f"], w=["wr_b"])
                    for which in range(2):
                        load_bcast(Ar[which][:, :], which, 4, f"Ar2_{which}")
                        load_bcast(Br[which][:, :], which, 3, f"Br2_{which}")
                        load_bcast(G2[which][:, :], which, 5, f"G2_{which}")
                    for ti in tiles_act:
                        which = 1 if ti < NCT else 0
                        i2 = ti % 2
                        L(lambda e: e.dma_start(out=xt[i2][:, :], in_=xall[ti * 128:(ti + 1) * 128, :]), w=[f"xtE{i2}"])
                        A(lambda e: e.activation(out=junk[:, :], in_=xt[i2][:, :], func=AF.Square, accum_out=st1[:, 0:1]), r=[f"xtE{i2}"], w=["junkE", "st1E"])
                        rstd_from_ss(st1[:, 0:1], st1[:, 1:2], 1.0 / D, ["st1E"], ["st1Eb"])
                        V(lambda e: e.scalar_tensor_tensor(out=hf[:, :], in0=xt[i2][:, :], scalar=st1[:, 1:2], in1=Ar[which][:, :], op0=ALU.mult, op1=ALU.mult),
                          r=[f"xtE{i2}", "st1Eb", f"Ar2_{which}"], w=["hfE"])
                        V(lambda e: e.tensor_tensor(out=hb[i2][:, :], in0=hf[:, :], in1=Br[which][:, :], op=ALU.add), r=["hfE", f"Br2_{which}"], w=[f"hbE{i2}"])
                        S(lambda e: e.dma_start(out=h2d[ti * 128:(ti + 1) * 128, :], in_=hb[i2][:, :]), r=[f"hbE{i2}"])
                        for kc in range(8):
                            M(lambda e: e.transpose(out=pT[:, kc * 128:(kc + 1) * 128], in_=hb[i2][:, kc * 128:(kc + 1) * 128], identity=ident[:, :]), r=[f"hbE{i2}", "ident"], w=["pTE"])
                        A(lambda e: e.copy(out=hT[:, :, :].rearrange("p k t -> p (k t)"), in_=pT[:, :]), r=["pTE"], w=["hTE"])
                        for kc in range(8):
                            M(lambda e: e.matmul(pR[:, 0:NE], lhsT=hT[:, kc, :], rhs=wr_b[:, kc, :], start=(kc == 0), stop=(kc == 7)), r=["hTE", "wr_b"], w=["pR"])
                        V(lambda e: e.tensor_reduce(out=st1[:, 2:3], in_=pR[:, 0:NE], axis=AX.X, op=ALU.max, negate=True), r=["pR"], w=["st1Ec"])
                        A(lambda e: e.activation(out=ex[:, :], in_=pR[:, 0:NE], func=AF.Exp, bias=st1[:, 2:3], accum_out=st1[:, 3:4]), r=["pR", "st1Ec"], w=["ex", "st1Ed"])
                        V(lambda e: e.reciprocal(out=st1[:, 4:5], in_=st1[:, 3:4]), r=["st1Ed"], w=["st1Ee"])
                        V(lambda e: e.tensor_scalar(out=aff[:, ti, :], in0=ex[:, :], scalar1=st1[:, 4:5], scalar2=None, op0=ALU.mult), r=["ex", "st1Ee"], w=["aff"])
                        M(lambda e: e.transpose(out=pAT[0:NE, 0:128], in_=aff[:, ti, :], identity=identf[:, :]), r=["aff", "identf"], w=["pAT"])
                        A(lambda e: e.copy(out=affT[:, ti * 128:(ti + 1) * 128], in_=pAT[0:NE, 0:128]), r=["pAT"], w=["affT"])
                P.barrier()
                with ExitStack() as p2:
                    work = tsb(p2, "work", [NE, CTX], F32)
                    mx = tsb(p2, "mx", [NE, 8], F32)
                    if not last:
                        V(lambda e: e.tensor_copy(out=work[:, :], in_=affT[:, 0:CTX]), r=["affT"], w=["work"])
                        for r_ in range(CAP_C // 8):
                            V(lambda e: e.max(out=mx[:, :], in_=work[:, :]), r=["work"], w=["mx"])
                            if r_ < CAP_C // 8 - 1:
                                V(lambda e: e.match_replace(out=work[:, :], in_to_replace=mx[:, :], in_values=work[:, :], imm_value=-1.0), r=["work", "mx"], w=["work"])
                        V(lambda e: e.tensor_scalar(out=maskT[:, 0:CTX], in0=affT[:, 0:CTX], scalar1=mx[:, 7:8], scalar2=None, op0=ALU.is_ge), r=["affT", "mx"], w=["maskT"])
                    A2 = tsb(p2, "A2", [128, 512], F32)
                    jk = tsb(p2, "jk2", [128, 512], F32)
                    mk2 = tsb(p2, "mk2", [128, 512], BF16)
                    Bsel = tsb(p2, "Bsel", [NE, 128], F32)
                    Gm = tsb(p2, "Gm", [128, 128], F32)
                    bs = tsb(p2, "bs", [128, 8], F32)
                    pG2 = tps(p2, "pGm", [128, 512])
                    pC2 = tps(p2, "pCnt", [128, 512])
                    S(lambda e: e.dma_start(out=aT_d, in_=affT[:, CTX:T]), r=["affT"], w=["aT_d"])
                    L(lambda e: e.dma_start(out=A2[:, :], in_=aT_d.rearrange("e (c t) -> (e c) t", c=8)), r=["aT_d"], w=["A2"])
                    G(lambda e: e.affine_select(out=Bsel[:, :], in_=onesf[0:NE, :], pattern=[[1, 128]], base=0, channel_multiplier=-8, compare_op=ALU.is_ge, fill=0.0), r=["onesf"], w=["Bsel"])
                    G(lambda e: e.affine_select(out=Bsel[:, :], in_=Bsel[:, :], pattern=[[-1, 128]], base=7, channel_multiplier=8, compare_op=ALU.is_ge, fill=0.0), r=["Bsel"], w=["Bsel"])
                    M(lambda e: e.matmul(pG2[:, 0:128], lhsT=Bsel[:, :], rhs=Bsel[:, :], start=True, stop=True), r=["Bsel"], w=["pGm"])
                    V(lambda e: e.tensor_copy(out=Gm[:, :], in_=pG2[:, 0:128]), r=["pGm"], w=["Gm"])
                    V(lambda e: e.memset(bs[:, 0:1], 0.0), w=["bs"])
                    V(lambda e: e.memset(bs[:, 1:2], 1.0), w=["bs"])
                    for it in range(32):
                        V(lambda e: e.tensor_scalar(out=bs[:, 2:3], in0=bs[:, 0:1], scalar1=bs[:, 1:2], scalar2=0.5, op0=ALU.add, op1=ALU.mult), r=["bs"], w=["bs"])
                        V(lambda e: e.tensor_scalar(out=jk[:, :], in0=A2[:, :], scalar1=bs[:, 2:3], scalar2=0.0, op0=ALU.is_ge, op1=ALU.add, accum_out=bs[:, 3:4]), r=["A2", "bs"], w=["jk2", "bs"])
                        M(lambda e: e.matmul(pC2[:, 0:1], lhsT=Gm[:, :], rhs=bs[:, 3:4], start=True, stop=True), r=["Gm", "bs"], w=["pCnt"])
                        V(lambda e: e.tensor_scalar(out=bs[:, 4:5], in0=pC2[:, 0:1], scalar1=float(CAP_L) - 0.5, scalar2=None, op0=ALU.is_ge), r=["pCnt"], w=["bs"])
                        V(lambda e: e.tensor_tensor(out=bs[:, 5:6], in0=bs[:, 4:5], in1=bs[:, 2:3], op=ALU.mult), r=["bs"], w=["bs"])
                        V(lambda e: e.tensor_tensor(out=bs[:, 0:1], in0=bs[:, 0:1], in1=bs[:, 5:6], op=ALU.max), r=["bs"], w=["bs"])
                        V(lambda e: e.scalar_tensor_tensor(out=bs[:, 6:7], in0=bs[:, 4:5], scalar=2.0, in1=bs[:, 2:3], op0=ALU.mult, op1=ALU.add), r=["bs"], w=["bs"])
                        V(lambda e: e.tensor_tensor(out=bs[:, 1:2], in0=bs[:, 1:2], in1=bs[:, 6:7], op=ALU.min), r=["bs"], w=["bs"])
                    V(lambda e: e.tensor_scalar(out=mk2[:, :], in0=A2[:, :], scalar1=bs[:, 0:1], scalar2=None, op0=ALU.is_ge), r=["A2", "bs"], w=["mk2"])
                    S(lambda e: e.dma_start(out=mk_d.rearrange("e (c t) -> (e c) t", c=8), in_=mk2[:, :]), r=["mk2"], w=["mk_d"])
                    L(lambda e: e.dma_start(out=maskT[:, CTX:T], in_=mk_d), r=["mk_d"], w=["maskT"])
                P.barrier()
                with ExitStack() as p3:
                    mk = tsb(p3, "mk", [128, NE], F32)
                    mkb = tsb(p3, "mkb", [128, NE], BF16)
                    Sf = tsb(p3, "Sf", [128, NE], F32)
                    Sb = tsb(p3, "Sb", [128, NE], BF16)
                    sl = tsb(p3, "sl", [128, NE], F32)
                    hb = [tsb(p3, f"hbS{i}", [128, XW], BF16) for i in range(2)]
                    tok_i = tsb(p3, "tok_i", [128, 2, NT], I32)
                    tokb = tsb(p3, "tokb", [128, NT, 2], BF16)
                    ghi = tsb(p3, "ghi", [128, NE], BF16)
                    zt = tsb(p3, "zt", [128, 2 * D], F32)
                    for hh in range(2):
                        G(lambda e: e.iota(tok_i[hh * 64:(hh + 1) * 64, 0, :], pattern=[[2, NT]], base=hh, channel_multiplier=0), w=["tok_i"])
                        G(lambda e: e.iota(tok_i[hh * 64:(hh + 1) * 64, 1, :], pattern=[[0, NT]], base=0, channel_multiplier=1), w=["tok_i"])
                    V(lambda e: e.tensor_copy(out=tokb[:, :, :].rearrange("p t j -> p j t"), in_=tok_i[:, :, :]), r=["tok_i"], w=["tokb"])
                    V(lambda e: e.memset(zt[:, :], 0.0), w=["zt"])
                    for i in range(2):
                        V(lambda e: e.memset(hb[i][:, D:XW], 0.0), w=[f"hbS{i}"])
                    for ti in range(0, NT, 2):
                        L(lambda e: e.dma_start(out=accd[ti * 128:(ti + 2) * 128, :].rearrange("(j p) c -> p j c", p=128), in_=zt[:, :].rearrange("p (j c) -> p j c", j=2)), r=["zt"])
                    pM = tps(p3, "pM", [128, 1024], BF16)
                    pPos = tps(p3, "pPos", [128, 512])
                    for ti in tiles_act:
                        isc = ti < NCT
                        i2 = ti % 2
                        if ti == 0 or ti == NCT:
                            V(lambda e: e.memset(Sf[:, :], 0.0), w=["Sf"])
                            V(lambda e: e.tensor_copy(out=Sb[:, :], in_=Sf[:, :]), r=["Sf"], w=["Sb"])
                        L(lambda e: e.dma_start(out=hb[i2][:, 0:D], in_=h2d[ti * 128:(ti + 1) * 128, :]), w=[f"hbS{i2}"])
                        M(lambda e: e.transpose(out=pM[:, 0:NE], in_=maskT[:, ti * 128:(ti + 1) * 128], identity=ident[0:NE, 0:NE]), r=["maskT", "ident"], w=["pM"])
                        V(lambda e: e.tensor_copy(out=mk[:, :], in_=pM[:, 0:NE]), r=["pM"], w=["mk"])
                        A(lambda e: e.copy(out=mkb[:, :], in_=pM[:, 0:NE]), r=["pM"], w=["mkb"])
                        M(lambda e: e.matmul(pPos[:, 0:NE], lhsT=Ub[:, :], rhs=mkb[:, :], start=True, stop=False), r=["Ub", "mkb"], w=["pPos"])
                        M(lambda e: e.matmul(pPos[:, 0:NE], lhsT=ones_b[:, :], rhs=Sb[:, :], start=False, stop=True), r=["ones_b", "Sb"], w=["pPos"])
                        V(lambda e: e.tensor_scalar(out=sl[:, :], in0=mk[:, :], scalar1=-BIG, scalar2=BIG, op0=ALU.mult, op1=ALU.add), r=["mk"], w=["sl"])
                        V(lambda e: e.tensor_tensor(out=sl[:, :], in0=pPos[:, 0:NE], in1=sl[:, :], op=ALU.add), r=["pPos", "sl"], w=["sl"])
                        V(lambda e: e.tensor_copy(out=sloti[:, ti, :], in_=sl[:, :]), r=["sl"], w=["sloti"])
                        V(lambda e: e.tensor_scalar(out=sl[:, :], in0=sl[:, :], scalar1=float((CAP_C if isc else CAP_L) - 1), scalar2=None, op0=ALU.min), r=["sl"], w=["sl"])
                        V(lambda e: e.tensor_copy(out=slotc[:, ti, :], in_=sl[:, :]), r=["sl"], w=["slotc"])
                        V(lambda e: e.tensor_tensor(out=gm[:, ti, :], in0=aff[:, ti, :], in1=mk[:, :], op=ALU.mult), r=["aff", "mk"], w=["gm"])
                        V(lambda e: e.tensor_tensor(out=Sf[:, :], in0=Sf[:, :], in1=mk[:, :], op=ALU.add), r=["Sf", "mk"], w=["Sf"])
                        V(lambda e: e.tensor_copy(out=Sb[:, :], in_=Sf[:, :]), r=["Sf"], w=["Sb"])
                        V(lambda e: e.tensor_copy(out=hb[i2][:, D:D + 2], in_=tokb[:, ti, :]), r=["tokb"], w=[f"hbS{i2}"])
                        V(lambda e: e.tensor_copy(out=ghi[:, :], in_=gm[:, ti, :]), r=["gm"], w=["ghi"])
                        V(lambda e: e.tensor_copy(out=hb[i2][:, D + 2:D + 18], in_=ghi[:, :]), r=["ghi"], w=[f"hbS{i2}"])
                        V(lambda e: e.tensor_tensor(out=hb[i2][:, D + 18:D + 34], in0=gm[:, ti, :], in1=ghi[:, :], op=ALU.subtract), r=["gm", "ghi"], w=[f"hbS{i2}"])
                        for ex_ in range(NE):
                            dst = xe_c[ex_] if isc else xe_l[ex_]
                            bnd = (CAP_C if isc else CAP_L) - 1
                            S(lambda e: e.indirect_dma_start(out=dst, out_offset=bass.IndirectOffsetOnAxis(ap=sloti[:, ti, ex_:ex_ + 1], axis=0),
                                                             in_=hb[i2][:, :], in_offset=None, bounds_check=REG[bnd], oob_is_err=False),
                              r=[f"hbS{i2}", "sloti"])
                    if "dbg_aff" in dbg:
                        S(lambda e: e.dma_start(out=dbg_aff, in_=aff[:, :, :].rearrange("p t e -> p (t e)")), r=["aff"])
                        S(lambda e: e.dma_start(out=dbg_slot, in_=sloti[:, :, :].rearrange("p t e -> p (t e)")), r=["sloti"])
                P.barrier()
                p13.close()
                if stop_after == ("E3", l):
                    break
                with ExitStack() as p4:
                    NJ = CAP_L + (CAP_C if not last else 0)
                    stg = [tsb(p4, f"stg{i}", [128, 8, D], F32) for i in range(2)]
                    wb = [tsb(p4, f"wb{i}", [128, 8, D], BF16) for i in range(4)]
                    xet = [tsb(p4, f"xet{i}", [128, XW], BF16) for i in range(5)]
                    sideb = [tsb(p4, f"sideb{i}", [128, 5, 36], BF16) for i in range(2)]
                    gatef = [tsb(p4, f"gatef{i}", [128, 5], F32) for i in range(2)]
                    idxf = [tsb(p4, f"idxf{i}", [128, 5], F32) for i in range(2)]
                    idxi = [tsb(p4, f"idxi{i}", [128, 5], I32) for i in range(2)]
                    xeT = [tsb(p4, f"xeT{i}", [128, 8, CAP_L + CAP_C], BF16) for i in range(2)]
                    hidT = tsb(p4, "hidT", [128, 8, CAP_L + CAP_C], BF16)
                    sg = [tsb(p4, f"sg{i}", [128, CAP_L + CAP_C], F32) for i in range(2)]
                    yeb = [tsb(p4, f"yeb{i}", [128, D], BF16) for i in range(2)]
                    pT = tps(p4, "pT4", [128, 1024], BF16)
                    pG = tps(p4, "pG", [128, 512]); pU = tps(p4, "pU", [128, 512])
                    pGc = tps(p4, "pGc", [128, 512])
                    pUc = tps(p4, "pUc", [128, 512])
                    pY = [tps(p4, f"pY4_{i}", [128, 512]) for i in range(2)]
                    cnt4 = {"w": 0, "s": 0, "x": 0, "g": 0, "y": 0}

                    def load_w(src):
                        si = cnt4["s"] % 2; cnt4["s"] += 1
                        wi = cnt4["w"] % 4; cnt4["w"] += 1
                        for h in range(4):
                            L(lambda e: e.dma_start(out=stg[si][:, 2 * h:2 * h + 2, :], in_=src[h * 256:(h + 1) * 256, :].rearrange("(k p) n -> p k n", p=128)), w=[f"stg{si}h{h}"])
                        V(lambda e: e.tensor_copy(out=wb[wi][:, 0:2, :], in_=stg[si][:, 0:2, :]), r=[f"stg{si}h0"], w=[f"wb{wi}a"])
                        A(lambda e: e.copy(out=wb[wi][:, 2:4, :], in_=stg[si][:, 2:4, :]), r=[f"stg{si}h1"], w=[f"wb{wi}b"])
                        V(lambda e: e.tensor_copy(out=wb[wi][:, 4:5, :], in_=stg[si][:, 4:5, :]), r=[f"stg{si}h2"], w=[f"wb{wi}c"])
                        A(lambda e: e.copy(out=wb[wi][:, 5:6, :], in_=stg[si][:, 5:6, :]), r=[f"stg{si}h2"], w=[f"wb{wi}d"])
                        G(lambda e: e.tensor_copy(out=wb[wi][:, 6:8, :], in_=stg[si][:, 6:8, :]), r=[f"stg{si}h3"], w=[f"wb{wi}e"])
                        return wb[wi], [f"wb{wi}{c}" for c in "abcde"]

                    def x_tiles(ex_):
                        jts = [(xe_l[ex_][j * 128:(j + 1) * 128, :], 128, j * 128) for j in range(4)]
                        if not last:
                            jts.append((xe_c[ex_][:, :], CAP_C, CAP_L))
                        return jts

                    def stage_x_load(ex_):
                        for jt, (src, nj, c0) in enumerate(x_tiles(ex_)):
                            L(lambda e: e.dma_start(out=xet[jt][0:nj, :], in_=src), w=[f"xet{jt}"])

                    def stage_x(ex_):
                        xT = xeT[ex_ % 2]; xk = f"xeT{ex_ % 2}"
                        for jt, (src, nj, c0) in enumerate(x_tiles(ex_)):
                            xi = jt
                            e2 = ex_ % 2
                            V(lambda e: e.tensor_copy(out=sideb[e2][0:nj, jt, :], in_=xet[xi][0:nj, D:XW]), r=[f"xet{xi}"], w=[f"sideb{e2}"])
                            V(lambda e: e.tensor_tensor(out=gatef[e2][0:nj, jt:jt + 1], in0=sideb[e2][0:nj, jt, 2 + ex_:3 + ex_], in1=sideb[e2][0:nj, jt, 18 + ex_:19 + ex_], op=ALU.add),
                              r=[f"sideb{e2}"], w=[f"gatef{e2}"])
                            V(lambda e: e.scalar_tensor_tensor(out=idxf[e2][0:nj, jt:jt + 1], in0=sideb[e2][0:nj, jt, 0:1], scalar=64.0, in1=sideb[e2][0:nj, jt, 1:2], op0=ALU.mult, op1=ALU.add),
                              r=[f"sideb{e2}"], w=[f"idxf{e2}"])
                            V(lambda e: e.tensor_copy(out=idxi[e2][0:nj, jt:jt + 1], in_=idxf[e2][0:nj, jt:jt + 1]), r=[f"idxf{e2}"], w=[f"idxi{e2}"])
                            for kc in range(8):
                                M(lambda e: e.transpose(out=pT[:, kc * 128:kc * 128 + nj], in_=xet[xi][0:nj, kc * 128:(kc + 1) * 128], identity=ident[0:nj, 0:nj]),
                                  r=[f"xet{xi}", "ident"], w=["pT4"])
                            V(lambda e: e.tensor_copy(out=xT[:, :, c0:c0 + nj], in_=pT[:, :].rearrange("p (k t) -> p k t", k=8)[:, :, 0:nj]), r=["pT4"], w=[xk])

                    def stage_hidden(ex_, wg, wgk, wu, wuk):
                        xT = xeT[ex_ % 2]; xk = f"xeT{ex_ % 2}"
                        for fc in range(8):
                            for kc in range(8):
                                M(lambda e: e.matmul(pG[:, :], lhsT=wg[:, kc, fc * 128:(fc + 1) * 128], rhs=xT[:, kc, 0:CAP_L], start=(kc == 0), stop=(kc == 7)), r=wgk + [xk], w=["pG"])
                            for kc in range(8):
                                M(lambda e: e.matmul(pU[:, :], lhsT=wu[:, kc, fc * 128:(fc + 1) * 128], rhs=xT[:, kc, 0:CAP_L], start=(kc == 0), stop=(kc == 7)), r=wuk + [xk], w=["pU"])
                            gi = cnt4["g"] % 2; cnt4["g"] += 1
                            A(lambda e: e.activation(out=sg[gi][:, 0:CAP_L], in_=pG[:, :], func=AF.Silu), r=["pG"], w=[f"sg{gi}"])
                            V(lambda e: e.tensor_tensor(out=hidT[:, fc, 0:CAP_L], in0=pU[:, :], in1=sg[gi][:, 0:CAP_L], op=ALU.mult), r=["pU", f"sg{gi}"], w=["hidT"])
                            if not last:
                                for kc in range(8):
                                    M(lambda e: e.matmul(pGc[:, 0:CAP_C], lhsT=wg[:, kc, fc * 128:(fc + 1) * 128], rhs=xT[:, kc, CAP_L:NJ], start=(kc == 0), stop=(kc == 7)), r=wgk + [xk], w=["pGc0"])
                                for kc in range(8):
                                    M(lambda e: e.matmul(pUc[:, 0:CAP_C], lhsT=wu[:, kc, fc * 128:(fc + 1) * 128], rhs=xT[:, kc, CAP_L:NJ], start=(kc == 0), stop=(kc == 7)), r=wuk + [xk], w=["pGc1"])
                                A(lambda e: e.activation(out=sg[gi][:, CAP_L:NJ], in_=pGc[:, 0:CAP_C], func=AF.Silu), r=["pGc0"], w=[f"sg{gi}c"])
                                V(lambda e: e.tensor_tensor(out=hidT[:, fc, CAP_L:NJ], in0=pUc[:, 0:CAP_C], in1=sg[gi][:, CAP_L:NJ], op=ALU.mult), r=["pGc1", f"sg{gi}c"], w=["hidTc"])

                    def stage_down(ex_, wd, wdk):
                        outs = [(None, 128, j * 128) for j in range(4)]
                        if not last:
                            outs.append((None, CAP_C, CAP_L))
                        e2 = ex_ % 2
                        for jt, (dst, nj, c0) in enumerate(outs):
                            gate_ap = gatef[e2][0:nj, jt:jt + 1]
                            idx_ap = idxi[e2][0:nj, jt:jt + 1]
                            yi = cnt4["y"] % 2; cnt4["y"] += 1
                            for dh in range(2):
                                for fc in range(8):
                                    M(lambda e: e.matmul(pY[dh][0:nj, :], lhsT=hidT[:, fc, c0:c0 + nj], rhs=wd[:, fc, dh * 512:(dh + 1) * 512], start=(fc == 0), stop=(fc == 7)),
                                      r=["hidT", "hidTc"] + wdk, w=[f"pY4_{dh}"])
                            A(lambda e: e.activation(out=yeb[yi][0:nj, 0:512], in_=pY[0][0:nj, :], func=AF.Identity, scale=gate_ap), r=["pY4_0", f"gatef{e2}"], w=[f"yeb{yi}a"])
                            V(lambda e: e.tensor_scalar(out=yeb[yi][0:nj, 512:1024], in0=pY[1][0:nj, :], scalar1=gate_ap, scalar2=None, op0=ALU.mult), r=["pY4_1", f"gatef{e2}"], w=[f"yeb{yi}b"])
                            S(lambda e: e.indirect_dma_start(out=accd[:, :], out_offset=bass.IndirectOffsetOnAxis(ap=idx_ap, axis=0),
                                                             in_=yeb[yi][0:nj, :], in_offset=None, compute_op=ALU.add),
                              r=[f"yeb{yi}a", f"yeb{yi}b", f"idxi{e2}"], w=["accd"])

                    stage_x_load(0)
                    wg, wgk = load_w(w_e_gate[l, 0])
                    wu, wuk = load_w(w_e_up[l, 0])
                    stage_x(0)
                    for ex_ in range(NE):
                        wd, wdk = load_w(w_e_down[l, ex_])
                        stage_hidden(ex_, wg, wgk, wu, wuk)
                        if ex_ + 1 < NE:
                            stage_x_load(ex_ + 1)
                            wg, wgk = load_w(w_e_gate[l, ex_ + 1])
                            wu, wuk = load_w(w_e_up[l, ex_ + 1])
                            stage_x(ex_ + 1)
                        stage_down(ex_, wd, wdk)
                P.barrier()
                if stop_after == ("E4", l):
                    break
                with ExitStack() as p5:
                    xt = [tsb(p5, f"xt5_{i}", [128, D], F32) for i in range(3)]
                    acc = [tsb(p5, f"acc{i}", [128, D], F32) for i in range(3)]
                    for n_, ti in enumerate(tiles_act):
                        which = 1 if ti < NCT else 0
                        i2 = n_ % 3
                        L(lambda e: e.dma_start(out=xt[i2][:, :], in_=xall[ti * 128:(ti + 1) * 128, :]), w=[f"xt5_{i2}"])
                        L(lambda e: e.dma_start(out=acc[i2][:, :], in_=accd[ti * 128:(ti + 1) * 128, :]), w=[f"acc{i2}"])
                        V(lambda e: e.tensor_tensor(out=acc[i2][:, :], in0=acc[i2][:, :], in1=G2[which][:, :], op=ALU.mult), r=[f"acc{i2}", f"G2_{which}"], w=[f"acc{i2}"])
                        V(lambda e: e.tensor_tensor(out=acc[i2][:, :], in0=acc[i2][:, :], in1=xt[i2][:, :], op=ALU.add), r=[f"acc{i2}", f"xt5_{i2}"], w=[f"acc{i2}"])
                        if last:
                            S(lambda e: e.dma_start(out=y_out[(ti - NCT) * 128:(ti - NCT + 1) * 128, :], in_=acc[i2][:, :]), r=[f"acc{i2}"])
                        else:
                            S(lambda e: e.dma_start(out=xall[ti * 128:(ti + 1) * 128, :], in_=acc[i2][:, :]), r=[f"acc{i2}"])
            P.barrier()
            if stop_after == ("L", l):
                break
        P.barrier()
        print("instr counts", P.ninstr, "total", P.total, flush=True)
    return nc


def _rope_table():
    t = np.arange(SEQ)
    row = (t // 64).astype(np.float64)
    col = (t % 64).astype(np.float64)
    out = np.zeros((SEQ, 192), np.float64)

    def fill(off, dim):
        ad = dim // 2
        inv = 10000.0 ** (-np.arange(0, ad, 2, dtype=np.float64) / ad)
        ar = row[:, None] * inv[None, :]
        ac = col[:, None] * inv[None, :]
        C = np.concatenate([np.cos(ar), np.cos(ar), np.cos(ac), np.cos(ac)], axis=1)
        S_ = np.concatenate([-np.sin(ar), np.sin(ar), -np.sin(ac), np.sin(ac)], axis=1)
        out[:, off:off + dim] = C
        out[:, off + dim:off + 2 * dim] = S_
    fill(0, 64)
    fill(128, 32)
    return out.astype(np.float32)


_NC_CACHE = {}


def kernel(**inputs):
    key = "full"
    if key not in _NC_CACHE:
        _NC_CACHE[key] = build()
    nc = _NC_CACHE[key]
    rope = _rope_table()
    shared = {k: np.ascontiguousarray(np.asarray(v, dtype=np.float32)) for k, v in inputs.items() if k not in ("x", "c", "ctx")}
    x = np.asarray(inputs["x"], dtype=np.float32); c = np.asarray(inputs["c"], dtype=np.float32); ctx = np.asarray(inputs["ctx"], dtype=np.float32)
    in_maps = []
    for b in range(8):
        m = dict(shared)
        m["x"] = np.ascontiguousarray(x[b]); m["ctx"] = np.ascontiguousarray(ctx[b]); m["c"] = np.ascontiguousarray(c[b])
        m["rope"] = rope
        in_maps.append(m)
    res = run_bass_kernel_spmd(nc, in_maps, core_ids=list(range(8)))
    return np.stack([np.asarray(r["y"], dtype=np.float32) for r in res.results], axis=0)
```
